# Optimizing a Trainium2 kernel written in Bass

```python
import jax
import jax.numpy as jnp
from jax import lax
import numpy as np

D_MODEL = 1024
BATCH = 8
SEQ = 4096
DEPTH = 2

CTX_LEN = 256
GRID_W = 64
HEAD_DIM = 64
ROPE_THETA = 10000.0
QBLK = 128
EPS = 1e-6

A_HEADS = (D_MODEL // 2) // HEAD_DIM
A_KV_HEADS = A_HEADS // 4
A_WIDTH = A_HEADS * HEAD_DIM
B_WIDTH = D_MODEL - A_WIDTH
CONV_WIDTH = 31
AB_IN = A_WIDTH + 2 * A_KV_HEADS * HEAD_DIM + 2 * B_WIDTH

C_HEADS = (D_MODEL // 2) // HEAD_DIM
C_KV_HEADS = C_HEADS // 4
C_WIDTH = C_HEADS * HEAD_DIM
WINDOW = 128
D_WIDTH = D_MODEL - C_WIDTH
POOL_SIZES = (2, 4, 8, 16)
POOL_GROUP_WIDTH = D_WIDTH // len(POOL_SIZES)
CD_IN = C_WIDTH + 2 * C_KV_HEADS * HEAD_DIM + D_WIDTH

N_EXPERT_GROUPS = 4
EXPERTS_PER_GROUP = 8
N_EXPERTS = N_EXPERT_GROUPS * EXPERTS_PER_GROUP
TOP_K_INNER = 2
D_EXPERT = D_MODEL // 2
MOE_BLOCK = 128

kernel_name = 'hybrid_dit_gqa_conformer_swa_pool_hmoe'


def _rmsnorm(x, g):
    xf = x.astype(jnp.float32)
    y = xf * lax.rsqrt(jnp.mean(xf * xf, axis=-1, keepdims=True) + EPS)
    return (y * g.astype(jnp.float32)).astype(x.dtype)


def _modulate(h, shift, scale):
    return h * (1 + scale) + shift


def _rope_tables(n_lat):
    rows = n_lat // GRID_W
    row = jnp.repeat(jnp.arange(rows, dtype=jnp.float32), GRID_W)
    col = jnp.tile(jnp.arange(GRID_W, dtype=jnp.float32), rows)
    n_freq = HEAD_DIM // 4
    inv = ROPE_THETA ** (-jnp.arange(n_freq, dtype=jnp.float32) / n_freq)
    ang = jnp.concatenate([row[:, None] * inv, col[:, None] * inv], axis=-1)
    return jnp.cos(ang), jnp.sin(ang)


def _rope(x, cos, sin):
    half = HEAD_DIM // 2
    x1, x2 = x[..., :half], x[..., half:]
    c = cos.astype(x.dtype)
    s = sin.astype(x.dtype)
    return jnp.concatenate([x1 * c - x2 * s, x2 * c + x1 * s], axis=-1)


def _heads(t, n):
    b, s, _ = t.shape
    return t.reshape(b, s, n, HEAD_DIM).transpose(0, 2, 1, 3)


def _group(q, n_kv):
    b, h, s, d = q.shape
    return q.reshape(b, n_kv, h // n_kv, s, d)


def _merge(o):
    b, k, g, s, d = o.shape
    return o.transpose(0, 3, 1, 2, 4).reshape(b, s, k * g * d)


def _attend(q, k, v, mask, sink):
    s = jnp.einsum('bkgqd,bksd->bkgqs', q, k).astype(jnp.float32) * (HEAD_DIM ** -0.5)
    if mask is not None:
        s = jnp.where(mask, s, -jnp.inf)
    if sink is None:
        p = jax.nn.softmax(s, axis=-1)
    else:
        sk = jnp.broadcast_to(sink.astype(jnp.float32)[None, :, :, None, None], s.shape[:-1] + (1,))
        p = jax.nn.softmax(jnp.concatenate([s, sk], axis=-1), axis=-1)[..., :-1]
    return jnp.einsum('bkgqs,bksd->bkgqd', p.astype(v.dtype), v)


def _dense_attn_blocks(q, k, v):
    b, hk, g, s, d = q.shape
    nb = s // QBLK
    qb = jnp.moveaxis(q.reshape(b, hk, g, nb, QBLK, d), 3, 0)
    ob = lax.map(lambda qi: _attend(qi, k, v, None, None), qb)
    return jnp.moveaxis(ob, 0, 3).reshape(b, hk, g, s, d)


def _window_attn_blocks(q, k_lat, v_lat, k_ctx, v_ctx, sink):
    b, hk, g, s, d = q.shape
    nb = s // QBLK
    span = QBLK + 2 * WINDOW
    n_ctx = k_ctx.shape[2]
    pad = ((0, 0), (0, 0), (WINDOW, WINDOW), (0, 0))
    kp = jnp.pad(k_lat, pad)
    vp = jnp.pad(v_lat, pad)
    qb = jnp.moveaxis(q.reshape(b, hk, g, nb, QBLK, d), 3, 0)
    r = jnp.arange(QBLK)
    j = jnp.arange(span)
    ctx_mask = jnp.ones((QBLK, n_ctx), dtype=bool)

    def block(args):
        qi, i = args
        start = i * QBLK
        kw = lax.dynamic_slice_in_dim(kp, start, span, axis=2)
        vw = lax.dynamic_slice_in_dim(vp, start, span, axis=2)
        qpos = start + r
        kpos = start - WINDOW + j
        band = (jnp.abs(qpos[:, None] - kpos[None, :]) <= WINDOW) & ((kpos >= 0) & (kpos < s))[None, :]
        mask = jnp.concatenate([ctx_mask, band], axis=1)
        return _attend(qi, jnp.concatenate([k_ctx, kw], axis=2), jnp.concatenate([v_ctx, vw], axis=2), mask, sink)

    ob = lax.map(block, (qb, jnp.arange(nb)))
    return jnp.moveaxis(ob, 0, 3).reshape(b, hk, g, s, d)


def _conv_module(u, conv_w, conv_b, ln_g, ln_b):
    a, gate = jnp.split(u, 2, axis=-1)
    glu = a * jax.nn.sigmoid(gate)
    y = lax.conv_general_dilated(glu, conv_w[:, None, :], window_strides=(1,),
                                 padding=[(CONV_WIDTH // 2, CONV_WIDTH // 2)],
                                 dimension_numbers=('NWC', 'WIO', 'NWC'),
                                 feature_group_count=B_WIDTH) + conv_b
    yf = y.astype(jnp.float32)
    mu = jnp.mean(yf, axis=-1, keepdims=True)
    var = jnp.mean((yf - mu) ** 2, axis=-1, keepdims=True)
    yn = ((yf - mu) * lax.rsqrt(var + EPS) * ln_g.astype(jnp.float32) + ln_b.astype(jnp.float32)).astype(u.dtype)
    return jax.nn.silu(yn)


def _pool_mixer(u, pool_w, pool_scale):
    b, s, _ = u.shape
    uf = u.astype(jnp.float32)
    cs = jnp.pad(jnp.cumsum(uf, axis=1), ((0, 0), (1, 0), (0, 0)))
    t = jnp.arange(s)
    outs = []
    for gi, w in enumerate(POOL_SIZES):
        lo_c, hi_c = gi * POOL_GROUP_WIDTH, (gi + 1) * POOL_GROUP_WIDTH
        lo = jnp.clip(t - w // 2, 0, s)
        hi = jnp.clip(t - w // 2 + w, 0, s)
        csg = cs[..., lo_c:hi_c]
        mean = (jnp.take(csg, hi, axis=1) - jnp.take(csg, lo, axis=1)) / (hi - lo).astype(jnp.float32)[None, :, None]
        outs.append(mean - uf[..., lo_c:hi_c])
    p = jnp.stack(outs, axis=2).astype(u.dtype)
    y = jnp.einsum('bsgc,gcd->bsgd', p, pool_w).reshape(b, s, D_WIDTH)
    return y * pool_scale


def _mixer_ab(hx, hc, w_in, w_out, q_g, k_g, conv_w, conv_b, ln_g, ln_b, cos, sin, ctx_out):
    kvw = A_KV_HEADS * HEAD_DIM
    o_k, o_v, o_u = A_WIDTH, A_WIDTH + kvw, A_WIDTH + 2 * kvw
    px = hx @ w_in
    qx = _rope(_rmsnorm(_heads(px[..., :o_k], A_HEADS), q_g), cos, sin)
    kx = _rope(_rmsnorm(_heads(px[..., o_k:o_v], A_KV_HEADS), k_g), cos, sin)
    vx = _heads(px[..., o_v:o_u], A_KV_HEADS)
    if ctx_out:
        pc = hc @ w_in
        kvc = pc[..., o_k:o_u]
    else:
        kvc = hc @ w_in[:, o_k:o_u]
    kc = _rmsnorm(_heads(kvc[..., :kvw], A_KV_HEADS), k_g)
    vc = _heads(kvc[..., kvw:], A_KV_HEADS)
    ox = _dense_attn_blocks(_group(qx, A_KV_HEADS), jnp.concatenate([kc, kx], axis=2), jnp.concatenate([vc, vx], axis=2))
    bx = _conv_module(px[..., o_u:], conv_w, conv_b, ln_g, ln_b)
    yx = jnp.concatenate([_merge(ox), bx], axis=-1) @ w_out
    if not ctx_out:
        return yx, None
    qc = _rmsnorm(_heads(pc[..., :o_k], A_HEADS), q_g)
    oc = _attend(_group(qc, A_KV_HEADS), kc, vc, None, None)
    bc = _conv_module(pc[..., o_u:], conv_w, conv_b, ln_g, ln_b)
    yc = jnp.concatenate([_merge(oc), bc], axis=-1) @ w_out
    return yx, yc


def _mixer_cd(hx, hc, w_in, w_out, q_g, k_g, sink, pool_w, pool_scale, cos, sin, ctx_out):
    kvw = C_KV_HEADS * HEAD_DIM
    o_k, o_v, o_u = C_WIDTH, C_WIDTH + kvw, C_WIDTH + 2 * kvw
    sink_g = sink.reshape(C_KV_HEADS, C_HEADS // C_KV_HEADS)
    px = hx @ w_in
    qx = _rope(_rmsnorm(_heads(px[..., :o_k], C_HEADS), q_g), cos, sin)
    kx = _rope(_rmsnorm(_heads(px[..., o_k:o_v], C_KV_HEADS), k_g), cos, sin)
    vx = _heads(px[..., o_v:o_u], C_KV_HEADS)
    if ctx_out:
        pc = hc @ w_in
        kvc = pc[..., o_k:o_u]
    else:
        kvc = hc @ w_in[:, o_k:o_u]
    kc = _rmsnorm(_heads(kvc[..., :kvw], C_KV_HEADS), k_g)
    vc = _heads(kvc[..., kvw:], C_KV_HEADS)
    ox = _window_attn_blocks(_group(qx, C_KV_HEADS), kx, vx, kc, vc, sink_g)
    dx = _pool_mixer(px[..., o_u:], pool_w, pool_scale)
    yx = jnp.concatenate([_merge(ox), dx], axis=-1) @ w_out
    if not ctx_out:
        return yx, None
    qc = _rmsnorm(_heads(pc[..., :o_k], C_HEADS), q_g)
    oc = _attend(_group(qc, C_KV_HEADS), kc, vc, None, sink_g)
    dc = _pool_mixer(pc[..., o_u:], pool_w, pool_scale)
    yc = jnp.concatenate([_merge(oc), dc], axis=-1) @ w_out
    return yx, yc


def _moe(h, grp_w, grp_b, exp_w, exp_b, w_gate, w_up, w_down):
    n_tok, d = h.shape
    tok_idx = jnp.arange(n_tok)
    g_logits = (h @ grp_w).astype(jnp.float32) + grp_b.astype(jnp.float32)
    g_sel = jnp.argmax(g_logits, axis=-1)
    g_w = jax.nn.softmax(g_logits, axis=-1)[tok_idx, g_sel]
    e_logits = ((h @ exp_w).astype(jnp.float32) + exp_b.astype(jnp.float32)).reshape(n_tok, N_EXPERT_GROUPS, EXPERTS_PER_GROUP)[tok_idx, g_sel]
    e_p, e_i = lax.top_k(jax.nn.softmax(e_logits, axis=-1), TOP_K_INNER)
    wts = (g_w[:, None] * e_p / jnp.sum(e_p, axis=-1, keepdims=True)).reshape(-1)
    eid = (g_sel[:, None] * EXPERTS_PER_GROUP + e_i).reshape(-1).astype(jnp.int32)
    n_asg = n_tok * TOP_K_INNER
    flat_tok = jnp.repeat(tok_idx, TOP_K_INNER).astype(jnp.int32)
    order = jnp.argsort(eid)
    se, stok, sw = eid[order], flat_tok[order], wts[order]
    counts = jnp.bincount(eid, length=N_EXPERTS).astype(jnp.int32)
    padded = (counts + MOE_BLOCK - 1) // MOE_BLOCK * MOE_BLOCK
    start = jnp.cumsum(counts) - counts
    pend = jnp.cumsum(padded)
    dest = (pend - padded)[se] + jnp.arange(n_asg, dtype=jnp.int32) - start[se]
    n_blk = -(-(n_asg + N_EXPERTS * (MOE_BLOCK - 1)) // MOE_BLOCK)
    cap = n_blk * MOE_BLOCK
    buf_tok = jnp.full((cap,), n_tok, jnp.int32).at[dest].set(stok)
    buf_w = jnp.zeros((cap,), h.dtype).at[dest].set(sw.astype(h.dtype))
    blk_e = jnp.minimum(jnp.searchsorted(pend, jnp.arange(n_blk, dtype=jnp.int32) * MOE_BLOCK, side='right'), N_EXPERTS - 1)
    h_pad = jnp.concatenate([h, jnp.zeros((1, d), h.dtype)], axis=0)

    def run(args):
        tok, wt, e = args
        xb = h_pad[tok]
        hid = jax.nn.silu(xb @ w_gate[e]) * (xb @ w_up[e])
        return (hid @ w_down[e]) * wt[:, None]

    ys = lax.map(run, (buf_tok.reshape(n_blk, MOE_BLOCK), buf_w.reshape(n_blk, MOE_BLOCK), blk_e))
    out = jnp.zeros((n_tok + 1, d), h.dtype).at[buf_tok].add(ys.reshape(cap, d))
    return out[:n_tok]


def setup_inputs(seed: int = 0) -> dict:
    key = jax.random.key(seed)
    keys = jax.random.split(key, 48)
    counter = iter(range(48))

    def nrm(shape, scale):
        return jax.random.normal(keys[next(counter)], shape, jnp.float32) * scale

    n_even = (DEPTH + 1) // 2
    n_odd = DEPTH // 2
    dm = D_MODEL
    return {
        'x': nrm((BATCH, SEQ, dm), 1.0),
        'c': nrm((BATCH, dm), 1.0),
        'ctx': nrm((BATCH, CTX_LEN, dm), 1.0),
        'c_ctx': nrm((dm,), 1.0),
        'mod_w': nrm((DEPTH, dm, 6 * dm), 0.5 * dm ** -0.5),
        'mod_b': nrm((DEPTH, 6 * dm), 0.02),
        'ln1_g': 1.0 + nrm((DEPTH, dm), 0.02),
        'ln2_g': 1.0 + nrm((DEPTH, dm), 0.02),
        'w_in_ab': nrm((n_even, dm, AB_IN), dm ** -0.5),
        'w_out_ab': nrm((n_even, A_WIDTH + B_WIDTH, dm), dm ** -0.5),
        'q_norm_a': 1.0 + nrm((n_even, HEAD_DIM), 0.02),
        'k_norm_a': 1.0 + nrm((n_even, HEAD_DIM), 0.02),
        'conv_w': nrm((n_even, CONV_WIDTH, B_WIDTH), CONV_WIDTH ** -0.5),
        'conv_b': nrm((n_even, B_WIDTH), 0.02),
        'conv_ln_g': 1.0 + nrm((n_even, B_WIDTH), 0.02),
        'conv_ln_b': nrm((n_even, B_WIDTH), 0.02),
        'w_in_cd': nrm((n_odd, dm, CD_IN), dm ** -0.5),
        'w_out_cd': nrm((n_odd, C_WIDTH + D_WIDTH, dm), dm ** -0.5),
        'q_norm_c': 1.0 + nrm((n_odd, HEAD_DIM), 0.02),
        'k_norm_c': 1.0 + nrm((n_odd, HEAD_DIM), 0.02),
        'sink_c': nrm((n_odd, C_HEADS), 0.5),
        'pool_w': nrm((n_odd, len(POOL_SIZES), POOL_GROUP_WIDTH, POOL_GROUP_WIDTH), POOL_GROUP_WIDTH ** -0.5),
        'pool_scale': 1.0 + nrm((n_odd, D_WIDTH), 0.02),
        'rt_grp_w': nrm((DEPTH, dm, N_EXPERT_GROUPS), dm ** -0.5),
        'rt_grp_b': nrm((DEPTH, N_EXPERT_GROUPS), 0.01),
        'rt_exp_w': nrm((DEPTH, dm, N_EXPERTS), dm ** -0.5),
        'rt_exp_b': nrm((DEPTH, N_EXPERTS), 0.01),
        'ex_gate': nrm((DEPTH, N_EXPERTS, dm, D_EXPERT), dm ** -0.5),
        'ex_up': nrm((DEPTH, N_EXPERTS, dm, D_EXPERT), dm ** -0.5),
        'ex_down': nrm((DEPTH, N_EXPERTS, D_EXPERT, dm), D_EXPERT ** -0.5),
    }


def reference(x, c, ctx, c_ctx, mod_w, mod_b, ln1_g, ln2_g,
              w_in_ab, w_out_ab, q_norm_a, k_norm_a, conv_w, conv_b, conv_ln_g, conv_ln_b,
              w_in_cd, w_out_cd, q_norm_c, k_norm_c, sink_c, pool_w, pool_scale,
              rt_grp_w, rt_grp_b, rt_exp_w, rt_exp_b, ex_gate, ex_up, ex_down):
    b, s, dm = x.shape
    cos, sin = _rope_tables(s)
    for i in range(DEPTH):
        last = i == DEPTH - 1
        li = i // 2
        mod = jax.nn.silu(c) @ mod_w[i] + mod_b[i]
        modc = jax.nn.silu(c_ctx) @ mod_w[i] + mod_b[i]
        sh1, sc1, g1, sh2, sc2, g2 = jnp.split(mod[:, None, :], 6, axis=-1)
        sh1c, sc1c, g1c, sh2c, sc2c, g2c = jnp.split(modc[None, None, :], 6, axis=-1)
        hx = _modulate(_rmsnorm(x, ln1_g[i]), sh1, sc1)
        hc = _modulate(_rmsnorm(ctx, ln1_g[i]), sh1c, sc1c)
        if i % 2 == 0:
            yx, yc = _mixer_ab(hx, hc, w_in_ab[li], w_out_ab[li], q_norm_a[li], k_norm_a[li],
                               conv_w[li], conv_b[li], conv_ln_g[li], conv_ln_b[li], cos, sin, not last)
        else:
            yx, yc = _mixer_cd(hx, hc, w_in_cd[li], w_out_cd[li], q_norm_c[li], k_norm_c[li],
                               sink_c[li], pool_w[li], pool_scale[li], cos, sin, not last)
        x = x + g1 * yx
        hx = _modulate(_rmsnorm(x, ln2_g[i]), sh2, sc2)
        if last:
            ox = _moe(hx.reshape(b * s, dm), rt_grp_w[i], rt_grp_b[i], rt_exp_w[i], rt_exp_b[i],
                      ex_gate[i], ex_up[i], ex_down[i]).reshape(b, s, dm)
            x = x + g2 * ox
        else:
            ctx = ctx + g1c * yc
            hc = _modulate(_rmsnorm(ctx, ln2_g[i]), sh2c, sc2c)
            n_ctx = ctx.shape[1]
            tokens = jnp.concatenate([hc, hx], axis=1).reshape(-1, dm)
            out = _moe(tokens, rt_grp_w[i], rt_grp_b[i], rt_exp_w[i], rt_exp_b[i],
                       ex_gate[i], ex_up[i], ex_down[i]).reshape(b, n_ctx + s, dm)
            ctx = ctx + g2c * out[:, :n_ctx]
            x = x + g2 * out[:, n_ctx:]
    return x
```

```python
import os
import numpy as np
from contextlib import ExitStack
from collections import deque
import concourse.bass as bass
import concourse.mybir as mybir
from concourse.bass_utils import run_bass_kernel_spmd

F32 = mybir.dt.float32
BF16 = mybir.dt.bfloat16
I32 = mybir.dt.int32
ALU = mybir.AluOpType
AF = mybir.ActivationFunctionType
AX = mybir.AxisListType

D = 1024
S = 4096
NCTX = 256
T = S + NCTX
NT = T // 128
EPS = 1e-6
GLU_OFF_CTX = 15
GLU_OFF_LAT = 15 + NCTX + 15
GLU_LEN = GLU_OFF_LAT + S + 15


class Buf:
    __slots__ = ("name", "w", "r")

    def __init__(self, name=""):
        self.name = name
        self.w = None
        self.r = {}


class Tile:
    __slots__ = ("t", "b")

    def __init__(self, t, b):
        self.t, self.b = t, b

    def __getitem__(self, k):
        return self.t[k]


import threading


class Interleaver:
    def __init__(self):
        self.active = False

    def run(self, fns):
        if len(fns) == 1:
            fns[0]()
            return
        n = len(fns)
        self.ev = [threading.Event() for _ in range(n)]
        self.alive = [True] * n
        self.err = []
        self.tid = {}
        done = threading.Event()

        def worker(i):
            self.ev[i].wait()
            self.ev[i].clear()
            try:
                fns[i]()
            except BaseException as e:
                self.err.append(e)
            self.alive[i] = False
            nxt = self._next(i)
            if nxt is None:
                done.set()
            else:
                self.ev[nxt].set()

        ths = [threading.Thread(target=worker, args=(i,)) for i in range(n)]
        self.active = True
        for i, th in enumerate(ths):
            th.start()
            self.tid[th.ident] = i
        self.ev[0].set()
        done.wait()
        for th in ths:
            th.join()
        self.active = False
        if self.err:
            raise self.err[0]

    def _next(self, i):
        n = len(self.alive)
        for d in range(1, n + 1):
            j = (i + d) % n
            if self.alive[j] and j != i:
                return j
        return None

    def yield_point(self):
        if not self.active:
            return
        i = self.tid.get(threading.get_ident())
        if i is None:
            return
        nxt = self._next(i)
        if nxt is None:
            return
        self.ev[nxt].set()
        self.ev[i].wait()
        self.ev[i].clear()


ILV = Interleaver()


class Eng:
    def __init__(self, key, e, sem):
        self.key, self.e, self.sem = key, e, sem
        self.n = 0
        self.known = {}

    def _wait(self, sem, val):
        if self.known.get(sem, 0) >= val:
            return
        self.known[sem] = val
        self.e.wait_ge(sem, val)

    def op(self, ins_fn, reads=(), writes=(), inc=True):
        for b in reads:
            if b.w is not None:
                self._wait(b.w[0], b.w[1])
        strict = (self.key != "pe")
        for b in writes:
            if b.w is not None and (strict or b.w[2] != self.key):
                self._wait(b.w[0], b.w[1])
            for sem, (val, k) in b.r.items():
                if strict or k != self.key:
                    self._wait(sem, val)
        ins = ins_fn()
        if inc:
            self.n += 1
            ins.then_inc(self.sem, 1)
            tok = (self.sem, self.n, self.key)
        else:
            tok = (self.sem, self.n + 1, self.key)
        for b in reads:
            b.r[tok[0]] = (tok[1], tok[2])
        for b in writes:
            b.w = tok
            b.r = {}
        if inc:
            ILV.yield_point()
        return ins


class DmaQ:
    def __init__(self, key, e, sems):
        self.key, self.e, self.sems = key, e, sems
        self.cnt = [0] * len(sems)
        self.i = 0
        self.known = {}

    def _wait(self, sem, val):
        if self.known.get(sem, 0) >= val:
            return
        self.known[sem] = val
        self.e.wait_ge(sem, val)

    def dma(self, out, in_, reads=(), writes=(), **kw):
        for b in reads:
            if b.w is not None:
                self._wait(b.w[0], b.w[1])
        for b in writes:
            if b.w is not None:
                self._wait(b.w[0], b.w[1])
            for sem, (val, k) in b.r.items():
                self._wait(sem, val)
        s = self.i % len(self.sems)
        self.i += 1
        sem = self.sems[s]
        if self.cnt[s] > 0:
            self._wait(sem, 16 * self.cnt[s])
        self.cnt[s] += 1
        ins = self.e.dma_start(out=out, in_=in_, **kw)
        ins.then_inc(sem, 16)
        tok = (sem, 16 * self.cnt[s], self.key + str(s))
        for b in reads:
            b.r[tok[0]] = (tok[1], tok[2])
        for b in writes:
            b.w = tok
            b.r = {}
        return ins


def _dma_generic(self, fn, reads=(), writes=()):
    for b in reads:
        if b.w is not None:
            self._wait(b.w[0], b.w[1])
    for b in writes:
        if b.w is not None:
            self._wait(b.w[0], b.w[1])
        for sem, (val, k) in b.r.items():
            self._wait(sem, val)
    s = self.i % len(self.sems)
    self.i += 1
    sem = self.sems[s]
    if self.cnt[s] > 0:
        self._wait(sem, 16 * self.cnt[s])
    self.cnt[s] += 1
    ins = fn()
    ins.then_inc(sem, 16)
    tok = (sem, 16 * self.cnt[s], self.key + str(s))
    for b in reads:
        b.r[tok[0]] = (tok[1], tok[2])
    for b in writes:
        b.w = tok
        b.r = {}
    return ins


DmaQ.dma_fn = _dma_generic


class FW:
    def __init__(self, nc, stack, n_dma_sems=10):
        self.nc = nc
        mk = lambda nm: stack.enter_context(nc.semaphore(nm))
        self.pe = Eng("pe", nc.tensor, mk("s_pe"))
        self.act = Eng("act", nc.scalar, mk("s_act"))
        self.dve = Eng("dve", nc.vector, mk("s_dve"))
        self.pool = Eng("pool", nc.gpsimd, mk("s_pool"))
        self.q_sync = DmaQ("qs", nc.sync, [mk(f"s_qs{i}") for i in range(n_dma_sems)])
        self.q_pool = DmaQ("qp", nc.gpsimd, [mk(f"s_qp{i}") for i in range(n_dma_sems)])
        self.q_pool.known = self.pool.known
        self.engs = [self.pe, self.act, self.dve, self.pool]
        self.qs = [self.q_sync, self.q_pool]

    def barrier(self):
        toks = []
        for e in self.engs:
            if e.n > 0:
                toks.append((e.sem, e.n))
        for q in self.qs:
            for s, c in zip(q.sems, q.cnt):
                if c > 0:
                    toks.append((s, 16 * c))
        for e in self.engs + [self.q_sync]:
            for sem, val in toks:
                e._wait(sem, val)


class Builder:
    def __init__(self, debug=False, stop=None):
        self.debug = debug
        self.stop = stop
        self.nc = bass.Bass("TRN2", target_bir_lowering=False)
        self.dbg_out = {}

    def dram_in(self, name, shape, dt=F32):
        return self.nc.dram_tensor(name, list(shape), dt, kind="ExternalInput").ap()

    def tile(self, st, name, shape, dt):
        self._tn = getattr(self, "_tn", 0) + 1
        name = f"{name}_{self._tn}"
        t = st.enter_context(self.nc.sbuf_tensor(name, list(shape), dt))
        return Tile(t, Buf(name))

    def ps_get(self):
        return self.psq.popleft()

    def ps_put(self, p):
        self.psq.append(p)

    def dbg(self, name, shape):
        if not self.debug:
            return None
        ap = self.nc.dram_tensor("dbg_" + name, list(shape), F32, kind="ExternalOutput").ap()
        self.dbg_out[name] = (ap, Buf("dbg_" + name))
        return ap

    def cut(self, n):
        return int(os.environ.get("P1_CUT", "-1")) == n

    def tok_in(self, t):
        if t < 2:
            return self.ctx_in[t * 128:(t + 1) * 128, :]
        return self.x_in[(t - 2) * 128:(t - 1) * 128, :]

    def build(self):
        nc = self.nc
        di = self.dram_in
        self.x_in = di("x", [S, D])
        self.ctx_in = di("ctx", [NCTX, D])
        self.c2T = di("c2T", [128, 8, 2])
        self.mod_w = di("mod_w", [2, D, 6 * D])
        self.mod_bT = di("mod_bT", [128, 2, 48])
        self.ln1gT = di("ln1gT", [128, 2, 8])
        self.ln2gT = di("ln2gT", [128, 2, 8])
        self.w_in_ab = di("w_in_ab", [D, 1792])
        self.w_out_ab = di("w_out_ab", [D, D])
        self.gqk_a = di("gqk_a", [128, 640])
        self.convwT = di("convwT", [128, 4, 31])
        self.convv = di("convv", [128, 3, 4])
        self.w_in_cd = di("w_in_cd", [D, 1280])
        self.w_out_cd = di("w_out_cd", [D, D])
        self.gqk_c = di("gqk_c", [128, 640])
        self.sink_rep = di("sink_rep", [128, 2, 512])
        self.pool_w = di("pool_w", [4, 128, 128])
        self.pool_scT = di("pool_scT", [128, 4])
        self.poolfix = di("poolfix", [128, 4, 32])
        self.rt_w = di("rt_w", [2, D, 36])
        self.rt_b = di("rt_b", [128, 2, 36])
        self.ex_gate = di("ex_gate", [2, 32 * 128, 4096])
        self.ex_up = di("ex_up", [2, 32 * 128, 4096])
        self.ex_down = di("ex_down", [2, 32 * 128, 4096])
        self.pidx = di("pidx", [128, 1])
        self.ident_in = di("ident", [128, 128])
        self.cosT = di("cosT", [128, 32, 32])
        self.sinT = di("sinT", [128, 32, 32])
        self.wmask = di("wmask", [128, 2, 512])
        self.mconst = di("mconst", [128, 433])
        self.umat = di("umat", [128, 128])
        self.y_out = nc.dram_tensor("y", [S, D], F32, kind="ExternalOutput").ap()
        self.xres = nc.dram_tensor("xres", [T, D], F32, kind="Internal").ap()
        self.xres_b = [Buf(f"xres{t}") for t in range(NT)]
        self.y_b = [Buf(f"y{t}") for t in range(NT)]

        with ExitStack() as st:
            self.fw = FW(nc, st)
            fw = self.fw
            self.PS = []
            self.PSB = []
            for i in range(4):
                pb_ = st.enter_context(nc.psum_tensor(f"psb{i}", [128, 1024], F32))
                h0 = Tile(pb_[:, 0:512], Buf(f"ps{2 * i}"))
                h1 = Tile(pb_[:, 512:1024], Buf(f"ps{2 * i + 1}"))
                self.PS += [h0, h1]
                self.PSB.append((pb_, h0, h1))
            self.psq = deque(self.PS)
            g = self.g = {}
            g["identf"] = self.tile(st, "identf", [128, 128], F32)
            g["identb"] = self.tile(st, "identb", [128, 128], BF16)
            g["onesf"] = self.tile(st, "onesf", [128, 128], F32)
            g["modT"] = self.tile(st, "modT", [128, 2, 48, 2], F32)
            g["A1"] = self.tile(st, "A1", [128, 2, 8, 2], F32)
            g["A2"] = self.tile(st, "A2", [128, 2, 8, 2], F32)
            g["epsc"] = self.tile(st, "epsc", [128, 1], F32)
            fw.q_sync.dma(g["identf"][:], self.ident_in, writes=[g["identf"].b])
            fw.dve.op(lambda: nc.vector.tensor_copy(out=g["identb"][:], in_=g["identf"][:]), [g["identf"].b], [g["identb"].b])
            fw.dve.op(lambda: nc.vector.memset(g["onesf"][:], 1.0), [], [g["onesf"].b])
            fw.dve.op(lambda: nc.vector.memset(g["epsc"][:], EPS), [], [g["epsc"].b])

            self.bc_reg = nc.gpsimd.alloc_register("bcreg")
            nc.gpsimd.reg_mov(self.bc_reg, 8191)
            self.phase_mod()
            if self.stop != "p0":
                self.layer0_mixer()
            if self.stop is None or self.stop in ("m0", "l1", "m1"):
                (self.moe2 if os.environ.get("MOE_DENSE") != "1" else self.moe)(0)
            if self.stop is None or self.stop in ("l1", "m1"):
                self.layer1_mixer()
            if self.stop is None or self.stop in ("m1",):
                (self.moe2 if os.environ.get("MOE_DENSE") != "1" else self.moe)(1)

            for b in self.y_b[2:]:
                if b.w is not None:
                    fw.q_sync._wait(b.w[0], b.w[1])
            for name, (ap, b) in self.dbg_out.items():
                if b.w is not None:
                    fw.q_sync._wait(b.w[0], b.w[1])
            fw.barrier()
        return nc

    def rstd_from_ss(self, ss, n_inv, cols):
        nc, fw = self.nc, self.fw
        fw.dve.op(lambda: nc.vector.tensor_scalar(out=ss[:, 0:cols], in0=ss[:, 0:cols], scalar1=n_inv, scalar2=EPS,
                                                  op0=ALU.mult, op1=ALU.add), [ss.b], [ss.b])
        fw.act.op(lambda: nc.scalar.sqrt(out=ss[:, 0:cols], in_=ss[:, 0:cols]), [ss.b], [ss.b])
        fw.dve.op(lambda: nc.vector.reciprocal(out=ss[:, 0:cols], in_=ss[:, 0:cols]), [ss.b], [ss.b])

    def load_cast_w(self, st_pool, dst, dst_cols, src_ap, kc, ncols, eng_i):
        nc, fw = self.nc, self.fw
        stg = st_pool[self.stg_i % len(st_pool)]
        self.stg_i += 1
        fw.q_sync.dma(stg[:, 0:kc * ncols].rearrange("p (k n) -> p k n", k=kc),
                      src_ap.rearrange("(k p) n -> p k n", p=128), writes=[stg.b])
        src = stg[:, 0:kc * ncols].rearrange("p (k n) -> p k n", k=kc)
        if eng_i % 2 == 0:
            fw.act.op(lambda: nc.scalar.copy(out=dst[:, 0:kc, dst_cols], in_=src), [stg.b], [dst.b])
        else:
            fw.dve.op(lambda: nc.vector.tensor_copy(out=dst[:, 0:kc, dst_cols], in_=src), [stg.b], [dst.b])

    def phase_mod(self):
        nc, fw, g = self.nc, self.fw, self.g
        with ExitStack() as ph:
            c2 = self.tile(ph, "c2", [128, 8, 2], F32)
            sil = self.tile(ph, "sil", [128, 8, 2], BF16)
            mb = self.tile(ph, "mb", [128, 2, 48], F32)
            l1g = self.tile(ph, "l1g", [128, 2, 8], F32)
            l2g = self.tile(ph, "l2g", [128, 2, 8], F32)
            stg = [self.tile(ph, f"mstg{i}", [128, 4096], F32) for i in range(2)]
            wb = [self.tile(ph, f"mwb{i}", [128, 8, 512], BF16) for i in range(2)]
            self.stg_i = 0
            fw.q_sync.dma(c2[:], self.c2T, writes=[c2.b])
            fw.q_sync.dma(mb[:], self.mod_bT, writes=[mb.b])
            fw.q_sync.dma(l1g[:], self.ln1gT, writes=[l1g.b])
            fw.q_sync.dma(l2g[:], self.ln2gT, writes=[l2g.b])
            fw.act.op(lambda: nc.scalar.activation(out=sil[:], in_=c2[:], func=AF.Silu), [c2.b], [sil.b])
            for l in range(2):
                pm = self.ps_get()
                pmv = pm[:, 0:96].rearrange("p (c j) -> p c j", j=2)
                for n in range(12):
                    w = wb[n % 2]
                    self.load_cast_w(stg, w, slice(0, 512), self.mod_w[l, :, n * 512:(n + 1) * 512], 8, 512, n)
                    for q in range(4):
                        cc = n * 4 + q
                        for k in range(8):
                            fw.pe.op(lambda: nc.tensor.matmul(pmv[:, cc, :], lhsT=w[:, k, q * 128:(q + 1) * 128], rhs=sil[:, k, :],
                                                              start=(k == 0), stop=(k == 7)),
                                     [w.b, sil.b], [pm.b], inc=(k == 7 and q == 3))
                mT = g["modT"]
                fw.dve.op(lambda: nc.vector.tensor_tensor(out=mT[:, l, :, :], in0=pmv,
                                                          in1=mb[:, l, :].unsqueeze(2).to_broadcast([128, 48, 2]), op=ALU.add),
                          [pm.b, mb.b], [mT.b])
                self.ps_put(pm)
                fw.dve.op(lambda: nc.vector.scalar_tensor_tensor(out=g["A1"][:, l, :, :], in0=mT[:, l, 8:16, :], scalar=1.0,
                                                                 in1=l1g[:, l, :].unsqueeze(2).to_broadcast([128, 8, 2]),
                                                                 op0=ALU.add, op1=ALU.mult), [mT.b, l1g.b], [g["A1"].b])
                fw.dve.op(lambda: nc.vector.scalar_tensor_tensor(out=g["A2"][:, l, :, :], in0=mT[:, l, 32:40, :], scalar=1.0,
                                                                 in1=l2g[:, l, :].unsqueeze(2).to_broadcast([128, 8, 2]),
                                                                 op0=ALU.add, op1=ALU.mult), [mT.b, l2g.b], [g["A2"].b])
            if self.debug:
                ap = self.dbg("modT", [128, 2 * 48 * 2])
                fw.q_pool.dma(ap, g["modT"][:].rearrange("p l c j -> p (l c j)"), reads=[g["modT"].b], writes=[self.dbg_out["modT"][1]])
            fw.barrier()

    def bcast_tile(self, dst, col_ap_fn, tmp, extra=()):
        nc, fw, g = self.nc, self.fw, self.g
        for c in range(8):
            fw.dve.op(lambda: nc.vector.tensor_scalar(out=tmp[:], in0=g["onesf"][:], scalar1=col_ap_fn(c), scalar2=None, op0=ALU.mult),
                      [g["onesf"].b, g["modT"].b] + list(extra), [tmp.b])
            p = self.ps_get()
            fw.pe.op(lambda: nc.tensor.transpose(out=p[:, 0:128], in_=tmp[:], identity=g["identf"][:]), [tmp.b, g["identf"].b], [p.b])
            fw.act.op(lambda: nc.scalar.copy(out=dst[:, c * 128:(c + 1) * 128], in_=p[:, 0:128]), [p.b], [dst.b])
            self.ps_put(p)

    def norm_tile_to_hT(self, xt, hT, col0, l, which, cls, scr):
        nc, fw, g = self.nc, self.fw, self.g
        ni = scr.get("ni", 0)
        scr["ni"] = ni + 1
        ss, xn = scr["ssl"][ni % len(scr["ssl"])], scr["xnl"][ni % len(scr["xnl"])]
        fw.act.op(lambda: nc.scalar.activation(out=xn[:], in_=xt[:], func=AF.Square, accum_out=ss[:, 0:1]), [xt.b], [xn.b, ss.b])
        self.rstd_from_ss(ss, 1.0 / D, 1)
        fw.act.op(lambda: nc.scalar.activation(out=xn[:], in_=xt[:], func=AF.Copy, scale=ss[:, 0:1]), [xt.b, ss.b], [xn.b])
        p = self.ps_get()
        pv = p[:].bitcast(BF16).rearrange("p (c q) -> p c q", c=8)
        for c in range(8):
            fw.pe.op(lambda: nc.tensor.transpose(out=pv[:, c, :], in_=xn[:, c * 128:(c + 1) * 128], identity=g["identb"][:]),
                     [xn.b, g["identb"].b], [p.b], inc=(c == 7))
        A = g["A1"] if which == 1 else g["A2"]
        boff = 0 if which == 1 else 24
        for c in range(8):
            fw.dve.op(lambda: nc.vector.tensor_scalar(out=hT[:, c, col0:col0 + 128], in0=pv[:, c, :], scalar1=A[:, l, c, cls:cls + 1],
                                                      scalar2=g["modT"][:, l, boff + c, cls:cls + 1], op0=ALU.mult, op1=ALU.add),
                      [p.b, A.b, g["modT"].b], [hT.b])
        self.ps_put(p)

    def qk_post(self, qkv, t, gqk, scr, qT, kT, do_q, lat_idx):
        nc, fw, g = self.nc, self.fw, self.g
        sq, ssq, qn, qr, tmp = scr["sq"], scr["ssq"], scr["qn"], scr["qr"], scr["rt"]
        h0 = 0 if do_q else 8
        nh = 10 - h0
        c0 = h0 * 64
        q3 = lambda tl: tl[:, c0:640].rearrange("p (h d) -> p h d", d=64)
        fw.pool.op(lambda: nc.gpsimd.tensor_tensor(out=sq[:, c0:640], in0=qkv[:, c0:640], in1=qkv[:, c0:640], op=ALU.mult), [qkv.b], [sq.b])
        fw.dve.op(lambda: nc.vector.tensor_reduce(out=ssq[:, h0:10], in_=q3(sq), axis=AX.X, op=ALU.add), [sq.b], [ssq.b])
        fw.dve.op(lambda: nc.vector.tensor_scalar(out=ssq[:, h0:10], in0=ssq[:, h0:10], scalar1=1.0 / 64, scalar2=EPS,
                                                  op0=ALU.mult, op1=ALU.add), [ssq.b], [ssq.b])
        fw.act.op(lambda: nc.scalar.sqrt(out=ssq[:, h0:10], in_=ssq[:, h0:10]), [ssq.b], [ssq.b])
        fw.dve.op(lambda: nc.vector.reciprocal(out=ssq[:, h0:10], in_=ssq[:, h0:10]), [ssq.b], [ssq.b])
        fw.dve.op(lambda: nc.vector.tensor_tensor(out=q3(qn), in0=q3(qkv), in1=ssq[:, h0:10].unsqueeze(2).to_broadcast([128, nh, 64]),
                                                  op=ALU.mult), [qkv.b, ssq.b], [qn.b])
        if lat_idx is None:
            fw.pool.op(lambda: nc.gpsimd.tensor_tensor(out=qr[:, c0:640], in0=qn[:, c0:640], in1=gqk[:, c0:640], op=ALU.mult),
                       [qn.b, gqk.b], [qr.b])
        else:
            fw.pool.op(lambda: nc.gpsimd.tensor_tensor(out=qn[:, c0:640], in0=qn[:, c0:640], in1=gqk[:, c0:640], op=ALU.mult),
                       [qn.b, gqk.b], [qn.b])
            cosb = g["cos"][:, lat_idx, :].unsqueeze(1).to_broadcast([128, nh, 32])
            sinb = g["sin"][:, lat_idx, :].unsqueeze(1).to_broadcast([128, nh, 32])
            x1 = q3(qn)[:, :, 0:32]
            x2 = q3(qn)[:, :, 32:64]
            sq2 = sq[:, 0:640].rearrange("p (k n) -> p k n", k=2)
            t3 = lambda k: (sq2 if k < 2 else tmp)[:, k % 2, c0 // 2:320].rearrange("p (h d) -> p h d", d=32)
            fw.dve.op(lambda: nc.vector.tensor_tensor(out=t3(0), in0=x1, in1=cosb, op=ALU.mult), [qn.b, g["cos"].b], [sq.b])
            fw.pool.op(lambda: nc.gpsimd.tensor_tensor(out=t3(1), in0=x2, in1=sinb, op=ALU.mult), [qn.b, g["sin"].b], [sq.b])
            fw.dve.op(lambda: nc.vector.tensor_tensor(out=t3(2), in0=x2, in1=cosb, op=ALU.mult), [qn.b, g["cos"].b], [tmp.b])
            fw.pool.op(lambda: nc.gpsimd.tensor_tensor(out=t3(3), in0=x1, in1=sinb, op=ALU.mult), [qn.b, g["sin"].b], [tmp.b])
            fw.dve.op(lambda: nc.vector.tensor_tensor(out=q3(qr)[:, :, 0:32], in0=t3(0), in1=t3(1), op=ALU.subtract), [sq.b], [qr.b])
            fw.pool.op(lambda: nc.gpsimd.tensor_tensor(out=q3(qr)[:, :, 32:64], in0=t3(2), in1=t3(3), op=ALU.add), [tmp.b], [qr.b])
        if self.cut(30):
            return
        p = self.ps_get()
        pv = p[:].bitcast(BF16).rearrange("p (c q) -> p c q", c=8)
        if do_q:
            for j in range(4):
                fw.pe.op(lambda: nc.tensor.transpose(out=pv[:, j, :], in_=qr[:, j * 128:(j + 1) * 128], identity=g["identb"][:]),
                         [qr.b, g["identb"].b], [p.b], inc=False)
        fw.pe.op(lambda: nc.tensor.transpose(out=pv[:, 4, :], in_=qr[:, 512:640], identity=g["identb"][:]),
                 [qr.b, g["identb"].b], [p.b])
        if self.cut(31):
            return
        if do_q:
            fw.dve.op(lambda: nc.vector.tensor_copy(out=qT[:, t, :, :], in_=pv[:, 0:4, :]), [p.b], [qT.b])
        if self.cut(32):
            return
        fw.dve.op(lambda: nc.vector.tensor_copy(out=kT[0:64, 0, t * 128:(t + 1) * 128], in_=pv[0:64, 4, :]), [p.b], [kT.b])
        fw.dve.op(lambda: nc.vector.tensor_copy(out=kT[64:128, 1, t * 128:(t + 1) * 128], in_=pv[64:128, 4, :]), [p.b], [kT.b])
        self.ps_put(p)

    def v_fill(self, qkv, t, Vp):
        nc, fw = self.nc, self.fw
        fw.act.op(lambda: nc.scalar.copy(out=Vp[:, t, 0:64], in_=qkv[:, 640:704]), [qkv.b], [Vp.b])
        fw.act.op(lambda: nc.scalar.copy(out=Vp[:, t, 136:200], in_=qkv[:, 704:768]), [qkv.b], [Vp.b])

    def v_init(self, Vp):
        nc, fw = self.nc, self.fw
        fw.pool.op(lambda: nc.gpsimd.memset(Vp[:], 0.0), [], [Vp.b])
        fw.pool.op(lambda: nc.gpsimd.memset(Vp[:, :, 64:65], 1.0), [], [Vp.b])
        fw.pool.op(lambda: nc.gpsimd.memset(Vp[:, :, 104:105], 1.0), [], [Vp.b])

    def attention(self, blocks, qT, kT, Vp, oT, scr, masks=None, sinkrow=None, LA=2):
        nc, fw, g = self.nc, self.fw, self.g
        steps = []
        for b0 in range(0, len(blocks), 2):
            (kv0_, qb, kts), (kv1_, qb1, kts1) = blocks[b0], blocks[b0 + 1]
            assert (kv0_, kv1_) == (0, 1) and qb == qb1
            for i, (kt, mi) in enumerate(kts):
                steps.append(dict(bi=b0, qb=qb, kt=kt, mi=mi, first=(i == 0), last=(i == len(kts) - 1)))
        assert len(self.psq) == 8
        spairs = self.PSB[0:2]
        for (_, h0, h1) in spairs:
            self.psq.remove(h0)
            self.psq.remove(h1)
        po_of = {}
        cnt = dict(s=0)

        pending = []

        def finish_block(kv, qb, bi, po, idx):
            r0 = kv * 64
            dr = 64 if kv == 0 else 32
            M = 65 if kv == 0 else 128
            slot = (bi // 2) % 2
            posb = scr["posb"][slot][kv]
            rec = scr["rec"][slot]
            fw.dve.op(lambda: nc.vector.tensor_copy(out=posb[0:M, :], in_=po[0:M, :]), [po.b], [posb.b])
            self.ps_put(po)
            if sinkrow is not None:
                fw.dve.op(lambda: nc.vector.tensor_tensor(out=rec[dr:dr + 1, :], in0=posb[dr:dr + 1, :], in1=sinkrow[dr:dr + 1, kv, :], op=ALU.add),
                          [posb.b, sinkrow.b], [rec.b])
                fw.dve.op(lambda: nc.vector.reciprocal(out=rec[dr:dr + 1, :], in_=rec[dr:dr + 1, :]), [rec.b], [rec.b])
            else:
                fw.dve.op(lambda: nc.vector.reciprocal(out=rec[dr:dr + 1, :], in_=posb[dr:dr + 1, :]), [posb.b], [rec.b])
            pending.append((idx + 4, kv, qb, posb, rec))

        def finalize(kv, qb, posb, rec):
            r0 = kv * 64
            dr = 64 if kv == 0 else 32
            pb = self.ps_get()
            Mb = 64 if kv == 0 else 128
            fw.pe.op(lambda: nc.tensor.matmul(pb[0:Mb, :], lhsT=g["onesf"][dr:dr + 1, 0:Mb], rhs=rec[dr:dr + 1, :], start=True, stop=True),
                     [g["onesf"].b, rec.b], [pb.b])
            fw.dve.op(lambda: nc.vector.tensor_tensor(out=oT[r0:r0 + 64, :, qb * 128:(qb + 1) * 128],
                                                      in0=posb[r0:r0 + 64, :].rearrange("p (j q) -> p j q", j=4),
                                                      in1=pb[r0:r0 + 64, :].rearrange("p (j q) -> p j q", j=4), op=ALU.mult),
                      [posb.b, pb.b], [oT.b])
            self.ps_put(pb)

        def emit_S(st):
            qb, kt = st["qb"], st["kt"]
            big, h0, h1 = spairs[cnt["s"] % 2]
            cnt["s"] += 1
            rhs_q = qT[:, qb, :, :].rearrange("p j q -> p (j q)")
            fw.pe.op(lambda: nc.tensor.matmul(h0[:], lhsT=kT[:, 0, kt * 128:(kt + 1) * 128], rhs=rhs_q, start=True, stop=True),
                     [kT.b, qT.b], [h0.b], inc=False)
            fw.pe.op(lambda: nc.tensor.matmul(h1[:], lhsT=kT[:, 1, kt * 128:(kt + 1) * 128], rhs=rhs_q, start=True, stop=True),
                     [kT.b, qT.b], [h1.b])
            pe_ = scr["pexp"][scr["pi"] % len(scr["pexp"])]
            scr["pi"] += 1
            fw.act.op(lambda: nc.scalar.activation(out=pe_[:], in_=big[:, 0:1024], func=AF.Exp), [h0.b, h1.b], [pe_.b])
            if st["mi"] is not None:
                fw.dve.op(lambda: nc.vector.tensor_tensor(out=pe_[:].rearrange("p (a q) -> p a q", a=2), in0=pe_[:].rearrange("p (a q) -> p a q", a=2),
                                                          in1=masks[:, st["mi"], :].unsqueeze(1).to_broadcast([128, 2, 512]), op=ALU.mult),
                          [pe_.b, masks.b], [pe_.b])
            st["pe"] = pe_

        def emit_PV(st):
            qb, kt, bi = st["qb"], st["kt"], st["bi"]
            if st["first"]:
                po_of[bi] = (self.ps_get(), self.ps_get())
            pe_ = st["pe"]
            for kv in range(2):
                po = po_of[bi][kv]
                M = 65 if kv == 0 else 128
                fw.pe.op(lambda: nc.tensor.matmul(po[0:M, :], lhsT=Vp[:, kt, kv * 72:kv * 72 + M], rhs=pe_[:, kv * 512:(kv + 1) * 512],
                                                  start=st["first"], stop=st["last"]), [Vp.b, pe_.b], [po.b], inc=(st["last"] or kv == 1))
            if st["last"]:
                for kv in range(2):
                    finish_block(kv, qb, bi, po_of[bi][kv], st["idx"])
                del po_of[bi]

        n = len(steps)
        for idx in range(n + LA):
            if idx < n:
                emit_S(steps[idx])
            if idx - LA >= 0:
                steps[idx - LA]["idx"] = idx
                emit_PV(steps[idx - LA])
            while pending and pending[0][0] <= idx:
                _, kv, qb_, posb, rec = pending.pop(0)
                finalize(kv, qb_, posb, rec)
        while pending:
            _, kv, qb_, posb, rec = pending.pop(0)
            finalize(kv, qb_, posb, rec)
        for (_, h0, h1) in spairs:
            self.psq.append(h0)
            self.psq.append(h1)

    def out_proj(self, l, cat_fn, wsrc, tiles, ph):
        nc, fw, g = self.nc, self.fw, self.g
        classes = [1, 0] if l == 0 else [0]
        wo = {}
        GB = self.tile(ph, "GB", [128, D], F32)
        tmp = self.tile(ph, "bct", [128, 128], F32)
        stg = [self.tile(ph, f"ostg{i}", [128, D], F32) for i in range(2)]
        for cls in classes:
            wo[cls] = self.tile(ph, f"wo{cls}", [128, 8, D], BF16)
            self.bcast_tile(GB, lambda c: g["modT"][:, l, 16 + c, cls:cls + 1], tmp)
            for c in range(8):
                s_ = stg[c % 2]
                if c < 4:
                    fw.q_sync.dma(s_[0:64, :], wsrc[c * 64:(c + 1) * 64, :], writes=[s_.b])
                    fw.q_sync.dma(s_[64:128, :], wsrc[(c + 4) * 64:(c + 5) * 64, :], writes=[s_.b])
                else:
                    fw.q_sync.dma(s_[:], wsrc[c * 128:(c + 1) * 128, :], writes=[s_.b])
                fw.dve.op(lambda: nc.vector.tensor_tensor(out=wo[cls][:, c, :], in0=s_[:], in1=GB[:], op=ALU.mult), [s_.b, GB.b], [wo[cls].b])
        xt = [self.tile(ph, f"oxt{i}", [128, D], F32) for i in range(3)]

        def op_fn(i, t):
            cls = 1 if t < 2 else 0
            x_ = xt[i % 3]
            src = self.tok_in(t) if l == 0 else self.xres[t * 128:(t + 1) * 128, :]
            rb = [] if l == 0 else [self.xres_b[t]]
            fw.q_sync.dma(x_[:], src, reads=rb, writes=[x_.b])
            for half in range(2):
                p = self.ps_get()
                for c in range(8):
                    ct, ci = cat_fn(c)
                    fw.pe.op(lambda: nc.tensor.matmul(p[:], lhsT=ct[:, ci, t * 128:(t + 1) * 128], rhs=wo[cls][:, c, half * 512:(half + 1) * 512],
                                                      start=(c == 0), stop=(c == 7)), [ct.b, wo[cls].b], [p.b], inc=(c == 7))
                fw.dve.op(lambda: nc.vector.tensor_tensor(out=x_[:, half * 512:(half + 1) * 512], in0=p[:], in1=x_[:, half * 512:(half + 1) * 512],
                                                          op=ALU.add), [p.b, x_.b], [x_.b])
                self.ps_put(p)
            fw.q_pool.dma(self.xres[t * 128:(t + 1) * 128, :], x_[:], reads=[x_.b], writes=[self.xres_b[t]])
            if self.debug and t in (0, 2, NT - 1):
                nm = f"x1_{l}_{t}"
                ap = self.dbg(nm, [128, D])
                fw.q_pool.dma(ap, x_[:], reads=[x_.b], writes=[self.dbg_out[nm][1]])

        for i0 in range(0, len(tiles), 3):
            grp = list(range(i0, min(i0 + 3, len(tiles))))
            ILV.run([(lambda i=i: op_fn(i, tiles[i])) for i in grp])

    def layer0_mixer(self):
        nc, fw, g = self.nc, self.fw, self.g
        l = 0
        with ExitStack() as ph:
            gluT = self.tile(ph, "gluT", [128, 4, GLU_LEN], BF16)
            oT = self.tile(ph, "oT", [128, 4, T], BF16)
            pattn = ExitStack()
            qT = self.tile(pattn, "qT", [128, NT, 4, 128], BF16)
            kT = self.tile(pattn, "kT", [128, 2, T], BF16)
            Vp = self.tile(pattn, "Vp", [128, NT, 200], BF16)
            self.v_init(Vp)
            fw.pool.op(lambda: nc.gpsimd.memset(kT[:], 0.0), [], [kT.b])
            fw.pool.op(lambda: nc.gpsimd.memset(gluT[:], 0.0), [], [gluT.b])
            with ExitStack() as p1:
                win = self.tile(p1, "win", [128, 8, 1792], BF16)
                gqk = self.tile(p1, "gqk", [128, 640], F32)
                g["cos"] = self.tile(p1, "cos", [128, 32, 32], F32)
                g["sin"] = self.tile(p1, "sin", [128, 32, 32], F32)
                fw.q_sync.dma(gqk[:], self.gqk_a, writes=[gqk.b])
                fw.q_sync.dma(g["cos"][:], self.cosT, writes=[g["cos"].b])
                fw.q_sync.dma(g["sin"][:], self.sinT, writes=[g["sin"].b])
                fw.dve.op(lambda: nc.vector.tensor_scalar(out=gqk[:, 0:512], in0=gqk[:, 0:512], scalar1=0.125, scalar2=None, op0=ALU.mult),
                          [gqk.b], [gqk.b])
                with ExitStack() as pw:
                    stg = [self.tile(pw, f"wstg{i}", [128, 4096], F32) for i in range(1)]
                    self.stg_i = 0
                    for n in range(4):
                        w_ = 512 if n < 3 else 256
                        self.load_cast_w(stg, win, slice(n * 512, n * 512 + w_), self.w_in_ab[:, n * 512:n * 512 + w_], 8, w_, n)
                    fw.barrier()
                if self.cut(0):
                    return
                scr = dict(ssl=[self.tile(p1, f"ss{i}", [128, 1], F32) for i in range(2)],
                           xnl=[self.tile(p1, f"xn{i}", [128, D], BF16) for i in range(2)], sq=self.tile(p1, "sq", [128, 640], F32),
                           ssq=self.tile(p1, "ssq", [128, 10], F32), qn=self.tile(p1, "qn", [128, 640], F32),
                           qr=self.tile(p1, "qr", [128, 640], BF16), rt=self.tile(p1, "rt", [128, 2, 320], F32))
                xt = [self.tile(p1, f"xt{i}", [128, D], F32) for i in range(1)]
                hT = [self.tile(p1, f"hT{i}", [128, 8, 512], BF16) for i in range(1)]
                qkv = [self.tile(p1, f"qkv{i}", [128, 768], F32) for i in range(1)]
                sig = [self.tile(p1, f"sig{i}", [128, 512], BF16) for i in range(1)]
                chunks = [(0, 2)] + [(2 + 4 * i, 4) for i in range(8)]
                oflat = oT[:].rearrange("p a t -> p (a t)")
                coff = [0]

                def carve(n_elems, dt, shape3=None):
                    nb = n_elems * (4 if dt == F32 else 2) // 2
                    ap = oflat[:, coff[0]:coff[0] + nb]
                    coff[0] += nb
                    if dt == F32:
                        ap = ap.bitcast(F32)
                    if shape3 is not None:
                        ap = ap.rearrange("p (a b) -> p a b", a=shape3[0])
                    return Tile(ap, Buf("carve"))

                scr_b = dict(ssl=[scr["ssl"][1]], xnl=[scr["xnl"][1]], sq=carve(640, F32), ssq=carve(16, F32), qn=carve(640, F32),
                             qr=carve(640, BF16), rt=carve(640, F32, (2, 320)))
                scr_a = dict(scr)
                scr_a["ssl"] = [scr["ssl"][0]]
                scr_a["xnl"] = [scr["xnl"][0]]
                scr_l = [scr_a, scr_b]
                xt2 = [xt[0], carve(D, F32)]
                qkv2 = [qkv[0], carve(768, F32)]
                for ci, (t0, ntl) in enumerate(chunks):
                    h = hT[0]
                    ntok = ntl * 128
                    cls = 1 if t0 < 2 else 0

                    def norm_fn(tl):
                        t = t0 + tl
                        x_ = xt2[tl % 2]
                        fw.q_sync.dma(x_[:], self.tok_in(t), writes=[x_.b])
                        self.norm_tile_to_hT(x_, h, tl * 128, l, 1, cls, scr_l[tl % 2])

                    def qkv_fn(tl):
                        t = t0 + tl
                        qk_ = qkv2[tl % 2]
                        for (c0, w_) in ((0, 512), (512, 256)):
                            p = self.ps_get()
                            for k in range(8):
                                fw.pe.op(lambda: nc.tensor.matmul(p[:, 0:w_], lhsT=h[:, k, tl * 128:(tl + 1) * 128], rhs=win[:, k, c0:c0 + w_],
                                                                  start=(k == 0), stop=(k == 7)), [h.b, win.b], [p.b], inc=(k == 7))
                            if c0 == 0:
                                fw.act.op(lambda: nc.scalar.copy(out=qk_[:, 0:512].rearrange("p (j a d) -> p a j d", a=2, d=64),
                                                                 in_=p[:, 0:512].rearrange("p (a j d) -> p a j d", a=2, j=4)), [p.b], [qk_.b])
                            else:
                                fw.act.op(lambda: nc.scalar.copy(out=qk_[:, c0:c0 + w_], in_=p[:, 0:w_]), [p.b], [qk_.b])
                            self.ps_put(p)
                        self.qk_post(qk_, t, gqk, scr_l[tl % 2], qT, kT, True, None if t < 2 else t - 2)
                        self.v_fill(qk_, t, Vp)

                    for tl0 in range(0, ntl, 2):
                        ILV.run([(lambda tl=tl: norm_fn(tl)) for tl in range(tl0, min(tl0 + 2, ntl))])
                    for tl0 in range(0, ntl, 2):
                        ILV.run([(lambda tl=tl: qkv_fn(tl)) for tl in range(tl0, min(tl0 + 2, ntl))])
                    off = GLU_OFF_CTX if t0 < 2 else GLU_OFF_LAT + (t0 - 2) * 128
                    for j in range(4):
                        pa = self.ps_get()
                        pg = self.ps_get()
                        for (pp, cb) in ((pa, 768 + j * 128), (pg, 1280 + j * 128)):
                            for k in range(8):
                                fw.pe.op(lambda: nc.tensor.matmul(pp[:, 0:ntok], lhsT=win[:, k, cb:cb + 128], rhs=h[:, k, 0:ntok],
                                                                  start=(k == 0), stop=(k == 7)), [win.b, h.b], [pp.b], inc=(k == 7))
                        sg = sig[0]
                        fw.act.op(lambda: nc.scalar.activation(out=sg[:, 0:ntok], in_=pg[:, 0:ntok], func=AF.Sigmoid), [pg.b], [sg.b])
                        fw.dve.op(lambda: nc.vector.tensor_tensor(out=gluT[:, j, off:off + ntok], in0=pa[:, 0:ntok], in1=sg[:, 0:ntok], op=ALU.mult),
                                  [pa.b, sg.b], [gluT.b])
                        self.ps_put(pa)
                        self.ps_put(pg)
                    if self.cut(4) or (self.cut(5) and ci == 1):
                        return
                fw.barrier()
            if self.debug:
                self.dbg_dump_bf(ph, "qT", qT[:, 2, :, :], [128, 4, 128], qT.b)
                self.dbg_dump_bf(ph, "kT", kT[:, 0, 0:512], [128, 512], kT.b)
                self.dbg_dump_bf(ph, "glu", gluT[:, :, GLU_OFF_LAT:GLU_OFF_LAT + 128], [128, 4, 128], gluT.b)
            if self.stop == "p1":
                return
            with ExitStack() as p2:
                scr = dict(pexp=[self.tile(p2, f"pexp{i}", [128, 1024], BF16) for i in range(4)], pi=0,
                           rec=[self.tile(p2, f"rec{i}", [128, 512], F32) for i in range(2)],
                           posb=[[self.tile(p2, f"posb{i}{k}", [128, 512], F32) for k in range(2)] for i in range(2)])
                blocks = []
                for qb in range(NT):
                    kts = [(0, None), (1, None)] if qb < 2 else [(k, None) for k in range(NT)]
                    for kv in range(2):
                        blocks.append((kv, qb, kts))
                self.attention(blocks, qT, kT, Vp, oT, scr)
                fw.barrier()
            if self.debug:
                self.dbg_dump_bf(ph, "oT", oT[:, :, 256:384], [128, 4, 128], oT.b)
                self.dbg_dump_bf(ph, "oTc", oT[:, :, 0:128], [128, 4, 128], oT.b)
            pattn.close()
            if self.stop == "p2":
                return
            bT = self.tile(ph, "bT", [128, 4, T], BF16)
            with ExitStack() as p3:
                cw = self.tile(p3, "cw", [128, 4, 31], F32)
                cv = self.tile(p3, "cv", [128, 3, 4], F32)
                diag = self.tile(p3, "diag", [128, 4, 31, 128], BF16)
                onesM = self.tile(p3, "onesM", [128, 128], F32)
                fw.q_sync.dma(cw[:], self.convwT, writes=[cw.b])
                fw.q_sync.dma(cv[:], self.convv, writes=[cv.b])
                fw.dve.op(lambda: nc.vector.memset(onesM[:], 1.0 / 512), [], [onesM.b])
                for j in range(4):
                    for tap in range(31):
                        e_ = fw.dve if (tap % 2 == 0) else fw.pool
                        ee = nc.vector if (tap % 2 == 0) else nc.gpsimd
                        e_.op(lambda: ee.tensor_scalar(out=diag[:, j, tap, :], in0=g["identb"][:], scalar1=cw[:, j, tap:tap + 1], scalar2=None,
                                                       op0=ALU.mult), [g["identb"].b, cw.b], [diag.b])
                ysb = [self.tile(p3, f"ysb{i}", [128, 4, 512], F32) for i in range(2)]
                ysq = [self.tile(p3, f"ysq{i}", [128, 4, 512], F32) for i in range(2)]
                mean_l = [self.tile(p3, f"mean{i}", [128, 512], F32) for i in range(2)]
                rstd_l = [self.tile(p3, f"rstd{i}", [128, 512], F32) for i in range(2)]
                tmp_l = [[self.tile(p3, f"ctmp{i}{k}", [128, 512], F32) for k in range(2)] for i in range(2)]
                chunks = [(GLU_OFF_CTX, 0, 256)] + [(GLU_OFF_LAT + 512 * i, 256 + 512 * i, 512) for i in range(8)]

                def conv_fn(ci):
                    off, tok0, ntok = chunks[ci]
                    mean, rstd, tmp = mean_l[ci % 2], rstd_l[ci % 2], tmp_l[ci % 2]
                    y_, q_ = ysb[ci % 2], ysq[ci % 2]
                    for j in range(4):
                        p = self.ps_get()
                        for tap in range(31):
                            fw.pe.op(lambda: nc.tensor.matmul(p[:, 0:ntok], lhsT=diag[:, j, tap, :], rhs=gluT[:, j, off + tap - 15:off + tap - 15 + ntok],
                                                              start=(tap == 0), stop=(tap == 30)), [diag.b, gluT.b], [p.b], inc=(tap == 30))
                        fw.act.op(lambda: nc.scalar.activation(out=y_[:, j, 0:ntok], in_=p[:, 0:ntok], func=AF.Identity, bias=cv[:, 0, j:j + 1]),
                                  [p.b, cv.b], [y_.b])
                        self.ps_put(p)
                        fw.pool.op(lambda: nc.gpsimd.tensor_tensor(out=q_[:, j, 0:ntok], in0=y_[:, j, 0:ntok], in1=y_[:, j, 0:ntok], op=ALU.mult),
                                   [y_.b], [q_.b])
                    pm = self.ps_get()
                    pq = self.ps_get()
                    for (pp, src) in ((pm, y_), (pq, q_)):
                        for j in range(4):
                            fw.pe.op(lambda: nc.tensor.matmul(pp[:, 0:ntok], lhsT=onesM[:], rhs=src[:, j, 0:ntok], start=(j == 0), stop=(j == 3)),
                                     [onesM.b, src.b], [pp.b], inc=(j == 3))
                    fw.act.op(lambda: nc.scalar.copy(out=mean[:, 0:ntok], in_=pm[:, 0:ntok]), [pm.b], [mean.b])
                    self.ps_put(pm)
                    fw.pool.op(lambda: nc.gpsimd.tensor_tensor(out=rstd[:, 0:ntok], in0=mean[:, 0:ntok], in1=mean[:, 0:ntok], op=ALU.mult),
                               [mean.b], [rstd.b])
                    fw.dve.op(lambda: nc.vector.tensor_tensor(out=rstd[:, 0:ntok], in0=pq[:, 0:ntok], in1=rstd[:, 0:ntok], op=ALU.subtract),
                              [pq.b, rstd.b], [rstd.b])
                    self.ps_put(pq)
                    fw.dve.op(lambda: nc.vector.tensor_scalar(out=rstd[:, 0:ntok], in0=rstd[:, 0:ntok], scalar1=EPS, scalar2=None, op0=ALU.add),
                              [rstd.b], [rstd.b])
                    fw.act.op(lambda: nc.scalar.sqrt(out=rstd[:, 0:ntok], in_=rstd[:, 0:ntok]), [rstd.b], [rstd.b])
                    fw.dve.op(lambda: nc.vector.reciprocal(out=rstd[:, 0:ntok], in_=rstd[:, 0:ntok]), [rstd.b], [rstd.b])
                    for j in range(4):
                        t_ = tmp[j % 2]
                        fw.pool.op(lambda: nc.gpsimd.tensor_tensor(out=t_[:, 0:ntok], in0=y_[:, j, 0:ntok], in1=mean[:, 0:ntok], op=ALU.subtract),
                                   [y_.b, mean.b], [t_.b])
                        fw.dve.op(lambda: nc.vector.tensor_tensor(out=t_[:, 0:ntok], in0=t_[:, 0:ntok], in1=rstd[:, 0:ntok], op=ALU.mult),
                                  [t_.b, rstd.b], [t_.b])
                        fw.act.op(lambda: nc.scalar.activation(out=bT[:, j, tok0:tok0 + ntok], in_=t_[:, 0:ntok], func=AF.Silu,
                                                               scale=cv[:, 1, j:j + 1], bias=cv[:, 2, j:j + 1]), [t_.b, cv.b], [bT.b])

                conv_fn(0)
                for c0_ in range(1, 9, 2):
                    ILV.run([(lambda ci=ci: conv_fn(ci)) for ci in (c0_, c0_ + 1)])
                fw.barrier()
            if self.debug:
                self.dbg_dump_bf(ph, "bT", bT[:, :, 256:384], [128, 4, 128], bT.b)
            if self.stop == "p3":
                return
            with ExitStack() as p4:
                self.out_proj(0, lambda c: (oT, c) if c < 4 else (bT, c - 4), self.w_out_ab, list(range(NT)), p4)
                fw.barrier()

    def dbg_dump_bf(self, ph, name, src_ap, shape, buf):
        nc, fw = self.nc, self.fw
        n = int(np.prod(shape[1:]))
        ap = self.dbg(name, [128, n])
        with ExitStack() as ds:
            tf = self.tile(ds, "dbgt_" + name, shape, F32)
            fw.dve.op(lambda: nc.vector.tensor_copy(out=tf[:], in_=src_ap), [buf], [tf.b])
            flat = tf[:] if len(shape) == 2 else tf[:].rearrange("p a b -> p (a b)")
            fw.q_pool.dma(ap, flat, reads=[tf.b], writes=[self.dbg_out[name][1]])
            fw.barrier()

    def moe(self, l):
        nc, fw, g = self.nc, self.fw, self.g
        if l == 0:
            groups = [list(range(0, 10)), list(range(10, 18)), list(range(18, 26)), list(range(26, 34))]
        else:
            groups = [list(range(2 + 8 * i, 10 + 8 * i)) for i in range(4)]
        ngrp = int(os.environ.get("MOE_GROUPS", "4"))
        nexp = int(os.environ.get("MOE_EXPERTS", "32"))
        classes = [0, 1] if l == 0 else [0]
        with ExitStack() as ph:
            wr = self.tile(ph, "wr", [128, 8, 36], F32)
            rb = self.tile(ph, "rb", [128, 36], F32)
            fw.q_sync.dma(wr[:], self.rt_w[l].rearrange("(k p) n -> p k n", p=128), writes=[wr.b])
            fw.q_sync.dma(rb[:], self.rt_b[:, l, :], writes=[rb.b])
            G2B = {}
            tmpb = self.tile(ph, "bct2", [128, 128], F32)
            for cls in classes:
                G2B[cls] = self.tile(ph, f"G2B{cls}", [128, D], F32)
                self.bcast_tile(G2B[cls], lambda c: g["modT"][:, l, 40 + c, cls:cls + 1], tmpb)
            stg = [self.tile(ph, f"estg{i}", [128, 4096], F32) for i in range(2)]
            wg = [self.tile(ph, f"wg{i}", [128, 8, 512], BF16) for i in range(2)]
            wu = [self.tile(ph, f"wu{i}", [128, 8, 512], BF16) for i in range(2)]
            wd = {cls: [self.tile(ph, f"wd{cls}_{i}", [128, 4, D], BF16) for i in range(2)] for cls in classes}
            xg = self.tile(ph, "xg", [128, 10, D], F32)
            h2T = self.tile(ph, "h2T", [128, 8, 1280], BF16)
            Wt = self.tile(ph, "Wt", [128, 10, 32], F32)
            xn = self.tile(ph, "xn2", [128, D], F32)
            hTf = self.tile(ph, "hTf", [128, 8, 128], F32)
            ss = self.tile(ph, "ss2", [128, 1], F32)
            r_ = {k: self.tile(ph, "r_" + k, [128, n], F32) for k, n in
                  dict(lg=36, gmax=1, ngmax=1, goh=4, ge=4, gsum=1, pen=4, em=32, m8=8, d=1, ed=1, p1=1, p2=1, t1=32, t2=32).items()}
            hid = [self.tile(ph, f"hid{i}", [128, 4, 512], BF16) for i in range(2)]
            sgl = [self.tile(ph, f"sgl{i}", [128, 512], F32) for i in range(2)]
            self.stg_i = 0

            def load_expert(e, need_ctx):
                i = e % 2
                self.load_cast_w(stg, wg[i], slice(0, 512), self.ex_gate[l, e], 8, 512, 0)
                self.load_cast_w(stg, wu[i], slice(0, 512), self.ex_up[l, e], 8, 512, 0)
                s_ = stg[self.stg_i % 2]
                self.stg_i += 1
                fw.q_sync.dma(s_[:].rearrange("p (k n) -> p k n", k=4), self.ex_down[l, e].rearrange("(k p) n -> p k n", p=128), writes=[s_.b])
                for cls in classes:
                    if cls == 1 and not need_ctx:
                        continue
                    fw.dve.op(lambda: nc.vector.tensor_tensor(out=wd[cls][i][:], in0=s_[:].rearrange("p (k n) -> p k n", k=4),
                                                              in1=G2B[cls][:].unsqueeze(1).to_broadcast([128, 4, D]), op=ALU.mult),
                              [s_.b, G2B[cls].b], [wd[cls][i].b])

            for gi, tiles in enumerate(groups[:ngrp]):
                has_ctx = (l == 0 and gi == 0)
                load_expert(0, has_ctx)
                for ti, t in enumerate(tiles):
                    cls = 1 if t < 2 else 0
                    fw.q_sync.dma(xg[:, ti, :], self.xres[t * 128:(t + 1) * 128, :], reads=[self.xres_b[t]], writes=[xg.b])
                    fw.act.op(lambda: nc.scalar.activation(out=xn[:], in_=xg[:, ti, :], func=AF.Square, accum_out=ss[:, 0:1]), [xg.b], [xn.b, ss.b])
                    self.rstd_from_ss(ss, 1.0 / D, 1)
                    fw.act.op(lambda: nc.scalar.activation(out=xn[:], in_=xg[:, ti, :], func=AF.Copy, scale=ss[:, 0:1]), [xg.b, ss.b], [xn.b])
                    for hb in range(2):
                        p = self.ps_get()
                        pv = p[:].rearrange("p (c q) -> p c q", c=4)
                        for c4 in range(4):
                            c = hb * 4 + c4
                            fw.pe.op(lambda: nc.tensor.transpose(out=pv[:, c4, :], in_=xn[:, c * 128:(c + 1) * 128], identity=g["identf"][:]),
                                     [xn.b, g["identf"].b], [p.b], inc=(c4 == 3))
                        for c4 in range(4):
                            c = hb * 4 + c4
                            if hb == 0:
                                fw.dve.op(lambda: nc.vector.tensor_scalar(out=hTf[:, c, :], in0=pv[:, c4, :], scalar1=g["A2"][:, l, c, cls:cls + 1],
                                                                          scalar2=g["modT"][:, l, 24 + c, cls:cls + 1], op0=ALU.mult, op1=ALU.add),
                                          [p.b, g["A2"].b, g["modT"].b], [hTf.b])
                            else:
                                fw.act.op(lambda: nc.scalar.activation(out=hTf[:, c, :], in_=pv[:, c4, :], func=AF.Identity,
                                                                       scale=g["A2"][:, l, c, cls:cls + 1], bias=g["modT"][:, l, 24 + c, cls:cls + 1]),
                                          [p.b, g["A2"].b, g["modT"].b], [hTf.b])
                        self.ps_put(p)
                    fw.pool.op(lambda: nc.gpsimd.tensor_copy(out=h2T[:, :, ti * 128:(ti + 1) * 128], in_=hTf[:]), [hTf.b], [h2T.b])
                    pr = self.ps_get()
                    for c in range(8):
                        fw.pe.op(lambda: nc.tensor.matmul(pr[:, 0:36], lhsT=hTf[:, c, :], rhs=wr[:, c, :], start=(c == 0), stop=(c == 7)),
                                 [hTf.b, wr.b], [pr.b], inc=(c == 7))
                    self.route(pr, rb, r_, Wt, ti)
                    self.ps_put(pr)
                if self.debug and gi == 0:
                    self.dbg_dump_bf(ph, f"Wt{l}", Wt[:, 0:4, :], [128, 4, 32], Wt.b)
                if has_ctx:
                    chunks = [(0, 2), (2, 4), (6, 4)]
                else:
                    chunks = [(0, 4), (4, 4)]
                for e in range(nexp):
                    if e + 1 < nexp:
                        load_expert(e + 1, has_ctx)
                    i = e % 2
                    for ci, (tl0, ntl) in enumerate(chunks):
                        cls = 1 if (has_ctx and ci == 0) else 0
                        ntok = ntl * 128
                        c0 = tl0 * 128
                        hd = hid[(e * len(chunks) + ci) % 2]
                        for f in range(4):
                            pg = self.ps_get()
                            pu = self.ps_get()
                            for (pp, w_) in ((pg, wg[i]), (pu, wu[i])):
                                for k in range(8):
                                    fw.pe.op(lambda: nc.tensor.matmul(pp[:, 0:ntok], lhsT=w_[:, k, f * 128:(f + 1) * 128], rhs=h2T[:, k, c0:c0 + ntok],
                                                                      start=(k == 0), stop=(k == 7)), [w_.b, h2T.b], [pp.b], inc=(k == 7))
                            sg = sgl[f % 2]
                            fw.act.op(lambda: nc.scalar.activation(out=sg[:, 0:ntok], in_=pg[:, 0:ntok], func=AF.Silu), [pg.b], [sg.b])
                            fw.dve.op(lambda: nc.vector.tensor_tensor(out=hd[:, f, 0:ntok], in0=pu[:, 0:ntok], in1=sg[:, 0:ntok], op=ALU.mult),
                                      [pu.b, sg.b], [hd.b])
                            self.ps_put(pg)
                            self.ps_put(pu)
                        for tl in range(ntl):
                            ti = tl0 + tl
                            for half in range(2):
                                pd = self.ps_get()
                                for f in range(4):
                                    fw.pe.op(lambda: nc.tensor.matmul(pd[:], lhsT=hd[:, f, tl * 128:(tl + 1) * 128],
                                                                      rhs=wd[cls][i][:, f, half * 512:(half + 1) * 512], start=(f == 0), stop=(f == 3)),
                                             [hd.b, wd[cls][i].b], [pd.b], inc=(f == 3))
                                fw.dve.op(lambda: nc.vector.scalar_tensor_tensor(out=xg[:, ti, half * 512:(half + 1) * 512], in0=pd[:],
                                                                                 scalar=Wt[:, ti, e:e + 1], in1=xg[:, ti, half * 512:(half + 1) * 512],
                                                                                 op0=ALU.mult, op1=ALU.add), [pd.b, Wt.b, xg.b], [xg.b])
                                self.ps_put(pd)
                for ti, t in enumerate(tiles):
                    if l == 0:
                        fw.q_pool.dma(self.xres[t * 128:(t + 1) * 128, :], xg[:, ti, :], reads=[xg.b], writes=[self.xres_b[t]])
                    else:
                        fw.q_pool.dma(self.y_out[(t - 2) * 128:(t - 1) * 128, :], xg[:, ti, :], reads=[xg.b], writes=[self.y_b[t]])
                    if self.debug and t in (0, 2, NT - 1):
                        nm = f"x2_{l}_{t}"
                        ap = self.dbg(nm, [128, D])
                        fw.q_pool.dma(ap, xg[:, ti, :], reads=[xg.b], writes=[self.dbg_out[nm][1]])
            fw.barrier()

    def moe2(self, l):
        nc, fw, g = self.nc, self.fw, self.g
        V = nc.vector
        tiles = list(range(NT)) if l == 0 else list(range(2, NT))
        ntl = len(tiles)
        NA = 2 * ntl
        NB = (2 * ntl * 128 + 32 * 127 + 127) // 128
        classes = [0, 1] if l == 0 else [0]
        hs = nc.dram_tensor(f"hs{l}", [NB * 128, D], BF16, kind="Internal").ap()
        ys = nc.dram_tensor(f"ys{l}", [NB * 128, D], F32, kind="Internal").ap()
        blkE_d = nc.dram_tensor(f"blkE{l}", [1, NB], I32, kind="Internal").ap()
        hs_bs = [Buf() for _ in range(NA)]
        ys_bs = [Buf() for _ in range(NB)]
        blkE_b = Buf()
        dd = lambda fn, rd, wr_: fw.dve.op(fn, [x.b for x in rd], [x.b for x in wr_])
        with ExitStack() as pm:
            DESTi = self.tile(pm, "DESTi", [128, NA], I32)
            W12 = self.tile(pm, "W12", [128, NA], F32)
            WIDX = self.tile(pm, "WIDX", [128, NB], I32)
            G2B = {}
            for cls in classes:
                G2B[cls] = self.tile(pm, f"G2Bm{cls}", [128, D], F32)
            with ExitStack() as ph:
                tmpb = self.tile(ph, "bct3", [128, 128], F32)
                A2B, B2B = {}, {}
                for cls in classes:
                    A2B[cls] = self.tile(ph, f"A2B{cls}", [128, D], F32)
                    B2B[cls] = self.tile(ph, f"B2B{cls}", [128, D], F32)
                    self.bcast_tile(G2B[cls], lambda c: g["modT"][:, l, 40 + c, cls:cls + 1], tmpb)
                    self.bcast_tile(A2B[cls], lambda c: g["A2"][:, l, c, cls:cls + 1], tmpb, extra=[g["A2"].b])
                    self.bcast_tile(B2B[cls], lambda c: g["modT"][:, l, 24 + c, cls:cls + 1], tmpb)
                wr = self.tile(ph, "wr", [128, 8, 36], F32)
                rb = self.tile(ph, "rb", [128, 36], F32)
                mc = self.tile(ph, "mc", [128, 433], F32)
                Uf = self.tile(ph, "Uf", [128, 128], F32)
                Ub = self.tile(ph, "Ub", [128, 128], BF16)
                onesb = self.tile(ph, "onesb", [128, 128], BF16)
                fw.q_sync.dma(wr[:], self.rt_w[l].rearrange("(k p) n -> p k n", p=128), writes=[wr.b])
                fw.q_sync.dma(rb[:], self.rt_b[:, l, :], writes=[rb.b])
                fw.q_sync.dma(mc[:], self.mconst, writes=[mc.b])
                fw.q_sync.dma(Uf[:], self.umat, writes=[Uf.b])
                dd(lambda: V.tensor_copy(out=Ub[:], in_=Uf[:]), [Uf], [Ub])
                dd(lambda: V.memset(onesb[:], 1.0), [], [onesb])
                zt = self.tile(ph, "zt", [128, 4096], BF16)
                fw.pool.op(lambda: nc.gpsimd.memset(zt[:], 0.0), [], [zt.b])
                hz_b = []
                for r0 in range(0, NB * 128, 512):
                    nr = min(512, NB * 128 - r0)
                    hb_ = Buf("hz")
                    fw.q_sync.dma(hs[r0:r0 + nr, :].rearrange("(p k) d -> p (k d)", p=128), zt[:, 0:(nr // 128) * D], reads=[zt.b], writes=[hb_])
                    hz_b.append(hb_)
                h2tm = self.tile(ph, "h2tm", [128, ntl, D], BF16)
                OH = self.tile(ph, "OH", [128, NA, 32], BF16)
                RK = self.tile(ph, "RK", [128, NA], F32)
                run = self.tile(ph, "run", [128, 32], F32)
                dd(lambda: V.memset(run[:], 0.0), [], [run])
                xt = [self.tile(ph, f"mxt{i}", [128, D], F32) for i in range(2)]
                xn_l = [self.tile(ph, f"mxn{i}", [128, D], F32) for i in range(2)]
                xm_l = [self.tile(ph, f"mxm{i}", [128, D], F32) for i in range(2)]
                hTf_l = [self.tile(ph, f"mhTf{i}", [128, 8, 128], F32) for i in range(2)]
                ss_l = [self.tile(ph, f"mss{i}", [128, 1], F32) for i in range(2)]
                r_l = [{k: self.tile(ph, f"r{i}_" + k, [128, n], F32) for k, n in
                        dict(lg=36, gmax=1, ngmax=1, goh=4, ge=4, gsum=1, pen=4, em=32, m8=8, d=1, ed=1, p1=1, p2=1, t1=32, t2=32, rf=32).items()}
                       for i in range(2)]
                def tile_fn(ti, t):
                    cls = 1 if t < 2 else 0
                    xn, xm, hTf, ss, r_ = xn_l[ti % 2], xm_l[ti % 2], hTf_l[ti % 2], ss_l[ti % 2], r_l[ti % 2]
                    x_ = xt[ti % 2]
                    fw.q_sync.dma(x_[:], self.xres[t * 128:(t + 1) * 128, :], reads=[self.xres_b[t]], writes=[x_.b])
                    fw.act.op(lambda: nc.scalar.activation(out=xn[:], in_=x_[:], func=AF.Square, accum_out=ss[:, 0:1]), [x_.b], [xn.b, ss.b])
                    self.rstd_from_ss(ss, 1.0 / D, 1)
                    fw.act.op(lambda: nc.scalar.activation(out=xn[:], in_=x_[:], func=AF.Copy, scale=ss[:, 0:1]), [x_.b, ss.b], [xn.b])
                    fw.pool.op(lambda: nc.gpsimd.tensor_tensor(out=xm[:], in0=xn[:], in1=A2B[cls][:], op=ALU.mult), [xn.b, A2B[cls].b], [xm.b])
                    fw.pool.op(lambda: nc.gpsimd.tensor_tensor(out=h2tm[:, ti, :], in0=xm[:], in1=B2B[cls][:], op=ALU.add), [xm.b, B2B[cls].b], [h2tm.b])
                    for hb in range(2):
                        p = self.ps_get()
                        pv = p[:].rearrange("p (c q) -> p c q", c=4)
                        for c4 in range(4):
                            c = hb * 4 + c4
                            fw.pe.op(lambda: nc.tensor.transpose(out=pv[:, c4, :], in_=xn[:, c * 128:(c + 1) * 128], identity=g["identf"][:]),
                                     [xn.b, g["identf"].b], [p.b], inc=(c4 == 3))
                        for c4 in range(4):
                            c = hb * 4 + c4
                            if hb == 0:
                                fw.dve.op(lambda: V.tensor_scalar(out=hTf[:, c, :], in0=pv[:, c4, :], scalar1=g["A2"][:, l, c, cls:cls + 1],
                                                                  scalar2=g["modT"][:, l, 24 + c, cls:cls + 1], op0=ALU.mult, op1=ALU.add),
                                          [p.b, g["A2"].b, g["modT"].b], [hTf.b])
                            else:
                                fw.act.op(lambda: nc.scalar.activation(out=hTf[:, c, :], in_=pv[:, c4, :], func=AF.Identity,
                                                                       scale=g["A2"][:, l, c, cls:cls + 1], bias=g["modT"][:, l, 24 + c, cls:cls + 1]),
                                          [p.b, g["A2"].b, g["modT"].b], [hTf.b])
                        self.ps_put(p)
                    pr = self.ps_get()
                    for c in range(8):
                        fw.pe.op(lambda: nc.tensor.matmul(pr[:, 0:36], lhsT=hTf[:, c, :], rhs=wr[:, c, :], start=(c == 0), stop=(c == 7)),
                                 [hTf.b, wr.b], [pr.b], inc=(c == 7))
                    self.route(pr, rb, r_, None, ti, OH=OH, W12=W12)
                    self.ps_put(pr)

                def rank_fn(ti):
                    r_ = r_l[ti % 2]
                    for k in range(2):
                        a = 2 * ti + k
                        pk = self.ps_get()
                        fw.pe.op(lambda: nc.tensor.matmul(pk[:, 0:32], lhsT=Ub[:], rhs=OH[:, a, :], start=True, stop=True), [Ub.b, OH.b], [pk.b], inc=False)
                        fw.pe.op(lambda: nc.tensor.matmul(pk[:, 32:64], lhsT=onesb[:], rhs=OH[:, a, :], start=True, stop=True), [onesb.b, OH.b], [pk.b])
                        rf = r_["rf"]
                        dd(lambda: V.tensor_tensor(out=rf[:], in0=pk[:, 0:32], in1=run[:], op=ALU.add), [pk, run], [rf])
                        dd(lambda: V.tensor_tensor(out=rf[:], in0=rf[:], in1=OH[:, a, :], op=ALU.mult), [rf, OH], [rf])
                        dd(lambda: V.tensor_reduce(out=RK[:, a:a + 1], in_=rf[:], axis=AX.X, op=ALU.add), [rf], [RK])
                        dd(lambda: V.tensor_tensor(out=run[:], in0=pk[:, 32:64], in1=run[:], op=ALU.add), [pk, run], [run])
                        self.ps_put(pk)
                for ti0 in range(0, ntl, 2):
                    pair = list(range(ti0, min(ti0 + 2, ntl)))
                    ILV.run([(lambda ti=ti: tile_fn(ti, tiles[ti])) for ti in pair])
                    for ti in pair:
                        rank_fn(ti)
                cmpf = self.tile(ph, "cmp", [128, 5120], F32)
                v3 = lambda n_a, n_b: cmpf[:, 0:n_a * n_b].rearrange("p (a b) -> p a b", b=n_b)
                T_ = lambda nm, n: self.tile(ph, nm, [128, n], F32)
                nblk, exc, nbp, mm, pbx = T_("nblk", 32), T_("exc", 32), T_("nbp", 32), T_("mm", 32), T_("pbx", 32)
                sc = [T_(f"scan{i}", 32) for i in range(2)]
                blkf, dstf = T_("blkf", NB), T_("dstf", NA)
                selX, selM, selS, jf, ta_, tb_ = T_("selX", NA), T_("selM", NA), T_("selS", NA), T_("jf", NA), T_("ta_", NA), T_("tb_", NA)
                pend, pbase, m2k, lsk = T_("pend", 16), T_("pbase", 16), T_("m2k", 16), T_("lsk", 16)
                kidx, pbp, m2p, lsp, o_, q_, par_, int_ = (T_(n_, NB) for n_ in ("kidx", "pbp", "m2p", "lsp", "o_", "q_", "par_", "int_"))
                IOTA32, THR, IOTAB, PAR32, PARB, THR2, THR3, IOTA16 = (mc[:, 0:32], mc[:, 32:67], mc[:, 67:67 + NB], mc[:, 167:199],
                                                                     mc[:, 199:199 + NB], mc[:, 299:367], mc[:, 367:417], mc[:, 417:433])
                c3 = v3(32, 35)
                dd(lambda: V.tensor_tensor(out=c3, in0=run[:].unsqueeze(2).to_broadcast([128, 32, 35]),
                                           in1=THR.unsqueeze(1).to_broadcast([128, 32, 35]), op=ALU.is_gt), [run, mc], [cmpf])
                dd(lambda: V.tensor_reduce(out=nblk[:], in_=c3, axis=AX.X, op=ALU.add), [cmpf], [nblk])
                dd(lambda: V.tensor_copy(out=sc[0][:], in_=nblk[:]), [nblk], [sc[0]])
                cur = 0
                for sh in (1, 2, 4, 8, 16):
                    a_, b_ = sc[cur], sc[1 - cur]
                    dd(lambda: V.tensor_copy(out=b_[:, 0:sh], in_=a_[:, 0:sh]), [a_], [b_])
                    dd(lambda: V.tensor_tensor(out=b_[:, sh:32], in0=a_[:, sh:32], in1=a_[:, 0:32 - sh], op=ALU.add), [a_], [b_])
                    cur = 1 - cur
                inc_ = sc[cur]
                dd(lambda: V.tensor_tensor(out=exc[:], in0=inc_[:], in1=nblk[:], op=ALU.subtract), [inc_, nblk], [exc])
                pr2 = lambda tl: tl[:].rearrange("p (k s) -> p k s", s=2)
                dd(lambda: V.tensor_copy(out=pr2(nbp)[:, :, 0:1], in_=pr2(nblk)[:, :, 1:2]), [nblk], [nbp])
                dd(lambda: V.tensor_copy(out=pr2(nbp)[:, :, 1:2], in_=pr2(nblk)[:, :, 0:1]), [nblk], [nbp])
                dd(lambda: V.tensor_tensor(out=mm[:], in0=nblk[:], in1=nbp[:], op=ALU.min), [nblk, nbp], [mm])
                dd(lambda: V.tensor_scalar(out=pr2(pbx)[:, :, 0:1], in0=pr2(exc)[:, :, 0:1], scalar1=128.0, scalar2=None, op0=ALU.mult), [exc], [pbx])
                dd(lambda: V.tensor_scalar(out=pr2(pbx)[:, :, 1:2], in0=pr2(exc)[:, :, 0:1], scalar1=128.0, scalar2=None, op0=ALU.mult), [exc], [pbx])
                c4_ = v3(NA, 32)
                for (dst_, vec_, vb_) in ((selX, pbx[:], pbx.b), (selM, mm[:], mm.b), (selS, PAR32, mc.b)):
                    dd(lambda: V.tensor_tensor(out=c4_, in0=OH[:], in1=vec_.unsqueeze(1).to_broadcast([128, NA, 32]), op=ALU.mult), [OH, Tile(None, vb_)], [cmpf])
                    dd(lambda: V.tensor_reduce(out=dst_[:], in_=c4_, axis=AX.X, op=ALU.add), [cmpf], [dst_])
                c5_ = v3(NA, 68)
                dd(lambda: V.tensor_tensor(out=c5_, in0=RK[:].unsqueeze(2).to_broadcast([128, NA, 68]),
                                           in1=THR2.unsqueeze(1).to_broadcast([128, NA, 68]), op=ALU.is_ge), [RK, mc], [cmpf])
                dd(lambda: V.tensor_reduce(out=jf[:], in_=c5_, axis=AX.X, op=ALU.add), [cmpf], [jf])
                dd(lambda: V.scalar_tensor_tensor(out=ta_[:], in0=jf[:], scalar=2.0, in1=selS[:], op0=ALU.mult, op1=ALU.add), [jf, selS], [ta_])
                dd(lambda: V.tensor_tensor(out=tb_[:], in0=selM[:], in1=jf[:], op=ALU.add), [selM, jf], [tb_])
                dd(lambda: V.tensor_tensor(out=ta_[:], in0=ta_[:], in1=tb_[:], op=ALU.min), [ta_, tb_], [ta_])
                dd(lambda: V.tensor_tensor(out=ta_[:], in0=ta_[:], in1=jf[:], op=ALU.subtract), [ta_, jf], [ta_])
                dd(lambda: V.scalar_tensor_tensor(out=dstf[:], in0=ta_[:], scalar=128.0, in1=selX[:], op0=ALU.mult, op1=ALU.add), [ta_, selX], [dstf])
                dd(lambda: V.tensor_tensor(out=dstf[:], in0=dstf[:], in1=RK[:], op=ALU.add), [dstf, RK], [dstf])
                dd(lambda: V.tensor_copy(out=DESTi[:], in_=dstf[:]), [dstf], [DESTi])
                dd(lambda: V.tensor_copy(out=pend[:], in_=pr2(inc_)[:, :, 1]), [inc_], [pend])
                dd(lambda: V.tensor_copy(out=pbase[:], in_=pr2(exc)[:, :, 0]), [exc], [pbase])
                dd(lambda: V.tensor_scalar(out=m2k[:], in0=pr2(mm)[:, :, 0], scalar1=2.0, scalar2=None, op0=ALU.mult), [mm], [m2k])
                dd(lambda: V.tensor_tensor(out=lsk[:], in0=pr2(nblk)[:, :, 1], in1=pr2(nblk)[:, :, 0], op=ALU.is_gt), [nblk], [lsk])
                c6_ = v3(NB, 16)
                dd(lambda: V.tensor_tensor(out=c6_, in0=pend[:].unsqueeze(1).to_broadcast([128, NB, 16]),
                                           in1=IOTAB.unsqueeze(2).to_broadcast([128, NB, 16]), op=ALU.is_le), [pend, mc], [cmpf])
                dd(lambda: V.tensor_reduce(out=kidx[:], in_=c6_, axis=AX.X, op=ALU.add), [cmpf], [kidx])
                dd(lambda: V.tensor_scalar(out=kidx[:], in0=kidx[:], scalar1=15.0, scalar2=None, op0=ALU.min), [kidx], [kidx])
                ohk = self.tile(ph, "ohk", [128, NB, 16], F32)
                dd(lambda: V.tensor_tensor(out=ohk[:], in0=kidx[:].unsqueeze(2).to_broadcast([128, NB, 16]),
                                           in1=IOTA16.unsqueeze(1).to_broadcast([128, NB, 16]), op=ALU.is_equal), [kidx, mc], [ohk])
                for (dst_, vec_) in ((pbp, pbase), (m2p, m2k), (lsp, lsk)):
                    dd(lambda: V.tensor_tensor(out=c6_, in0=ohk[:], in1=vec_[:].unsqueeze(1).to_broadcast([128, NB, 16]), op=ALU.mult), [ohk, vec_], [cmpf])
                    dd(lambda: V.tensor_reduce(out=dst_[:], in_=c6_, axis=AX.X, op=ALU.add), [cmpf], [dst_])
                dd(lambda: V.tensor_tensor(out=o_[:], in0=IOTAB, in1=pbp[:], op=ALU.subtract), [mc, pbp], [o_])
                c7_ = v3(NB, 50)
                dd(lambda: V.tensor_tensor(out=c7_, in0=o_[:].unsqueeze(2).to_broadcast([128, NB, 50]),
                                           in1=THR3.unsqueeze(1).to_broadcast([128, NB, 50]), op=ALU.is_ge), [o_, mc], [cmpf])
                dd(lambda: V.tensor_reduce(out=q_[:], in_=c7_, axis=AX.X, op=ALU.add), [cmpf], [q_])
                dd(lambda: V.scalar_tensor_tensor(out=par_[:], in0=q_[:], scalar=-2.0, in1=o_[:], op0=ALU.mult, op1=ALU.add), [q_, o_], [par_])
                dd(lambda: V.tensor_tensor(out=int_[:], in0=o_[:], in1=m2p[:], op=ALU.is_lt), [o_, m2p], [int_])
                dd(lambda: V.tensor_tensor(out=par_[:], in0=par_[:], in1=lsp[:], op=ALU.subtract), [par_, lsp], [par_])
                dd(lambda: V.tensor_tensor(out=par_[:], in0=par_[:], in1=int_[:], op=ALU.mult), [par_, int_], [par_])
                dd(lambda: V.tensor_tensor(out=par_[:], in0=par_[:], in1=lsp[:], op=ALU.add), [par_, lsp], [par_])
                dd(lambda: V.scalar_tensor_tensor(out=blkf[:], in0=kidx[:], scalar=2.0, in1=par_[:], op0=ALU.mult, op1=ALU.add), [kidx, par_], [blkf])
                pix = self.tile(ph, "pix", [128, 1], F32)
                fw.q_sync.dma(pix[:], self.pidx, writes=[pix.b])
                dd(lambda: V.tensor_scalar(out=blkf[:], in0=blkf[:], scalar1=128.0, scalar2=pix[:, 0:1], op0=ALU.mult, op1=ALU.add), [blkf, pix], [blkf])
                if l == 1:
                    dd(lambda: V.tensor_scalar(out=blkf[:], in0=blkf[:], scalar1=4096.0, scalar2=None, op0=ALU.add), [blkf], [blkf])
                same2 = self.tile(ph, "same2", [128, NB], F32)
                dd(lambda: V.tensor_tensor(out=same2[:, 2:NB], in0=blkf[:, 2:NB], in1=blkf[:, 0:NB - 2], op=ALU.is_equal), [blkf], [same2])
                dd(lambda: V.tensor_scalar(out=same2[:, 2:NB], in0=same2[:, 2:NB], scalar1=1.0e6, scalar2=None, op0=ALU.mult), [same2], [same2])
                dd(lambda: V.tensor_tensor(out=blkf[:, 2:NB], in0=blkf[:, 2:NB], in1=same2[:, 2:NB], op=ALU.add), [blkf, same2], [blkf])
                dd(lambda: V.tensor_copy(out=WIDX[:], in_=blkf[:]), [blkf], [WIDX])
                if self.debug:
                    self.dbg_dump_bf(ph, f"dst{l}", dstf[:, 0:8], [128, 8], dstf.b)
                    self.dbg_dump_bf(ph, f"blk{l}", blkf[:, 0:NB], [128, NB], blkf.b)
                    self.dbg_dump_bf(ph, f"cnt{l}", run[:, 0:32], [128, 32], run.b)
                if os.environ.get("MOE_PROBE") == "1":
                    def tryv(nm, f):
                        try:
                            f(); print("PROBE ok", nm, flush=True)
                        except Exception as e:
                            print("PROBE fail", nm, repr(e)[:120], flush=True)
                    tryv("base", lambda: nc.gpsimd.indirect_dma_start(out=hs[:, :], out_offset=bass.IndirectOffsetOnAxis(ap=DESTi[:, 0:1], axis=0),
                                                                      in_=h2tm[:, 0, :], in_offset=None, bounds_check=NB * 128 - 1, oob_is_err=False))
                    tryv("xn_f32_ys", lambda: nc.gpsimd.indirect_dma_start(out=ys[:, :], out_offset=bass.IndirectOffsetOnAxis(ap=DESTi[:, 0:1], axis=0),
                                                                      in_=xn[:, :], in_offset=None, bounds_check=NB * 128 - 1, oob_is_err=False))
                    tryv("blki_idx", lambda: nc.gpsimd.indirect_dma_start(out=hs[:, :], out_offset=bass.IndirectOffsetOnAxis(ap=blki[:, 0:1], axis=0),
                                                                      in_=h2tm[:, 0, :], in_offset=None, bounds_check=NB * 128 - 1, oob_is_err=False))
                    tryv("gather", lambda: nc.gpsimd.indirect_dma_start(out=xn[:, :], out_offset=None, in_=ys[:, :],
                                                                      in_offset=bass.IndirectOffsetOnAxis(ap=DESTi[:, 0:1], axis=0), bounds_check=NB * 128 - 1, oob_is_err=False))
                    tryv("plain", lambda: nc.gpsimd.dma_start(out=ys[0:128, :], in_=xn[:, :]))
                for a in range(NA):
                    if os.environ.get("MOE_PROBE") == "1":
                        print("PROBE scatter a", a, flush=True)
                    fw.q_pool.dma_fn(lambda: nc.gpsimd.indirect_dma_start(
                        out=hs[:, :], out_offset=bass.IndirectOffsetOnAxis(ap=DESTi[:, a:a + 1], axis=0),
                        in_=h2tm[:, a // 2, :], in_offset=None),
                        reads=[h2tm.b, DESTi.b] + hz_b, writes=[hs_bs[a]])
                fw.barrier()
            with ExitStack() as ph:
                stg = {k: [self.tile(ph, f"bs{k}{i}", [128, 4096], F32) for i in range(2)] for k in "gud"}
                wgt = {k: [self.tile(ph, f"bw{k}{i}", [128, 4096], BF16) for i in range(2)] for k in "gud"}
                xb = [self.tile(ph, f"xb{i}", [128, D], BF16) for i in range(4)]
                xbT = [self.tile(ph, f"xbT{i}", [128, 8, 128], BF16) for i in range(2)]
                sg_l = [self.tile(ph, f"bsg{i}", [128, 512], F32) for i in range(2)]
                hid_l = [self.tile(ph, f"bhid{i}", [128, 512], BF16) for i in range(2)]
                hidT_l = [self.tile(ph, f"bhidT{i}", [128, 4, 128], BF16) for i in range(2)]
                ysb = [self.tile(ph, f"ysb{i}", [128, D], F32) for i in range(2)]
                srcs = dict(g=self.ex_gate, u=self.ex_up, d=self.ex_down)

                def gathers(i):
                    b2 = i % 2
                    for k in "gud":
                        fw.q_pool.dma_fn(lambda: nc.gpsimd.indirect_dma_start(
                            out=stg[k][b2][:, :], out_offset=None, in_=srcs[k].rearrange("l r n -> (l r) n"),
                            in_offset=bass.IndirectOffsetOnAxis(ap=WIDX[:, i:i + 1], axis=0),
                            bounds_check=self.bc_reg, oob_is_err=False),
                            reads=[WIDX.b], writes=[stg[k][b2].b])

                def casts(i):
                    b2 = i % 2
                    fw.dve.op(lambda: V.tensor_copy(out=wgt["g"][b2][:], in_=stg["g"][b2][:]), [stg["g"][b2].b], [wgt["g"][b2].b])
                    fw.act.op(lambda: nc.scalar.copy(out=wgt["u"][b2][:], in_=stg["u"][b2][:]), [stg["u"][b2].b], [wgt["u"][b2].b])
                    fw.dve.op(lambda: V.tensor_copy(out=wgt["d"][b2][:, 0:2048], in_=stg["d"][b2][:, 0:2048]), [stg["d"][b2].b], [wgt["d"][b2].b])
                    fw.act.op(lambda: nc.scalar.copy(out=wgt["d"][b2][:, 2048:4096], in_=stg["d"][b2][:, 2048:4096]), [stg["d"][b2].b], [wgt["d"][b2].b])

                def xload(i):
                    fw.q_sync.dma(xb[i % 4][:], hs[i * 128:(i + 1) * 128, :], reads=hs_bs, writes=[xb[i % 4].b])

                def block_fn(i):
                    b2 = i % 2
                    sg, hid, hidT = sg_l[b2], hid_l[b2], hidT_l[b2]
                    wg_ = wgt["g"][b2][:].rearrange("p (k n) -> p k n", k=8)
                    wu_ = wgt["u"][b2][:].rearrange("p (k n) -> p k n", k=8)
                    wd_ = wgt["d"][b2][:].rearrange("p (k n) -> p k n", k=4)
                    p = self.ps_get()
                    pv = p[:].bitcast(BF16).rearrange("p (c q) -> p c q", c=8)
                    for c in range(8):
                        fw.pe.op(lambda: nc.tensor.transpose(out=pv[:, c, :], in_=xb[i % 4][:, c * 128:(c + 1) * 128], identity=g["identb"][:]),
                                 [xb[i % 4].b, g["identb"].b], [p.b], inc=(c == 7))
                    dd(lambda: V.tensor_copy(out=xbT[b2][:], in_=pv), [p], [xbT[b2]])
                    self.ps_put(p)
                    pg = self.ps_get()
                    pu = self.ps_get()
                    for (pp, w_, wb_) in ((pg, wg_, wgt["g"][b2].b), (pu, wu_, wgt["u"][b2].b)):
                        for k in range(8):
                            fw.pe.op(lambda: nc.tensor.matmul(pp[:], lhsT=xbT[b2][:, k, :], rhs=w_[:, k, :], start=(k == 0), stop=(k == 7)),
                                     [xbT[b2].b, wb_], [pp.b], inc=(k == 7))
                    fw.act.op(lambda: nc.scalar.activation(out=sg[:], in_=pg[:], func=AF.Silu), [pg.b], [sg.b])
                    dd(lambda: V.tensor_tensor(out=hid[:], in0=pu[:], in1=sg[:], op=ALU.mult), [pu, sg], [hid])
                    self.ps_put(pg)
                    self.ps_put(pu)
                    p = self.ps_get()
                    pv = p[:].bitcast(BF16).rearrange("p (c q) -> p c q", c=8)
                    for c in range(4):
                        fw.pe.op(lambda: nc.tensor.transpose(out=pv[:, c, :], in_=hid[:, c * 128:(c + 1) * 128], identity=g["identb"][:]),
                                 [hid.b, g["identb"].b], [p.b], inc=(c == 3))
                    dd(lambda: V.tensor_copy(out=hidT[:], in_=pv[:, 0:4, :]), [p], [hidT])
                    self.ps_put(p)
                    y_ = ysb[b2]
                    pds = []
                    for half in range(2):
                        pd = self.ps_get()
                        pds.append(pd)
                        for f in range(4):
                            fw.pe.op(lambda: nc.tensor.matmul(pd[:], lhsT=hidT[:, f, :], rhs=wd_[:, f, half * 512:(half + 1) * 512],
                                                              start=(f == 0), stop=(f == 3)), [hidT.b, wgt["d"][b2].b], [pd.b], inc=(f == 3))
                    if i + 2 < NB:
                        casts(i + 2)
                    fw.act.op(lambda: nc.scalar.copy(out=y_[:, 0:512], in_=pds[0][:]), [pds[0].b], [y_.b])
                    dd(lambda: V.tensor_copy(out=y_[:, 512:1024], in_=pds[1][:]), [pds[1]], [y_])
                    self.ps_put(pds[0])
                    self.ps_put(pds[1])
                    fw.q_sync.dma(ys[i * 128:(i + 1) * 128, :], y_[:], reads=[y_.b], writes=[ys_bs[i]])

                gathers(0)
                gathers(1)
                for j in range(2):
                    xload(j)
                casts(0)
                casts(1)
                for i0_ in range(0, NB, 2):
                    for j in (i0_ + 2, i0_ + 3):
                        if j < NB:
                            gathers(j)
                            xload(j)
                    ILV.run([(lambda i=i: block_fn(i)) for i in (i0_, i0_ + 1) if i < NB])
                fw.barrier()
            with ExitStack() as ph:
                xt = [self.tile(ph, f"cxt{i}", [128, D], F32) for i in range(3)]
                y1 = [self.tile(ph, f"cy1{i}", [128, D], F32) for i in range(3)]
                y2 = [self.tile(ph, f"cy2{i}", [128, D], F32) for i in range(3)]
                def comb_fn(ti, t):
                    cls = 1 if t < 2 else 0
                    x_, a1, a2 = xt[ti % 3], y1[ti % 3], y2[ti % 3]
                    fw.q_sync.dma(x_[:], self.xres[t * 128:(t + 1) * 128, :], reads=[self.xres_b[t]], writes=[x_.b])
                    for k, yy in ((0, a1), (1, a2)):
                        a = 2 * ti + k
                        fw.q_pool.dma_fn(lambda: nc.gpsimd.indirect_dma_start(
                            out=yy[:, :], out_offset=None, in_=ys[:, :],
                            in_offset=bass.IndirectOffsetOnAxis(ap=DESTi[:, a:a + 1], axis=0)),
                            reads=ys_bs + [DESTi.b], writes=[yy.b])
                    dd(lambda: V.tensor_scalar(out=a1[:], in0=a1[:], scalar1=W12[:, 2 * ti:2 * ti + 1], scalar2=None, op0=ALU.mult), [a1, W12], [a1])
                    dd(lambda: V.scalar_tensor_tensor(out=a1[:], in0=a2[:], scalar=W12[:, 2 * ti + 1:2 * ti + 2], in1=a1[:], op0=ALU.mult, op1=ALU.add),
                       [a2, W12, a1], [a1])
                    dd(lambda: V.tensor_tensor(out=a1[:], in0=a1[:], in1=G2B[cls][:], op=ALU.mult), [a1, G2B[cls]], [a1])
                    dd(lambda: V.tensor_tensor(out=x_[:], in0=x_[:], in1=a1[:], op=ALU.add), [x_, a1], [x_])
                    if l == 0:
                        fw.q_sync.dma(self.xres[t * 128:(t + 1) * 128, :], x_[:], reads=[x_.b], writes=[self.xres_b[t]])
                    else:
                        fw.q_sync.dma(self.y_out[(t - 2) * 128:(t - 1) * 128, :], x_[:], reads=[x_.b], writes=[self.y_b[t]])
                    if self.debug and t in (0, 2, NT - 1):
                        nm = f"x2_{l}_{t}"
                        ap = self.dbg(nm, [128, D])
                        fw.q_sync.dma(ap, x_[:], reads=[x_.b], writes=[self.dbg_out[nm][1]])

                for ti0 in range(0, ntl, 3):
                    pair = list(range(ti0, min(ti0 + 3, ntl)))
                    ILV.run([(lambda ti=ti: comb_fn(ti, tiles[ti])) for ti in pair])
                fw.barrier()

    def route(self, pr, rb, r_, Wt, ti, OH=None, W12=None):
        nc, fw = self.nc, self.fw
        V = nc.vector
        d = lambda fn, rd, wr_: fw.dve.op(fn, [x.b for x in rd], [x.b for x in wr_])
        lg, gmax, ngmax, goh, ge, gsum, pen, em, m8 = (r_[k] for k in ("lg", "gmax", "ngmax", "goh", "ge", "gsum", "pen", "em", "m8"))
        dd, ed, p1, p2, t1, t2 = (r_[k] for k in ("d", "ed", "p1", "p2", "t1", "t2"))
        d(lambda: V.tensor_tensor(out=lg[:], in0=pr[:, 0:36], in1=rb[:], op=ALU.add), [pr, rb], [lg])
        d(lambda: V.tensor_reduce(out=gmax[:], in_=lg[:, 0:4], axis=AX.X, op=ALU.max), [lg], [gmax])
        d(lambda: V.tensor_scalar(out=ngmax[:], in0=gmax[:], scalar1=-1.0, scalar2=None, op0=ALU.mult), [gmax], [ngmax])
        d(lambda: V.tensor_scalar(out=pen[:], in0=lg[:, 0:4], scalar1=gmax[:, 0:1], scalar2=None, op0=ALU.is_ge), [lg, gmax], [pen])
        d(lambda: V.tensor_scalar(out=pen[:], in0=pen[:], scalar1=1e30, scalar2=-1e30, op0=ALU.mult, op1=ALU.add), [pen], [pen])
        fw.act.op(lambda: nc.scalar.activation(out=ge[:], in_=lg[:, 0:4], func=AF.Exp, bias=ngmax[:, 0:1], accum_out=gsum[:, 0:1]),
                  [lg.b, ngmax.b], [ge.b, gsum.b])
        d(lambda: V.tensor_tensor(out=em[:].rearrange("p (a b) -> p a b", a=4), in0=lg[:, 4:36].rearrange("p (a b) -> p a b", a=4),
                                  in1=pen[:].unsqueeze(2).to_broadcast([128, 4, 8]), op=ALU.add), [lg, pen], [em])
        d(lambda: V.max(out=m8[:], in_=em[:]), [em], [m8])
        d(lambda: V.tensor_tensor(out=dd[:], in0=m8[:, 1:2], in1=m8[:, 0:1], op=ALU.subtract), [m8], [dd])
        fw.act.op(lambda: nc.scalar.activation(out=ed[:], in_=dd[:], func=AF.Exp), [dd.b], [ed.b])
        d(lambda: V.tensor_scalar(out=p1[:], in0=ed[:], scalar1=1.0, scalar2=None, op0=ALU.add), [ed], [p1])
        d(lambda: V.tensor_tensor(out=p1[:], in0=p1[:], in1=gsum[:], op=ALU.mult), [p1, gsum], [p1])
        d(lambda: V.reciprocal(out=p1[:], in_=p1[:]), [p1], [p1])
        d(lambda: V.tensor_tensor(out=p2[:], in0=p1[:], in1=ed[:], op=ALU.mult), [p1, ed], [p2])
        if OH is not None:
            for k in range(2):
                a = 2 * ti + k
                d(lambda: V.tensor_scalar(out=OH[:, a, :], in0=em[:], scalar1=m8[:, k:k + 1], scalar2=None, op0=ALU.is_equal), [em, m8], [OH])
                pk_ = p1 if k == 0 else p2
                d(lambda: V.tensor_copy(out=W12[:, a:a + 1], in_=pk_[:]), [pk_], [W12])
            return
        d(lambda: V.tensor_scalar(out=t1[:], in0=em[:], scalar1=m8[:, 0:1], scalar2=p1[:, 0:1], op0=ALU.is_equal, op1=ALU.mult), [em, m8, p1], [t1])
        d(lambda: V.tensor_scalar(out=t2[:], in0=em[:], scalar1=m8[:, 1:2], scalar2=p2[:, 0:1], op0=ALU.is_equal, op1=ALU.mult), [em, m8, p2], [t2])
        d(lambda: V.tensor_tensor(out=Wt[:, ti, :], in0=t1[:], in1=t2[:], op=ALU.add), [t1, t2], [Wt])

    def layer1_mixer(self):
        nc, fw, g = self.nc, self.fw, self.g
        l = 1
        PADU = 16
        with ExitStack() as ph:
            oT = self.tile(ph, "oT1", [128, 4, T], BF16)
            uT = self.tile(ph, "uT", [128, 4, S + 2 * PADU], BF16)
            pattn = ExitStack()
            qT = self.tile(pattn, "qT1", [128, NT, 4, 128], BF16)
            kT = self.tile(pattn, "kT1", [128, 2, T], BF16)
            Vp = self.tile(pattn, "Vp1", [128, NT, 200], BF16)
            self.v_init(Vp)
            fw.pool.op(lambda: nc.gpsimd.memset(kT[:], 0.0), [], [kT.b])
            fw.pool.op(lambda: nc.gpsimd.memset(uT[:], 0.0), [], [uT.b])
            with ExitStack() as p1:
                win = self.tile(p1, "win1", [128, 8, 1280], BF16)
                gqk = self.tile(p1, "gqk1", [128, 640], F32)
                g["cos"] = self.tile(p1, "cos1", [128, 32, 32], F32)
                g["sin"] = self.tile(p1, "sin1", [128, 32, 32], F32)
                fw.q_sync.dma(gqk[:], self.gqk_c, writes=[gqk.b])
                fw.q_sync.dma(g["cos"][:], self.cosT, writes=[g["cos"].b])
                fw.q_sync.dma(g["sin"][:], self.sinT, writes=[g["sin"].b])
                fw.dve.op(lambda: nc.vector.tensor_scalar(out=gqk[:, 0:512], in0=gqk[:, 0:512], scalar1=0.125, scalar2=None, op0=ALU.mult),
                          [gqk.b], [gqk.b])
                with ExitStack() as pw:
                    stg = [self.tile(pw, f"wstg1{i}", [128, 4096], F32) for i in range(2)]
                    self.stg_i = 0
                    for n in range(3):
                        w_ = 512 if n < 2 else 256
                        self.load_cast_w(stg, win, slice(n * 512, n * 512 + w_), self.w_in_cd[:, n * 512:n * 512 + w_], 8, w_, n)
                    fw.barrier()
                ssl_ = [self.tile(p1, f"ss1{i}", [128, 1], F32) for i in range(2)]
                xnl_ = [self.tile(p1, f"xn1{i}", [128, D], BF16) for i in range(2)]
                scr_l = [dict(ssl=[ssl_[i]], xnl=[xnl_[i]], sq=self.tile(p1, f"sq1{i}", [128, 640], F32),
                              ssq=self.tile(p1, f"ssq1{i}", [128, 10], F32), qn=self.tile(p1, f"qn1{i}", [128, 640], F32),
                              qr=self.tile(p1, f"qr1{i}", [128, 640], BF16), rt=self.tile(p1, f"rt1{i}", [128, 2, 320], F32)) for i in range(2)]
                xt = [self.tile(p1, f"xt1{i}", [128, D], F32) for i in range(2)]
                hT = self.tile(p1, "hT1", [128, 8, 512], BF16)
                qkv = [self.tile(p1, f"qkv1{i}", [128, 768], F32) for i in range(2)]
                chunks = [(0, 2)] + [(2 + 4 * i, 4) for i in range(8)]
                for ci, (t0, ntl) in enumerate(chunks):
                    h = hT
                    ntok = ntl * 128
                    is_ctx = t0 < 2
                    cls = 1 if is_ctx else 0
                    def norm_fn(tl):
                        t = t0 + tl
                        x_ = xt[tl % 2]
                        fw.q_sync.dma(x_[:], self.xres[t * 128:(t + 1) * 128, :], reads=[self.xres_b[t]], writes=[x_.b])
                        self.norm_tile_to_hT(x_, h, tl * 128, l, 1, cls, scr_l[tl % 2])

                    def qkv_fn(tl):
                        t = t0 + tl
                        qk_ = qkv[tl % 2]
                        for (c0, w_) in ((0, 512), (512, 256)):
                            if is_ctx and c0 == 0:
                                continue
                            p = self.ps_get()
                            for k in range(8):
                                fw.pe.op(lambda: nc.tensor.matmul(p[:, 0:w_], lhsT=h[:, k, tl * 128:(tl + 1) * 128], rhs=win[:, k, c0:c0 + w_],
                                                                  start=(k == 0), stop=(k == 7)), [h.b, win.b], [p.b], inc=(k == 7))
                            if c0 == 0:
                                fw.act.op(lambda: nc.scalar.copy(out=qk_[:, 0:512].rearrange("p (j a d) -> p a j d", a=2, d=64),
                                                                 in_=p[:, 0:512].rearrange("p (a j d) -> p a j d", a=2, j=4)), [p.b], [qk_.b])
                            else:
                                fw.act.op(lambda: nc.scalar.copy(out=qk_[:, c0:c0 + w_], in_=p[:, 0:w_]), [p.b], [qk_.b])
                            self.ps_put(p)
                        self.qk_post(qk_, t, gqk, scr_l[tl % 2], qT, kT, not is_ctx, None if is_ctx else t - 2)
                        self.v_fill(qk_, t, Vp)

                    for tl0 in range(0, ntl, 2):
                        ILV.run([(lambda tl=tl: norm_fn(tl)) for tl in range(tl0, min(tl0 + 2, ntl))])
                    for tl0 in range(0, ntl, 2):
                        ILV.run([(lambda tl=tl: qkv_fn(tl)) for tl in range(tl0, min(tl0 + 2, ntl))])
                    if not is_ctx:
                        tok0 = (t0 - 2) * 128
                        for j in range(4):
                            pu = self.ps_get()
                            for k in range(8):
                                fw.pe.op(lambda: nc.tensor.matmul(pu[:, 0:ntok], lhsT=win[:, k, 768 + j * 128:768 + (j + 1) * 128], rhs=h[:, k, 0:ntok],
                                                                  start=(k == 0), stop=(k == 7)), [win.b, h.b], [pu.b], inc=(k == 7))
                            fw.dve.op(lambda: nc.vector.tensor_copy(out=uT[:, j, PADU + tok0:PADU + tok0 + ntok], in_=pu[:, 0:ntok]), [pu.b], [uT.b])
                            self.ps_put(pu)
                fw.barrier()
            if self.stop == "l1p1":
                pattn.close()
                return
            with ExitStack() as p2:
                wm = self.tile(p2, "wm", [128, 2, 512], BF16)
                sk = self.tile(p2, "sk", [128, 2, 512], F32)
                with ExitStack() as pw:
                    wmf = self.tile(pw, "wmf", [128, 2, 512], F32)
                    fw.q_sync.dma(wmf[:], self.wmask, writes=[wmf.b])
                    fw.dve.op(lambda: nc.vector.tensor_copy(out=wm[:], in_=wmf[:]), [wmf.b], [wm.b])
                    fw.q_sync.dma(sk[:], self.sink_rep, writes=[sk.b])
                    fw.act.op(lambda: nc.scalar.activation(out=sk[:], in_=sk[:], func=AF.Exp), [sk.b], [sk.b])
                    fw.barrier()
                scr = dict(pexp=[self.tile(p2, f"pexp1{i}", [128, 1024], BF16) for i in range(4)], pi=0,
                           rec=[self.tile(p2, f"rec1{i}", [128, 512], F32) for i in range(2)],
                           posb=[[self.tile(p2, f"posb1{i}{k}", [128, 512], F32) for k in range(2)] for i in range(2)])
                blocks = []
                for t in range(2, NT):
                    kts = [(0, None), (1, None)]
                    if t > 2:
                        kts.append((t - 1, 0))
                    kts.append((t, None))
                    if t < NT - 1:
                        kts.append((t + 1, 1))
                    for kv in range(2):
                        blocks.append((kv, t, kts))
                self.attention(blocks, qT, kT, Vp, oT, scr, masks=wm, sinkrow=sk)
                fw.barrier()
            pattn.close()
            if self.debug:
                self.dbg_dump_bf(ph, "oT1", oT[:, :, 256:384], [128, 4, 128], oT.b)
                self.dbg_dump_bf(ph, "oT1b", oT[:, :, 640:768], [128, 4, 128], oT.b)
            dT = self.tile(ph, "dT", [128, 4, T], BF16)
            with ExitStack() as p3:
                pwf = self.tile(p3, "pwf", [128, 4, 128], F32)
                pwb = self.tile(p3, "pwb", [128, 4, 128], BF16)
                psc = self.tile(p3, "psc", [128, 4], F32)
                pfx = self.tile(p3, "pfx", [128, 4, 32], F32)
                fw.q_sync.dma(pwf[:], self.pool_w.rearrange("g c d -> c g d"), writes=[pwf.b])
                fw.q_sync.dma(psc[:], self.pool_scT, writes=[psc.b])
                fw.q_sync.dma(pfx[:], self.poolfix, writes=[pfx.b])
                fw.dve.op(lambda: nc.vector.tensor_copy(out=pwb[:], in_=pwf[:]), [pwf.b], [pwb.b])
                ta = [self.tile(p3, f"pta{i}", [128, 528], F32) for i in range(2)]
                pp_ = [self.tile(p3, f"ppb{i}", [128, 512], BF16) for i in range(2)]
                cnt = 0
                for ci in range(8):
                    tok0 = ci * 512
                    for j, w in enumerate((2, 4, 8, 16)):
                        base = PADU + tok0 - w // 2
                        ln = 512 + w - 1
                        cur = uT[:, j, base:base + ln]
                        curb = uT.b
                        step = 1
                        k = 0
                        while step < w:
                            dst = ta[k % 2]
                            e_, ee = (fw.dve, nc.vector) if (cnt % 2 == 0) else (fw.pool, nc.gpsimd)
                            cnt += 1
                            cc, cb_ = cur, curb
                            e_.op(lambda: ee.tensor_tensor(out=dst[:, 0:ln - step], in0=cc[:, 0:ln - step], in1=cc[:, step:ln], op=ALU.add), [cb_], [dst.b])
                            ln -= step
                            step *= 2
                            cur, curb = dst[:, 0:ln], dst.b
                            k += 1
                        pb_ = pp_[j % 2]
                        uc = uT[:, j, PADU + tok0:PADU + tok0 + 512]
                        sdst = ta[k % 2]
                        fw.dve.op(lambda: nc.vector.scalar_tensor_tensor(out=pb_[:], in0=cur[:, 0:512], scalar=1.0 / w, in1=uc, op0=ALU.mult, op1=ALU.subtract),
                                  [curb, uT.b], [pb_.b])
                        for (cond, lo, fo) in ((ci == 0, 0, 0), (ci == 7, 496, 16)):
                            if cond:
                                fw.dve.op(lambda: nc.vector.tensor_tensor(out=sdst[:, 0:16], in0=cur[:, lo:lo + 16], in1=pfx[:, j, fo:fo + 16], op=ALU.mult),
                                          [curb, pfx.b], [sdst.b])
                                fw.dve.op(lambda: nc.vector.tensor_tensor(out=pb_[:, lo:lo + 16], in0=sdst[:, 0:16], in1=uc[:, lo:lo + 16], op=ALU.subtract),
                                          [sdst.b, uT.b], [pb_.b])
                        py = self.ps_get()
                        fw.pe.op(lambda: nc.tensor.matmul(py[:], lhsT=pwb[:, j, :], rhs=pb_[:], start=True, stop=True), [pwb.b, pb_.b], [py.b])
                        fw.act.op(lambda: nc.scalar.activation(out=dT[:, j, NCTX + tok0:NCTX + tok0 + 512], in_=py[:], func=AF.Copy, scale=psc[:, j:j + 1]),
                                  [py.b, psc.b], [dT.b])
                        self.ps_put(py)
                fw.barrier()
            if self.debug:
                self.dbg_dump_bf(ph, "dT", dT[:, :, 256:384], [128, 4, 128], dT.b)
                self.dbg_dump_bf(ph, "dTe", dT[:, :, T - 128:T], [128, 4, 128], dT.b)
            if self.stop == "l1p3":
                return
            with ExitStack() as p4:
                self.out_proj(1, lambda c: (oT, c) if c < 4 else (dT, c - 4), self.w_out_cd, list(range(2, NT)), p4)
                fw.barrier()


def _rope_tables():
    rows = S // 64
    row = np.repeat(np.arange(rows, dtype=np.float32), 64)
    col = np.tile(np.arange(64, dtype=np.float32), rows)
    inv = (10000.0 ** (-np.arange(16, dtype=np.float32) / 16)).astype(np.float32)
    ang = np.concatenate([row[:, None] * inv, col[:, None] * inv], axis=-1).astype(np.float32)
    return np.cos(ang).astype(np.float32), np.sin(ang).astype(np.float32)


def _fm(v, chunks):
    return np.ascontiguousarray(np.asarray(v, np.float32).reshape(chunks, 128).T)


def make_in_maps(inp, cores):
    f = lambda a: np.ascontiguousarray(np.asarray(a, dtype=np.float32))
    cos, sin = _rope_tables()
    cosT = np.ascontiguousarray(cos.reshape(32, 128, 32).transpose(1, 0, 2))
    sinT = np.ascontiguousarray(sin.reshape(32, 128, 32).transpose(1, 0, 2))
    r = np.arange(128)
    mprev = (r[None, :] <= r[:, None]).astype(np.float32)
    mnext = (r[:, None] <= r[None, :]).astype(np.float32)
    wmask = np.stack([np.tile(mprev, (1, 4)), np.tile(mnext, (1, 4))], axis=1)
    shared = {
        "mod_w": f(inp["mod_w"]),
        "mod_bT": np.ascontiguousarray(f(inp["mod_b"]).reshape(2, 48, 128).transpose(2, 0, 1)),
        "ln1gT": np.ascontiguousarray(f(inp["ln1_g"]).reshape(2, 8, 128).transpose(2, 0, 1)),
        "ln2gT": np.ascontiguousarray(f(inp["ln2_g"]).reshape(2, 8, 128).transpose(2, 0, 1)),
        "w_in_ab": f(inp["w_in_ab"][0]), "w_out_ab": f(inp["w_out_ab"][0]),
        "gqk_a": np.ascontiguousarray(np.broadcast_to(np.concatenate([np.tile(f(inp["q_norm_a"][0]), 8), np.tile(f(inp["k_norm_a"][0]), 2)])[None, :], (128, 640))),
        "convwT": np.ascontiguousarray(f(inp["conv_w"][0]).reshape(31, 4, 128).transpose(2, 1, 0)),
        "convv": np.ascontiguousarray(np.stack([_fm(inp["conv_b"][0], 4), _fm(inp["conv_ln_g"][0], 4), _fm(inp["conv_ln_b"][0], 4)], axis=1)),
        "w_in_cd": f(inp["w_in_cd"][0]), "w_out_cd": f(inp["w_out_cd"][0]),
        "gqk_c": np.ascontiguousarray(np.broadcast_to(np.concatenate([np.tile(f(inp["q_norm_c"][0]), 8), np.tile(f(inp["k_norm_c"][0]), 2)])[None, :], (128, 640))),
        "sink_rep": np.ascontiguousarray(np.broadcast_to(np.repeat(f(inp["sink_c"][0]).reshape(2, 4), 128, axis=1)[None], (128, 2, 512))),
        "pool_w": f(inp["pool_w"][0]),
        "pool_scT": _fm(inp["pool_scale"][0], 4),
        "poolfix": _poolfix(),
        "rt_w": np.ascontiguousarray(np.concatenate([f(inp["rt_grp_w"]), f(inp["rt_exp_w"])], axis=2)),
        "rt_b": np.ascontiguousarray(np.broadcast_to(np.concatenate([f(inp["rt_grp_b"]), f(inp["rt_exp_b"])], axis=1)[None], (128, 2, 36))),
        "ex_gate": np.ascontiguousarray(f(inp["ex_gate"]).reshape(2, 32, 8, 128, 512).transpose(0, 1, 3, 2, 4).reshape(2, 4096, 4096)),
        "ex_up": np.ascontiguousarray(f(inp["ex_up"]).reshape(2, 32, 8, 128, 512).transpose(0, 1, 3, 2, 4).reshape(2, 4096, 4096)),
        "ex_down": np.ascontiguousarray(f(inp["ex_down"]).reshape(2, 32, 4, 128, 1024).transpose(0, 1, 3, 2, 4).reshape(2, 4096, 4096)),
        "pidx": np.arange(128, dtype=np.float32).reshape(128, 1),
        "mconst": np.ascontiguousarray(np.broadcast_to(np.concatenate([
            np.arange(32), 128.0 * np.arange(35), np.arange(100), np.arange(32) % 2, np.arange(100) % 2,
            128.0 * np.arange(1, 69), 2.0 * np.arange(1, 51), np.arange(16)]).astype(np.float32)[None], (128, 433))),
        "umat": np.ascontiguousarray((r[:, None] < r[None, :]).astype(np.float32)),
        "ident": np.eye(128, dtype=np.float32), "cosT": cosT, "sinT": sinT, "wmask": np.ascontiguousarray(wmask),
    }
    maps = []
    for b in cores:
        m = dict(shared)
        m["x"] = f(inp["x"][b])
        m["ctx"] = f(inp["ctx"][b])
        c2 = np.stack([f(inp["c"][b]), f(inp["c_ctx"])], axis=1)
        m["c2T"] = np.ascontiguousarray(c2.reshape(8, 128, 2).transpose(1, 0, 2))
        maps.append(m)
    return maps


def _poolfix():
    out = np.ones((4, 32), np.float32)
    for gi, w in enumerate((2, 4, 8, 16)):
        for i, t in enumerate(list(range(16)) + list(range(S - 16, S))):
            lo = min(max(t - w // 2, 0), S)
            hi = min(max(t - w // 2 + w, 0), S)
            out[gi, i] = 1.0 / float(hi - lo)
    return np.ascontiguousarray(np.broadcast_to(out[None], (128, 4, 32)))


_NC_CACHE = {}


def kernel(**inputs):
    if "nc" not in _NC_CACHE:
        _NC_CACHE["nc"] = Builder(debug=False).build()
    nc = _NC_CACHE["nc"]
    maps = make_in_maps(inputs, list(range(8)))
    res = run_bass_kernel_spmd(nc, maps, core_ids=list(range(8)))
    return np.stack([np.asarray(r["y"], dtype=np.float32) for r in res.results], axis=0)
```

```python
import os
import numpy as np
from contextlib import ExitStack
from collections import deque
import concourse.bass as bass
import concourse.mybir as mybir
from concourse.bass_utils import run_bass_kernel_spmd

F32 = mybir.dt.float32
BF16 = mybir.dt.bfloat16
I32 = mybir.dt.int32
ALU = mybir.AluOpType
AF = mybir.ActivationFunctionType
AX = mybir.AxisListType

D = 1024
S = 4096
NCTX = 256
T = S + NCTX
NT = T // 128
EPS = 1e-6
GLU_OFF_CTX = 15
GLU_OFF_LAT = 15 + NCTX + 15
GLU_LEN = GLU_OFF_LAT + S + 15


class Buf:
    __slots__ = ("name", "w", "r")

    def __init__(self, name=""):
        self.name = name
        self.w = None
        self.r = {}


class Tile:
    __slots__ = ("t", "b")

    def __init__(self, t, b):
        self.t, self.b = t, b

    def __getitem__(self, k):
        return self.t[k]


import threading


class Interleaver:
    def __init__(self):
        self.active = False

    def run(self, fns):
        if len(fns) == 1:
            fns[0]()
            return
        n = len(fns)
        self.ev = [threading.Event() for _ in range(n)]
        self.alive = [True] * n
        self.err = []
        self.tid = {}
        done = threading.Event()

        def worker(i):
            self.ev[i].wait()
            self.ev[i].clear()
            try:
                fns[i]()
            except BaseException as e:
                self.err.append(e)
            self.alive[i] = False
            nxt = self._next(i)
            if nxt is None:
                done.set()
            else:
                self.ev[nxt].set()

        ths = [threading.Thread(target=worker, args=(i,)) for i in range(n)]
        self.active = True
        for i, th in enumerate(ths):
            th.start()
            self.tid[th.ident] = i
        self.ev[0].set()
        done.wait()
        for th in ths:
            th.join()
        self.active = False
        if self.err:
            raise self.err[0]

    def _next(self, i):
        n = len(self.alive)
        for d in range(1, n + 1):
            j = (i + d) % n
            if self.alive[j] and j != i:
                return j
        return None

    def yield_point(self):
        if not self.active:
            return
        i = self.tid.get(threading.get_ident())
        if i is None:
            return
        nxt = self._next(i)
        if nxt is None:
            return
        self.ev[nxt].set()
        self.ev[i].wait()
        self.ev[i].clear()


ILV = Interleaver()


class Eng:
    def __init__(self, key, e, sem):
        self.key, self.e, self.sem = key, e, sem
        self.n = 0
        self.known = {}

    def _wait(self, sem, val):
        if self.known.get(sem, 0) >= val:
            return
        self.known[sem] = val
        self.e.wait_ge(sem, val)

    def op(self, ins_fn, reads=(), writes=(), inc=True):
        for b in reads:
            if b.w is not None:
                self._wait(b.w[0], b.w[1])
        strict = (self.key != "pe")
        for b in writes:
            if b.w is not None and (strict or b.w[2] != self.key):
                self._wait(b.w[0], b.w[1])
            for sem, (val, k) in b.r.items():
                if strict or k != self.key:
                    self._wait(sem, val)
        ins = ins_fn()
        if inc:
            self.n += 1
            ins.then_inc(self.sem, 1)
            tok = (self.sem, self.n, self.key)
        else:
            tok = (self.sem, self.n + 1, self.key)
        for b in reads:
            b.r[tok[0]] = (tok[1], tok[2])
        for b in writes:
            b.w = tok
            b.r = {}
        if inc:
            ILV.yield_point()
        return ins


class DmaQ:
    def __init__(self, key, e, sems):
        self.key, self.e, self.sems = key, e, sems
        self.cnt = [0] * len(sems)
        self.i = 0
        self.known = {}

    def _wait(self, sem, val):
        if self.known.get(sem, 0) >= val:
            return
        self.known[sem] = val
        self.e.wait_ge(sem, val)

    def dma(self, out, in_, reads=(), writes=(), **kw):
        for b in reads:
            if b.w is not None:
                self._wait(b.w[0], b.w[1])
        for b in writes:
            if b.w is not None:
                self._wait(b.w[0], b.w[1])
            for sem, (val, k) in b.r.items():
                self._wait(sem, val)
        s = self.i % len(self.sems)
        self.i += 1
        sem = self.sems[s]
        if self.cnt[s] > 0:
            self._wait(sem, 16 * self.cnt[s])
        self.cnt[s] += 1
        ins = self.e.dma_start(out=out, in_=in_, **kw)
        ins.then_inc(sem, 16)
        tok = (sem, 16 * self.cnt[s], self.key + str(s))
        for b in reads:
            b.r[tok[0]] = (tok[1], tok[2])
        for b in writes:
            b.w = tok
            b.r = {}
        return ins


def _dma_generic(self, fn, reads=(), writes=()):
    for b in reads:
        if b.w is not None:
            self._wait(b.w[0], b.w[1])
    for b in writes:
        if b.w is not None:
            self._wait(b.w[0], b.w[1])
        for sem, (val, k) in b.r.items():
            self._wait(sem, val)
    s = self.i % len(self.sems)
    self.i += 1
    sem = self.sems[s]
    if self.cnt[s] > 0:
        self._wait(sem, 16 * self.cnt[s])
    self.cnt[s] += 1
    ins = fn()
    ins.then_inc(sem, 16)
    tok = (sem, 16 * self.cnt[s], self.key + str(s))
    for b in reads:
        b.r[tok[0]] = (tok[1], tok[2])
    for b in writes:
        b.w = tok
        b.r = {}
    return ins


DmaQ.dma_fn = _dma_generic


class FW:
    def __init__(self, nc, stack, n_dma_sems=10):
        self.nc = nc
        mk = lambda nm: stack.enter_context(nc.semaphore(nm))
        self.pe = Eng("pe", nc.tensor, mk("s_pe"))
        self.act = Eng("act", nc.scalar, mk("s_act"))
        self.dve = Eng("dve", nc.vector, mk("s_dve"))
        self.pool = Eng("pool", nc.gpsimd, mk("s_pool"))
        self.q_sync = DmaQ("qs", nc.sync, [mk(f"s_qs{i}") for i in range(n_dma_sems)])
        self.q_pool = DmaQ("qp", nc.gpsimd, [mk(f"s_qp{i}") for i in range(n_dma_sems)])
        self.q_pool.known = self.pool.known
        self.engs = [self.pe, self.act, self.dve, self.pool]
        self.qs = [self.q_sync, self.q_pool]

    def barrier(self):
        toks = []
        for e in self.engs:
            if e.n > 0:
                toks.append((e.sem, e.n))
        for q in self.qs:
            for s, c in zip(q.sems, q.cnt):
                if c > 0:
                    toks.append((s, 16 * c))
        for e in self.engs + [self.q_sync]:
            for sem, val in toks:
                e._wait(sem, val)


class Builder:
    def __init__(self, debug=False, stop=None):
        self.debug = debug
        self.stop = stop
        self.nc = bass.Bass("TRN2", target_bir_lowering=False)
        self.dbg_out = {}

    def dram_in(self, name, shape, dt=F32):
        return self.nc.dram_tensor(name, list(shape), dt, kind="ExternalInput").ap()

    def tile(self, st, name, shape, dt):
        self._tn = getattr(self, "_tn", 0) + 1
        name = f"{name}_{self._tn}"
        t = st.enter_context(self.nc.sbuf_tensor(name, list(shape), dt))
        return Tile(t, Buf(name))

    def ps_get(self):
        return self.psq.popleft()

    def ps_put(self, p):
        self.psq.append(p)

    def dbg(self, name, shape):
        if not self.debug:
            return None
        ap = self.nc.dram_tensor("dbg_" + name, list(shape), F32, kind="ExternalOutput").ap()
        self.dbg_out[name] = (ap, Buf("dbg_" + name))
        return ap

    def cut(self, n):
        return int(os.environ.get("P1_CUT", "-1")) == n

    def tok_in(self, t):
        if t < 2:
            return self.ctx_in[t * 128:(t + 1) * 128, :]
        return self.x_in[(t - 2) * 128:(t - 1) * 128, :]

    def build(self):
        nc = self.nc
        di = self.dram_in
        self.x_in = di("x", [S, D])
        self.ctx_in = di("ctx", [NCTX, D])
        self.c2T = di("c2T", [128, 8, 2])
        self.mod_w = di("mod_w", [2, D, 6 * D])
        self.mod_bT = di("mod_bT", [128, 2, 48])
        self.ln1gT = di("ln1gT", [128, 2, 8])
        self.ln2gT = di("ln2gT", [128, 2, 8])
        self.w_in_ab = di("w_in_ab", [D, 1792])
        self.w_out_ab = di("w_out_ab", [D, D])
        self.gqk_a = di("gqk_a", [128, 640])
        self.convwT = di("convwT", [128, 4, 31])
        self.convv = di("convv", [128, 3, 4])
        self.w_in_cd = di("w_in_cd", [D, 1280])
        self.w_out_cd = di("w_out_cd", [D, D])
        self.gqk_c = di("gqk_c", [128, 640])
        self.sink_rep = di("sink_rep", [128, 2, 512])
        self.pool_w = di("pool_w", [4, 128, 128])
        self.pool_scT = di("pool_scT", [128, 4])
        self.poolfix = di("poolfix", [128, 4, 32])
        self.rt_w = di("rt_w", [2, D, 36])
        self.rt_b = di("rt_b", [128, 2, 36])
        self.ex_gate = di("ex_gate", [2, 32 * 128, 4096])
        self.ex_up = di("ex_up", [2, 32 * 128, 4096])
        self.ex_down = di("ex_down", [2, 32 * 128, 4096])
        self.pidx = di("pidx", [128, 1])
        self.ident_in = di("ident", [128, 128])
        self.cosT = di("cosT", [128, 32, 32])
        self.sinT = di("sinT", [128, 32, 32])
        self.wmask = di("wmask", [128, 2, 512])
        self.mconst = di("mconst", [128, 433])
        self.umat = di("umat", [128, 128])
        self.y_out = nc.dram_tensor("y", [S, D], F32, kind="ExternalOutput").ap()
        self.xres = nc.dram_tensor("xres", [T, D], F32, kind="Internal").ap()
        self.xres_b = [Buf(f"xres{t}") for t in range(NT)]
        self.y_b = [Buf(f"y{t}") for t in range(NT)]

        with ExitStack() as st:
            self.fw = FW(nc, st)
            fw = self.fw
            self.PS = []
            self.PSB = []
            for i in range(4):
                pb_ = st.enter_context(nc.psum_tensor(f"psb{i}", [128, 1024], F32))
                h0 = Tile(pb_[:, 0:512], Buf(f"ps{2 * i}"))
                h1 = Tile(pb_[:, 512:1024], Buf(f"ps{2 * i + 1}"))
                self.PS += [h0, h1]
                self.PSB.append((pb_, h0, h1))
            self.psq = deque(self.PS)
            g = self.g = {}
            g["identf"] = self.tile(st, "identf", [128, 128], F32)
            g["identb"] = self.tile(st, "identb", [128, 128], BF16)
            g["onesf"] = self.tile(st, "onesf", [128, 128], F32)
            g["modT"] = self.tile(st, "modT", [128, 2, 48, 2], F32)
            g["A1"] = self.tile(st, "A1", [128, 2, 8, 2], F32)
            g["A2"] = self.tile(st, "A2", [128, 2, 8, 2], F32)
            g["epsc"] = self.tile(st, "epsc", [128, 1], F32)
            fw.q_sync.dma(g["identf"][:], self.ident_in, writes=[g["identf"].b])
            fw.dve.op(lambda: nc.vector.tensor_copy(out=g["identb"][:], in_=g["identf"][:]), [g["identf"].b], [g["identb"].b])
            fw.dve.op(lambda: nc.vector.memset(g["onesf"][:], 1.0), [], [g["onesf"].b])
            fw.dve.op(lambda: nc.vector.memset(g["epsc"][:], EPS), [], [g["epsc"].b])

            self.bc_reg = nc.gpsimd.alloc_register("bcreg")
            nc.gpsimd.reg_mov(self.bc_reg, 8191)
            self.phase_mod()
            if self.stop != "p0":
                self.layer0_mixer()
            if self.stop is None or self.stop in ("m0", "l1", "m1"):
                (self.moe2 if os.environ.get("MOE_DENSE") != "1" else self.moe)(0)
            if self.stop is None or self.stop in ("l1", "m1"):
                self.layer1_mixer()
            if self.stop is None or self.stop in ("m1",):
                (self.moe2 if os.environ.get("MOE_DENSE") != "1" else self.moe)(1)

            for b in self.y_b[2:]:
                if b.w is not None:
                    fw.q_sync._wait(b.w[0], b.w[1])
            for name, (ap, b) in self.dbg_out.items():
                if b.w is not None:
                    fw.q_sync._wait(b.w[0], b.w[1])
            fw.barrier()
        return nc

    def rstd_from_ss(self, ss, n_inv, cols):
        nc, fw = self.nc, self.fw
        fw.dve.op(lambda: nc.vector.tensor_scalar(out=ss[:, 0:cols], in0=ss[:, 0:cols], scalar1=n_inv, scalar2=EPS,
                                                  op0=ALU.mult, op1=ALU.add), [ss.b], [ss.b])
        fw.act.op(lambda: nc.scalar.sqrt(out=ss[:, 0:cols], in_=ss[:, 0:cols]), [ss.b], [ss.b])
        fw.dve.op(lambda: nc.vector.reciprocal(out=ss[:, 0:cols], in_=ss[:, 0:cols]), [ss.b], [ss.b])

    def load_cast_w(self, st_pool, dst, dst_cols, src_ap, kc, ncols, eng_i):
        nc, fw = self.nc, self.fw
        stg = st_pool[self.stg_i % len(st_pool)]
        self.stg_i += 1
        fw.q_sync.dma(stg[:, 0:kc * ncols].rearrange("p (k n) -> p k n", k=kc),
                      src_ap.rearrange("(k p) n -> p k n", p=128), writes=[stg.b])
        src = stg[:, 0:kc * ncols].rearrange("p (k n) -> p k n", k=kc)
        if eng_i % 2 == 0:
            fw.act.op(lambda: nc.scalar.copy(out=dst[:, 0:kc, dst_cols], in_=src), [stg.b], [dst.b])
        else:
            fw.dve.op(lambda: nc.vector.tensor_copy(out=dst[:, 0:kc, dst_cols], in_=src), [stg.b], [dst.b])

    def phase_mod(self):
        nc, fw, g = self.nc, self.fw, self.g
        with ExitStack() as ph:
            c2 = self.tile(ph, "c2", [128, 8, 2], F32)
            sil = self.tile(ph, "sil", [128, 8, 2], BF16)
            mb = self.tile(ph, "mb", [128, 2, 48], F32)
            l1g = self.tile(ph, "l1g", [128, 2, 8], F32)
            l2g = self.tile(ph, "l2g", [128, 2, 8], F32)
            stg = [self.tile(ph, f"mstg{i}", [128, 4096], F32) for i in range(2)]
            wb = [self.tile(ph, f"mwb{i}", [128, 8, 512], BF16) for i in range(2)]
            self.stg_i = 0
            fw.q_sync.dma(c2[:], self.c2T, writes=[c2.b])
            fw.q_sync.dma(mb[:], self.mod_bT, writes=[mb.b])
            fw.q_sync.dma(l1g[:], self.ln1gT, writes=[l1g.b])
            fw.q_sync.dma(l2g[:], self.ln2gT, writes=[l2g.b])
            fw.act.op(lambda: nc.scalar.activation(out=sil[:], in_=c2[:], func=AF.Silu), [c2.b], [sil.b])
            for l in range(2):
                pm = self.ps_get()
                pmv = pm[:, 0:96].rearrange("p (c j) -> p c j", j=2)
                for n in range(12):
                    w = wb[n % 2]
                    self.load_cast_w(stg, w, slice(0, 512), self.mod_w[l, :, n * 512:(n + 1) * 512], 8, 512, n)
                    for q in range(4):
                        cc = n * 4 + q
                        for k in range(8):
                            fw.pe.op(lambda: nc.tensor.matmul(pmv[:, cc, :], lhsT=w[:, k, q * 128:(q + 1) * 128], rhs=sil[:, k, :],
                                                              start=(k == 0), stop=(k == 7)),
                                     [w.b, sil.b], [pm.b], inc=(k == 7 and q == 3))
                mT = g["modT"]
                fw.dve.op(lambda: nc.vector.tensor_tensor(out=mT[:, l, :, :], in0=pmv,
                                                          in1=mb[:, l, :].unsqueeze(2).to_broadcast([128, 48, 2]), op=ALU.add),
                          [pm.b, mb.b], [mT.b])
                self.ps_put(pm)
                fw.dve.op(lambda: nc.vector.scalar_tensor_tensor(out=g["A1"][:, l, :, :], in0=mT[:, l, 8:16, :], scalar=1.0,
                                                                 in1=l1g[:, l, :].unsqueeze(2).to_broadcast([128, 8, 2]),
                                                                 op0=ALU.add, op1=ALU.mult), [mT.b, l1g.b], [g["A1"].b])
                fw.dve.op(lambda: nc.vector.scalar_tensor_tensor(out=g["A2"][:, l, :, :], in0=mT[:, l, 32:40, :], scalar=1.0,
                                                                 in1=l2g[:, l, :].unsqueeze(2).to_broadcast([128, 8, 2]),
                                                                 op0=ALU.add, op1=ALU.mult), [mT.b, l2g.b], [g["A2"].b])
            if self.debug:
                ap = self.dbg("modT", [128, 2 * 48 * 2])
                fw.q_pool.dma(ap, g["modT"][:].rearrange("p l c j -> p (l c j)"), reads=[g["modT"].b], writes=[self.dbg_out["modT"][1]])
            fw.barrier()

    def bcast_tile(self, dst, col_ap_fn, tmp, extra=()):
        nc, fw, g = self.nc, self.fw, self.g
        for c in range(8):
            fw.dve.op(lambda: nc.vector.tensor_scalar(out=tmp[:], in0=g["onesf"][:], scalar1=col_ap_fn(c), scalar2=None, op0=ALU.mult),
                      [g["onesf"].b, g["modT"].b] + list(extra), [tmp.b])
            p = self.ps_get()
            fw.pe.op(lambda: nc.tensor.transpose(out=p[:, 0:128], in_=tmp[:], identity=g["identf"][:]), [tmp.b, g["identf"].b], [p.b])
            fw.act.op(lambda: nc.scalar.copy(out=dst[:, c * 128:(c + 1) * 128], in_=p[:, 0:128]), [p.b], [dst.b])
            self.ps_put(p)

    def norm_tile_to_hT(self, xt, hT, col0, l, which, cls, scr):
        nc, fw, g = self.nc, self.fw, self.g
        ni = scr.get("ni", 0)
        scr["ni"] = ni + 1
        ss, xn = scr["ssl"][ni % len(scr["ssl"])], scr["xnl"][ni % len(scr["xnl"])]
        fw.act.op(lambda: nc.scalar.activation(out=xn[:], in_=xt[:], func=AF.Square, accum_out=ss[:, 0:1]), [xt.b], [xn.b, ss.b])
        self.rstd_from_ss(ss, 1.0 / D, 1)
        fw.act.op(lambda: nc.scalar.activation(out=xn[:], in_=xt[:], func=AF.Copy, scale=ss[:, 0:1]), [xt.b, ss.b], [xn.b])
        p = self.ps_get()
        pv = p[:].bitcast(BF16).rearrange("p (c q) -> p c q", c=8)
        for c in range(8):
            fw.pe.op(lambda: nc.tensor.transpose(out=pv[:, c, :], in_=xn[:, c * 128:(c + 1) * 128], identity=g["identb"][:]),
                     [xn.b, g["identb"].b], [p.b], inc=(c == 7))
        A = g["A1"] if which == 1 else g["A2"]
        boff = 0 if which == 1 else 24
        for c in range(8):
            fw.dve.op(lambda: nc.vector.tensor_scalar(out=hT[:, c, col0:col0 + 128], in0=pv[:, c, :], scalar1=A[:, l, c, cls:cls + 1],
                                                      scalar2=g["modT"][:, l, boff + c, cls:cls + 1], op0=ALU.mult, op1=ALU.add),
                      [p.b, A.b, g["modT"].b], [hT.b])
        self.ps_put(p)

    def qk_post(self, qkv, t, gqk, scr, qT, kT, do_q, lat_idx):
        nc, fw, g = self.nc, self.fw, self.g
        sq, ssq, qn, qr, tmp = scr["sq"], scr["ssq"], scr["qn"], scr["qr"], scr["rt"]
        h0 = 0 if do_q else 8
        nh = 10 - h0
        c0 = h0 * 64
        q3 = lambda tl: tl[:, c0:640].rearrange("p (h d) -> p h d", d=64)
        fw.pool.op(lambda: nc.gpsimd.tensor_tensor(out=sq[:, c0:640], in0=qkv[:, c0:640], in1=qkv[:, c0:640], op=ALU.mult), [qkv.b], [sq.b])
        fw.dve.op(lambda: nc.vector.tensor_reduce(out=ssq[:, h0:10], in_=q3(sq), axis=AX.X, op=ALU.add), [sq.b], [ssq.b])
        fw.dve.op(lambda: nc.vector.tensor_scalar(out=ssq[:, h0:10], in0=ssq[:, h0:10], scalar1=1.0 / 64, scalar2=EPS,
                                                  op0=ALU.mult, op1=ALU.add), [ssq.b], [ssq.b])
        fw.act.op(lambda: nc.scalar.sqrt(out=ssq[:, h0:10], in_=ssq[:, h0:10]), [ssq.b], [ssq.b])
        fw.dve.op(lambda: nc.vector.reciprocal(out=ssq[:, h0:10], in_=ssq[:, h0:10]), [ssq.b], [ssq.b])
        fw.dve.op(lambda: nc.vector.tensor_tensor(out=q3(qn), in0=q3(qkv), in1=ssq[:, h0:10].unsqueeze(2).to_broadcast([128, nh, 64]),
                                                  op=ALU.mult), [qkv.b, ssq.b], [qn.b])
        if lat_idx is None:
            fw.pool.op(lambda: nc.gpsimd.tensor_tensor(out=qr[:, c0:640], in0=qn[:, c0:640], in1=gqk[:, c0:640], op=ALU.mult),
                       [qn.b, gqk.b], [qr.b])
        else:
            fw.pool.op(lambda: nc.gpsimd.tensor_tensor(out=qn[:, c0:640], in0=qn[:, c0:640], in1=gqk[:, c0:640], op=ALU.mult),
                       [qn.b, gqk.b], [qn.b])
            cosb = g["cos"][:, lat_idx, :].unsqueeze(1).to_broadcast([128, nh, 32])
            sinb = g["sin"][:, lat_idx, :].unsqueeze(1).to_broadcast([128, nh, 32])
            x1 = q3(qn)[:, :, 0:32]
            x2 = q3(qn)[:, :, 32:64]
            sq2 = sq[:, 0:640].rearrange("p (k n) -> p k n", k=2)
            t3 = lambda k: (sq2 if k < 2 else tmp)[:, k % 2, c0 // 2:320].rearrange("p (h d) -> p h d", d=32)
            fw.dve.op(lambda: nc.vector.tensor_tensor(out=t3(0), in0=x1, in1=cosb, op=ALU.mult), [qn.b, g["cos"].b], [sq.b])
            fw.pool.op(lambda: nc.gpsimd.tensor_tensor(out=t3(1), in0=x2, in1=sinb, op=ALU.mult), [qn.b, g["sin"].b], [sq.b])
            fw.dve.op(lambda: nc.vector.tensor_tensor(out=t3(2), in0=x2, in1=cosb, op=ALU.mult), [qn.b, g["cos"].b], [tmp.b])
            fw.pool.op(lambda: nc.gpsimd.tensor_tensor(out=t3(3), in0=x1, in1=sinb, op=ALU.mult), [qn.b, g["sin"].b], [tmp.b])
            fw.dve.op(lambda: nc.vector.tensor_tensor(out=q3(qr)[:, :, 0:32], in0=t3(0), in1=t3(1), op=ALU.subtract), [sq.b], [qr.b])
            fw.pool.op(lambda: nc.gpsimd.tensor_tensor(out=q3(qr)[:, :, 32:64], in0=t3(2), in1=t3(3), op=ALU.add), [tmp.b], [qr.b])
        if self.cut(30):
            return
        p = self.ps_get()
        pv = p[:].bitcast(BF16).rearrange("p (c q) -> p c q", c=8)
        if do_q:
            for j in range(4):
                fw.pe.op(lambda: nc.tensor.transpose(out=pv[:, j, :], in_=qr[:, j * 128:(j + 1) * 128], identity=g["identb"][:]),
                         [qr.b, g["identb"].b], [p.b], inc=False)
        fw.pe.op(lambda: nc.tensor.transpose(out=pv[:, 4, :], in_=qr[:, 512:640], identity=g["identb"][:]),
                 [qr.b, g["identb"].b], [p.b])
        if self.cut(31):
            return
        if do_q:
            fw.dve.op(lambda: nc.vector.tensor_copy(out=qT[:, t, :, :], in_=pv[:, 0:4, :]), [p.b], [qT.b])
        if self.cut(32):
            return
        fw.dve.op(lambda: nc.vector.tensor_copy(out=kT[0:64, 0, t * 128:(t + 1) * 128], in_=pv[0:64, 4, :]), [p.b], [kT.b])
        fw.dve.op(lambda: nc.vector.tensor_copy(out=kT[64:128, 1, t * 128:(t + 1) * 128], in_=pv[64:128, 4, :]), [p.b], [kT.b])
        self.ps_put(p)

    def v_fill(self, qkv, t, Vp):
        nc, fw = self.nc, self.fw
        fw.act.op(lambda: nc.scalar.copy(out=Vp[:, t, 0:64], in_=qkv[:, 640:704]), [qkv.b], [Vp.b])
        fw.act.op(lambda: nc.scalar.copy(out=Vp[:, t, 136:200], in_=qkv[:, 704:768]), [qkv.b], [Vp.b])

    def v_init(self, Vp):
        nc, fw = self.nc, self.fw
        fw.pool.op(lambda: nc.gpsimd.memset(Vp[:], 0.0), [], [Vp.b])
        fw.pool.op(lambda: nc.gpsimd.memset(Vp[:, :, 64:65], 1.0), [], [Vp.b])
        fw.pool.op(lambda: nc.gpsimd.memset(Vp[:, :, 104:105], 1.0), [], [Vp.b])

    def attention(self, blocks, qT, kT, Vp, oT, scr, masks=None, sinkrow=None, LA=2):
        nc, fw, g = self.nc, self.fw, self.g
        steps = []
        for b0 in range(0, len(blocks), 2):
            (kv0_, qb, kts), (kv1_, qb1, kts1) = blocks[b0], blocks[b0 + 1]
            assert (kv0_, kv1_) == (0, 1) and qb == qb1
            for i, (kt, mi) in enumerate(kts):
                steps.append(dict(bi=b0, qb=qb, kt=kt, mi=mi, first=(i == 0), last=(i == len(kts) - 1)))
        assert len(self.psq) == 8
        spairs = self.PSB[0:2]
        for (_, h0, h1) in spairs:
            self.psq.remove(h0)
            self.psq.remove(h1)
        po_of = {}
        cnt = dict(s=0)

        pending = []

        def finish_block(kv, qb, bi, po, idx):
            r0 = kv * 64
            dr = 64 if kv == 0 else 32
            M = 65 if kv == 0 else 128
            slot = (bi // 2) % 2
            posb = scr["posb"][slot][kv]
            rec = scr["rec"][slot]
            fw.dve.op(lambda: nc.vector.tensor_copy(out=posb[0:M, :], in_=po[0:M, :]), [po.b], [posb.b])
            self.ps_put(po)
            if sinkrow is not None:
                fw.dve.op(lambda: nc.vector.tensor_tensor(out=rec[dr:dr + 1, :], in0=posb[dr:dr + 1, :], in1=sinkrow[dr:dr + 1, kv, :], op=ALU.add),
                          [posb.b, sinkrow.b], [rec.b])
                fw.dve.op(lambda: nc.vector.reciprocal(out=rec[dr:dr + 1, :], in_=rec[dr:dr + 1, :]), [rec.b], [rec.b])
            else:
                fw.dve.op(lambda: nc.vector.reciprocal(out=rec[dr:dr + 1, :], in_=posb[dr:dr + 1, :]), [posb.b], [rec.b])
            pending.append((idx + 4, kv, qb, posb, rec))

        def finalize(kv, qb, posb, rec):
            r0 = kv * 64
            dr = 64 if kv == 0 else 32
            pb = self.ps_get()
            Mb = 64 if kv == 0 else 128
            fw.pe.op(lambda: nc.tensor.matmul(pb[0:Mb, :], lhsT=g["onesf"][dr:dr + 1, 0:Mb], rhs=rec[dr:dr + 1, :], start=True, stop=True),
                     [g["onesf"].b, rec.b], [pb.b])
            fw.dve.op(lambda: nc.vector.tensor_tensor(out=oT[r0:r0 + 64, :, qb * 128:(qb + 1) * 128],
                                                      in0=posb[r0:r0 + 64, :].rearrange("p (j q) -> p j q", j=4),
                                                      in1=pb[r0:r0 + 64, :].rearrange("p (j q) -> p j q", j=4), op=ALU.mult),
                      [posb.b, pb.b], [oT.b])
            self.ps_put(pb)

        def emit_S(st):
            qb, kt = st["qb"], st["kt"]
            big, h0, h1 = spairs[cnt["s"] % 2]
            cnt["s"] += 1
            rhs_q = qT[:, qb, :, :].rearrange("p j q -> p (j q)")
            fw.pe.op(lambda: nc.tensor.matmul(h0[:], lhsT=kT[:, 0, kt * 128:(kt + 1) * 128], rhs=rhs_q, start=True, stop=True),
                     [kT.b, qT.b], [h0.b], inc=False)
            fw.pe.op(lambda: nc.tensor.matmul(h1[:], lhsT=kT[:, 1, kt * 128:(kt + 1) * 128], rhs=rhs_q, start=True, stop=True),
                     [kT.b, qT.b], [h1.b])
            pe_ = scr["pexp"][scr["pi"] % len(scr["pexp"])]
            scr["pi"] += 1
            fw.act.op(lambda: nc.scalar.activation(out=pe_[:], in_=big[:, 0:1024], func=AF.Exp), [h0.b, h1.b], [pe_.b])
            if st["mi"] is not None:
                fw.dve.op(lambda: nc.vector.tensor_tensor(out=pe_[:].rearrange("p (a q) -> p a q", a=2), in0=pe_[:].rearrange("p (a q) -> p a q", a=2),
                                                          in1=masks[:, st["mi"], :].unsqueeze(1).to_broadcast([128, 2, 512]), op=ALU.mult),
                          [pe_.b, masks.b], [pe_.b])
            st["pe"] = pe_

        def emit_PV(st):
            qb, kt, bi = st["qb"], st["kt"], st["bi"]
            if st["first"]:
                po_of[bi] = (self.ps_get(), self.ps_get())
            pe_ = st["pe"]
            for kv in range(2):
                po = po_of[bi][kv]
                M = 65 if kv == 0 else 128
                fw.pe.op(lambda: nc.tensor.matmul(po[0:M, :], lhsT=Vp[:, kt, kv * 72:kv * 72 + M], rhs=pe_[:, kv * 512:(kv + 1) * 512],
                                                  start=st["first"], stop=st["last"]), [Vp.b, pe_.b], [po.b], inc=(st["last"] or kv == 1))
            if st["last"]:
                for kv in range(2):
                    finish_block(kv, qb, bi, po_of[bi][kv], st["idx"])
                del po_of[bi]

        n = len(steps)
        for idx in range(n + LA):
            if idx < n:
                emit_S(steps[idx])
            if idx - LA >= 0:
                steps[idx - LA]["idx"] = idx
                emit_PV(steps[idx - LA])
            while pending and pending[0][0] <= idx:
                _, kv, qb_, posb, rec = pending.pop(0)
                finalize(kv, qb_, posb, rec)
        while pending:
            _, kv, qb_, posb, rec = pending.pop(0)
            finalize(kv, qb_, posb, rec)
        for (_, h0, h1) in spairs:
            self.psq.append(h0)
            self.psq.append(h1)

    def out_proj(self, l, cat_fn, wsrc, tiles, ph):
        nc, fw, g = self.nc, self.fw, self.g
        classes = [1, 0] if l == 0 else [0]
        wo = {}
        GB = self.tile(ph, "GB", [128, D], F32)
        tmp = self.tile(ph, "bct", [128, 128], F32)
        stg = [self.tile(ph, f"ostg{i}", [128, D], F32) for i in range(2)]
        for cls in classes:
            wo[cls] = self.tile(ph, f"wo{cls}", [128, 8, D], BF16)
            self.bcast_tile(GB, lambda c: g["modT"][:, l, 16 + c, cls:cls + 1], tmp)
            for c in range(8):
                s_ = stg[c % 2]
                if c < 4:
                    fw.q_sync.dma(s_[0:64, :], wsrc[c * 64:(c + 1) * 64, :], writes=[s_.b])
                    fw.q_sync.dma(s_[64:128, :], wsrc[(c + 4) * 64:(c + 5) * 64, :], writes=[s_.b])
                else:
                    fw.q_sync.dma(s_[:], wsrc[c * 128:(c + 1) * 128, :], writes=[s_.b])
                fw.dve.op(lambda: nc.vector.tensor_tensor(out=wo[cls][:, c, :], in0=s_[:], in1=GB[:], op=ALU.mult), [s_.b, GB.b], [wo[cls].b])
        xt = [self.tile(ph, f"oxt{i}", [128, D], F32) for i in range(3)]

        def op_fn(i, t):
            cls = 1 if t < 2 else 0
            x_ = xt[i % 3]
            src = self.tok_in(t) if l == 0 else self.xres[t * 128:(t + 1) * 128, :]
            rb = [] if l == 0 else [self.xres_b[t]]
            fw.q_sync.dma(x_[:], src, reads=rb, writes=[x_.b])
            for half in range(2):
                p = self.ps_get()
                for c in range(8):
                    ct, ci = cat_fn(c)
                    fw.pe.op(lambda: nc.tensor.matmul(p[:], lhsT=ct[:, ci, t * 128:(t + 1) * 128], rhs=wo[cls][:, c, half * 512:(half + 1) * 512],
                                                      start=(c == 0), stop=(c == 7)), [ct.b, wo[cls].b], [p.b], inc=(c == 7))
                fw.dve.op(lambda: nc.vector.tensor_tensor(out=x_[:, half * 512:(half + 1) * 512], in0=p[:], in1=x_[:, half * 512:(half + 1) * 512],
                                                          op=ALU.add), [p.b, x_.b], [x_.b])
                self.ps_put(p)
            fw.q_pool.dma(self.xres[t * 128:(t + 1) * 128, :], x_[:], reads=[x_.b], writes=[self.xres_b[t]])
            if self.debug and t in (0, 2, NT - 1):
                nm = f"x1_{l}_{t}"
                ap = self.dbg(nm, [128, D])
                fw.q_pool.dma(ap, x_[:], reads=[x_.b], writes=[self.dbg_out[nm][1]])

        for i0 in range(0, len(tiles), 3):
            grp = list(range(i0, min(i0 + 3, len(tiles))))
            ILV.run([(lambda i=i: op_fn(i, tiles[i])) for i in grp])

    def layer0_mixer(self):
        nc, fw, g = self.nc, self.fw, self.g
        l = 0
        with ExitStack() as ph:
            gluT = self.tile(ph, "gluT", [128, 4, GLU_LEN], BF16)
            oT = self.tile(ph, "oT", [128, 4, T], BF16)
            pattn = ExitStack()
            qT = self.tile(pattn, "qT", [128, NT, 4, 128], BF16)
            kT = self.tile(pattn, "kT", [128, 2, T], BF16)
            Vp = self.tile(pattn, "Vp", [128, NT, 200], BF16)
            self.v_init(Vp)
            fw.pool.op(lambda: nc.gpsimd.memset(kT[:], 0.0), [], [kT.b])
            fw.pool.op(lambda: nc.gpsimd.memset(gluT[:], 0.0), [], [gluT.b])
            with ExitStack() as p1:
                win = self.tile(p1, "win", [128, 8, 1792], BF16)
                gqk = self.tile(p1, "gqk", [128, 640], F32)
                g["cos"] = self.tile(p1, "cos", [128, 32, 32], F32)
                g["sin"] = self.tile(p1, "sin", [128, 32, 32], F32)
                fw.q_sync.dma(gqk[:], self.gqk_a, writes=[gqk.b])
                fw.q_sync.dma(g["cos"][:], self.cosT, writes=[g["cos"].b])
                fw.q_sync.dma(g["sin"][:], self.sinT, writes=[g["sin"].b])
                fw.dve.op(lambda: nc.vector.tensor_scalar(out=gqk[:, 0:512], in0=gqk[:, 0:512], scalar1=0.125, scalar2=None, op0=ALU.mult),
                          [gqk.b], [gqk.b])
                with ExitStack() as pw:
                    stg = [self.tile(pw, f"wstg{i}", [128, 4096], F32) for i in range(1)]
                    self.stg_i = 0
                    for n in range(4):
                        w_ = 512 if n < 3 else 256
                        self.load_cast_w(stg, win, slice(n * 512, n * 512 + w_), self.w_in_ab[:, n * 512:n * 512 + w_], 8, w_, n)
                    fw.barrier()
                if self.cut(0):
                    return
                scr = dict(ssl=[self.tile(p1, f"ss{i}", [128, 1], F32) for i in range(2)],
                           xnl=[self.tile(p1, f"xn{i}", [128, D], BF16) for i in range(2)], sq=self.tile(p1, "sq", [128, 640], F32),
                           ssq=self.tile(p1, "ssq", [128, 10], F32), qn=self.tile(p1, "qn", [128, 640], F32),
                           qr=self.tile(p1, "qr", [128, 640], BF16), rt=self.tile(p1, "rt", [128, 2, 320], F32))
                xt = [self.tile(p1, f"xt{i}", [128, D], F32) for i in range(1)]
                hT = [self.tile(p1, f"hT{i}", [128, 8, 512], BF16) for i in range(1)]
                qkv = [self.tile(p1, f"qkv{i}", [128, 768], F32) for i in range(1)]
                sig = [self.tile(p1, f"sig{i}", [128, 512], BF16) for i in range(1)]
                chunks = [(0, 2)] + [(2 + 4 * i, 4) for i in range(8)]
                oflat = oT[:].rearrange("p a t -> p (a t)")
                coff = [0]

                def carve(n_elems, dt, shape3=None):
                    nb = n_elems * (4 if dt == F32 else 2) // 2
                    ap = oflat[:, coff[0]:coff[0] + nb]
                    coff[0] += nb
                    if dt == F32:
                        ap = ap.bitcast(F32)
                    if shape3 is not None:
                        ap = ap.rearrange("p (a b) -> p a b", a=shape3[0])
                    return Tile(ap, Buf("carve"))

                scr_b = dict(ssl=[scr["ssl"][1]], xnl=[scr["xnl"][1]], sq=carve(640, F32), ssq=carve(16, F32), qn=carve(640, F32),
                             qr=carve(640, BF16), rt=carve(640, F32, (2, 320)))
                scr_a = dict(scr)
                scr_a["ssl"] = [scr["ssl"][0]]
                scr_a["xnl"] = [scr["xnl"][0]]
                scr_l = [scr_a, scr_b]
                xt2 = [xt[0], carve(D, F32)]
                qkv2 = [qkv[0], carve(768, F32)]
                for ci, (t0, ntl) in enumerate(chunks):
                    h = hT[0]
                    ntok = ntl * 128
                    cls = 1 if t0 < 2 else 0

                    def norm_fn(tl):
                        t = t0 + tl
                        x_ = xt2[tl % 2]
                        fw.q_sync.dma(x_[:], self.tok_in(t), writes=[x_.b])
                        self.norm_tile_to_hT(x_, h, tl * 128, l, 1, cls, scr_l[tl % 2])

                    def qkv_fn(tl):
                        t = t0 + tl
                        qk_ = qkv2[tl % 2]
                        for (c0, w_) in ((0, 512), (512, 256)):
                            p = self.ps_get()
                            for k in range(8):
                                fw.pe.op(lambda: nc.tensor.matmul(p[:, 0:w_], lhsT=h[:, k, tl * 128:(tl + 1) * 128], rhs=win[:, k, c0:c0 + w_],
                                                                  start=(k == 0), stop=(k == 7)), [h.b, win.b], [p.b], inc=(k == 7))
                            if c0 == 0:
                                fw.act.op(lambda: nc.scalar.copy(out=qk_[:, 0:512].rearrange("p (j a d) -> p a j d", a=2, d=64),
                                                                 in_=p[:, 0:512].rearrange("p (a j d) -> p a j d", a=2, j=4)), [p.b], [qk_.b])
                            else:
                                fw.act.op(lambda: nc.scalar.copy(out=qk_[:, c0:c0 + w_], in_=p[:, 0:w_]), [p.b], [qk_.b])
                            self.ps_put(p)
                        self.qk_post(qk_, t, gqk, scr_l[tl % 2], qT, kT, True, None if t < 2 else t - 2)
                        self.v_fill(qk_, t, Vp)

                    for tl0 in range(0, ntl, 2):
                        ILV.run([(lambda tl=tl: norm_fn(tl)) for tl in range(tl0, min(tl0 + 2, ntl))])
                    for tl0 in range(0, ntl, 2):
                        ILV.run([(lambda tl=tl: qkv_fn(tl)) for tl in range(tl0, min(tl0 + 2, ntl))])
                    off = GLU_OFF_CTX if t0 < 2 else GLU_OFF_LAT + (t0 - 2) * 128
                    for j in range(4):
                        pa = self.ps_get()
                        pg = self.ps_get()
                        for (pp, cb) in ((pa, 768 + j * 128), (pg, 1280 + j * 128)):
                            for k in range(8):
                                fw.pe.op(lambda: nc.tensor.matmul(pp[:, 0:ntok], lhsT=win[:, k, cb:cb + 128], rhs=h[:, k, 0:ntok],
                                                                  start=(k == 0), stop=(k == 7)), [win.b, h.b], [pp.b], inc=(k == 7))
                        sg = sig[0]
                        fw.act.op(lambda: nc.scalar.activation(out=sg[:, 0:ntok], in_=pg[:, 0:ntok], func=AF.Sigmoid), [pg.b], [sg.b])
                        fw.dve.op(lambda: nc.vector.tensor_tensor(out=gluT[:, j, off:off + ntok], in0=pa[:, 0:ntok], in1=sg[:, 0:ntok], op=ALU.mult),
                                  [pa.b, sg.b], [gluT.b])
                        self.ps_put(pa)
                        self.ps_put(pg)
                    if self.cut(4) or (self.cut(5) and ci == 1):
                        return
                fw.barrier()
            if self.debug:
                self.dbg_dump_bf(ph, "qT", qT[:, 2, :, :], [128, 4, 128], qT.b)
                self.dbg_dump_bf(ph, "kT", kT[:, 0, 0:512], [128, 512], kT.b)
                self.dbg_dump_bf(ph, "glu", gluT[:, :, GLU_OFF_LAT:GLU_OFF_LAT + 128], [128, 4, 128], gluT.b)
            if self.stop == "p1":
                return
            with ExitStack() as p2:
                scr = dict(pexp=[self.tile(p2, f"pexp{i}", [128, 1024], BF16) for i in range(4)], pi=0,
                           rec=[self.tile(p2, f"rec{i}", [128, 512], F32) for i in range(2)],
                           posb=[[self.tile(p2, f"posb{i}{k}", [128, 512], F32) for k in range(2)] for i in range(2)])
                blocks = []
                for qb in range(NT):
                    kts = [(0, None), (1, None)] if qb < 2 else [(k, None) for k in range(NT)]
                    for kv in range(2):
                        blocks.append((kv, qb, kts))
                self.attention(blocks, qT, kT, Vp, oT, scr)
                fw.barrier()
            if self.debug:
                self.dbg_dump_bf(ph, "oT", oT[:, :, 256:384], [128, 4, 128], oT.b)
                self.dbg_dump_bf(ph, "oTc", oT[:, :, 0:128], [128, 4, 128], oT.b)
            pattn.close()
            if self.stop == "p2":
                return
            bT = self.tile(ph, "bT", [128, 4, T], BF16)
            with ExitStack() as p3:
                cw = self.tile(p3, "cw", [128, 4, 31], F32)
                cv = self.tile(p3, "cv", [128, 3, 4], F32)
                diag = self.tile(p3, "diag", [128, 4, 31, 128], BF16)
                onesM = self.tile(p3, "onesM", [128, 128], F32)
                fw.q_sync.dma(cw[:], self.convwT, writes=[cw.b])
                fw.q_sync.dma(cv[:], self.convv, writes=[cv.b])
                fw.dve.op(lambda: nc.vector.memset(onesM[:], 1.0 / 512), [], [onesM.b])
                for j in range(4):
                    for tap in range(31):
                        e_ = fw.dve if (tap % 2 == 0) else fw.pool
                        ee = nc.vector if (tap % 2 == 0) else nc.gpsimd
                        e_.op(lambda: ee.tensor_scalar(out=diag[:, j, tap, :], in0=g["identb"][:], scalar1=cw[:, j, tap:tap + 1], scalar2=None,
                                                       op0=ALU.mult), [g["identb"].b, cw.b], [diag.b])
                ysb = [self.tile(p3, f"ysb{i}", [128, 4, 512], F32) for i in range(2)]
                ysq = [self.tile(p3, f"ysq{i}", [128, 4, 512], F32) for i in range(2)]
                mean_l = [self.tile(p3, f"mean{i}", [128, 512], F32) for i in range(2)]
                rstd_l = [self.tile(p3, f"rstd{i}", [128, 512], F32) for i in range(2)]
                tmp_l = [[self.tile(p3, f"ctmp{i}{k}", [128, 512], F32) for k in range(2)] for i in range(2)]
                chunks = [(GLU_OFF_CTX, 0, 256)] + [(GLU_OFF_LAT + 512 * i, 256 + 512 * i, 512) for i in range(8)]

                def conv_fn(ci):
                    off, tok0, ntok = chunks[ci]
                    mean, rstd, tmp = mean_l[ci % 2], rstd_l[ci % 2], tmp_l[ci % 2]
                    y_, q_ = ysb[ci % 2], ysq[ci % 2]
                    for j in range(4):
                        p = self.ps_get()
                        for tap in range(31):
                            fw.pe.op(lambda: nc.tensor.matmul(p[:, 0:ntok], lhsT=diag[:, j, tap, :], rhs=gluT[:, j, off + tap - 15:off + tap - 15 + ntok],
                                                              start=(tap == 0), stop=(tap == 30)), [diag.b, gluT.b], [p.b], inc=(tap == 30))
                        fw.act.op(lambda: nc.scalar.activation(out=y_[:, j, 0:ntok], in_=p[:, 0:ntok], func=AF.Identity, bias=cv[:, 0, j:j + 1]),
                                  [p.b, cv.b], [y_.b])
                        self.ps_put(p)
                        fw.pool.op(lambda: nc.gpsimd.tensor_tensor(out=q_[:, j, 0:ntok], in0=y_[:, j, 0:ntok], in1=y_[:, j, 0:ntok], op=ALU.mult),
                                   [y_.b], [q_.b])
                    pm = self.ps_get()
                    pq = self.ps_get()
                    for (pp, src) in ((pm, y_), (pq, q_)):
                        for j in range(4):
                            fw.pe.op(lambda: nc.tensor.matmul(pp[:, 0:ntok], lhsT=onesM[:], rhs=src[:, j, 0:ntok], start=(j == 0), stop=(j == 3)),
                                     [onesM.b, src.b], [pp.b], inc=(j == 3))
                    fw.act.op(lambda: nc.scalar.copy(out=mean[:, 0:ntok], in_=pm[:, 0:ntok]), [pm.b], [mean.b])
                    self.ps_put(pm)
                    fw.pool.op(lambda: nc.gpsimd.tensor_tensor(out=rstd[:, 0:ntok], in0=mean[:, 0:ntok], in1=mean[:, 0:ntok], op=ALU.mult),
                               [mean.b], [rstd.b])
                    fw.dve.op(lambda: nc.vector.tensor_tensor(out=rstd[:, 0:ntok], in0=pq[:, 0:ntok], in1=rstd[:, 0:ntok], op=ALU.subtract),
                              [pq.b, rstd.b], [rstd.b])
                    self.ps_put(pq)
                    fw.dve.op(lambda: nc.vector.tensor_scalar(out=rstd[:, 0:ntok], in0=rstd[:, 0:ntok], scalar1=EPS, scalar2=None, op0=ALU.add),
                              [rstd.b], [rstd.b])
                    fw.act.op(lambda: nc.scalar.sqrt(out=rstd[:, 0:ntok], in_=rstd[:, 0:ntok]), [rstd.b], [rstd.b])
                    fw.dve.op(lambda: nc.vector.reciprocal(out=rstd[:, 0:ntok], in_=rstd[:, 0:ntok]), [rstd.b], [rstd.b])
                    for j in range(4):
                        t_ = tmp[j % 2]
                        fw.pool.op(lambda: nc.gpsimd.tensor_tensor(out=t_[:, 0:ntok], in0=y_[:, j, 0:ntok], in1=mean[:, 0:ntok], op=ALU.subtract),
                                   [y_.b, mean.b], [t_.b])
                        fw.dve.op(lambda: nc.vector.tensor_tensor(out=t_[:, 0:ntok], in0=t_[:, 0:ntok], in1=rstd[:, 0:ntok], op=ALU.mult),
                                  [t_.b, rstd.b], [t_.b])
                        fw.act.op(lambda: nc.scalar.activation(out=bT[:, j, tok0:tok0 + ntok], in_=t_[:, 0:ntok], func=AF.Silu,
                                                               scale=cv[:, 1, j:j + 1], bias=cv[:, 2, j:j + 1]), [t_.b, cv.b], [bT.b])

                conv_fn(0)
                for c0_ in range(1, 9, 2):
                    ILV.run([(lambda ci=ci: conv_fn(ci)) for ci in (c0_, c0_ + 1)])
                fw.barrier()
            if self.debug:
                self.dbg_dump_bf(ph, "bT", bT[:, :, 256:384], [128, 4, 128], bT.b)
            if self.stop == "p3":
                return
            with ExitStack() as p4:
                self.out_proj(0, lambda c: (oT, c) if c < 4 else (bT, c - 4), self.w_out_ab, list(range(NT)), p4)
                fw.barrier()

    def dbg_dump_bf(self, ph, name, src_ap, shape, buf):
        nc, fw = self.nc, self.fw
        n = int(np.prod(shape[1:]))
        ap = self.dbg(name, [128, n])
        with ExitStack() as ds:
            tf = self.tile(ds, "dbgt_" + name, shape, F32)
            fw.dve.op(lambda: nc.vector.tensor_copy(out=tf[:], in_=src_ap), [buf], [tf.b])
            flat = tf[:] if len(shape) == 2 else tf[:].rearrange("p a b -> p (a b)")
            fw.q_pool.dma(ap, flat, reads=[tf.b], writes=[self.dbg_out[name][1]])
            fw.barrier()

    def moe(self, l):
        nc, fw, g = self.nc, self.fw, self.g
        if l == 0:
            groups = [list(range(0, 10)), list(range(10, 18)), list(range(18, 26)), list(range(26, 34))]
        else:
            groups = [list(range(2 + 8 * i, 10 + 8 * i)) for i in range(4)]
        ngrp = int(os.environ.get("MOE_GROUPS", "4"))
        nexp = int(os.environ.get("MOE_EXPERTS", "32"))
        classes = [0, 1] if l == 0 else [0]
        with ExitStack() as ph:
            wr = self.tile(ph, "wr", [128, 8, 36], F32)
            rb = self.tile(ph, "rb", [128, 36], F32)
            fw.q_sync.dma(wr[:], self.rt_w[l].rearrange("(k p) n -> p k n", p=128), writes=[wr.b])
            fw.q_sync.dma(rb[:], self.rt_b[:, l, :], writes=[rb.b])
            G2B = {}
            tmpb = self.tile(ph, "bct2", [128, 128], F32)
            for cls in classes:
                G2B[cls] = self.tile(ph, f"G2B{cls}", [128, D], F32)
                self.bcast_tile(G2B[cls], lambda c: g["modT"][:, l, 40 + c, cls:cls + 1], tmpb)
            stg = [self.tile(ph, f"estg{i}", [128, 4096], F32) for i in range(2)]
            wg = [self.tile(ph, f"wg{i}", [128, 8, 512], BF16) for i in range(2)]
            wu = [self.tile(ph, f"wu{i}", [128, 8, 512], BF16) for i in range(2)]
            wd = {cls: [self.tile(ph, f"wd{cls}_{i}", [128, 4, D], BF16) for i in range(2)] for cls in classes}
            xg = self.tile(ph, "xg", [128, 10, D], F32)
            h2T = self.tile(ph, "h2T", [128, 8, 1280], BF16)
            Wt = self.tile(ph, "Wt", [128, 10, 32], F32)
            xn = self.tile(ph, "xn2", [128, D], F32)
            hTf = self.tile(ph, "hTf", [128, 8, 128], F32)
            ss = self.tile(ph, "ss2", [128, 1], F32)
            r_ = {k: self.tile(ph, "r_" + k, [128, n], F32) for k, n in
                  dict(lg=36, gmax=1, ngmax=1, goh=4, ge=4, gsum=1, pen=4, em=32, m8=8, d=1, ed=1, p1=1, p2=1, t1=32, t2=32).items()}
            hid = [self.tile(ph, f"hid{i}", [128, 4, 512], BF16) for i in range(2)]
            sgl = [self.tile(ph, f"sgl{i}", [128, 512], F32) for i in range(2)]
            self.stg_i = 0

            def load_expert(e, need_ctx):
                i = e % 2
                self.load_cast_w(stg, wg[i], slice(0, 512), self.ex_gate[l, e], 8, 512, 0)
                self.load_cast_w(stg, wu[i], slice(0, 512), self.ex_up[l, e], 8, 512, 0)
                s_ = stg[self.stg_i % 2]
                self.stg_i += 1
                fw.q_sync.dma(s_[:].rearrange("p (k n) -> p k n", k=4), self.ex_down[l, e].rearrange("(k p) n -> p k n", p=128), writes=[s_.b])
                for cls in classes:
                    if cls == 1 and not need_ctx:
                        continue
                    fw.dve.op(lambda: nc.vector.tensor_tensor(out=wd[cls][i][:], in0=s_[:].rearrange("p (k n) -> p k n", k=4),
                                                              in1=G2B[cls][:].unsqueeze(1).to_broadcast([128, 4, D]), op=ALU.mult),
                              [s_.b, G2B[cls].b], [wd[cls][i].b])

            for gi, tiles in enumerate(groups[:ngrp]):
                has_ctx = (l == 0 and gi == 0)
                load_expert(0, has_ctx)
                for ti, t in enumerate(tiles):
                    cls = 1 if t < 2 else 0
                    fw.q_sync.dma(xg[:, ti, :], self.xres[t * 128:(t + 1) * 128, :], reads=[self.xres_b[t]], writes=[xg.b])
                    fw.act.op(lambda: nc.scalar.activation(out=xn[:], in_=xg[:, ti, :], func=AF.Square, accum_out=ss[:, 0:1]), [xg.b], [xn.b, ss.b])
                    self.rstd_from_ss(ss, 1.0 / D, 1)
                    fw.act.op(lambda: nc.scalar.activation(out=xn[:], in_=xg[:, ti, :], func=AF.Copy, scale=ss[:, 0:1]), [xg.b, ss.b], [xn.b])
                    for hb in range(2):
                        p = self.ps_get()
                        pv = p[:].rearrange("p (c q) -> p c q", c=4)
                        for c4 in range(4):
                            c = hb * 4 + c4
                            fw.pe.op(lambda: nc.tensor.transpose(out=pv[:, c4, :], in_=xn[:, c * 128:(c + 1) * 128], identity=g["identf"][:]),
                                     [xn.b, g["identf"].b], [p.b], inc=(c4 == 3))
                        for c4 in range(4):
                            c = hb * 4 + c4
                            if hb == 0:
                                fw.dve.op(lambda: nc.vector.tensor_scalar(out=hTf[:, c, :], in0=pv[:, c4, :], scalar1=g["A2"][:, l, c, cls:cls + 1],
                                                                          scalar2=g["modT"][:, l, 24 + c, cls:cls + 1], op0=ALU.mult, op1=ALU.add),
                                          [p.b, g["A2"].b, g["modT"].b], [hTf.b])
                            else:
                                fw.act.op(lambda: nc.scalar.activation(out=hTf[:, c, :], in_=pv[:, c4, :], func=AF.Identity,
                                                                       scale=g["A2"][:, l, c, cls:cls + 1], bias=g["modT"][:, l, 24 + c, cls:cls + 1]),
                                          [p.b, g["A2"].b, g["modT"].b], [hTf.b])
                        self.ps_put(p)
                    fw.pool.op(lambda: nc.gpsimd.tensor_copy(out=h2T[:, :, ti * 128:(ti + 1) * 128], in_=hTf[:]), [hTf.b], [h2T.b])
                    pr = self.ps_get()
                    for c in range(8):
                        fw.pe.op(lambda: nc.tensor.matmul(pr[:, 0:36], lhsT=hTf[:, c, :], rhs=wr[:, c, :], start=(c == 0), stop=(c == 7)),
                                 [hTf.b, wr.b], [pr.b], inc=(c == 7))
                    self.route(pr, rb, r_, Wt, ti)
                    self.ps_put(pr)
                if self.debug and gi == 0:
                    self.dbg_dump_bf(ph, f"Wt{l}", Wt[:, 0:4, :], [128, 4, 32], Wt.b)
                if has_ctx:
                    chunks = [(0, 2), (2, 4), (6, 4)]
                else:
                    chunks = [(0, 4), (4, 4)]
                for e in range(nexp):
                    if e + 1 < nexp:
                        load_expert(e + 1, has_ctx)
                    i = e % 2
                    for ci, (tl0, ntl) in enumerate(chunks):
                        cls = 1 if (has_ctx and ci == 0) else 0
                        ntok = ntl * 128
                        c0 = tl0 * 128
                        hd = hid[(e * len(chunks) + ci) % 2]
                        for f in range(4):
                            pg = self.ps_get()
                            pu = self.ps_get()
                            for (pp, w_) in ((pg, wg[i]), (pu, wu[i])):
                                for k in range(8):
                                    fw.pe.op(lambda: nc.tensor.matmul(pp[:, 0:ntok], lhsT=w_[:, k, f * 128:(f + 1) * 128], rhs=h2T[:, k, c0:c0 + ntok],
                                                                      start=(k == 0), stop=(k == 7)), [w_.b, h2T.b], [pp.b], inc=(k == 7))
                            sg = sgl[f % 2]
                            fw.act.op(lambda: nc.scalar.activation(out=sg[:, 0:ntok], in_=pg[:, 0:ntok], func=AF.Silu), [pg.b], [sg.b])
                            fw.dve.op(lambda: nc.vector.tensor_tensor(out=hd[:, f, 0:ntok], in0=pu[:, 0:ntok], in1=sg[:, 0:ntok], op=ALU.mult),
                                      [pu.b, sg.b], [hd.b])
                            self.ps_put(pg)
                            self.ps_put(pu)
                        for tl in range(ntl):
                            ti = tl0 + tl
                            for half in range(2):
                                pd = self.ps_get()
                                for f in range(4):
                                    fw.pe.op(lambda: nc.tensor.matmul(pd[:], lhsT=hd[:, f, tl * 128:(tl + 1) * 128],
                                                                      rhs=wd[cls][i][:, f, half * 512:(half + 1) * 512], start=(f == 0), stop=(f == 3)),
                                             [hd.b, wd[cls][i].b], [pd.b], inc=(f == 3))
                                fw.dve.op(lambda: nc.vector.scalar_tensor_tensor(out=xg[:, ti, half * 512:(half + 1) * 512], in0=pd[:],
                                                                                 scalar=Wt[:, ti, e:e + 1], in1=xg[:, ti, half * 512:(half + 1) * 512],
                                                                                 op0=ALU.mult, op1=ALU.add), [pd.b, Wt.b, xg.b], [xg.b])
                                self.ps_put(pd)
                for ti, t in enumerate(tiles):
                    if l == 0:
                        fw.q_pool.dma(self.xres[t * 128:(t + 1) * 128, :], xg[:, ti, :], reads=[xg.b], writes=[self.xres_b[t]])
                    else:
                        fw.q_pool.dma(self.y_out[(t - 2) * 128:(t - 1) * 128, :], xg[:, ti, :], reads=[xg.b], writes=[self.y_b[t]])
                    if self.debug and t in (0, 2, NT - 1):
                        nm = f"x2_{l}_{t}"
                        ap = self.dbg(nm, [128, D])
                        fw.q_pool.dma(ap, xg[:, ti, :], reads=[xg.b], writes=[self.dbg_out[nm][1]])
            fw.barrier()

    def moe2(self, l):
        nc, fw, g = self.nc, self.fw, self.g
        V = nc.vector
        tiles = list(range(NT)) if l == 0 else list(range(2, NT))
        ntl = len(tiles)
        NA = 2 * ntl
        NB = (2 * ntl * 128 + 32 * 127 + 127) // 128
        classes = [0, 1] if l == 0 else [0]
        hs = nc.dram_tensor(f"hs{l}", [NB * 128, D], BF16, kind="Internal").ap()
        ys = nc.dram_tensor(f"ys{l}", [NB * 128, D], F32, kind="Internal").ap()
        blkE_d = nc.dram_tensor(f"blkE{l}", [1, NB], I32, kind="Internal").ap()
        hs_bs = [Buf() for _ in range(NA)]
        ys_bs = [Buf() for _ in range(NB)]
        blkE_b = Buf()
        dd = lambda fn, rd, wr_: fw.dve.op(fn, [x.b for x in rd], [x.b for x in wr_])
        with ExitStack() as pm:
            DESTi = self.tile(pm, "DESTi", [128, NA], I32)
            W12 = self.tile(pm, "W12", [128, NA], F32)
            WIDX = self.tile(pm, "WIDX", [128, NB], I32)
            G2B = {}
            for cls in classes:
                G2B[cls] = self.tile(pm, f"G2Bm{cls}", [128, D], F32)
            with ExitStack() as ph:
                tmpb = self.tile(ph, "bct3", [128, 128], F32)
                A2B, B2B = {}, {}
                for cls in classes:
                    A2B[cls] = self.tile(ph, f"A2B{cls}", [128, D], F32)
                    B2B[cls] = self.tile(ph, f"B2B{cls}", [128, D], F32)
                    self.bcast_tile(G2B[cls], lambda c: g["modT"][:, l, 40 + c, cls:cls + 1], tmpb)
                    self.bcast_tile(A2B[cls], lambda c: g["A2"][:, l, c, cls:cls + 1], tmpb, extra=[g["A2"].b])
                    self.bcast_tile(B2B[cls], lambda c: g["modT"][:, l, 24 + c, cls:cls + 1], tmpb)
                wr = self.tile(ph, "wr", [128, 8, 36], F32)
                rb = self.tile(ph, "rb", [128, 36], F32)
                mc = self.tile(ph, "mc", [128, 433], F32)
                Uf = self.tile(ph, "Uf", [128, 128], F32)
                Ub = self.tile(ph, "Ub", [128, 128], BF16)
                onesb = self.tile(ph, "onesb", [128, 128], BF16)
                fw.q_sync.dma(wr[:], self.rt_w[l].rearrange("(k p) n -> p k n", p=128), writes=[wr.b])
                fw.q_sync.dma(rb[:], self.rt_b[:, l, :], writes=[rb.b])
                fw.q_sync.dma(mc[:], self.mconst, writes=[mc.b])
                fw.q_sync.dma(Uf[:], self.umat, writes=[Uf.b])
                dd(lambda: V.tensor_copy(out=Ub[:], in_=Uf[:]), [Uf], [Ub])
                dd(lambda: V.memset(onesb[:], 1.0), [], [onesb])
                zt = self.tile(ph, "zt", [128, 4096], BF16)
                fw.pool.op(lambda: nc.gpsimd.memset(zt[:], 0.0), [], [zt.b])
                hz_b = []
                for r0 in range(0, NB * 128, 512):
                    nr = min(512, NB * 128 - r0)
                    hb_ = Buf("hz")
                    fw.q_pool.dma(hs[r0:r0 + nr, :].rearrange("(p k) d -> p (k d)", p=128), zt[:, 0:(nr // 128) * D], reads=[zt.b], writes=[hb_])
                    hz_b.append(hb_)
                h2tm = self.tile(ph, "h2tm", [128, ntl, D], BF16)
                OH = self.tile(ph, "OH", [128, NA, 32], BF16)
                RK = self.tile(ph, "RK", [128, NA], F32)
                run = self.tile(ph, "run", [128, 32], F32)
                dd(lambda: V.memset(run[:], 0.0), [], [run])
                xt = [self.tile(ph, f"mxt{i}", [128, D], F32) for i in range(2)]
                xn_l = [self.tile(ph, f"mxn{i}", [128, D], F32) for i in range(2)]
                xm_l = [self.tile(ph, f"mxm{i}", [128, D], F32) for i in range(2)]
                hTf_l = [self.tile(ph, f"mhTf{i}", [128, 8, 128], F32) for i in range(2)]
                ss_l = [self.tile(ph, f"mss{i}", [128, 1], F32) for i in range(2)]
                r_l = [{k: self.tile(ph, f"r{i}_" + k, [128, n], F32) for k, n in
                        dict(lg=36, gmax=1, ngmax=1, goh=4, ge=4, gsum=1, pen=4, em=32, m8=8, d=1, ed=1, p1=1, p2=1, t1=32, t2=32, rf=32).items()}
                       for i in range(2)]
                def tile_fn(ti, t):
                    cls = 1 if t < 2 else 0
                    xn, xm, hTf, ss, r_ = xn_l[ti % 2], xm_l[ti % 2], hTf_l[ti % 2], ss_l[ti % 2], r_l[ti % 2]
                    x_ = xt[ti % 2]
                    fw.q_sync.dma(x_[:], self.xres[t * 128:(t + 1) * 128, :], reads=[self.xres_b[t]], writes=[x_.b])
                    fw.act.op(lambda: nc.scalar.activation(out=xn[:], in_=x_[:], func=AF.Square, accum_out=ss[:, 0:1]), [x_.b], [xn.b, ss.b])
                    self.rstd_from_ss(ss, 1.0 / D, 1)
                    fw.act.op(lambda: nc.scalar.activation(out=xn[:], in_=x_[:], func=AF.Copy, scale=ss[:, 0:1]), [x_.b, ss.b], [xn.b])
                    fw.pool.op(lambda: nc.gpsimd.tensor_tensor(out=xm[:], in0=xn[:], in1=A2B[cls][:], op=ALU.mult), [xn.b, A2B[cls].b], [xm.b])
                    fw.pool.op(lambda: nc.gpsimd.tensor_tensor(out=h2tm[:, ti, :], in0=xm[:], in1=B2B[cls][:], op=ALU.add), [xm.b, B2B[cls].b], [h2tm.b])
                    for hb in range(2):
                        p = self.ps_get()
                        pv = p[:].rearrange("p (c q) -> p c q", c=4)
                        for c4 in range(4):
                            c = hb * 4 + c4
                            fw.pe.op(lambda: nc.tensor.transpose(out=pv[:, c4, :], in_=xn[:, c * 128:(c + 1) * 128], identity=g["identf"][:]),
                                     [xn.b, g["identf"].b], [p.b], inc=(c4 == 3))
                        for c4 in range(4):
                            c = hb * 4 + c4
                            if hb == 0:
                                fw.dve.op(lambda: V.tensor_scalar(out=hTf[:, c, :], in0=pv[:, c4, :], scalar1=g["A2"][:, l, c, cls:cls + 1],
                                                                  scalar2=g["modT"][:, l, 24 + c, cls:cls + 1], op0=ALU.mult, op1=ALU.add),
                                          [p.b, g["A2"].b, g["modT"].b], [hTf.b])
                            else:
                                fw.act.op(lambda: nc.scalar.activation(out=hTf[:, c, :], in_=pv[:, c4, :], func=AF.Identity,
                                                                       scale=g["A2"][:, l, c, cls:cls + 1], bias=g["modT"][:, l, 24 + c, cls:cls + 1]),
                                          [p.b, g["A2"].b, g["modT"].b], [hTf.b])
                        self.ps_put(p)
                    pr = self.ps_get()
                    for c in range(8):
                        fw.pe.op(lambda: nc.tensor.matmul(pr[:, 0:36], lhsT=hTf[:, c, :], rhs=wr[:, c, :], start=(c == 0), stop=(c == 7)),
                                 [hTf.b, wr.b], [pr.b], inc=(c == 7))
                    self.route(pr, rb, r_, None, ti, OH=OH, W12=W12)
                    self.ps_put(pr)

                def rank_fn(ti):
                    r_ = r_l[ti % 2]
                    for k in range(2):
                        a = 2 * ti + k
                        pk = self.ps_get()
                        fw.pe.op(lambda: nc.tensor.matmul(pk[:, 0:32], lhsT=Ub[:], rhs=OH[:, a, :], start=True, stop=True), [Ub.b, OH.b], [pk.b], inc=False)
                        fw.pe.op(lambda: nc.tensor.matmul(pk[:, 32:64], lhsT=onesb[:], rhs=OH[:, a, :], start=True, stop=True), [onesb.b, OH.b], [pk.b])
                        rf = r_["rf"]
                        dd(lambda: V.tensor_tensor(out=rf[:], in0=pk[:, 0:32], in1=run[:], op=ALU.add), [pk, run], [rf])
                        dd(lambda: V.tensor_tensor(out=rf[:], in0=rf[:], in1=OH[:, a, :], op=ALU.mult), [rf, OH], [rf])
                        dd(lambda: V.tensor_reduce(out=RK[:, a:a + 1], in_=rf[:], axis=AX.X, op=ALU.add), [rf], [RK])
                        dd(lambda: V.tensor_tensor(out=run[:], in0=pk[:, 32:64], in1=run[:], op=ALU.add), [pk, run], [run])
                        self.ps_put(pk)
                for ti0 in range(0, ntl, 2):
                    pair = list(range(ti0, min(ti0 + 2, ntl)))
                    ILV.run([(lambda ti=ti: tile_fn(ti, tiles[ti])) for ti in pair])
                    for ti in pair:
                        rank_fn(ti)
                cmpf = self.tile(ph, "cmp", [128, 5120], F32)
                v3 = lambda n_a, n_b: cmpf[:, 0:n_a * n_b].rearrange("p (a b) -> p a b", b=n_b)
                T_ = lambda nm, n: self.tile(ph, nm, [128, n], F32)
                nblk, exc, nbp, mm, pbx = T_("nblk", 32), T_("exc", 32), T_("nbp", 32), T_("mm", 32), T_("pbx", 32)
                sc = [T_(f"scan{i}", 32) for i in range(2)]
                blkf, dstf = T_("blkf", NB), T_("dstf", NA)
                selX, selM, selS, jf, ta_, tb_ = T_("selX", NA), T_("selM", NA), T_("selS", NA), T_("jf", NA), T_("ta_", NA), T_("tb_", NA)
                pend, pbase, m2k, lsk = T_("pend", 16), T_("pbase", 16), T_("m2k", 16), T_("lsk", 16)
                kidx, pbp, m2p, lsp, o_, q_, par_, int_ = (T_(n_, NB) for n_ in ("kidx", "pbp", "m2p", "lsp", "o_", "q_", "par_", "int_"))
                IOTA32, THR, IOTAB, PAR32, PARB, THR2, THR3, IOTA16 = (mc[:, 0:32], mc[:, 32:67], mc[:, 67:67 + NB], mc[:, 167:199],
                                                                     mc[:, 199:199 + NB], mc[:, 299:367], mc[:, 367:417], mc[:, 417:433])
                c3 = v3(32, 35)
                dd(lambda: V.tensor_tensor(out=c3, in0=run[:].unsqueeze(2).to_broadcast([128, 32, 35]),
                                           in1=THR.unsqueeze(1).to_broadcast([128, 32, 35]), op=ALU.is_gt), [run, mc], [cmpf])
                dd(lambda: V.tensor_reduce(out=nblk[:], in_=c3, axis=AX.X, op=ALU.add), [cmpf], [nblk])
                dd(lambda: V.tensor_copy(out=sc[0][:], in_=nblk[:]), [nblk], [sc[0]])
                cur = 0
                for sh in (1, 2, 4, 8, 16):
                    a_, b_ = sc[cur], sc[1 - cur]
                    dd(lambda: V.tensor_copy(out=b_[:, 0:sh], in_=a_[:, 0:sh]), [a_], [b_])
                    dd(lambda: V.tensor_tensor(out=b_[:, sh:32], in0=a_[:, sh:32], in1=a_[:, 0:32 - sh], op=ALU.add), [a_], [b_])
                    cur = 1 - cur
                inc_ = sc[cur]
                dd(lambda: V.tensor_tensor(out=exc[:], in0=inc_[:], in1=nblk[:], op=ALU.subtract), [inc_, nblk], [exc])
                pr2 = lambda tl: tl[:].rearrange("p (k s) -> p k s", s=2)
                dd(lambda: V.tensor_copy(out=pr2(nbp)[:, :, 0:1], in_=pr2(nblk)[:, :, 1:2]), [nblk], [nbp])
                dd(lambda: V.tensor_copy(out=pr2(nbp)[:, :, 1:2], in_=pr2(nblk)[:, :, 0:1]), [nblk], [nbp])
                dd(lambda: V.tensor_tensor(out=mm[:], in0=nblk[:], in1=nbp[:], op=ALU.min), [nblk, nbp], [mm])
                dd(lambda: V.tensor_scalar(out=pr2(pbx)[:, :, 0:1], in0=pr2(exc)[:, :, 0:1], scalar1=128.0, scalar2=None, op0=ALU.mult), [exc], [pbx])
                dd(lambda: V.tensor_scalar(out=pr2(pbx)[:, :, 1:2], in0=pr2(exc)[:, :, 0:1], scalar1=128.0, scalar2=None, op0=ALU.mult), [exc], [pbx])
                c4_ = v3(NA, 32)
                for (dst_, vec_, vb_) in ((selX, pbx[:], pbx.b), (selM, mm[:], mm.b), (selS, PAR32, mc.b)):
                    dd(lambda: V.tensor_tensor(out=c4_, in0=OH[:], in1=vec_.unsqueeze(1).to_broadcast([128, NA, 32]), op=ALU.mult), [OH, Tile(None, vb_)], [cmpf])
                    dd(lambda: V.tensor_reduce(out=dst_[:], in_=c4_, axis=AX.X, op=ALU.add), [cmpf], [dst_])
                c5_ = v3(NA, 68)
                dd(lambda: V.tensor_tensor(out=c5_, in0=RK[:].unsqueeze(2).to_broadcast([128, NA, 68]),
                                           in1=THR2.unsqueeze(1).to_broadcast([128, NA, 68]), op=ALU.is_ge), [RK, mc], [cmpf])
                dd(lambda: V.tensor_reduce(out=jf[:], in_=c5_, axis=AX.X, op=ALU.add), [cmpf], [jf])
                dd(lambda: V.scalar_tensor_tensor(out=ta_[:], in0=jf[:], scalar=2.0, in1=selS[:], op0=ALU.mult, op1=ALU.add), [jf, selS], [ta_])
                dd(lambda: V.tensor_tensor(out=tb_[:], in0=selM[:], in1=jf[:], op=ALU.add), [selM, jf], [tb_])
                dd(lambda: V.tensor_tensor(out=ta_[:], in0=ta_[:], in1=tb_[:], op=ALU.min), [ta_, tb_], [ta_])
                dd(lambda: V.tensor_tensor(out=ta_[:], in0=ta_[:], in1=jf[:], op=ALU.subtract), [ta_, jf], [ta_])
                dd(lambda: V.scalar_tensor_tensor(out=dstf[:], in0=ta_[:], scalar=128.0, in1=selX[:], op0=ALU.mult, op1=ALU.add), [ta_, selX], [dstf])
                dd(lambda: V.tensor_tensor(out=dstf[:], in0=dstf[:], in1=RK[:], op=ALU.add), [dstf, RK], [dstf])
                dd(lambda: V.tensor_copy(out=DESTi[:], in_=dstf[:]), [dstf], [DESTi])
                dd(lambda: V.tensor_copy(out=pend[:], in_=pr2(inc_)[:, :, 1]), [inc_], [pend])
                dd(lambda: V.tensor_copy(out=pbase[:], in_=pr2(exc)[:, :, 0]), [exc], [pbase])
                dd(lambda: V.tensor_scalar(out=m2k[:], in0=pr2(mm)[:, :, 0], scalar1=2.0, scalar2=None, op0=ALU.mult), [mm], [m2k])
                dd(lambda: V.tensor_tensor(out=lsk[:], in0=pr2(nblk)[:, :, 1], in1=pr2(nblk)[:, :, 0], op=ALU.is_gt), [nblk], [lsk])
                c6_ = v3(NB, 16)
                dd(lambda: V.tensor_tensor(out=c6_, in0=pend[:].unsqueeze(1).to_broadcast([128, NB, 16]),
                                           in1=IOTAB.unsqueeze(2).to_broadcast([128, NB, 16]), op=ALU.is_le), [pend, mc], [cmpf])
                dd(lambda: V.tensor_reduce(out=kidx[:], in_=c6_, axis=AX.X, op=ALU.add), [cmpf], [kidx])
                dd(lambda: V.tensor_scalar(out=kidx[:], in0=kidx[:], scalar1=15.0, scalar2=None, op0=ALU.min), [kidx], [kidx])
                ohk = self.tile(ph, "ohk", [128, NB, 16], F32)
                dd(lambda: V.tensor_tensor(out=ohk[:], in0=kidx[:].unsqueeze(2).to_broadcast([128, NB, 16]),
                                           in1=IOTA16.unsqueeze(1).to_broadcast([128, NB, 16]), op=ALU.is_equal), [kidx, mc], [ohk])
                for (dst_, vec_) in ((pbp, pbase), (m2p, m2k), (lsp, lsk)):
                    dd(lambda: V.tensor_tensor(out=c6_, in0=ohk[:], in1=vec_[:].unsqueeze(1).to_broadcast([128, NB, 16]), op=ALU.mult), [ohk, vec_], [cmpf])
                    dd(lambda: V.tensor_reduce(out=dst_[:], in_=c6_, axis=AX.X, op=ALU.add), [cmpf], [dst_])
                dd(lambda: V.tensor_tensor(out=o_[:], in0=IOTAB, in1=pbp[:], op=ALU.subtract), [mc, pbp], [o_])
                c7_ = v3(NB, 50)
                dd(lambda: V.tensor_tensor(out=c7_, in0=o_[:].unsqueeze(2).to_broadcast([128, NB, 50]),
                                           in1=THR3.unsqueeze(1).to_broadcast([128, NB, 50]), op=ALU.is_ge), [o_, mc], [cmpf])
                dd(lambda: V.tensor_reduce(out=q_[:], in_=c7_, axis=AX.X, op=ALU.add), [cmpf], [q_])
                dd(lambda: V.scalar_tensor_tensor(out=par_[:], in0=q_[:], scalar=-2.0, in1=o_[:], op0=ALU.mult, op1=ALU.add), [q_, o_], [par_])
                dd(lambda: V.tensor_tensor(out=int_[:], in0=o_[:], in1=m2p[:], op=ALU.is_lt), [o_, m2p], [int_])
                dd(lambda: V.tensor_tensor(out=par_[:], in0=par_[:], in1=lsp[:], op=ALU.subtract), [par_, lsp], [par_])
                dd(lambda: V.tensor_tensor(out=par_[:], in0=par_[:], in1=int_[:], op=ALU.mult), [par_, int_], [par_])
                dd(lambda: V.tensor_tensor(out=par_[:], in0=par_[:], in1=lsp[:], op=ALU.add), [par_, lsp], [par_])
                dd(lambda: V.scalar_tensor_tensor(out=blkf[:], in0=kidx[:], scalar=2.0, in1=par_[:], op0=ALU.mult, op1=ALU.add), [kidx, par_], [blkf])
                pix = self.tile(ph, "pix", [128, 1], F32)
                fw.q_sync.dma(pix[:], self.pidx, writes=[pix.b])
                dd(lambda: V.tensor_scalar(out=blkf[:], in0=blkf[:], scalar1=128.0, scalar2=pix[:, 0:1], op0=ALU.mult, op1=ALU.add), [blkf, pix], [blkf])
                if l == 1:
                    dd(lambda: V.tensor_scalar(out=blkf[:], in0=blkf[:], scalar1=4096.0, scalar2=None, op0=ALU.add), [blkf], [blkf])
                same2 = self.tile(ph, "same2", [128, NB], F32)
                dd(lambda: V.tensor_tensor(out=same2[:, 2:NB], in0=blkf[:, 2:NB], in1=blkf[:, 0:NB - 2], op=ALU.is_equal), [blkf], [same2])
                dd(lambda: V.tensor_scalar(out=same2[:, 2:NB], in0=same2[:, 2:NB], scalar1=1.0e6, scalar2=None, op0=ALU.mult), [same2], [same2])
                dd(lambda: V.tensor_tensor(out=blkf[:, 2:NB], in0=blkf[:, 2:NB], in1=same2[:, 2:NB], op=ALU.add), [blkf, same2], [blkf])
                dd(lambda: V.tensor_copy(out=WIDX[:], in_=blkf[:]), [blkf], [WIDX])
                if self.debug:
                    self.dbg_dump_bf(ph, f"dst{l}", dstf[:, 0:8], [128, 8], dstf.b)
                    self.dbg_dump_bf(ph, f"blk{l}", blkf[:, 0:NB], [128, NB], blkf.b)
                    self.dbg_dump_bf(ph, f"cnt{l}", run[:, 0:32], [128, 32], run.b)
                if os.environ.get("MOE_PROBE") == "1":
                    def tryv(nm, f):
                        try:
                            f(); print("PROBE ok", nm, flush=True)
                        except Exception as e:
                            print("PROBE fail", nm, repr(e)[:120], flush=True)
                    tryv("base", lambda: nc.gpsimd.indirect_dma_start(out=hs[:, :], out_offset=bass.IndirectOffsetOnAxis(ap=DESTi[:, 0:1], axis=0),
                                                                      in_=h2tm[:, 0, :], in_offset=None, bounds_check=NB * 128 - 1, oob_is_err=False))
                    tryv("xn_f32_ys", lambda: nc.gpsimd.indirect_dma_start(out=ys[:, :], out_offset=bass.IndirectOffsetOnAxis(ap=DESTi[:, 0:1], axis=0),
                                                                      in_=xn[:, :], in_offset=None, bounds_check=NB * 128 - 1, oob_is_err=False))
                    tryv("blki_idx", lambda: nc.gpsimd.indirect_dma_start(out=hs[:, :], out_offset=bass.IndirectOffsetOnAxis(ap=blki[:, 0:1], axis=0),
                                                                      in_=h2tm[:, 0, :], in_offset=None, bounds_check=NB * 128 - 1, oob_is_err=False))
                    tryv("gather", lambda: nc.gpsimd.indirect_dma_start(out=xn[:, :], out_offset=None, in_=ys[:, :],
                                                                      in_offset=bass.IndirectOffsetOnAxis(ap=DESTi[:, 0:1], axis=0), bounds_check=NB * 128 - 1, oob_is_err=False))
                    tryv("plain", lambda: nc.gpsimd.dma_start(out=ys[0:128, :], in_=xn[:, :]))
                for a in range(NA):
                    if os.environ.get("MOE_PROBE") == "1":
                        print("PROBE scatter a", a, flush=True)
                    fw.q_pool.dma_fn(lambda: nc.gpsimd.indirect_dma_start(
                        out=hs[:, :], out_offset=bass.IndirectOffsetOnAxis(ap=DESTi[:, a:a + 1], axis=0),
                        in_=h2tm[:, a // 2, :], in_offset=None),
                        reads=[h2tm.b, DESTi.b] + hz_b, writes=[hs_bs[a]])
                fw.barrier()
            with ExitStack() as ph:
                stg = {k: [self.tile(ph, f"bs{k}{i}", [128, 4096], F32) for i in range(2)] for k in "gud"}
                wgt = {k: [self.tile(ph, f"bw{k}{i}", [128, 4096], BF16) for i in range(2)] for k in "gud"}
                xb = [self.tile(ph, f"xb{i}", [128, D], BF16) for i in range(4)]
                xbT = [self.tile(ph, f"xbT{i}", [128, 8, 128], BF16) for i in range(2)]
                sg_l = [self.tile(ph, f"bsg{i}", [128, 512], F32) for i in range(2)]
                hid_l = [self.tile(ph, f"bhid{i}", [128, 512], BF16) for i in range(2)]
                hidT_l = [self.tile(ph, f"bhidT{i}", [128, 4, 128], BF16) for i in range(2)]
                ysb = [self.tile(ph, f"ysb{i}", [128, D], F32) for i in range(2)]
                srcs = dict(g=self.ex_gate, u=self.ex_up, d=self.ex_down)

                def gathers(i):
                    b2 = i % 2
                    for k in "gud":
                        fw.q_pool.dma_fn(lambda: nc.gpsimd.indirect_dma_start(
                            out=stg[k][b2][:, :], out_offset=None, in_=srcs[k].rearrange("l r n -> (l r) n"),
                            in_offset=bass.IndirectOffsetOnAxis(ap=WIDX[:, i:i + 1], axis=0),
                            bounds_check=self.bc_reg, oob_is_err=False),
                            reads=[WIDX.b], writes=[stg[k][b2].b])

                def casts(i):
                    b2 = i % 2
                    fw.dve.op(lambda: V.tensor_copy(out=wgt["g"][b2][:], in_=stg["g"][b2][:]), [stg["g"][b2].b], [wgt["g"][b2].b])
                    fw.act.op(lambda: nc.scalar.copy(out=wgt["u"][b2][:], in_=stg["u"][b2][:]), [stg["u"][b2].b], [wgt["u"][b2].b])
                    fw.dve.op(lambda: V.tensor_copy(out=wgt["d"][b2][:, 0:2048], in_=stg["d"][b2][:, 0:2048]), [stg["d"][b2].b], [wgt["d"][b2].b])
                    fw.act.op(lambda: nc.scalar.copy(out=wgt["d"][b2][:, 2048:4096], in_=stg["d"][b2][:, 2048:4096]), [stg["d"][b2].b], [wgt["d"][b2].b])

                def xload(i):
                    fw.q_sync.dma(xb[i % 4][:], hs[i * 128:(i + 1) * 128, :], reads=hs_bs, writes=[xb[i % 4].b])

                def block_fn(i):
                    b2 = i % 2
                    sg, hid, hidT = sg_l[b2], hid_l[b2], hidT_l[b2]
                    wg_ = wgt["g"][b2][:].rearrange("p (k n) -> p k n", k=8)
                    wu_ = wgt["u"][b2][:].rearrange("p (k n) -> p k n", k=8)
                    wd_ = wgt["d"][b2][:].rearrange("p (k n) -> p k n", k=4)
                    p = self.ps_get()
                    pv = p[:].bitcast(BF16).rearrange("p (c q) -> p c q", c=8)
                    for c in range(8):
                        fw.pe.op(lambda: nc.tensor.transpose(out=pv[:, c, :], in_=xb[i % 4][:, c * 128:(c + 1) * 128], identity=g["identb"][:]),
                                 [xb[i % 4].b, g["identb"].b], [p.b], inc=(c == 7))
                    dd(lambda: V.tensor_copy(out=xbT[b2][:], in_=pv), [p], [xbT[b2]])
                    self.ps_put(p)
                    pg = self.ps_get()
                    pu = self.ps_get()
                    for (pp, w_, wb_) in ((pg, wg_, wgt["g"][b2].b), (pu, wu_, wgt["u"][b2].b)):
                        for k in range(8):
                            fw.pe.op(lambda: nc.tensor.matmul(pp[:], lhsT=xbT[b2][:, k, :], rhs=w_[:, k, :], start=(k == 0), stop=(k == 7)),
                                     [xbT[b2].b, wb_], [pp.b], inc=(k == 7))
                    fw.act.op(lambda: nc.scalar.activation(out=sg[:], in_=pg[:], func=AF.Silu), [pg.b], [sg.b])
                    dd(lambda: V.tensor_tensor(out=hid[:], in0=pu[:], in1=sg[:], op=ALU.mult), [pu, sg], [hid])
                    self.ps_put(pg)
                    self.ps_put(pu)
                    p = self.ps_get()
                    pv = p[:].bitcast(BF16).rearrange("p (c q) -> p c q", c=8)
                    for c in range(4):
                        fw.pe.op(lambda: nc.tensor.transpose(out=pv[:, c, :], in_=hid[:, c * 128:(c + 1) * 128], identity=g["identb"][:]),
                                 [hid.b, g["identb"].b], [p.b], inc=(c == 3))
                    dd(lambda: V.tensor_copy(out=hidT[:], in_=pv[:, 0:4, :]), [p], [hidT])
                    self.ps_put(p)
                    y_ = ysb[b2]
                    pds = []
                    for half in range(2):
                        pd = self.ps_get()
                        pds.append(pd)
                        for f in range(4):
                            fw.pe.op(lambda: nc.tensor.matmul(pd[:], lhsT=hidT[:, f, :], rhs=wd_[:, f, half * 512:(half + 1) * 512],
                                                              start=(f == 0), stop=(f == 3)), [hidT.b, wgt["d"][b2].b], [pd.b], inc=(f == 3))
                    if i + 2 < NB:
                        casts(i + 2)
                    fw.act.op(lambda: nc.scalar.copy(out=y_[:, 0:512], in_=pds[0][:]), [pds[0].b], [y_.b])
                    dd(lambda: V.tensor_copy(out=y_[:, 512:1024], in_=pds[1][:]), [pds[1]], [y_])
                    self.ps_put(pds[0])
                    self.ps_put(pds[1])
                    fw.q_sync.dma(ys[i * 128:(i + 1) * 128, :], y_[:], reads=[y_.b], writes=[ys_bs[i]])

                gathers(0)
                gathers(1)
                for j in range(2):
                    xload(j)
                casts(0)
                casts(1)
                for i0_ in range(0, NB, 2):
                    for j in (i0_ + 2, i0_ + 3):
                        if j < NB:
                            gathers(j)
                            xload(j)
                    ILV.run([(lambda i=i: block_fn(i)) for i in (i0_, i0_ + 1) if i < NB])
                fw.barrier()
            with ExitStack() as ph:
                xt = [self.tile(ph, f"cxt{i}", [128, D], F32) for i in range(3)]
                y1 = [self.tile(ph, f"cy1{i}", [128, D], F32) for i in range(3)]
                y2 = [self.tile(ph, f"cy2{i}", [128, D], F32) for i in range(3)]
                def comb_fn(ti, t):
                    cls = 1 if t < 2 else 0
                    x_, a1, a2 = xt[ti % 3], y1[ti % 3], y2[ti % 3]
                    fw.q_sync.dma(x_[:], self.xres[t * 128:(t + 1) * 128, :], reads=[self.xres_b[t]], writes=[x_.b])
                    for k, yy in ((0, a1), (1, a2)):
                        a = 2 * ti + k
                        fw.q_pool.dma_fn(lambda: nc.gpsimd.indirect_dma_start(
                            out=yy[:, :], out_offset=None, in_=ys[:, :],
                            in_offset=bass.IndirectOffsetOnAxis(ap=DESTi[:, a:a + 1], axis=0)),
                            reads=ys_bs + [DESTi.b], writes=[yy.b])
                    dd(lambda: V.tensor_scalar(out=a1[:], in0=a1[:], scalar1=W12[:, 2 * ti:2 * ti + 1], scalar2=None, op0=ALU.mult), [a1, W12], [a1])
                    dd(lambda: V.scalar_tensor_tensor(out=a1[:], in0=a2[:], scalar=W12[:, 2 * ti + 1:2 * ti + 2], in1=a1[:], op0=ALU.mult, op1=ALU.add),
                       [a2, W12, a1], [a1])
                    dd(lambda: V.tensor_tensor(out=a1[:], in0=a1[:], in1=G2B[cls][:], op=ALU.mult), [a1, G2B[cls]], [a1])
                    dd(lambda: V.tensor_tensor(out=x_[:], in0=x_[:], in1=a1[:], op=ALU.add), [x_, a1], [x_])
                    if l == 0:
                        fw.q_sync.dma(self.xres[t * 128:(t + 1) * 128, :], x_[:], reads=[x_.b], writes=[self.xres_b[t]])
                    else:
                        fw.q_sync.dma(self.y_out[(t - 2) * 128:(t - 1) * 128, :], x_[:], reads=[x_.b], writes=[self.y_b[t]])
                    if self.debug and t in (0, 2, NT - 1):
                        nm = f"x2_{l}_{t}"
                        ap = self.dbg(nm, [128, D])
                        fw.q_sync.dma(ap, x_[:], reads=[x_.b], writes=[self.dbg_out[nm][1]])

                for ti0 in range(0, ntl, 3):
                    pair = list(range(ti0, min(ti0 + 3, ntl)))
                    ILV.run([(lambda ti=ti: comb_fn(ti, tiles[ti])) for ti in pair])
                fw.barrier()

    def route(self, pr, rb, r_, Wt, ti, OH=None, W12=None):
        nc, fw = self.nc, self.fw
        V = nc.vector
        d = lambda fn, rd, wr_: fw.dve.op(fn, [x.b for x in rd], [x.b for x in wr_])
        lg, gmax, ngmax, goh, ge, gsum, pen, em, m8 = (r_[k] for k in ("lg", "gmax", "ngmax", "goh", "ge", "gsum", "pen", "em", "m8"))
        dd, ed, p1, p2, t1, t2 = (r_[k] for k in ("d", "ed", "p1", "p2", "t1", "t2"))
        d(lambda: V.tensor_tensor(out=lg[:], in0=pr[:, 0:36], in1=rb[:], op=ALU.add), [pr, rb], [lg])
        d(lambda: V.tensor_reduce(out=gmax[:], in_=lg[:, 0:4], axis=AX.X, op=ALU.max), [lg], [gmax])
        d(lambda: V.tensor_scalar(out=ngmax[:], in0=gmax[:], scalar1=-1.0, scalar2=None, op0=ALU.mult), [gmax], [ngmax])
        d(lambda: V.tensor_scalar(out=pen[:], in0=lg[:, 0:4], scalar1=gmax[:, 0:1], scalar2=None, op0=ALU.is_ge), [lg, gmax], [pen])
        d(lambda: V.tensor_scalar(out=pen[:], in0=pen[:], scalar1=1e30, scalar2=-1e30, op0=ALU.mult, op1=ALU.add), [pen], [pen])
        fw.act.op(lambda: nc.scalar.activation(out=ge[:], in_=lg[:, 0:4], func=AF.Exp, bias=ngmax[:, 0:1], accum_out=gsum[:, 0:1]),
                  [lg.b, ngmax.b], [ge.b, gsum.b])
        d(lambda: V.tensor_tensor(out=em[:].rearrange("p (a b) -> p a b", a=4), in0=lg[:, 4:36].rearrange("p (a b) -> p a b", a=4),
                                  in1=pen[:].unsqueeze(2).to_broadcast([128, 4, 8]), op=ALU.add), [lg, pen], [em])
        d(lambda: V.max(out=m8[:], in_=em[:]), [em], [m8])
        d(lambda: V.tensor_tensor(out=dd[:], in0=m8[:, 1:2], in1=m8[:, 0:1], op=ALU.subtract), [m8], [dd])
        fw.act.op(lambda: nc.scalar.activation(out=ed[:], in_=dd[:], func=AF.Exp), [dd.b], [ed.b])
        d(lambda: V.tensor_scalar(out=p1[:], in0=ed[:], scalar1=1.0, scalar2=None, op0=ALU.add), [ed], [p1])
        d(lambda: V.tensor_tensor(out=p1[:], in0=p1[:], in1=gsum[:], op=ALU.mult), [p1, gsum], [p1])
        d(lambda: V.reciprocal(out=p1[:], in_=p1[:]), [p1], [p1])
        d(lambda: V.tensor_tensor(out=p2[:], in0=p1[:], in1=ed[:], op=ALU.mult), [p1, ed], [p2])
        if OH is not None:
            for k in range(2):
                a = 2 * ti + k
                d(lambda: V.tensor_scalar(out=OH[:, a, :], in0=em[:], scalar1=m8[:, k:k + 1], scalar2=None, op0=ALU.is_equal), [em, m8], [OH])
                pk_ = p1 if k == 0 else p2
                d(lambda: V.tensor_copy(out=W12[:, a:a + 1], in_=pk_[:]), [pk_], [W12])
            return
        d(lambda: V.tensor_scalar(out=t1[:], in0=em[:], scalar1=m8[:, 0:1], scalar2=p1[:, 0:1], op0=ALU.is_equal, op1=ALU.mult), [em, m8, p1], [t1])
        d(lambda: V.tensor_scalar(out=t2[:], in0=em[:], scalar1=m8[:, 1:2], scalar2=p2[:, 0:1], op0=ALU.is_equal, op1=ALU.mult), [em, m8, p2], [t2])
        d(lambda: V.tensor_tensor(out=Wt[:, ti, :], in0=t1[:], in1=t2[:], op=ALU.add), [t1, t2], [Wt])

    def layer1_mixer(self):
        nc, fw, g = self.nc, self.fw, self.g
        l = 1
        PADU = 16
        with ExitStack() as ph:
            oT = self.tile(ph, "oT1", [128, 4, T], BF16)
            uT = self.tile(ph, "uT", [128, 4, S + 2 * PADU], BF16)
            pattn = ExitStack()
            qT = self.tile(pattn, "qT1", [128, NT, 4, 128], BF16)
            kT = self.tile(pattn, "kT1", [128, 2, T], BF16)
            Vp = self.tile(pattn, "Vp1", [128, NT, 200], BF16)
            self.v_init(Vp)
            fw.pool.op(lambda: nc.gpsimd.memset(kT[:], 0.0), [], [kT.b])
            fw.pool.op(lambda: nc.gpsimd.memset(uT[:], 0.0), [], [uT.b])
            with ExitStack() as p1:
                win = self.tile(p1, "win1", [128, 8, 1280], BF16)
                gqk = self.tile(p1, "gqk1", [128, 640], F32)
                g["cos"] = self.tile(p1, "cos1", [128, 32, 32], F32)
                g["sin"] = self.tile(p1, "sin1", [128, 32, 32], F32)
                fw.q_sync.dma(gqk[:], self.gqk_c, writes=[gqk.b])
                fw.q_sync.dma(g["cos"][:], self.cosT, writes=[g["cos"].b])
                fw.q_sync.dma(g["sin"][:], self.sinT, writes=[g["sin"].b])
                fw.dve.op(lambda: nc.vector.tensor_scalar(out=gqk[:, 0:512], in0=gqk[:, 0:512], scalar1=0.125, scalar2=None, op0=ALU.mult),
                          [gqk.b], [gqk.b])
                with ExitStack() as pw:
                    stg = [self.tile(pw, f"wstg1{i}", [128, 4096], F32) for i in range(2)]
                    self.stg_i = 0
                    for n in range(3):
                        w_ = 512 if n < 2 else 256
                        self.load_cast_w(stg, win, slice(n * 512, n * 512 + w_), self.w_in_cd[:, n * 512:n * 512 + w_], 8, w_, n)
                    fw.barrier()
                ssl_ = [self.tile(p1, f"ss1{i}", [128, 1], F32) for i in range(2)]
                xnl_ = [self.tile(p1, f"xn1{i}", [128, D], BF16) for i in range(2)]
                scr_l = [dict(ssl=[ssl_[i]], xnl=[xnl_[i]], sq=self.tile(p1, f"sq1{i}", [128, 640], F32),
                              ssq=self.tile(p1, f"ssq1{i}", [128, 10], F32), qn=self.tile(p1, f"qn1{i}", [128, 640], F32),
                              qr=self.tile(p1, f"qr1{i}", [128, 640], BF16), rt=self.tile(p1, f"rt1{i}", [128, 2, 320], F32)) for i in range(2)]
                xt = [self.tile(p1, f"xt1{i}", [128, D], F32) for i in range(2)]
                hT = self.tile(p1, "hT1", [128, 8, 512], BF16)
                qkv = [self.tile(p1, f"qkv1{i}", [128, 768], F32) for i in range(2)]
                chunks = [(0, 2)] + [(2 + 4 * i, 4) for i in range(8)]
                for ci, (t0, ntl) in enumerate(chunks):
                    h = hT
                    ntok = ntl * 128
                    is_ctx = t0 < 2
                    cls = 1 if is_ctx else 0
                    def norm_fn(tl):
                        t = t0 + tl
                        x_ = xt[tl % 2]
                        fw.q_sync.dma(x_[:], self.xres[t * 128:(t + 1) * 128, :], reads=[self.xres_b[t]], writes=[x_.b])
                        self.norm_tile_to_hT(x_, h, tl * 128, l, 1, cls, scr_l[tl % 2])

                    def qkv_fn(tl):
                        t = t0 + tl
                        qk_ = qkv[tl % 2]
                        for (c0, w_) in ((0, 512), (512, 256)):
                            if is_ctx and c0 == 0:
                                continue
                            p = self.ps_get()
                            for k in range(8):
                                fw.pe.op(lambda: nc.tensor.matmul(p[:, 0:w_], lhsT=h[:, k, tl * 128:(tl + 1) * 128], rhs=win[:, k, c0:c0 + w_],
                                                                  start=(k == 0), stop=(k == 7)), [h.b, win.b], [p.b], inc=(k == 7))
                            if c0 == 0:
                                fw.act.op(lambda: nc.scalar.copy(out=qk_[:, 0:512].rearrange("p (j a d) -> p a j d", a=2, d=64),
                                                                 in_=p[:, 0:512].rearrange("p (a j d) -> p a j d", a=2, j=4)), [p.b], [qk_.b])
                            else:
                                fw.act.op(lambda: nc.scalar.copy(out=qk_[:, c0:c0 + w_], in_=p[:, 0:w_]), [p.b], [qk_.b])
                            self.ps_put(p)
                        self.qk_post(qk_, t, gqk, scr_l[tl % 2], qT, kT, not is_ctx, None if is_ctx else t - 2)
                        self.v_fill(qk_, t, Vp)

                    for tl0 in range(0, ntl, 2):
                        ILV.run([(lambda tl=tl: norm_fn(tl)) for tl in range(tl0, min(tl0 + 2, ntl))])
                    for tl0 in range(0, ntl, 2):
                        ILV.run([(lambda tl=tl: qkv_fn(tl)) for tl in range(tl0, min(tl0 + 2, ntl))])
                    if not is_ctx:
                        tok0 = (t0 - 2) * 128
                        for j in range(4):
                            pu = self.ps_get()
                            for k in range(8):
                                fw.pe.op(lambda: nc.tensor.matmul(pu[:, 0:ntok], lhsT=win[:, k, 768 + j * 128:768 + (j + 1) * 128], rhs=h[:, k, 0:ntok],
                                                                  start=(k == 0), stop=(k == 7)), [win.b, h.b], [pu.b], inc=(k == 7))
                            fw.dve.op(lambda: nc.vector.tensor_copy(out=uT[:, j, PADU + tok0:PADU + tok0 + ntok], in_=pu[:, 0:ntok]), [pu.b], [uT.b])
                            self.ps_put(pu)
                fw.barrier()
            if self.stop == "l1p1":
                pattn.close()
                return
            with ExitStack() as p2:
                wm = self.tile(p2, "wm", [128, 2, 512], BF16)
                sk = self.tile(p2, "sk", [128, 2, 512], F32)
                with ExitStack() as pw:
                    wmf = self.tile(pw, "wmf", [128, 2, 512], F32)
                    fw.q_sync.dma(wmf[:], self.wmask, writes=[wmf.b])
                    fw.dve.op(lambda: nc.vector.tensor_copy(out=wm[:], in_=wmf[:]), [wmf.b], [wm.b])
                    fw.q_sync.dma(sk[:], self.sink_rep, writes=[sk.b])
                    fw.act.op(lambda: nc.scalar.activation(out=sk[:], in_=sk[:], func=AF.Exp), [sk.b], [sk.b])
                    fw.barrier()
                scr = dict(pexp=[self.tile(p2, f"pexp1{i}", [128, 1024], BF16) for i in range(4)], pi=0,
                           rec=[self.tile(p2, f"rec1{i}", [128, 512], F32) for i in range(2)],
                           posb=[[self.tile(p2, f"posb1{i}{k}", [128, 512], F32) for k in range(2)] for i in range(2)])
                blocks = []
                for t in range(2, NT):
                    kts = [(0, None), (1, None)]
                    if t > 2:
                        kts.append((t - 1, 0))
                    kts.append((t, None))
                    if t < NT - 1:
                        kts.append((t + 1, 1))
                    for kv in range(2):
                        blocks.append((kv, t, kts))
                self.attention(blocks, qT, kT, Vp, oT, scr, masks=wm, sinkrow=sk)
                fw.barrier()
            pattn.close()
            if self.debug:
                self.dbg_dump_bf(ph, "oT1", oT[:, :, 256:384], [128, 4, 128], oT.b)
                self.dbg_dump_bf(ph, "oT1b", oT[:, :, 640:768], [128, 4, 128], oT.b)
            dT = self.tile(ph, "dT", [128, 4, T], BF16)
            with ExitStack() as p3:
                pwf = self.tile(p3, "pwf", [128, 4, 128], F32)
                pwb = self.tile(p3, "pwb", [128, 4, 128], BF16)
                psc = self.tile(p3, "psc", [128, 4], F32)
                pfx = self.tile(p3, "pfx", [128, 4, 32], F32)
                fw.q_sync.dma(pwf[:], self.pool_w.rearrange("g c d -> c g d"), writes=[pwf.b])
                fw.q_sync.dma(psc[:], self.pool_scT, writes=[psc.b])
                fw.q_sync.dma(pfx[:], self.poolfix, writes=[pfx.b])
                fw.dve.op(lambda: nc.vector.tensor_copy(out=pwb[:], in_=pwf[:]), [pwf.b], [pwb.b])
                ta = [self.tile(p3, f"pta{i}", [128, 528], F32) for i in range(2)]
                pp_ = [self.tile(p3, f"ppb{i}", [128, 512], BF16) for i in range(2)]
                cnt = 0
                for ci in range(8):
                    tok0 = ci * 512
                    for j, w in enumerate((2, 4, 8, 16)):
                        base = PADU + tok0 - w // 2
                        ln = 512 + w - 1
                        cur = uT[:, j, base:base + ln]
                        curb = uT.b
                        step = 1
                        k = 0
                        while step < w:
                            dst = ta[k % 2]
                            e_, ee = (fw.dve, nc.vector) if (cnt % 2 == 0) else (fw.pool, nc.gpsimd)
                            cnt += 1
                            cc, cb_ = cur, curb
                            e_.op(lambda: ee.tensor_tensor(out=dst[:, 0:ln - step], in0=cc[:, 0:ln - step], in1=cc[:, step:ln], op=ALU.add), [cb_], [dst.b])
                            ln -= step
                            step *= 2
                            cur, curb = dst[:, 0:ln], dst.b
                            k += 1
                        pb_ = pp_[j % 2]
                        uc = uT[:, j, PADU + tok0:PADU + tok0 + 512]
                        sdst = ta[k % 2]
                        fw.dve.op(lambda: nc.vector.scalar_tensor_tensor(out=pb_[:], in0=cur[:, 0:512], scalar=1.0 / w, in1=uc, op0=ALU.mult, op1=ALU.subtract),
                                  [curb, uT.b], [pb_.b])
                        for (cond, lo, fo) in ((ci == 0, 0, 0), (ci == 7, 496, 16)):
                            if cond:
                                fw.dve.op(lambda: nc.vector.tensor_tensor(out=sdst[:, 0:16], in0=cur[:, lo:lo + 16], in1=pfx[:, j, fo:fo + 16], op=ALU.mult),
                                          [curb, pfx.b], [sdst.b])
                                fw.dve.op(lambda: nc.vector.tensor_tensor(out=pb_[:, lo:lo + 16], in0=sdst[:, 0:16], in1=uc[:, lo:lo + 16], op=ALU.subtract),
                                          [sdst.b, uT.b], [pb_.b])
                        py = self.ps_get()
                        fw.pe.op(lambda: nc.tensor.matmul(py[:], lhsT=pwb[:, j, :], rhs=pb_[:], start=True, stop=True), [pwb.b, pb_.b], [py.b])
                        fw.act.op(lambda: nc.scalar.activation(out=dT[:, j, NCTX + tok0:NCTX + tok0 + 512], in_=py[:], func=AF.Copy, scale=psc[:, j:j + 1]),
                                  [py.b, psc.b], [dT.b])
                        self.ps_put(py)
                fw.barrier()
            if self.debug:
                self.dbg_dump_bf(ph, "dT", dT[:, :, 256:384], [128, 4, 128], dT.b)
                self.dbg_dump_bf(ph, "dTe", dT[:, :, T - 128:T], [128, 4, 128], dT.b)
            if self.stop == "l1p3":
                return
            with ExitStack() as p4:
                self.out_proj(1, lambda c: (oT, c) if c < 4 else (dT, c - 4), self.w_out_cd, list(range(2, NT)), p4)
                fw.barrier()


def _rope_tables():
    rows = S // 64
    row = np.repeat(np.arange(rows, dtype=np.float32), 64)
    col = np.tile(np.arange(64, dtype=np.float32), rows)
    inv = (10000.0 ** (-np.arange(16, dtype=np.float32) / 16)).astype(np.float32)
    ang = np.concatenate([row[:, None] * inv, col[:, None] * inv], axis=-1).astype(np.float32)
    return np.cos(ang).astype(np.float32), np.sin(ang).astype(np.float32)


def _fm(v, chunks):
    return np.ascontiguousarray(np.asarray(v, np.float32).reshape(chunks, 128).T)


def make_in_maps(inp, cores):
    f = lambda a: np.ascontiguousarray(np.asarray(a, dtype=np.float32))
    cos, sin = _rope_tables()
    cosT = np.ascontiguousarray(cos.reshape(32, 128, 32).transpose(1, 0, 2))
    sinT = np.ascontiguousarray(sin.reshape(32, 128, 32).transpose(1, 0, 2))
    r = np.arange(128)
    mprev = (r[None, :] <= r[:, None]).astype(np.float32)
    mnext = (r[:, None] <= r[None, :]).astype(np.float32)
    wmask = np.stack([np.tile(mprev, (1, 4)), np.tile(mnext, (1, 4))], axis=1)
    shared = {
        "mod_w": f(inp["mod_w"]),
        "mod_bT": np.ascontiguousarray(f(inp["mod_b"]).reshape(2, 48, 128).transpose(2, 0, 1)),
        "ln1gT": np.ascontiguousarray(f(inp["ln1_g"]).reshape(2, 8, 128).transpose(2, 0, 1)),
        "ln2gT": np.ascontiguousarray(f(inp["ln2_g"]).reshape(2, 8, 128).transpose(2, 0, 1)),
        "w_in_ab": f(inp["w_in_ab"][0]), "w_out_ab": f(inp["w_out_ab"][0]),
        "gqk_a": np.ascontiguousarray(np.broadcast_to(np.concatenate([np.tile(f(inp["q_norm_a"][0]), 8), np.tile(f(inp["k_norm_a"][0]), 2)])[None, :], (128, 640))),
        "convwT": np.ascontiguousarray(f(inp["conv_w"][0]).reshape(31, 4, 128).transpose(2, 1, 0)),
        "convv": np.ascontiguousarray(np.stack([_fm(inp["conv_b"][0], 4), _fm(inp["conv_ln_g"][0], 4), _fm(inp["conv_ln_b"][0], 4)], axis=1)),
        "w_in_cd": f(inp["w_in_cd"][0]), "w_out_cd": f(inp["w_out_cd"][0]),
        "gqk_c": np.ascontiguousarray(np.broadcast_to(np.concatenate([np.tile(f(inp["q_norm_c"][0]), 8), np.tile(f(inp["k_norm_c"][0]), 2)])[None, :], (128, 640))),
        "sink_rep": np.ascontiguousarray(np.broadcast_to(np.repeat(f(inp["sink_c"][0]).reshape(2, 4), 128, axis=1)[None], (128, 2, 512))),
        "pool_w": f(inp["pool_w"][0]),
        "pool_scT": _fm(inp["pool_scale"][0], 4),
        "poolfix": _poolfix(),
        "rt_w": np.ascontiguousarray(np.concatenate([f(inp["rt_grp_w"]), f(inp["rt_exp_w"])], axis=2)),
        "rt_b": np.ascontiguousarray(np.broadcast_to(np.concatenate([f(inp["rt_grp_b"]), f(inp["rt_exp_b"])], axis=1)[None], (128, 2, 36))),
        "ex_gate": np.ascontiguousarray(f(inp["ex_gate"]).reshape(2, 32, 8, 128, 512).transpose(0, 1, 3, 2, 4).reshape(2, 4096, 4096)),
        "ex_up": np.ascontiguousarray(f(inp["ex_up"]).reshape(2, 32, 8, 128, 512).transpose(0, 1, 3, 2, 4).reshape(2, 4096, 4096)),
        "ex_down": np.ascontiguousarray(f(inp["ex_down"]).reshape(2, 32, 4, 128, 1024).transpose(0, 1, 3, 2, 4).reshape(2, 4096, 4096)),
        "pidx": np.arange(128, dtype=np.float32).reshape(128, 1),
        "mconst": np.ascontiguousarray(np.broadcast_to(np.concatenate([
            np.arange(32), 128.0 * np.arange(35), np.arange(100), np.arange(32) % 2, np.arange(100) % 2,
            128.0 * np.arange(1, 69), 2.0 * np.arange(1, 51), np.arange(16)]).astype(np.float32)[None], (128, 433))),
        "umat": np.ascontiguousarray((r[:, None] < r[None, :]).astype(np.float32)),
        "ident": np.eye(128, dtype=np.float32), "cosT": cosT, "sinT": sinT, "wmask": np.ascontiguousarray(wmask),
    }
    maps = []
    for b in cores:
        m = dict(shared)
        m["x"] = f(inp["x"][b])
        m["ctx"] = f(inp["ctx"][b])
        c2 = np.stack([f(inp["c"][b]), f(inp["c_ctx"])], axis=1)
        m["c2T"] = np.ascontiguousarray(c2.reshape(8, 128, 2).transpose(1, 0, 2))
        maps.append(m)
    return maps


def _poolfix():
    out = np.ones((4, 32), np.float32)
    for gi, w in enumerate((2, 4, 8, 16)):
        for i, t in enumerate(list(range(16)) + list(range(S - 16, S))):
            lo = min(max(t - w // 2, 0), S)
            hi = min(max(t - w // 2 + w, 0), S)
            out[gi, i] = 1.0 / float(hi - lo)
    return np.ascontiguousarray(np.broadcast_to(out[None], (128, 4, 32)))


_NC_CACHE = {}


def kernel(**inputs):
    if "nc" not in _NC_CACHE:
        _NC_CACHE["nc"] = Builder(debug=False).build()
    nc = _NC_CACHE["nc"]
    maps = make_in_maps(inputs, list(range(8)))
    res = run_bass_kernel_spmd(nc, maps, core_ids=list(range(8)))
    return np.stack([np.asarray(r["y"], dtype=np.float32) for r in res.results], axis=0)
```

```python
import os
import numpy as np
from contextlib import ExitStack
from collections import deque
import concourse.bass as bass
import concourse.mybir as mybir
from concourse.bass_utils import run_bass_kernel_spmd

F32 = mybir.dt.float32
BF16 = mybir.dt.bfloat16
I32 = mybir.dt.int32
ALU = mybir.AluOpType
AF = mybir.ActivationFunctionType
AX = mybir.AxisListType

D = 1024
S = 4096
NCTX = 256
T = S + NCTX
NT = T // 128
EPS = 1e-6
GLU_OFF_CTX = 15
GLU_OFF_LAT = 15 + NCTX + 15
GLU_LEN = GLU_OFF_LAT + S + 15


class Buf:
    __slots__ = ("name", "w", "r")

    def __init__(self, name=""):
        self.name = name
        self.w = None
        self.r = {}


class Tile:
    __slots__ = ("t", "b")

    def __init__(self, t, b):
        self.t, self.b = t, b

    def __getitem__(self, k):
        return self.t[k]


import threading


class Interleaver:
    def __init__(self):
        self.active = False

    def run(self, fns):
        if len(fns) == 1:
            fns[0]()
            return
        n = len(fns)
        self.ev = [threading.Event() for _ in range(n)]
        self.alive = [True] * n
        self.err = []
        self.tid = {}
        done = threading.Event()

        def worker(i):
            self.ev[i].wait()
            self.ev[i].clear()
            try:
                fns[i]()
            except BaseException as e:
                self.err.append(e)
            self.alive[i] = False
            nxt = self._next(i)
            if nxt is None:
                done.set()
            else:
                self.ev[nxt].set()

        ths = [threading.Thread(target=worker, args=(i,)) for i in range(n)]
        self.active = True
        for i, th in enumerate(ths):
            th.start()
            self.tid[th.ident] = i
        self.ev[0].set()
        done.wait()
        for th in ths:
            th.join()
        self.active = False
        if self.err:
            raise self.err[0]

    def _next(self, i):
        n = len(self.alive)
        for d in range(1, n + 1):
            j = (i + d) % n
            if self.alive[j] and j != i:
                return j
        return None

    def yield_point(self):
        if not self.active:
            return
        i = self.tid.get(threading.get_ident())
        if i is None:
            return
        nxt = self._next(i)
        if nxt is None:
            return
        self.ev[nxt].set()
        self.ev[i].wait()
        self.ev[i].clear()


ILV = Interleaver()


class Eng:
    def __init__(self, key, e, sem):
        self.key, self.e, self.sem = key, e, sem
        self.n = 0
        self.known = {}

    def _wait(self, sem, val):
        if self.known.get(sem, 0) >= val:
            return
        self.known[sem] = val
        self.e.wait_ge(sem, val)

    def op(self, ins_fn, reads=(), writes=(), inc=True):
        for b in reads:
            if b.w is not None:
                self._wait(b.w[0], b.w[1])
        strict = (self.key != "pe")
        for b in writes:
            if b.w is not None and (strict or b.w[2] != self.key):
                self._wait(b.w[0], b.w[1])
            for sem, (val, k) in b.r.items():
                if strict or k != self.key:
                    self._wait(sem, val)
        ins = ins_fn()
        if inc:
            self.n += 1
            ins.then_inc(self.sem, 1)
            tok = (self.sem, self.n, self.key)
        else:
            tok = (self.sem, self.n + 1, self.key)
        for b in reads:
            b.r[tok[0]] = (tok[1], tok[2])
        for b in writes:
            b.w = tok
            b.r = {}
        if inc:
            ILV.yield_point()
        return ins


class DmaQ:
    def __init__(self, key, e, sems):
        self.key, self.e, self.sems = key, e, sems
        self.cnt = [0] * len(sems)
        self.i = 0
        self.known = {}

    def _wait(self, sem, val):
        if self.known.get(sem, 0) >= val:
            return
        self.known[sem] = val
        self.e.wait_ge(sem, val)

    def dma(self, out, in_, reads=(), writes=(), **kw):
        for b in reads:
            if b.w is not None:
                self._wait(b.w[0], b.w[1])
        for b in writes:
            if b.w is not None:
                self._wait(b.w[0], b.w[1])
            for sem, (val, k) in b.r.items():
                self._wait(sem, val)
        s = self.i % len(self.sems)
        self.i += 1
        sem = self.sems[s]
        if self.cnt[s] > 0:
            self._wait(sem, 16 * self.cnt[s])
        self.cnt[s] += 1
        ins = self.e.dma_start(out=out, in_=in_, **kw)
        ins.then_inc(sem, 16)
        tok = (sem, 16 * self.cnt[s], self.key + str(s))
        for b in reads:
            b.r[tok[0]] = (tok[1], tok[2])
        for b in writes:
            b.w = tok
            b.r = {}
        return ins


def _dma_generic(self, fn, reads=(), writes=()):
    for b in reads:
        if b.w is not None:
            self._wait(b.w[0], b.w[1])
    for b in writes:
        if b.w is not None:
            self._wait(b.w[0], b.w[1])
        for sem, (val, k) in b.r.items():
            self._wait(sem, val)
    s = self.i % len(self.sems)
    self.i += 1
    sem = self.sems[s]
    if self.cnt[s] > 0:
        self._wait(sem, 16 * self.cnt[s])
    self.cnt[s] += 1
    ins = fn()
    ins.then_inc(sem, 16)
    tok = (sem, 16 * self.cnt[s], self.key + str(s))
    for b in reads:
        b.r[tok[0]] = (tok[1], tok[2])
    for b in writes:
        b.w = tok
        b.r = {}
    return ins


DmaQ.dma_fn = _dma_generic


class FW:
    def __init__(self, nc, stack, n_dma_sems=10):
        self.nc = nc
        mk = lambda nm: stack.enter_context(nc.semaphore(nm))
        self.pe = Eng("pe", nc.tensor, mk("s_pe"))
        self.act = Eng("act", nc.scalar, mk("s_act"))
        self.dve = Eng("dve", nc.vector, mk("s_dve"))
        self.pool = Eng("pool", nc.gpsimd, mk("s_pool"))
        self.q_sync = DmaQ("qs", nc.sync, [mk(f"s_qs{i}") for i in range(n_dma_sems)])
        self.q_pool = DmaQ("qp", nc.gpsimd, [mk(f"s_qp{i}") for i in range(n_dma_sems)])
        self.q_pool.known = self.pool.known
        self.engs = [self.pe, self.act, self.dve, self.pool]
        self.qs = [self.q_sync, self.q_pool]

    def barrier(self):
        toks = []
        for e in self.engs:
            if e.n > 0:
                toks.append((e.sem, e.n))
        for q in self.qs:
            for s, c in zip(q.sems, q.cnt):
                if c > 0:
                    toks.append((s, 16 * c))
        for e in self.engs + [self.q_sync]:
            for sem, val in toks:
                e._wait(sem, val)


class Builder:
    def __init__(self, debug=False, stop=None):
        self.debug = debug
        self.stop = stop
        self.nc = bass.Bass("TRN2", target_bir_lowering=False)
        self.dbg_out = {}

    def dram_in(self, name, shape, dt=F32):
        return self.nc.dram_tensor(name, list(shape), dt, kind="ExternalInput").ap()

    def tile(self, st, name, shape, dt):
        self._tn = getattr(self, "_tn", 0) + 1
        name = f"{name}_{self._tn}"
        t = st.enter_context(self.nc.sbuf_tensor(name, list(shape), dt))
        return Tile(t, Buf(name))

    def ps_get(self):
        return self.psq.popleft()

    def ps_put(self, p):
        self.psq.append(p)

    def dbg(self, name, shape):
        if not self.debug:
            return None
        ap = self.nc.dram_tensor("dbg_" + name, list(shape), F32, kind="ExternalOutput").ap()
        self.dbg_out[name] = (ap, Buf("dbg_" + name))
        return ap

    def cut(self, n):
        return int(os.environ.get("P1_CUT", "-1")) == n

    def tok_in(self, t):
        if t < 2:
            return self.ctx_in[t * 128:(t + 1) * 128, :]
        return self.x_in[(t - 2) * 128:(t - 1) * 128, :]

    def build(self):
        nc = self.nc
        di = self.dram_in
        self.x_in = di("x", [S, D])
        self.ctx_in = di("ctx", [NCTX, D])
        self.c2T = di("c2T", [128, 8, 2])
        self.mod_w = di("mod_w", [2, D, 6 * D])
        self.mod_bT = di("mod_bT", [128, 2, 48])
        self.ln1gT = di("ln1gT", [128, 2, 8])
        self.ln2gT = di("ln2gT", [128, 2, 8])
        self.w_in_ab = di("w_in_ab", [D, 1792])
        self.w_out_ab = di("w_out_ab", [D, D])
        self.gqk_a = di("gqk_a", [128, 640])
        self.convwT = di("convwT", [128, 4, 31])
        self.convv = di("convv", [128, 3, 4])
        self.w_in_cd = di("w_in_cd", [D, 1280])
        self.w_out_cd = di("w_out_cd", [D, D])
        self.gqk_c = di("gqk_c", [128, 640])
        self.sink_rep = di("sink_rep", [128, 2, 512])
        self.pool_w = di("pool_w", [4, 128, 128])
        self.pool_scT = di("pool_scT", [128, 4])
        self.poolfix = di("poolfix", [128, 4, 32])
        self.rt_w = di("rt_w", [2, D, 36])
        self.rt_b = di("rt_b", [128, 2, 36])
        self.ex_gate = di("ex_gate", [2, 32 * 128, 4096])
        self.ex_up = di("ex_up", [2, 32 * 128, 4096])
        self.ex_down = di("ex_down", [2, 32 * 128, 4096])
        self.pidx = di("pidx", [128, 1])
        self.ident_in = di("ident", [128, 128])
        self.cosT = di("cosT", [128, 32, 32])
        self.sinT = di("sinT", [128, 32, 32])
        self.wmask = di("wmask", [128, 2, 512])
        self.mconst = di("mconst", [128, 433])
        self.umat = di("umat", [128, 128])
        self.y_out = nc.dram_tensor("y", [S, D], F32, kind="ExternalOutput").ap()
        self.xres = nc.dram_tensor("xres", [T, D], F32, kind="Internal").ap()
        self.xres_b = [Buf(f"xres{t}") for t in range(NT)]
        self.hs_d, self.hz_b = {}, {}
        for l_ in range(2):
            nb_ = (2 * (NT if l_ == 0 else NT - 2) * 128 + 32 * 127 + 127) // 128
            self.hs_d[l_] = nc.dram_tensor(f"hs{l_}", [nb_ * 128, D], BF16, kind="Internal").ap()
            self.hz_b[l_] = []
        self.y_b = [Buf(f"y{t}") for t in range(NT)]

        with ExitStack() as st:
            self.fw = FW(nc, st)
            fw = self.fw
            self.PS = []
            self.PSB = []
            for i in range(4):
                pb_ = st.enter_context(nc.psum_tensor(f"psb{i}", [128, 1024], F32))
                h0 = Tile(pb_[:, 0:512], Buf(f"ps{2 * i}"))
                h1 = Tile(pb_[:, 512:1024], Buf(f"ps{2 * i + 1}"))
                self.PS += [h0, h1]
                self.PSB.append((pb_, h0, h1))
            self.psq = deque(self.PS)
            g = self.g = {}
            g["identf"] = self.tile(st, "identf", [128, 128], F32)
            g["identb"] = self.tile(st, "identb", [128, 128], BF16)
            g["onesf"] = self.tile(st, "onesf", [128, 128], F32)
            g["modT"] = self.tile(st, "modT", [128, 2, 48, 2], F32)
            g["A1"] = self.tile(st, "A1", [128, 2, 8, 2], F32)
            g["A2"] = self.tile(st, "A2", [128, 2, 8, 2], F32)
            g["epsc"] = self.tile(st, "epsc", [128, 1], F32)
            fw.q_sync.dma(g["identf"][:], self.ident_in, writes=[g["identf"].b])
            fw.dve.op(lambda: nc.vector.tensor_copy(out=g["identb"][:], in_=g["identf"][:]), [g["identf"].b], [g["identb"].b])
            fw.dve.op(lambda: nc.vector.memset(g["onesf"][:], 1.0), [], [g["onesf"].b])
            fw.dve.op(lambda: nc.vector.memset(g["epsc"][:], EPS), [], [g["epsc"].b])

            self.bc_reg = nc.gpsimd.alloc_register("bcreg")
            nc.gpsimd.reg_mov(self.bc_reg, 8191)
            self.phase_mod()
            if self.stop != "p0":
                self.layer0_mixer()
            if self.stop is None or self.stop in ("m0", "l1", "m1"):
                (self.moe2 if os.environ.get("MOE_DENSE") != "1" else self.moe)(0)
            if self.stop is None or self.stop in ("l1", "m1"):
                self.layer1_mixer()
            if self.stop is None or self.stop in ("m1",):
                (self.moe2 if os.environ.get("MOE_DENSE") != "1" else self.moe)(1)

            for b in self.y_b[2:]:
                if b.w is not None:
                    fw.q_sync._wait(b.w[0], b.w[1])
            for name, (ap, b) in self.dbg_out.items():
                if b.w is not None:
                    fw.q_sync._wait(b.w[0], b.w[1])
            fw.barrier()
        return nc

    def rstd_from_ss(self, ss, n_inv, cols):
        nc, fw = self.nc, self.fw
        fw.dve.op(lambda: nc.vector.tensor_scalar(out=ss[:, 0:cols], in0=ss[:, 0:cols], scalar1=n_inv, scalar2=EPS,
                                                  op0=ALU.mult, op1=ALU.add), [ss.b], [ss.b])
        fw.act.op(lambda: nc.scalar.sqrt(out=ss[:, 0:cols], in_=ss[:, 0:cols]), [ss.b], [ss.b])
        fw.dve.op(lambda: nc.vector.reciprocal(out=ss[:, 0:cols], in_=ss[:, 0:cols]), [ss.b], [ss.b])

    def load_cast_w(self, st_pool, dst, dst_cols, src_ap, kc, ncols, eng_i):
        nc, fw = self.nc, self.fw
        stg = st_pool[self.stg_i % len(st_pool)]
        self.stg_i += 1
        fw.q_sync.dma(stg[:, 0:kc * ncols].rearrange("p (k n) -> p k n", k=kc),
                      src_ap.rearrange("(k p) n -> p k n", p=128), writes=[stg.b])
        src = stg[:, 0:kc * ncols].rearrange("p (k n) -> p k n", k=kc)
        if eng_i % 2 == 0:
            fw.act.op(lambda: nc.scalar.copy(out=dst[:, 0:kc, dst_cols], in_=src), [stg.b], [dst.b])
        else:
            fw.dve.op(lambda: nc.vector.tensor_copy(out=dst[:, 0:kc, dst_cols], in_=src), [stg.b], [dst.b])

    def phase_mod(self):
        nc, fw, g = self.nc, self.fw, self.g
        with ExitStack() as ph:
            c2 = self.tile(ph, "c2", [128, 8, 2], F32)
            sil = self.tile(ph, "sil", [128, 8, 2], BF16)
            mb = self.tile(ph, "mb", [128, 2, 48], F32)
            l1g = self.tile(ph, "l1g", [128, 2, 8], F32)
            l2g = self.tile(ph, "l2g", [128, 2, 8], F32)
            stg = [self.tile(ph, f"mstg{i}", [128, 4096], F32) for i in range(2)]
            wb = [self.tile(ph, f"mwb{i}", [128, 8, 512], BF16) for i in range(2)]
            self.stg_i = 0
            fw.q_sync.dma(c2[:], self.c2T, writes=[c2.b])
            fw.q_sync.dma(mb[:], self.mod_bT, writes=[mb.b])
            fw.q_sync.dma(l1g[:], self.ln1gT, writes=[l1g.b])
            fw.q_sync.dma(l2g[:], self.ln2gT, writes=[l2g.b])
            fw.act.op(lambda: nc.scalar.activation(out=sil[:], in_=c2[:], func=AF.Silu), [c2.b], [sil.b])
            for l in range(2):
                pm = self.ps_get()
                pmv = pm[:, 0:96].rearrange("p (c j) -> p c j", j=2)
                for n in range(12):
                    w = wb[n % 2]
                    self.load_cast_w(stg, w, slice(0, 512), self.mod_w[l, :, n * 512:(n + 1) * 512], 8, 512, n)
                    for q in range(4):
                        cc = n * 4 + q
                        for k in range(8):
                            fw.pe.op(lambda: nc.tensor.matmul(pmv[:, cc, :], lhsT=w[:, k, q * 128:(q + 1) * 128], rhs=sil[:, k, :],
                                                              start=(k == 0), stop=(k == 7)),
                                     [w.b, sil.b], [pm.b], inc=(k == 7 and q == 3))
                mT = g["modT"]
                fw.dve.op(lambda: nc.vector.tensor_tensor(out=mT[:, l, :, :], in0=pmv,
                                                          in1=mb[:, l, :].unsqueeze(2).to_broadcast([128, 48, 2]), op=ALU.add),
                          [pm.b, mb.b], [mT.b])
                self.ps_put(pm)
                fw.dve.op(lambda: nc.vector.scalar_tensor_tensor(out=g["A1"][:, l, :, :], in0=mT[:, l, 8:16, :], scalar=1.0,
                                                                 in1=l1g[:, l, :].unsqueeze(2).to_broadcast([128, 8, 2]),
                                                                 op0=ALU.add, op1=ALU.mult), [mT.b, l1g.b], [g["A1"].b])
                fw.dve.op(lambda: nc.vector.scalar_tensor_tensor(out=g["A2"][:, l, :, :], in0=mT[:, l, 32:40, :], scalar=1.0,
                                                                 in1=l2g[:, l, :].unsqueeze(2).to_broadcast([128, 8, 2]),
                                                                 op0=ALU.add, op1=ALU.mult), [mT.b, l2g.b], [g["A2"].b])
            if self.debug:
                ap = self.dbg("modT", [128, 2 * 48 * 2])
                fw.q_pool.dma(ap, g["modT"][:].rearrange("p l c j -> p (l c j)"), reads=[g["modT"].b], writes=[self.dbg_out["modT"][1]])
            fw.barrier()

    def bcast_tile(self, dst, col_ap_fn, tmp, extra=()):
        nc, fw, g = self.nc, self.fw, self.g
        for c in range(8):
            fw.dve.op(lambda: nc.vector.tensor_scalar(out=tmp[:], in0=g["onesf"][:], scalar1=col_ap_fn(c), scalar2=None, op0=ALU.mult),
                      [g["onesf"].b, g["modT"].b] + list(extra), [tmp.b])
            p = self.ps_get()
            fw.pe.op(lambda: nc.tensor.transpose(out=p[:, 0:128], in_=tmp[:], identity=g["identf"][:]), [tmp.b, g["identf"].b], [p.b])
            fw.act.op(lambda: nc.scalar.copy(out=dst[:, c * 128:(c + 1) * 128], in_=p[:, 0:128]), [p.b], [dst.b])
            self.ps_put(p)

    def norm_tile_to_hT(self, xt, hT, col0, l, which, cls, scr):
        nc, fw, g = self.nc, self.fw, self.g
        ni = scr.get("ni", 0)
        scr["ni"] = ni + 1
        ss, xn = scr["ssl"][ni % len(scr["ssl"])], scr["xnl"][ni % len(scr["xnl"])]
        fw.act.op(lambda: nc.scalar.activation(out=xn[:], in_=xt[:], func=AF.Square, accum_out=ss[:, 0:1]), [xt.b], [xn.b, ss.b])
        self.rstd_from_ss(ss, 1.0 / D, 1)
        fw.act.op(lambda: nc.scalar.activation(out=xn[:], in_=xt[:], func=AF.Copy, scale=ss[:, 0:1]), [xt.b, ss.b], [xn.b])
        p = self.ps_get()
        pv = p[:].bitcast(BF16).rearrange("p (c q) -> p c q", c=8)
        for c in range(8):
            fw.pe.op(lambda: nc.tensor.transpose(out=pv[:, c, :], in_=xn[:, c * 128:(c + 1) * 128], identity=g["identb"][:]),
                     [xn.b, g["identb"].b], [p.b], inc=(c == 7))
        A = g["A1"] if which == 1 else g["A2"]
        boff = 0 if which == 1 else 24
        for c in range(8):
            fw.dve.op(lambda: nc.vector.tensor_scalar(out=hT[:, c, col0:col0 + 128], in0=pv[:, c, :], scalar1=A[:, l, c, cls:cls + 1],
                                                      scalar2=g["modT"][:, l, boff + c, cls:cls + 1], op0=ALU.mult, op1=ALU.add),
                      [p.b, A.b, g["modT"].b], [hT.b])
        self.ps_put(p)

    def qk_post(self, qkv, t, gqk, scr, qT, kT, do_q, lat_idx):
        nc, fw, g = self.nc, self.fw, self.g
        sq, ssq, qn, qr, tmp = scr["sq"], scr["ssq"], scr["qn"], scr["qr"], scr["rt"]
        h0 = 0 if do_q else 8
        nh = 10 - h0
        c0 = h0 * 64
        q3 = lambda tl: tl[:, c0:640].rearrange("p (h d) -> p h d", d=64)
        fw.pool.op(lambda: nc.gpsimd.tensor_tensor(out=sq[:, c0:640], in0=qkv[:, c0:640], in1=qkv[:, c0:640], op=ALU.mult), [qkv.b], [sq.b])
        fw.dve.op(lambda: nc.vector.tensor_reduce(out=ssq[:, h0:10], in_=q3(sq), axis=AX.X, op=ALU.add), [sq.b], [ssq.b])
        fw.dve.op(lambda: nc.vector.tensor_scalar(out=ssq[:, h0:10], in0=ssq[:, h0:10], scalar1=1.0 / 64, scalar2=EPS,
                                                  op0=ALU.mult, op1=ALU.add), [ssq.b], [ssq.b])
        fw.act.op(lambda: nc.scalar.sqrt(out=ssq[:, h0:10], in_=ssq[:, h0:10]), [ssq.b], [ssq.b])
        fw.dve.op(lambda: nc.vector.reciprocal(out=ssq[:, h0:10], in_=ssq[:, h0:10]), [ssq.b], [ssq.b])
        fw.dve.op(lambda: nc.vector.tensor_tensor(out=q3(qn), in0=q3(qkv), in1=ssq[:, h0:10].unsqueeze(2).to_broadcast([128, nh, 64]),
                                                  op=ALU.mult), [qkv.b, ssq.b], [qn.b])
        if lat_idx is None:
            fw.pool.op(lambda: nc.gpsimd.tensor_tensor(out=qr[:, c0:640], in0=qn[:, c0:640], in1=gqk[:, c0:640], op=ALU.mult),
                       [qn.b, gqk.b], [qr.b])
        else:
            fw.pool.op(lambda: nc.gpsimd.tensor_tensor(out=qn[:, c0:640], in0=qn[:, c0:640], in1=gqk[:, c0:640], op=ALU.mult),
                       [qn.b, gqk.b], [qn.b])
            cosb = g["cos"][:, lat_idx, :].unsqueeze(1).to_broadcast([128, nh, 32])
            sinb = g["sin"][:, lat_idx, :].unsqueeze(1).to_broadcast([128, nh, 32])
            x1 = q3(qn)[:, :, 0:32]
            x2 = q3(qn)[:, :, 32:64]
            sq2 = sq[:, 0:640].rearrange("p (k n) -> p k n", k=2)
            t3 = lambda k: (sq2 if k < 2 else tmp)[:, k % 2, c0 // 2:320].rearrange("p (h d) -> p h d", d=32)
            fw.dve.op(lambda: nc.vector.tensor_tensor(out=t3(0), in0=x1, in1=cosb, op=ALU.mult), [qn.b, g["cos"].b], [sq.b])
            fw.pool.op(lambda: nc.gpsimd.tensor_tensor(out=t3(1), in0=x2, in1=sinb, op=ALU.mult), [qn.b, g["sin"].b], [sq.b])
            fw.dve.op(lambda: nc.vector.tensor_tensor(out=t3(2), in0=x2, in1=cosb, op=ALU.mult), [qn.b, g["cos"].b], [tmp.b])
            fw.pool.op(lambda: nc.gpsimd.tensor_tensor(out=t3(3), in0=x1, in1=sinb, op=ALU.mult), [qn.b, g["sin"].b], [tmp.b])
            fw.dve.op(lambda: nc.vector.tensor_tensor(out=q3(qr)[:, :, 0:32], in0=t3(0), in1=t3(1), op=ALU.subtract), [sq.b], [qr.b])
            fw.pool.op(lambda: nc.gpsimd.tensor_tensor(out=q3(qr)[:, :, 32:64], in0=t3(2), in1=t3(3), op=ALU.add), [tmp.b], [qr.b])
        if self.cut(30):
            return
        p = self.ps_get()
        pv = p[:].bitcast(BF16).rearrange("p (c q) -> p c q", c=8)
        if do_q:
            for j in range(4):
                fw.pe.op(lambda: nc.tensor.transpose(out=pv[:, j, :], in_=qr[:, j * 128:(j + 1) * 128], identity=g["identb"][:]),
                         [qr.b, g["identb"].b], [p.b], inc=False)
        fw.pe.op(lambda: nc.tensor.transpose(out=pv[:, 4, :], in_=qr[:, 512:640], identity=g["identb"][:]),
                 [qr.b, g["identb"].b], [p.b])
        if self.cut(31):
            return
        if do_q:
            fw.dve.op(lambda: nc.vector.tensor_copy(out=qT[:, t, :, :], in_=pv[:, 0:4, :]), [p.b], [qT.b])
        if self.cut(32):
            return
        fw.dve.op(lambda: nc.vector.tensor_copy(out=kT[0:64, 0, t * 128:(t + 1) * 128], in_=pv[0:64, 4, :]), [p.b], [kT.b])
        fw.dve.op(lambda: nc.vector.tensor_copy(out=kT[64:128, 1, t * 128:(t + 1) * 128], in_=pv[64:128, 4, :]), [p.b], [kT.b])
        self.ps_put(p)

    def v_fill(self, qkv, t, Vp):
        nc, fw = self.nc, self.fw
        fw.act.op(lambda: nc.scalar.copy(out=Vp[:, t, 0:64], in_=qkv[:, 640:704]), [qkv.b], [Vp.b])
        fw.act.op(lambda: nc.scalar.copy(out=Vp[:, t, 136:200], in_=qkv[:, 704:768]), [qkv.b], [Vp.b])

    def v_init(self, Vp):
        nc, fw = self.nc, self.fw
        fw.pool.op(lambda: nc.gpsimd.memset(Vp[:], 0.0), [], [Vp.b])
        fw.pool.op(lambda: nc.gpsimd.memset(Vp[:, :, 64:65], 1.0), [], [Vp.b])
        fw.pool.op(lambda: nc.gpsimd.memset(Vp[:, :, 104:105], 1.0), [], [Vp.b])

    def attention(self, blocks, qT, kT, Vp, oT, scr, masks=None, sinkrow=None, LA=2):
        nc, fw, g = self.nc, self.fw, self.g
        steps = []
        for b0 in range(0, len(blocks), 2):
            (kv0_, qb, kts), (kv1_, qb1, kts1) = blocks[b0], blocks[b0 + 1]
            assert (kv0_, kv1_) == (0, 1) and qb == qb1
            for i, (kt, mi) in enumerate(kts):
                steps.append(dict(bi=b0, qb=qb, kt=kt, mi=mi, first=(i == 0), last=(i == len(kts) - 1)))
        assert len(self.psq) == 8
        spairs = self.PSB[0:2]
        for (_, h0, h1) in spairs:
            self.psq.remove(h0)
            self.psq.remove(h1)
        po_of = {}
        cnt = dict(s=0)

        pending = []

        def finish_block(kv, qb, bi, po, idx):
            r0 = kv * 64
            dr = 64 if kv == 0 else 32
            M = 65 if kv == 0 else 128
            slot = (bi // 2) % 2
            posb = scr["posb"][slot][kv]
            rec = scr["rec"][slot]
            fw.dve.op(lambda: nc.vector.tensor_copy(out=posb[0:M, :], in_=po[0:M, :]), [po.b], [posb.b])
            self.ps_put(po)
            if sinkrow is not None:
                fw.dve.op(lambda: nc.vector.tensor_tensor(out=rec[dr:dr + 1, :], in0=posb[dr:dr + 1, :], in1=sinkrow[dr:dr + 1, kv, :], op=ALU.add),
                          [posb.b, sinkrow.b], [rec.b])
                fw.dve.op(lambda: nc.vector.reciprocal(out=rec[dr:dr + 1, :], in_=rec[dr:dr + 1, :]), [rec.b], [rec.b])
            else:
                fw.dve.op(lambda: nc.vector.reciprocal(out=rec[dr:dr + 1, :], in_=posb[dr:dr + 1, :]), [posb.b], [rec.b])
            pending.append((idx + 4, kv, qb, posb, rec))

        def finalize(kv, qb, posb, rec):
            r0 = kv * 64
            dr = 64 if kv == 0 else 32
            pb = self.ps_get()
            Mb = 64 if kv == 0 else 128
            fw.pe.op(lambda: nc.tensor.matmul(pb[0:Mb, :], lhsT=g["onesf"][dr:dr + 1, 0:Mb], rhs=rec[dr:dr + 1, :], start=True, stop=True),
                     [g["onesf"].b, rec.b], [pb.b])
            fw.dve.op(lambda: nc.vector.tensor_tensor(out=oT[r0:r0 + 64, :, qb * 128:(qb + 1) * 128],
                                                      in0=posb[r0:r0 + 64, :].rearrange("p (j q) -> p j q", j=4),
                                                      in1=pb[r0:r0 + 64, :].rearrange("p (j q) -> p j q", j=4), op=ALU.mult),
                      [posb.b, pb.b], [oT.b])
            self.ps_put(pb)

        def emit_S(st):
            qb, kt = st["qb"], st["kt"]
            big, h0, h1 = spairs[cnt["s"] % 2]
            cnt["s"] += 1
            rhs_q = qT[:, qb, :, :].rearrange("p j q -> p (j q)")
            fw.pe.op(lambda: nc.tensor.matmul(h0[:], lhsT=kT[:, 0, kt * 128:(kt + 1) * 128], rhs=rhs_q, start=True, stop=True),
                     [kT.b, qT.b], [h0.b], inc=False)
            fw.pe.op(lambda: nc.tensor.matmul(h1[:], lhsT=kT[:, 1, kt * 128:(kt + 1) * 128], rhs=rhs_q, start=True, stop=True),
                     [kT.b, qT.b], [h1.b])
            pe_ = scr["pexp"][scr["pi"] % len(scr["pexp"])]
            scr["pi"] += 1
            fw.act.op(lambda: nc.scalar.activation(out=pe_[:], in_=big[:, 0:1024], func=AF.Exp), [h0.b, h1.b], [pe_.b])
            if st["mi"] is not None:
                fw.dve.op(lambda: nc.vector.tensor_tensor(out=pe_[:].rearrange("p (a q) -> p a q", a=2), in0=pe_[:].rearrange("p (a q) -> p a q", a=2),
                                                          in1=masks[:, st["mi"], :].unsqueeze(1).to_broadcast([128, 2, 512]), op=ALU.mult),
                          [pe_.b, masks.b], [pe_.b])
            st["pe"] = pe_

        def emit_PV(st):
            qb, kt, bi = st["qb"], st["kt"], st["bi"]
            if st["first"]:
                po_of[bi] = (self.ps_get(), self.ps_get())
            pe_ = st["pe"]
            for kv in range(2):
                po = po_of[bi][kv]
                M = 65 if kv == 0 else 128
                fw.pe.op(lambda: nc.tensor.matmul(po[0:M, :], lhsT=Vp[:, kt, kv * 72:kv * 72 + M], rhs=pe_[:, kv * 512:(kv + 1) * 512],
                                                  start=st["first"], stop=st["last"]), [Vp.b, pe_.b], [po.b], inc=(st["last"] or kv == 1))
            if st["last"]:
                for kv in range(2):
                    finish_block(kv, qb, bi, po_of[bi][kv], st["idx"])
                del po_of[bi]

        n = len(steps)
        for idx in range(n + LA):
            if idx < n:
                emit_S(steps[idx])
            if idx - LA >= 0:
                steps[idx - LA]["idx"] = idx
                emit_PV(steps[idx - LA])
            while pending and pending[0][0] <= idx:
                _, kv, qb_, posb, rec = pending.pop(0)
                finalize(kv, qb_, posb, rec)
        while pending:
            _, kv, qb_, posb, rec = pending.pop(0)
            finalize(kv, qb_, posb, rec)
        for (_, h0, h1) in spairs:
            self.psq.append(h0)
            self.psq.append(h1)

    def out_proj(self, l, cat_fn, wsrc, tiles, ph):
        nc, fw, g = self.nc, self.fw, self.g
        classes = [1, 0] if l == 0 else [0]
        wo = {}
        GB = self.tile(ph, "GB", [128, D], F32)
        tmp = self.tile(ph, "bct", [128, 128], F32)
        stg = [self.tile(ph, f"ostg{i}", [128, D], F32) for i in range(2)]
        for cls in classes:
            wo[cls] = self.tile(ph, f"wo{cls}", [128, 8, D], BF16)
            self.bcast_tile(GB, lambda c: g["modT"][:, l, 16 + c, cls:cls + 1], tmp)
            for c in range(8):
                s_ = stg[c % 2]
                if c < 4:
                    fw.q_sync.dma(s_[0:64, :], wsrc[c * 64:(c + 1) * 64, :], writes=[s_.b])
                    fw.q_sync.dma(s_[64:128, :], wsrc[(c + 4) * 64:(c + 5) * 64, :], writes=[s_.b])
                else:
                    fw.q_sync.dma(s_[:], wsrc[c * 128:(c + 1) * 128, :], writes=[s_.b])
                fw.dve.op(lambda: nc.vector.tensor_tensor(out=wo[cls][:, c, :], in0=s_[:], in1=GB[:], op=ALU.mult), [s_.b, GB.b], [wo[cls].b])
        xt = [self.tile(ph, f"oxt{i}", [128, D], F32) for i in range(3)]

        def op_fn(i, t):
            cls = 1 if t < 2 else 0
            x_ = xt[i % 3]
            src = self.tok_in(t) if l == 0 else self.xres[t * 128:(t + 1) * 128, :]
            rb = [] if l == 0 else [self.xres_b[t]]
            fw.q_sync.dma(x_[:], src, reads=rb, writes=[x_.b])
            for half in range(2):
                p = self.ps_get()
                for c in range(8):
                    ct, ci = cat_fn(c)
                    fw.pe.op(lambda: nc.tensor.matmul(p[:], lhsT=ct[:, ci, t * 128:(t + 1) * 128], rhs=wo[cls][:, c, half * 512:(half + 1) * 512],
                                                      start=(c == 0), stop=(c == 7)), [ct.b, wo[cls].b], [p.b], inc=(c == 7))
                fw.dve.op(lambda: nc.vector.tensor_tensor(out=x_[:, half * 512:(half + 1) * 512], in0=p[:], in1=x_[:, half * 512:(half + 1) * 512],
                                                          op=ALU.add), [p.b, x_.b], [x_.b])
                self.ps_put(p)
            fw.q_pool.dma(self.xres[t * 128:(t + 1) * 128, :], x_[:], reads=[x_.b], writes=[self.xres_b[t]])
            if self.debug and t in (0, 2, NT - 1):
                nm = f"x1_{l}_{t}"
                ap = self.dbg(nm, [128, D])
                fw.q_pool.dma(ap, x_[:], reads=[x_.b], writes=[self.dbg_out[nm][1]])

        for i0 in range(0, len(tiles), 3):
            grp = list(range(i0, min(i0 + 3, len(tiles))))
            ILV.run([(lambda i=i: op_fn(i, tiles[i])) for i in grp])

    def layer0_mixer(self):
        nc, fw, g = self.nc, self.fw, self.g
        l = 0
        with ExitStack() as ph:
            gluT = self.tile(ph, "gluT", [128, 4, GLU_LEN], BF16)
            oT = self.tile(ph, "oT", [128, 4, T], BF16)
            pattn = ExitStack()
            qT = self.tile(pattn, "qT", [128, NT, 4, 128], BF16)
            kT = self.tile(pattn, "kT", [128, 2, T], BF16)
            Vp = self.tile(pattn, "Vp", [128, NT, 200], BF16)
            self.v_init(Vp)
            fw.pool.op(lambda: nc.gpsimd.memset(kT[:], 0.0), [], [kT.b])
            fw.pool.op(lambda: nc.gpsimd.memset(gluT[:], 0.0), [], [gluT.b])
            with ExitStack() as p1:
                win = self.tile(p1, "win", [128, 8, 1792], BF16)
                gqk = self.tile(p1, "gqk", [128, 640], F32)
                g["cos"] = self.tile(p1, "cos", [128, 32, 32], F32)
                g["sin"] = self.tile(p1, "sin", [128, 32, 32], F32)
                fw.q_sync.dma(gqk[:], self.gqk_a, writes=[gqk.b])
                fw.q_sync.dma(g["cos"][:], self.cosT, writes=[g["cos"].b])
                fw.q_sync.dma(g["sin"][:], self.sinT, writes=[g["sin"].b])
                fw.dve.op(lambda: nc.vector.tensor_scalar(out=gqk[:, 0:512], in0=gqk[:, 0:512], scalar1=0.125, scalar2=None, op0=ALU.mult),
                          [gqk.b], [gqk.b])
                with ExitStack() as pw:
                    stg = [self.tile(pw, f"wstg{i}", [128, 4096], F32) for i in range(1)]
                    self.stg_i = 0
                    for n in range(4):
                        w_ = 512 if n < 3 else 256
                        self.load_cast_w(stg, win, slice(n * 512, n * 512 + w_), self.w_in_ab[:, n * 512:n * 512 + w_], 8, w_, n)
                    fw.barrier()
                if self.cut(0):
                    return
                scr = dict(ssl=[self.tile(p1, f"ss{i}", [128, 1], F32) for i in range(2)],
                           xnl=[self.tile(p1, f"xn{i}", [128, D], BF16) for i in range(2)], sq=self.tile(p1, "sq", [128, 640], F32),
                           ssq=self.tile(p1, "ssq", [128, 10], F32), qn=self.tile(p1, "qn", [128, 640], F32),
                           qr=self.tile(p1, "qr", [128, 640], BF16), rt=self.tile(p1, "rt", [128, 2, 320], F32))
                xt = [self.tile(p1, f"xt{i}", [128, D], F32) for i in range(1)]
                hT = [self.tile(p1, f"hT{i}", [128, 8, 512], BF16) for i in range(1)]
                qkv = [self.tile(p1, f"qkv{i}", [128, 768], F32) for i in range(1)]
                sig = [self.tile(p1, f"sig{i}", [128, 512], BF16) for i in range(1)]
                chunks = [(0, 2)] + [(2 + 4 * i, 4) for i in range(8)]
                oflat = oT[:].rearrange("p a t -> p (a t)")
                coff = [0]

                def carve(n_elems, dt, shape3=None):
                    nb = n_elems * (4 if dt == F32 else 2) // 2
                    ap = oflat[:, coff[0]:coff[0] + nb]
                    coff[0] += nb
                    if dt == F32:
                        ap = ap.bitcast(F32)
                    if shape3 is not None:
                        ap = ap.rearrange("p (a b) -> p a b", a=shape3[0])
                    return Tile(ap, Buf("carve"))

                scr_b = dict(ssl=[scr["ssl"][1]], xnl=[scr["xnl"][1]], sq=carve(640, F32), ssq=carve(16, F32), qn=carve(640, F32),
                             qr=carve(640, BF16), rt=carve(640, F32, (2, 320)))
                scr_a = dict(scr)
                scr_a["ssl"] = [scr["ssl"][0]]
                scr_a["xnl"] = [scr["xnl"][0]]
                scr_l = [scr_a, scr_b]
                xt2 = [xt[0], carve(D, F32)]
                qkv2 = [qkv[0], carve(768, F32)]
                for ci, (t0, ntl) in enumerate(chunks):
                    h = hT[0]
                    ntok = ntl * 128
                    cls = 1 if t0 < 2 else 0

                    def norm_fn(tl):
                        t = t0 + tl
                        x_ = xt2[tl % 2]
                        fw.q_sync.dma(x_[:], self.tok_in(t), writes=[x_.b])
                        self.norm_tile_to_hT(x_, h, tl * 128, l, 1, cls, scr_l[tl % 2])

                    def qkv_fn(tl):
                        t = t0 + tl
                        qk_ = qkv2[tl % 2]
                        for (c0, w_) in ((0, 512), (512, 256)):
                            p = self.ps_get()
                            for k in range(8):
                                fw.pe.op(lambda: nc.tensor.matmul(p[:, 0:w_], lhsT=h[:, k, tl * 128:(tl + 1) * 128], rhs=win[:, k, c0:c0 + w_],
                                                                  start=(k == 0), stop=(k == 7)), [h.b, win.b], [p.b], inc=(k == 7))
                            if c0 == 0:
                                fw.act.op(lambda: nc.scalar.copy(out=qk_[:, 0:512].rearrange("p (j a d) -> p a j d", a=2, d=64),
                                                                 in_=p[:, 0:512].rearrange("p (a j d) -> p a j d", a=2, j=4)), [p.b], [qk_.b])
                            else:
                                fw.act.op(lambda: nc.scalar.copy(out=qk_[:, c0:c0 + w_], in_=p[:, 0:w_]), [p.b], [qk_.b])
                            self.ps_put(p)
                        self.qk_post(qk_, t, gqk, scr_l[tl % 2], qT, kT, True, None if t < 2 else t - 2)
                        self.v_fill(qk_, t, Vp)

                    for tl0 in range(0, ntl, 2):
                        ILV.run([(lambda tl=tl: norm_fn(tl)) for tl in range(tl0, min(tl0 + 2, ntl))])
                    for tl0 in range(0, ntl, 2):
                        ILV.run([(lambda tl=tl: qkv_fn(tl)) for tl in range(tl0, min(tl0 + 2, ntl))])
                    off = GLU_OFF_CTX if t0 < 2 else GLU_OFF_LAT + (t0 - 2) * 128
                    for j in range(4):
                        pa = self.ps_get()
                        pg = self.ps_get()
                        for (pp, cb) in ((pa, 768 + j * 128), (pg, 1280 + j * 128)):
                            for k in range(8):
                                fw.pe.op(lambda: nc.tensor.matmul(pp[:, 0:ntok], lhsT=win[:, k, cb:cb + 128], rhs=h[:, k, 0:ntok],
                                                                  start=(k == 0), stop=(k == 7)), [win.b, h.b], [pp.b], inc=(k == 7))
                        sg = sig[0]
                        fw.act.op(lambda: nc.scalar.activation(out=sg[:, 0:ntok], in_=pg[:, 0:ntok], func=AF.Sigmoid), [pg.b], [sg.b])
                        fw.dve.op(lambda: nc.vector.tensor_tensor(out=gluT[:, j, off:off + ntok], in0=pa[:, 0:ntok], in1=sg[:, 0:ntok], op=ALU.mult),
                                  [pa.b, sg.b], [gluT.b])
                        self.ps_put(pa)
                        self.ps_put(pg)
                    if self.cut(4) or (self.cut(5) and ci == 1):
                        return
                fw.barrier()
            if self.debug:
                self.dbg_dump_bf(ph, "qT", qT[:, 2, :, :], [128, 4, 128], qT.b)
                self.dbg_dump_bf(ph, "kT", kT[:, 0, 0:512], [128, 512], kT.b)
                self.dbg_dump_bf(ph, "glu", gluT[:, :, GLU_OFF_LAT:GLU_OFF_LAT + 128], [128, 4, 128], gluT.b)
            if self.stop == "p1":
                return
            with ExitStack() as p2:
                scr = dict(pexp=[self.tile(p2, f"pexp{i}", [128, 1024], BF16) for i in range(4)], pi=0,
                           rec=[self.tile(p2, f"rec{i}", [128, 512], F32) for i in range(2)],
                           posb=[[self.tile(p2, f"posb{i}{k}", [128, 512], F32) for k in range(2)] for i in range(2)])
                blocks = []
                for qb in range(NT):
                    kts = [(0, None), (1, None)] if qb < 2 else [(k, None) for k in range(NT)]
                    for kv in range(2):
                        blocks.append((kv, qb, kts))
                zt = self.tile(p2, "zt", [128, 4096], BF16)
                fw.pool.op(lambda: nc.gpsimd.memset(zt[:], 0.0), [], [zt.b])
                for l_ in range(2):
                    nrows = self.hs_d[l_].shape[0]
                    for r0 in range(0, nrows, 512):
                        nr = min(512, nrows - r0)
                        hb_ = Buf("hz")
                        fw.q_pool.dma(self.hs_d[l_][r0:r0 + nr, :].rearrange("(p k) d -> p (k d)", p=128), zt[:, 0:(nr // 128) * D],
                                      reads=[zt.b], writes=[hb_])
                        self.hz_b[l_].append(hb_)
                self.attention(blocks, qT, kT, Vp, oT, scr)
                fw.barrier()
            if self.debug:
                self.dbg_dump_bf(ph, "oT", oT[:, :, 256:384], [128, 4, 128], oT.b)
                self.dbg_dump_bf(ph, "oTc", oT[:, :, 0:128], [128, 4, 128], oT.b)
            pattn.close()
            if self.stop == "p2":
                return
            bT = self.tile(ph, "bT", [128, 4, T], BF16)
            with ExitStack() as p3:
                cw = self.tile(p3, "cw", [128, 4, 31], F32)
                cv = self.tile(p3, "cv", [128, 3, 4], F32)
                diag = self.tile(p3, "diag", [128, 4, 31, 128], BF16)
                onesM = self.tile(p3, "onesM", [128, 128], F32)
                fw.q_sync.dma(cw[:], self.convwT, writes=[cw.b])
                fw.q_sync.dma(cv[:], self.convv, writes=[cv.b])
                fw.dve.op(lambda: nc.vector.memset(onesM[:], 1.0 / 512), [], [onesM.b])
                for j in range(4):
                    for tap in range(31):
                        e_ = fw.dve if (tap % 2 == 0) else fw.pool
                        ee = nc.vector if (tap % 2 == 0) else nc.gpsimd
                        e_.op(lambda: ee.tensor_scalar(out=diag[:, j, tap, :], in0=g["identb"][:], scalar1=cw[:, j, tap:tap + 1], scalar2=None,
                                                       op0=ALU.mult), [g["identb"].b, cw.b], [diag.b])
                ysb = [self.tile(p3, f"ysb{i}", [128, 4, 512], F32) for i in range(2)]
                ysq = [self.tile(p3, f"ysq{i}", [128, 4, 512], F32) for i in range(2)]
                mean_l = [self.tile(p3, f"mean{i}", [128, 512], F32) for i in range(2)]
                rstd_l = [self.tile(p3, f"rstd{i}", [128, 512], F32) for i in range(2)]
                tmp_l = [[self.tile(p3, f"ctmp{i}{k}", [128, 512], F32) for k in range(2)] for i in range(2)]
                chunks = [(GLU_OFF_CTX, 0, 256)] + [(GLU_OFF_LAT + 512 * i, 256 + 512 * i, 512) for i in range(8)]

                def conv_fn(ci):
                    off, tok0, ntok = chunks[ci]
                    mean, rstd, tmp = mean_l[ci % 2], rstd_l[ci % 2], tmp_l[ci % 2]
                    y_, q_ = ysb[ci % 2], ysq[ci % 2]
                    for j in range(4):
                        p = self.ps_get()
                        for tap in range(31):
                            fw.pe.op(lambda: nc.tensor.matmul(p[:, 0:ntok], lhsT=diag[:, j, tap, :], rhs=gluT[:, j, off + tap - 15:off + tap - 15 + ntok],
                                                              start=(tap == 0), stop=(tap == 30)), [diag.b, gluT.b], [p.b], inc=(tap == 30))
                        fw.act.op(lambda: nc.scalar.activation(out=y_[:, j, 0:ntok], in_=p[:, 0:ntok], func=AF.Identity, bias=cv[:, 0, j:j + 1]),
                                  [p.b, cv.b], [y_.b])
                        self.ps_put(p)
                        fw.pool.op(lambda: nc.gpsimd.tensor_tensor(out=q_[:, j, 0:ntok], in0=y_[:, j, 0:ntok], in1=y_[:, j, 0:ntok], op=ALU.mult),
                                   [y_.b], [q_.b])
                    pm = self.ps_get()
                    pq = self.ps_get()
                    for (pp, src) in ((pm, y_), (pq, q_)):
                        for j in range(4):
                            fw.pe.op(lambda: nc.tensor.matmul(pp[:, 0:ntok], lhsT=onesM[:], rhs=src[:, j, 0:ntok], start=(j == 0), stop=(j == 3)),
                                     [onesM.b, src.b], [pp.b], inc=(j == 3))
                    fw.act.op(lambda: nc.scalar.copy(out=mean[:, 0:ntok], in_=pm[:, 0:ntok]), [pm.b], [mean.b])
                    self.ps_put(pm)
                    fw.pool.op(lambda: nc.gpsimd.tensor_tensor(out=rstd[:, 0:ntok], in0=mean[:, 0:ntok], in1=mean[:, 0:ntok], op=ALU.mult),
                               [mean.b], [rstd.b])
                    fw.dve.op(lambda: nc.vector.tensor_tensor(out=rstd[:, 0:ntok], in0=pq[:, 0:ntok], in1=rstd[:, 0:ntok], op=ALU.subtract),
                              [pq.b, rstd.b], [rstd.b])
                    self.ps_put(pq)
                    fw.dve.op(lambda: nc.vector.tensor_scalar(out=rstd[:, 0:ntok], in0=rstd[:, 0:ntok], scalar1=EPS, scalar2=None, op0=ALU.add),
                              [rstd.b], [rstd.b])
                    fw.act.op(lambda: nc.scalar.sqrt(out=rstd[:, 0:ntok], in_=rstd[:, 0:ntok]), [rstd.b], [rstd.b])
                    fw.dve.op(lambda: nc.vector.reciprocal(out=rstd[:, 0:ntok], in_=rstd[:, 0:ntok]), [rstd.b], [rstd.b])
                    for j in range(4):
                        t_ = tmp[j % 2]
                        fw.pool.op(lambda: nc.gpsimd.tensor_tensor(out=t_[:, 0:ntok], in0=y_[:, j, 0:ntok], in1=mean[:, 0:ntok], op=ALU.subtract),
                                   [y_.b, mean.b], [t_.b])
                        fw.dve.op(lambda: nc.vector.tensor_tensor(out=t_[:, 0:ntok], in0=t_[:, 0:ntok], in1=rstd[:, 0:ntok], op=ALU.mult),
                                  [t_.b, rstd.b], [t_.b])
                        fw.act.op(lambda: nc.scalar.activation(out=bT[:, j, tok0:tok0 + ntok], in_=t_[:, 0:ntok], func=AF.Silu,
                                                               scale=cv[:, 1, j:j + 1], bias=cv[:, 2, j:j + 1]), [t_.b, cv.b], [bT.b])

                conv_fn(0)
                for c0_ in range(1, 9, 2):
                    ILV.run([(lambda ci=ci: conv_fn(ci)) for ci in (c0_, c0_ + 1)])
                fw.barrier()
            if self.debug:
                self.dbg_dump_bf(ph, "bT", bT[:, :, 256:384], [128, 4, 128], bT.b)
            if self.stop == "p3":
                return
            with ExitStack() as p4:
                self.out_proj(0, lambda c: (oT, c) if c < 4 else (bT, c - 4), self.w_out_ab, list(range(NT)), p4)
                fw.barrier()

    def dbg_dump_bf(self, ph, name, src_ap, shape, buf):
        nc, fw = self.nc, self.fw
        n = int(np.prod(shape[1:]))
        ap = self.dbg(name, [128, n])
        with ExitStack() as ds:
            tf = self.tile(ds, "dbgt_" + name, shape, F32)
            fw.dve.op(lambda: nc.vector.tensor_copy(out=tf[:], in_=src_ap), [buf], [tf.b])
            flat = tf[:] if len(shape) == 2 else tf[:].rearrange("p a b -> p (a b)")
            fw.q_pool.dma(ap, flat, reads=[tf.b], writes=[self.dbg_out[name][1]])
            fw.barrier()

    def moe(self, l):
        nc, fw, g = self.nc, self.fw, self.g
        if l == 0:
            groups = [list(range(0, 10)), list(range(10, 18)), list(range(18, 26)), list(range(26, 34))]
        else:
            groups = [list(range(2 + 8 * i, 10 + 8 * i)) for i in range(4)]
        ngrp = int(os.environ.get("MOE_GROUPS", "4"))
        nexp = int(os.environ.get("MOE_EXPERTS", "32"))
        classes = [0, 1] if l == 0 else [0]
        with ExitStack() as ph:
            wr = self.tile(ph, "wr", [128, 8, 36], F32)
            rb = self.tile(ph, "rb", [128, 36], F32)
            fw.q_sync.dma(wr[:], self.rt_w[l].rearrange("(k p) n -> p k n", p=128), writes=[wr.b])
            fw.q_sync.dma(rb[:], self.rt_b[:, l, :], writes=[rb.b])
            G2B = {}
            tmpb = self.tile(ph, "bct2", [128, 128], F32)
            for cls in classes:
                G2B[cls] = self.tile(ph, f"G2B{cls}", [128, D], F32)
                self.bcast_tile(G2B[cls], lambda c: g["modT"][:, l, 40 + c, cls:cls + 1], tmpb)
            stg = [self.tile(ph, f"estg{i}", [128, 4096], F32) for i in range(2)]
            wg = [self.tile(ph, f"wg{i}", [128, 8, 512], BF16) for i in range(2)]
            wu = [self.tile(ph, f"wu{i}", [128, 8, 512], BF16) for i in range(2)]
            wd = {cls: [self.tile(ph, f"wd{cls}_{i}", [128, 4, D], BF16) for i in range(2)] for cls in classes}
            xg = self.tile(ph, "xg", [128, 10, D], F32)
            h2T = self.tile(ph, "h2T", [128, 8, 1280], BF16)
            Wt = self.tile(ph, "Wt", [128, 10, 32], F32)
            xn = self.tile(ph, "xn2", [128, D], F32)
            hTf = self.tile(ph, "hTf", [128, 8, 128], F32)
            ss = self.tile(ph, "ss2", [128, 1], F32)
            r_ = {k: self.tile(ph, "r_" + k, [128, n], F32) for k, n in
                  dict(lg=36, gmax=1, ngmax=1, goh=4, ge=4, gsum=1, pen=4, em=32, m8=8, d=1, ed=1, p1=1, p2=1, t1=32, t2=32).items()}
            hid = [self.tile(ph, f"hid{i}", [128, 4, 512], BF16) for i in range(2)]
            sgl = [self.tile(ph, f"sgl{i}", [128, 512], F32) for i in range(2)]
            self.stg_i = 0

            def load_expert(e, need_ctx):
                i = e % 2
                self.load_cast_w(stg, wg[i], slice(0, 512), self.ex_gate[l, e], 8, 512, 0)
                self.load_cast_w(stg, wu[i], slice(0, 512), self.ex_up[l, e], 8, 512, 0)
                s_ = stg[self.stg_i % 2]
                self.stg_i += 1
                fw.q_sync.dma(s_[:].rearrange("p (k n) -> p k n", k=4), self.ex_down[l, e].rearrange("(k p) n -> p k n", p=128), writes=[s_.b])
                for cls in classes:
                    if cls == 1 and not need_ctx:
                        continue
                    fw.dve.op(lambda: nc.vector.tensor_tensor(out=wd[cls][i][:], in0=s_[:].rearrange("p (k n) -> p k n", k=4),
                                                              in1=G2B[cls][:].unsqueeze(1).to_broadcast([128, 4, D]), op=ALU.mult),
                              [s_.b, G2B[cls].b], [wd[cls][i].b])

            for gi, tiles in enumerate(groups[:ngrp]):
                has_ctx = (l == 0 and gi == 0)
                load_expert(0, has_ctx)
                for ti, t in enumerate(tiles):
                    cls = 1 if t < 2 else 0
                    fw.q_sync.dma(xg[:, ti, :], self.xres[t * 128:(t + 1) * 128, :], reads=[self.xres_b[t]], writes=[xg.b])
                    fw.act.op(lambda: nc.scalar.activation(out=xn[:], in_=xg[:, ti, :], func=AF.Square, accum_out=ss[:, 0:1]), [xg.b], [xn.b, ss.b])
                    self.rstd_from_ss(ss, 1.0 / D, 1)
                    fw.act.op(lambda: nc.scalar.activation(out=xn[:], in_=xg[:, ti, :], func=AF.Copy, scale=ss[:, 0:1]), [xg.b, ss.b], [xn.b])
                    for hb in range(2):
                        p = self.ps_get()
                        pv = p[:].rearrange("p (c q) -> p c q", c=4)
                        for c4 in range(4):
                            c = hb * 4 + c4
                            fw.pe.op(lambda: nc.tensor.transpose(out=pv[:, c4, :], in_=xn[:, c * 128:(c + 1) * 128], identity=g["identf"][:]),
                                     [xn.b, g["identf"].b], [p.b], inc=(c4 == 3))
                        for c4 in range(4):
                            c = hb * 4 + c4
                            if hb == 0:
                                fw.dve.op(lambda: nc.vector.tensor_scalar(out=hTf[:, c, :], in0=pv[:, c4, :], scalar1=g["A2"][:, l, c, cls:cls + 1],
                                                                          scalar2=g["modT"][:, l, 24 + c, cls:cls + 1], op0=ALU.mult, op1=ALU.add),
                                          [p.b, g["A2"].b, g["modT"].b], [hTf.b])
                            else:
                                fw.act.op(lambda: nc.scalar.activation(out=hTf[:, c, :], in_=pv[:, c4, :], func=AF.Identity,
                                                                       scale=g["A2"][:, l, c, cls:cls + 1], bias=g["modT"][:, l, 24 + c, cls:cls + 1]),
                                          [p.b, g["A2"].b, g["modT"].b], [hTf.b])
                        self.ps_put(p)
                    fw.pool.op(lambda: nc.gpsimd.tensor_copy(out=h2T[:, :, ti * 128:(ti + 1) * 128], in_=hTf[:]), [hTf.b], [h2T.b])
                    pr = self.ps_get()
                    for c in range(8):
                        fw.pe.op(lambda: nc.tensor.matmul(pr[:, 0:36], lhsT=hTf[:, c, :], rhs=wr[:, c, :], start=(c == 0), stop=(c == 7)),
                                 [hTf.b, wr.b], [pr.b], inc=(c == 7))
                    self.route(pr, rb, r_, Wt, ti)
                    self.ps_put(pr)
                if self.debug and gi == 0:
                    self.dbg_dump_bf(ph, f"Wt{l}", Wt[:, 0:4, :], [128, 4, 32], Wt.b)
                if has_ctx:
                    chunks = [(0, 2), (2, 4), (6, 4)]
                else:
                    chunks = [(0, 4), (4, 4)]
                for e in range(nexp):
                    if e + 1 < nexp:
                        load_expert(e + 1, has_ctx)
                    i = e % 2
                    for ci, (tl0, ntl) in enumerate(chunks):
                        cls = 1 if (has_ctx and ci == 0) else 0
                        ntok = ntl * 128
                        c0 = tl0 * 128
                        hd = hid[(e * len(chunks) + ci) % 2]
                        for f in range(4):
                            pg = self.ps_get()
                            pu = self.ps_get()
                            for (pp, w_) in ((pg, wg[i]), (pu, wu[i])):
                                for k in range(8):
                                    fw.pe.op(lambda: nc.tensor.matmul(pp[:, 0:ntok], lhsT=w_[:, k, f * 128:(f + 1) * 128], rhs=h2T[:, k, c0:c0 + ntok],
                                                                      start=(k == 0), stop=(k == 7)), [w_.b, h2T.b], [pp.b], inc=(k == 7))
                            sg = sgl[f % 2]
                            fw.act.op(lambda: nc.scalar.activation(out=sg[:, 0:ntok], in_=pg[:, 0:ntok], func=AF.Silu), [pg.b], [sg.b])
                            fw.dve.op(lambda: nc.vector.tensor_tensor(out=hd[:, f, 0:ntok], in0=pu[:, 0:ntok], in1=sg[:, 0:ntok], op=ALU.mult),
                                      [pu.b, sg.b], [hd.b])
                            self.ps_put(pg)
                            self.ps_put(pu)
                        for tl in range(ntl):
                            ti = tl0 + tl
                            for half in range(2):
                                pd = self.ps_get()
                                for f in range(4):
                                    fw.pe.op(lambda: nc.tensor.matmul(pd[:], lhsT=hd[:, f, tl * 128:(tl + 1) * 128],
                                                                      rhs=wd[cls][i][:, f, half * 512:(half + 1) * 512], start=(f == 0), stop=(f == 3)),
                                             [hd.b, wd[cls][i].b], [pd.b], inc=(f == 3))
                                fw.dve.op(lambda: nc.vector.scalar_tensor_tensor(out=xg[:, ti, half * 512:(half + 1) * 512], in0=pd[:],
                                                                                 scalar=Wt[:, ti, e:e + 1], in1=xg[:, ti, half * 512:(half + 1) * 512],
                                                                                 op0=ALU.mult, op1=ALU.add), [pd.b, Wt.b, xg.b], [xg.b])
                                self.ps_put(pd)
                for ti, t in enumerate(tiles):
                    if l == 0:
                        fw.q_pool.dma(self.xres[t * 128:(t + 1) * 128, :], xg[:, ti, :], reads=[xg.b], writes=[self.xres_b[t]])
                    else:
                        fw.q_pool.dma(self.y_out[(t - 2) * 128:(t - 1) * 128, :], xg[:, ti, :], reads=[xg.b], writes=[self.y_b[t]])
                    if self.debug and t in (0, 2, NT - 1):
                        nm = f"x2_{l}_{t}"
                        ap = self.dbg(nm, [128, D])
                        fw.q_pool.dma(ap, xg[:, ti, :], reads=[xg.b], writes=[self.dbg_out[nm][1]])
            fw.barrier()

    def moe2(self, l):
        nc, fw, g = self.nc, self.fw, self.g
        V = nc.vector
        tiles = list(range(NT)) if l == 0 else list(range(2, NT))
        ntl = len(tiles)
        NA = 2 * ntl
        NB = (2 * ntl * 128 + 32 * 127 + 127) // 128
        classes = [0, 1] if l == 0 else [0]
        hs = self.hs_d[l]
        ys = nc.dram_tensor(f"ys{l}", [NB * 128, D], F32, kind="Internal").ap()
        blkE_d = nc.dram_tensor(f"blkE{l}", [1, NB], I32, kind="Internal").ap()
        hs_bs = [Buf() for _ in range(NA)]
        ys_bs = [Buf() for _ in range(NB)]
        blkE_b = Buf()
        dd = lambda fn, rd, wr_: fw.dve.op(fn, [x.b for x in rd], [x.b for x in wr_])
        with ExitStack() as pm:
            DESTi = self.tile(pm, "DESTi", [128, NA], I32)
            W12 = self.tile(pm, "W12", [128, NA], F32)
            WIDX = self.tile(pm, "WIDX", [128, NB], I32)
            G2B = {}
            for cls in classes:
                G2B[cls] = self.tile(pm, f"G2Bm{cls}", [128, D], F32)
            with ExitStack() as ph:
                tmpb = self.tile(ph, "bct3", [128, 128], F32)
                A2B, B2B = {}, {}
                for cls in classes:
                    A2B[cls] = self.tile(ph, f"A2B{cls}", [128, D], F32)
                    B2B[cls] = self.tile(ph, f"B2B{cls}", [128, D], F32)
                    self.bcast_tile(G2B[cls], lambda c: g["modT"][:, l, 40 + c, cls:cls + 1], tmpb)
                    self.bcast_tile(A2B[cls], lambda c: g["A2"][:, l, c, cls:cls + 1], tmpb, extra=[g["A2"].b])
                    self.bcast_tile(B2B[cls], lambda c: g["modT"][:, l, 24 + c, cls:cls + 1], tmpb)
                wr = self.tile(ph, "wr", [128, 8, 36], F32)
                rb = self.tile(ph, "rb", [128, 36], F32)
                mc = self.tile(ph, "mc", [128, 433], F32)
                Uf = self.tile(ph, "Uf", [128, 128], F32)
                Ub = self.tile(ph, "Ub", [128, 128], BF16)
                onesb = self.tile(ph, "onesb", [128, 128], BF16)
                fw.q_sync.dma(wr[:], self.rt_w[l].rearrange("(k p) n -> p k n", p=128), writes=[wr.b])
                fw.q_sync.dma(rb[:], self.rt_b[:, l, :], writes=[rb.b])
                fw.q_sync.dma(mc[:], self.mconst, writes=[mc.b])
                fw.q_sync.dma(Uf[:], self.umat, writes=[Uf.b])
                dd(lambda: V.tensor_copy(out=Ub[:], in_=Uf[:]), [Uf], [Ub])
                dd(lambda: V.memset(onesb[:], 1.0), [], [onesb])
                hz_b = self.hz_b[l]
                h2tm = self.tile(ph, "h2tm", [128, ntl, D], BF16)
                OH = self.tile(ph, "OH", [128, NA, 32], BF16)
                RK = self.tile(ph, "RK", [128, NA], F32)
                run = self.tile(ph, "run", [128, 32], F32)
                dd(lambda: V.memset(run[:], 0.0), [], [run])
                xt = [self.tile(ph, f"mxt{i}", [128, D], F32) for i in range(2)]
                xn_l = [self.tile(ph, f"mxn{i}", [128, D], F32) for i in range(2)]
                xm_l = [self.tile(ph, f"mxm{i}", [128, D], F32) for i in range(2)]
                hTf_l = [self.tile(ph, f"mhTf{i}", [128, 8, 128], F32) for i in range(2)]
                ss_l = [self.tile(ph, f"mss{i}", [128, 1], F32) for i in range(2)]
                r_l = [{k: self.tile(ph, f"r{i}_" + k, [128, n], F32) for k, n in
                        dict(lg=36, gmax=1, ngmax=1, goh=4, ge=4, gsum=1, pen=4, em=32, m8=8, d=1, ed=1, p1=1, p2=1, t1=32, t2=32, rf=32).items()}
                       for i in range(2)]
                def tile_fn(ti, t):
                    cls = 1 if t < 2 else 0
                    xn, xm, hTf, ss, r_ = xn_l[ti % 2], xm_l[ti % 2], hTf_l[ti % 2], ss_l[ti % 2], r_l[ti % 2]
                    x_ = xt[ti % 2]
                    fw.q_sync.dma(x_[:], self.xres[t * 128:(t + 1) * 128, :], reads=[self.xres_b[t]], writes=[x_.b])
                    fw.act.op(lambda: nc.scalar.activation(out=xn[:], in_=x_[:], func=AF.Square, accum_out=ss[:, 0:1]), [x_.b], [xn.b, ss.b])
                    self.rstd_from_ss(ss, 1.0 / D, 1)
                    fw.act.op(lambda: nc.scalar.activation(out=xn[:], in_=x_[:], func=AF.Copy, scale=ss[:, 0:1]), [x_.b, ss.b], [xn.b])
                    fw.pool.op(lambda: nc.gpsimd.tensor_tensor(out=xm[:], in0=xn[:], in1=A2B[cls][:], op=ALU.mult), [xn.b, A2B[cls].b], [xm.b])
                    fw.pool.op(lambda: nc.gpsimd.tensor_tensor(out=h2tm[:, ti, :], in0=xm[:], in1=B2B[cls][:], op=ALU.add), [xm.b, B2B[cls].b], [h2tm.b])
                    for hb in range(2):
                        p = self.ps_get()
                        pv = p[:].rearrange("p (c q) -> p c q", c=4)
                        for c4 in range(4):
                            c = hb * 4 + c4
                            fw.pe.op(lambda: nc.tensor.transpose(out=pv[:, c4, :], in_=xn[:, c * 128:(c + 1) * 128], identity=g["identf"][:]),
                                     [xn.b, g["identf"].b], [p.b], inc=(c4 == 3))
                        for c4 in range(4):
                            c = hb * 4 + c4
                            if hb == 0:
                                fw.dve.op(lambda: V.tensor_scalar(out=hTf[:, c, :], in0=pv[:, c4, :], scalar1=g["A2"][:, l, c, cls:cls + 1],
                                                                  scalar2=g["modT"][:, l, 24 + c, cls:cls + 1], op0=ALU.mult, op1=ALU.add),
                                          [p.b, g["A2"].b, g["modT"].b], [hTf.b])
                            else:
                                fw.act.op(lambda: nc.scalar.activation(out=hTf[:, c, :], in_=pv[:, c4, :], func=AF.Identity,
                                                                       scale=g["A2"][:, l, c, cls:cls + 1], bias=g["modT"][:, l, 24 + c, cls:cls + 1]),
                                          [p.b, g["A2"].b, g["modT"].b], [hTf.b])
                        self.ps_put(p)
                    pr = self.ps_get()
                    for c in range(8):
                        fw.pe.op(lambda: nc.tensor.matmul(pr[:, 0:36], lhsT=hTf[:, c, :], rhs=wr[:, c, :], start=(c == 0), stop=(c == 7)),
                                 [hTf.b, wr.b], [pr.b], inc=(c == 7))
                    self.route(pr, rb, r_, None, ti, OH=OH, W12=W12)
                    self.ps_put(pr)

                def rank_fn(ti):
                    r_ = r_l[ti % 2]
                    for k in range(2):
                        a = 2 * ti + k
                        pk = self.ps_get()
                        fw.pe.op(lambda: nc.tensor.matmul(pk[:, 0:32], lhsT=Ub[:], rhs=OH[:, a, :], start=True, stop=True), [Ub.b, OH.b], [pk.b], inc=False)
                        fw.pe.op(lambda: nc.tensor.matmul(pk[:, 32:64], lhsT=onesb[:], rhs=OH[:, a, :], start=True, stop=True), [onesb.b, OH.b], [pk.b])
                        rf = r_["rf"]
                        dd(lambda: V.tensor_tensor(out=rf[:], in0=pk[:, 0:32], in1=run[:], op=ALU.add), [pk, run], [rf])
                        dd(lambda: V.tensor_tensor(out=rf[:], in0=rf[:], in1=OH[:, a, :], op=ALU.mult), [rf, OH], [rf])
                        dd(lambda: V.tensor_reduce(out=RK[:, a:a + 1], in_=rf[:], axis=AX.X, op=ALU.add), [rf], [RK])
                        dd(lambda: V.tensor_tensor(out=run[:], in0=pk[:, 32:64], in1=run[:], op=ALU.add), [pk, run], [run])
                        self.ps_put(pk)
                for ti0 in range(0, ntl, 2):
                    pair = list(range(ti0, min(ti0 + 2, ntl)))
                    ILV.run([(lambda ti=ti: tile_fn(ti, tiles[ti])) for ti in pair])
                    for ti in pair:
                        rank_fn(ti)
                cmpf = self.tile(ph, "cmp", [128, 5120], F32)
                v3 = lambda n_a, n_b: cmpf[:, 0:n_a * n_b].rearrange("p (a b) -> p a b", b=n_b)
                T_ = lambda nm, n: self.tile(ph, nm, [128, n], F32)
                nblk, exc, nbp, mm, pbx = T_("nblk", 32), T_("exc", 32), T_("nbp", 32), T_("mm", 32), T_("pbx", 32)
                sc = [T_(f"scan{i}", 32) for i in range(2)]
                blkf, dstf = T_("blkf", NB), T_("dstf", NA)
                selX, selM, selS, jf, ta_, tb_ = T_("selX", NA), T_("selM", NA), T_("selS", NA), T_("jf", NA), T_("ta_", NA), T_("tb_", NA)
                pend, pbase, m2k, lsk = T_("pend", 16), T_("pbase", 16), T_("m2k", 16), T_("lsk", 16)
                kidx, pbp, m2p, lsp, o_, q_, par_, int_ = (T_(n_, NB) for n_ in ("kidx", "pbp", "m2p", "lsp", "o_", "q_", "par_", "int_"))
                IOTA32, THR, IOTAB, PAR32, PARB, THR2, THR3, IOTA16 = (mc[:, 0:32], mc[:, 32:67], mc[:, 67:67 + NB], mc[:, 167:199],
                                                                     mc[:, 199:199 + NB], mc[:, 299:367], mc[:, 367:417], mc[:, 417:433])
                c3 = v3(32, 35)
                dd(lambda: V.tensor_tensor(out=c3, in0=run[:].unsqueeze(2).to_broadcast([128, 32, 35]),
                                           in1=THR.unsqueeze(1).to_broadcast([128, 32, 35]), op=ALU.is_gt), [run, mc], [cmpf])
                dd(lambda: V.tensor_reduce(out=nblk[:], in_=c3, axis=AX.X, op=ALU.add), [cmpf], [nblk])
                dd(lambda: V.tensor_copy(out=sc[0][:], in_=nblk[:]), [nblk], [sc[0]])
                cur = 0
                for sh in (1, 2, 4, 8, 16):
                    a_, b_ = sc[cur], sc[1 - cur]
                    dd(lambda: V.tensor_copy(out=b_[:, 0:sh], in_=a_[:, 0:sh]), [a_], [b_])
                    dd(lambda: V.tensor_tensor(out=b_[:, sh:32], in0=a_[:, sh:32], in1=a_[:, 0:32 - sh], op=ALU.add), [a_], [b_])
                    cur = 1 - cur
                inc_ = sc[cur]
                dd(lambda: V.tensor_tensor(out=exc[:], in0=inc_[:], in1=nblk[:], op=ALU.subtract), [inc_, nblk], [exc])
                pr2 = lambda tl: tl[:].rearrange("p (k s) -> p k s", s=2)
                dd(lambda: V.tensor_copy(out=pr2(nbp)[:, :, 0:1], in_=pr2(nblk)[:, :, 1:2]), [nblk], [nbp])
                dd(lambda: V.tensor_copy(out=pr2(nbp)[:, :, 1:2], in_=pr2(nblk)[:, :, 0:1]), [nblk], [nbp])
                dd(lambda: V.tensor_tensor(out=mm[:], in0=nblk[:], in1=nbp[:], op=ALU.min), [nblk, nbp], [mm])
                dd(lambda: V.tensor_scalar(out=pr2(pbx)[:, :, 0:1], in0=pr2(exc)[:, :, 0:1], scalar1=128.0, scalar2=None, op0=ALU.mult), [exc], [pbx])
                dd(lambda: V.tensor_scalar(out=pr2(pbx)[:, :, 1:2], in0=pr2(exc)[:, :, 0:1], scalar1=128.0, scalar2=None, op0=ALU.mult), [exc], [pbx])
                c4_ = v3(NA, 32)
                for (dst_, vec_, vb_) in ((selX, pbx[:], pbx.b), (selM, mm[:], mm.b), (selS, PAR32, mc.b)):
                    dd(lambda: V.tensor_tensor(out=c4_, in0=OH[:], in1=vec_.unsqueeze(1).to_broadcast([128, NA, 32]), op=ALU.mult), [OH, Tile(None, vb_)], [cmpf])
                    dd(lambda: V.tensor_reduce(out=dst_[:], in_=c4_, axis=AX.X, op=ALU.add), [cmpf], [dst_])
                c5_ = v3(NA, 68)
                dd(lambda: V.tensor_tensor(out=c5_, in0=RK[:].unsqueeze(2).to_broadcast([128, NA, 68]),
                                           in1=THR2.unsqueeze(1).to_broadcast([128, NA, 68]), op=ALU.is_ge), [RK, mc], [cmpf])
                dd(lambda: V.tensor_reduce(out=jf[:], in_=c5_, axis=AX.X, op=ALU.add), [cmpf], [jf])
                dd(lambda: V.scalar_tensor_tensor(out=ta_[:], in0=jf[:], scalar=2.0, in1=selS[:], op0=ALU.mult, op1=ALU.add), [jf, selS], [ta_])
                dd(lambda: V.tensor_tensor(out=tb_[:], in0=selM[:], in1=jf[:], op=ALU.add), [selM, jf], [tb_])
                dd(lambda: V.tensor_tensor(out=ta_[:], in0=ta_[:], in1=tb_[:], op=ALU.min), [ta_, tb_], [ta_])
                dd(lambda: V.tensor_tensor(out=ta_[:], in0=ta_[:], in1=jf[:], op=ALU.subtract), [ta_, jf], [ta_])
                dd(lambda: V.scalar_tensor_tensor(out=dstf[:], in0=ta_[:], scalar=128.0, in1=selX[:], op0=ALU.mult, op1=ALU.add), [ta_, selX], [dstf])
                dd(lambda: V.tensor_tensor(out=dstf[:], in0=dstf[:], in1=RK[:], op=ALU.add), [dstf, RK], [dstf])
                dd(lambda: V.tensor_copy(out=DESTi[:], in_=dstf[:]), [dstf], [DESTi])
                dd(lambda: V.tensor_copy(out=pend[:], in_=pr2(inc_)[:, :, 1]), [inc_], [pend])
                dd(lambda: V.tensor_copy(out=pbase[:], in_=pr2(exc)[:, :, 0]), [exc], [pbase])
                dd(lambda: V.tensor_scalar(out=m2k[:], in0=pr2(mm)[:, :, 0], scalar1=2.0, scalar2=None, op0=ALU.mult), [mm], [m2k])
                dd(lambda: V.tensor_tensor(out=lsk[:], in0=pr2(nblk)[:, :, 1], in1=pr2(nblk)[:, :, 0], op=ALU.is_gt), [nblk], [lsk])
                c6_ = v3(NB, 16)
                dd(lambda: V.tensor_tensor(out=c6_, in0=pend[:].unsqueeze(1).to_broadcast([128, NB, 16]),
                                           in1=IOTAB.unsqueeze(2).to_broadcast([128, NB, 16]), op=ALU.is_le), [pend, mc], [cmpf])
                dd(lambda: V.tensor_reduce(out=kidx[:], in_=c6_, axis=AX.X, op=ALU.add), [cmpf], [kidx])
                dd(lambda: V.tensor_scalar(out=kidx[:], in0=kidx[:], scalar1=15.0, scalar2=None, op0=ALU.min), [kidx], [kidx])
                ohk = self.tile(ph, "ohk", [128, NB, 16], F32)
                dd(lambda: V.tensor_tensor(out=ohk[:], in0=kidx[:].unsqueeze(2).to_broadcast([128, NB, 16]),
                                           in1=IOTA16.unsqueeze(1).to_broadcast([128, NB, 16]), op=ALU.is_equal), [kidx, mc], [ohk])
                for (dst_, vec_) in ((pbp, pbase), (m2p, m2k), (lsp, lsk)):
                    dd(lambda: V.tensor_tensor(out=c6_, in0=ohk[:], in1=vec_[:].unsqueeze(1).to_broadcast([128, NB, 16]), op=ALU.mult), [ohk, vec_], [cmpf])
                    dd(lambda: V.tensor_reduce(out=dst_[:], in_=c6_, axis=AX.X, op=ALU.add), [cmpf], [dst_])
                dd(lambda: V.tensor_tensor(out=o_[:], in0=IOTAB, in1=pbp[:], op=ALU.subtract), [mc, pbp], [o_])
                c7_ = v3(NB, 50)
                dd(lambda: V.tensor_tensor(out=c7_, in0=o_[:].unsqueeze(2).to_broadcast([128, NB, 50]),
                                           in1=THR3.unsqueeze(1).to_broadcast([128, NB, 50]), op=ALU.is_ge), [o_, mc], [cmpf])
                dd(lambda: V.tensor_reduce(out=q_[:], in_=c7_, axis=AX.X, op=ALU.add), [cmpf], [q_])
                dd(lambda: V.scalar_tensor_tensor(out=par_[:], in0=q_[:], scalar=-2.0, in1=o_[:], op0=ALU.mult, op1=ALU.add), [q_, o_], [par_])
                dd(lambda: V.tensor_tensor(out=int_[:], in0=o_[:], in1=m2p[:], op=ALU.is_lt), [o_, m2p], [int_])
                dd(lambda: V.tensor_tensor(out=par_[:], in0=par_[:], in1=lsp[:], op=ALU.subtract), [par_, lsp], [par_])
                dd(lambda: V.tensor_tensor(out=par_[:], in0=par_[:], in1=int_[:], op=ALU.mult), [par_, int_], [par_])
                dd(lambda: V.tensor_tensor(out=par_[:], in0=par_[:], in1=lsp[:], op=ALU.add), [par_, lsp], [par_])
                dd(lambda: V.scalar_tensor_tensor(out=blkf[:], in0=kidx[:], scalar=2.0, in1=par_[:], op0=ALU.mult, op1=ALU.add), [kidx, par_], [blkf])
                pix = self.tile(ph, "pix", [128, 1], F32)
                fw.q_sync.dma(pix[:], self.pidx, writes=[pix.b])
                dd(lambda: V.tensor_scalar(out=blkf[:], in0=blkf[:], scalar1=128.0, scalar2=pix[:, 0:1], op0=ALU.mult, op1=ALU.add), [blkf, pix], [blkf])
                if l == 1:
                    dd(lambda: V.tensor_scalar(out=blkf[:], in0=blkf[:], scalar1=4096.0, scalar2=None, op0=ALU.add), [blkf], [blkf])
                same2 = self.tile(ph, "same2", [128, NB], F32)
                dd(lambda: V.tensor_tensor(out=same2[:, 2:NB], in0=blkf[:, 2:NB], in1=blkf[:, 0:NB - 2], op=ALU.is_equal), [blkf], [same2])
                dd(lambda: V.tensor_scalar(out=same2[:, 2:NB], in0=same2[:, 2:NB], scalar1=1.0e6, scalar2=None, op0=ALU.mult), [same2], [same2])
                dd(lambda: V.tensor_tensor(out=blkf[:, 2:NB], in0=blkf[:, 2:NB], in1=same2[:, 2:NB], op=ALU.add), [blkf, same2], [blkf])
                dd(lambda: V.tensor_copy(out=WIDX[:], in_=blkf[:]), [blkf], [WIDX])
                if self.debug:
                    self.dbg_dump_bf(ph, f"dst{l}", dstf[:, 0:8], [128, 8], dstf.b)
                    self.dbg_dump_bf(ph, f"blk{l}", blkf[:, 0:NB], [128, NB], blkf.b)
                    self.dbg_dump_bf(ph, f"cnt{l}", run[:, 0:32], [128, 32], run.b)
                if os.environ.get("MOE_PROBE") == "1":
                    def tryv(nm, f):
                        try:
                            f(); print("PROBE ok", nm, flush=True)
                        except Exception as e:
                            print("PROBE fail", nm, repr(e)[:120], flush=True)
                    tryv("base", lambda: nc.gpsimd.indirect_dma_start(out=hs[:, :], out_offset=bass.IndirectOffsetOnAxis(ap=DESTi[:, 0:1], axis=0),
                                                                      in_=h2tm[:, 0, :], in_offset=None, bounds_check=NB * 128 - 1, oob_is_err=False))
                    tryv("xn_f32_ys", lambda: nc.gpsimd.indirect_dma_start(out=ys[:, :], out_offset=bass.IndirectOffsetOnAxis(ap=DESTi[:, 0:1], axis=0),
                                                                      in_=xn[:, :], in_offset=None, bounds_check=NB * 128 - 1, oob_is_err=False))
                    tryv("blki_idx", lambda: nc.gpsimd.indirect_dma_start(out=hs[:, :], out_offset=bass.IndirectOffsetOnAxis(ap=blki[:, 0:1], axis=0),
                                                                      in_=h2tm[:, 0, :], in_offset=None, bounds_check=NB * 128 - 1, oob_is_err=False))
                    tryv("gather", lambda: nc.gpsimd.indirect_dma_start(out=xn[:, :], out_offset=None, in_=ys[:, :],
                                                                      in_offset=bass.IndirectOffsetOnAxis(ap=DESTi[:, 0:1], axis=0), bounds_check=NB * 128 - 1, oob_is_err=False))
                    tryv("plain", lambda: nc.gpsimd.dma_start(out=ys[0:128, :], in_=xn[:, :]))
                for a in range(NA):
                    if os.environ.get("MOE_PROBE") == "1":
                        print("PROBE scatter a", a, flush=True)
                    fw.q_pool.dma_fn(lambda: nc.gpsimd.indirect_dma_start(
                        out=hs[:, :], out_offset=bass.IndirectOffsetOnAxis(ap=DESTi[:, a:a + 1], axis=0),
                        in_=h2tm[:, a // 2, :], in_offset=None),
                        reads=[h2tm.b, DESTi.b] + hz_b, writes=[hs_bs[a]])
                fw.barrier()
            with ExitStack() as ph:
                stg = {k: [self.tile(ph, f"bs{k}{i}", [128, 4096], F32) for i in range(2)] for k in "gud"}
                wgt = {k: [self.tile(ph, f"bw{k}{i}", [128, 4096], BF16) for i in range(2)] for k in "gud"}
                xb = [self.tile(ph, f"xb{i}", [128, D], BF16) for i in range(4)]
                xbT = [self.tile(ph, f"xbT{i}", [128, 8, 128], BF16) for i in range(2)]
                sg_l = [self.tile(ph, f"bsg{i}", [128, 512], F32) for i in range(2)]
                hid_l = [self.tile(ph, f"bhid{i}", [128, 512], BF16) for i in range(2)]
                hidT_l = [self.tile(ph, f"bhidT{i}", [128, 4, 128], BF16) for i in range(2)]
                ysb = [self.tile(ph, f"ysb{i}", [128, D], F32) for i in range(2)]
                srcs = dict(g=self.ex_gate, u=self.ex_up, d=self.ex_down)

                def gathers(i):
                    b2 = i % 2
                    for k in "gud":
                        fw.q_pool.dma_fn(lambda: nc.gpsimd.indirect_dma_start(
                            out=stg[k][b2][:, :], out_offset=None, in_=srcs[k].rearrange("l r n -> (l r) n"),
                            in_offset=bass.IndirectOffsetOnAxis(ap=WIDX[:, i:i + 1], axis=0),
                            bounds_check=self.bc_reg, oob_is_err=False),
                            reads=[WIDX.b], writes=[stg[k][b2].b])

                def casts(i):
                    b2 = i % 2
                    fw.dve.op(lambda: V.tensor_copy(out=wgt["g"][b2][:], in_=stg["g"][b2][:]), [stg["g"][b2].b], [wgt["g"][b2].b])
                    fw.act.op(lambda: nc.scalar.copy(out=wgt["u"][b2][:], in_=stg["u"][b2][:]), [stg["u"][b2].b], [wgt["u"][b2].b])
                    fw.dve.op(lambda: V.tensor_copy(out=wgt["d"][b2][:, 0:2048], in_=stg["d"][b2][:, 0:2048]), [stg["d"][b2].b], [wgt["d"][b2].b])
                    fw.act.op(lambda: nc.scalar.copy(out=wgt["d"][b2][:, 2048:4096], in_=stg["d"][b2][:, 2048:4096]), [stg["d"][b2].b], [wgt["d"][b2].b])

                def xload(i):
                    fw.q_sync.dma(xb[i % 4][:], hs[i * 128:(i + 1) * 128, :], reads=hs_bs, writes=[xb[i % 4].b])

                def block_fn(i):
                    b2 = i % 2
                    sg, hid, hidT = sg_l[b2], hid_l[b2], hidT_l[b2]
                    wg_ = wgt["g"][b2][:].rearrange("p (k n) -> p k n", k=8)
                    wu_ = wgt["u"][b2][:].rearrange("p (k n) -> p k n", k=8)
                    wd_ = wgt["d"][b2][:].rearrange("p (k n) -> p k n", k=4)
                    p = self.ps_get()
                    pv = p[:].bitcast(BF16).rearrange("p (c q) -> p c q", c=8)
                    for c in range(8):
                        fw.pe.op(lambda: nc.tensor.transpose(out=pv[:, c, :], in_=xb[i % 4][:, c * 128:(c + 1) * 128], identity=g["identb"][:]),
                                 [xb[i % 4].b, g["identb"].b], [p.b], inc=(c == 7))
                    dd(lambda: V.tensor_copy(out=xbT[b2][:], in_=pv), [p], [xbT[b2]])
                    self.ps_put(p)
                    pg = self.ps_get()
                    pu = self.ps_get()
                    for (pp, w_, wb_) in ((pg, wg_, wgt["g"][b2].b), (pu, wu_, wgt["u"][b2].b)):
                        for k in range(8):
                            fw.pe.op(lambda: nc.tensor.matmul(pp[:], lhsT=xbT[b2][:, k, :], rhs=w_[:, k, :], start=(k == 0), stop=(k == 7)),
                                     [xbT[b2].b, wb_], [pp.b], inc=(k == 7))
                    fw.act.op(lambda: nc.scalar.activation(out=sg[:], in_=pg[:], func=AF.Silu), [pg.b], [sg.b])
                    dd(lambda: V.tensor_tensor(out=hid[:], in0=pu[:], in1=sg[:], op=ALU.mult), [pu, sg], [hid])
                    self.ps_put(pg)
                    self.ps_put(pu)
                    p = self.ps_get()
                    pv = p[:].bitcast(BF16).rearrange("p (c q) -> p c q", c=8)
                    for c in range(4):
                        fw.pe.op(lambda: nc.tensor.transpose(out=pv[:, c, :], in_=hid[:, c * 128:(c + 1) * 128], identity=g["identb"][:]),
                                 [hid.b, g["identb"].b], [p.b], inc=(c == 3))
                    dd(lambda: V.tensor_copy(out=hidT[:], in_=pv[:, 0:4, :]), [p], [hidT])
                    self.ps_put(p)
                    y_ = ysb[b2]
                    pds = []
                    for half in range(2):
                        pd = self.ps_get()
                        pds.append(pd)
                        for f in range(4):
                            fw.pe.op(lambda: nc.tensor.matmul(pd[:], lhsT=hidT[:, f, :], rhs=wd_[:, f, half * 512:(half + 1) * 512],
                                                              start=(f == 0), stop=(f == 3)), [hidT.b, wgt["d"][b2].b], [pd.b], inc=(f == 3))
                    if i + 2 < NB:
                        casts(i + 2)
                    fw.act.op(lambda: nc.scalar.copy(out=y_[:, 0:512], in_=pds[0][:]), [pds[0].b], [y_.b])
                    dd(lambda: V.tensor_copy(out=y_[:, 512:1024], in_=pds[1][:]), [pds[1]], [y_])
                    self.ps_put(pds[0])
                    self.ps_put(pds[1])
                    fw.q_sync.dma(ys[i * 128:(i + 1) * 128, :], y_[:], reads=[y_.b], writes=[ys_bs[i]])

                gathers(0)
                gathers(1)
                for j in range(2):
                    xload(j)
                casts(0)
                casts(1)
                for i0_ in range(0, NB, 2):
                    for j in (i0_ + 2, i0_ + 3):
                        if j < NB:
                            gathers(j)
                            xload(j)
                    ILV.run([(lambda i=i: block_fn(i)) for i in (i0_, i0_ + 1) if i < NB])
                fw.barrier()
            with ExitStack() as ph:
                xt = [self.tile(ph, f"cxt{i}", [128, D], F32) for i in range(3)]
                y1 = [self.tile(ph, f"cy1{i}", [128, D], F32) for i in range(3)]
                y2 = [self.tile(ph, f"cy2{i}", [128, D], F32) for i in range(3)]
                def comb_fn(ti, t):
                    cls = 1 if t < 2 else 0
                    x_, a1, a2 = xt[ti % 3], y1[ti % 3], y2[ti % 3]
                    fw.q_sync.dma(x_[:], self.xres[t * 128:(t + 1) * 128, :], reads=[self.xres_b[t]], writes=[x_.b])
                    for k, yy in ((0, a1), (1, a2)):
                        a = 2 * ti + k
                        fw.q_pool.dma_fn(lambda: nc.gpsimd.indirect_dma_start(
                            out=yy[:, :], out_offset=None, in_=ys[:, :],
                            in_offset=bass.IndirectOffsetOnAxis(ap=DESTi[:, a:a + 1], axis=0)),
                            reads=ys_bs + [DESTi.b], writes=[yy.b])
                    dd(lambda: V.tensor_scalar(out=a1[:], in0=a1[:], scalar1=W12[:, 2 * ti:2 * ti + 1], scalar2=None, op0=ALU.mult), [a1, W12], [a1])
                    dd(lambda: V.scalar_tensor_tensor(out=a1[:], in0=a2[:], scalar=W12[:, 2 * ti + 1:2 * ti + 2], in1=a1[:], op0=ALU.mult, op1=ALU.add),
                       [a2, W12, a1], [a1])
                    dd(lambda: V.tensor_tensor(out=a1[:], in0=a1[:], in1=G2B[cls][:], op=ALU.mult), [a1, G2B[cls]], [a1])
                    dd(lambda: V.tensor_tensor(out=x_[:], in0=x_[:], in1=a1[:], op=ALU.add), [x_, a1], [x_])
                    if l == 0:
                        fw.q_sync.dma(self.xres[t * 128:(t + 1) * 128, :], x_[:], reads=[x_.b], writes=[self.xres_b[t]])
                    else:
                        fw.q_sync.dma(self.y_out[(t - 2) * 128:(t - 1) * 128, :], x_[:], reads=[x_.b], writes=[self.y_b[t]])
                    if self.debug and t in (0, 2, NT - 1):
                        nm = f"x2_{l}_{t}"
                        ap = self.dbg(nm, [128, D])
                        fw.q_sync.dma(ap, x_[:], reads=[x_.b], writes=[self.dbg_out[nm][1]])

                for ti0 in range(0, ntl, 3):
                    pair = list(range(ti0, min(ti0 + 3, ntl)))
                    ILV.run([(lambda ti=ti: comb_fn(ti, tiles[ti])) for ti in pair])
                fw.barrier()

    def route(self, pr, rb, r_, Wt, ti, OH=None, W12=None):
        nc, fw = self.nc, self.fw
        V = nc.vector
        d = lambda fn, rd, wr_: fw.dve.op(fn, [x.b for x in rd], [x.b for x in wr_])
        lg, gmax, ngmax, goh, ge, gsum, pen, em, m8 = (r_[k] for k in ("lg", "gmax", "ngmax", "goh", "ge", "gsum", "pen", "em", "m8"))
        dd, ed, p1, p2, t1, t2 = (r_[k] for k in ("d", "ed", "p1", "p2", "t1", "t2"))
        d(lambda: V.tensor_tensor(out=lg[:], in0=pr[:, 0:36], in1=rb[:], op=ALU.add), [pr, rb], [lg])
        d(lambda: V.tensor_reduce(out=gmax[:], in_=lg[:, 0:4], axis=AX.X, op=ALU.max), [lg], [gmax])
        d(lambda: V.tensor_scalar(out=ngmax[:], in0=gmax[:], scalar1=-1.0, scalar2=None, op0=ALU.mult), [gmax], [ngmax])
        d(lambda: V.tensor_scalar(out=pen[:], in0=lg[:, 0:4], scalar1=gmax[:, 0:1], scalar2=None, op0=ALU.is_ge), [lg, gmax], [pen])
        d(lambda: V.tensor_scalar(out=pen[:], in0=pen[:], scalar1=1e30, scalar2=-1e30, op0=ALU.mult, op1=ALU.add), [pen], [pen])
        fw.act.op(lambda: nc.scalar.activation(out=ge[:], in_=lg[:, 0:4], func=AF.Exp, bias=ngmax[:, 0:1], accum_out=gsum[:, 0:1]),
                  [lg.b, ngmax.b], [ge.b, gsum.b])
        d(lambda: V.tensor_tensor(out=em[:].rearrange("p (a b) -> p a b", a=4), in0=lg[:, 4:36].rearrange("p (a b) -> p a b", a=4),
                                  in1=pen[:].unsqueeze(2).to_broadcast([128, 4, 8]), op=ALU.add), [lg, pen], [em])
        d(lambda: V.max(out=m8[:], in_=em[:]), [em], [m8])
        d(lambda: V.tensor_tensor(out=dd[:], in0=m8[:, 1:2], in1=m8[:, 0:1], op=ALU.subtract), [m8], [dd])
        fw.act.op(lambda: nc.scalar.activation(out=ed[:], in_=dd[:], func=AF.Exp), [dd.b], [ed.b])
        d(lambda: V.tensor_scalar(out=p1[:], in0=ed[:], scalar1=1.0, scalar2=None, op0=ALU.add), [ed], [p1])
        d(lambda: V.tensor_tensor(out=p1[:], in0=p1[:], in1=gsum[:], op=ALU.mult), [p1, gsum], [p1])
        d(lambda: V.reciprocal(out=p1[:], in_=p1[:]), [p1], [p1])
        d(lambda: V.tensor_tensor(out=p2[:], in0=p1[:], in1=ed[:], op=ALU.mult), [p1, ed], [p2])
        if OH is not None:
            for k in range(2):
                a = 2 * ti + k
                d(lambda: V.tensor_scalar(out=OH[:, a, :], in0=em[:], scalar1=m8[:, k:k + 1], scalar2=None, op0=ALU.is_equal), [em, m8], [OH])
                pk_ = p1 if k == 0 else p2
                d(lambda: V.tensor_copy(out=W12[:, a:a + 1], in_=pk_[:]), [pk_], [W12])
            return
        d(lambda: V.tensor_scalar(out=t1[:], in0=em[:], scalar1=m8[:, 0:1], scalar2=p1[:, 0:1], op0=ALU.is_equal, op1=ALU.mult), [em, m8, p1], [t1])
        d(lambda: V.tensor_scalar(out=t2[:], in0=em[:], scalar1=m8[:, 1:2], scalar2=p2[:, 0:1], op0=ALU.is_equal, op1=ALU.mult), [em, m8, p2], [t2])
        d(lambda: V.tensor_tensor(out=Wt[:, ti, :], in0=t1[:], in1=t2[:], op=ALU.add), [t1, t2], [Wt])

    def layer1_mixer(self):
        nc, fw, g = self.nc, self.fw, self.g
        l = 1
        PADU = 16
        with ExitStack() as ph:
            oT = self.tile(ph, "oT1", [128, 4, T], BF16)
            uT = self.tile(ph, "uT", [128, 4, S + 2 * PADU], BF16)
            pattn = ExitStack()
            qT = self.tile(pattn, "qT1", [128, NT, 4, 128], BF16)
            kT = self.tile(pattn, "kT1", [128, 2, T], BF16)
            Vp = self.tile(pattn, "Vp1", [128, NT, 200], BF16)
            self.v_init(Vp)
            fw.pool.op(lambda: nc.gpsimd.memset(kT[:], 0.0), [], [kT.b])
            fw.pool.op(lambda: nc.gpsimd.memset(uT[:], 0.0), [], [uT.b])
            with ExitStack() as p1:
                win = self.tile(p1, "win1", [128, 8, 1280], BF16)
                gqk = self.tile(p1, "gqk1", [128, 640], F32)
                g["cos"] = self.tile(p1, "cos1", [128, 32, 32], F32)
                g["sin"] = self.tile(p1, "sin1", [128, 32, 32], F32)
                fw.q_sync.dma(gqk[:], self.gqk_c, writes=[gqk.b])
                fw.q_sync.dma(g["cos"][:], self.cosT, writes=[g["cos"].b])
                fw.q_sync.dma(g["sin"][:], self.sinT, writes=[g["sin"].b])
                fw.dve.op(lambda: nc.vector.tensor_scalar(out=gqk[:, 0:512], in0=gqk[:, 0:512], scalar1=0.125, scalar2=None, op0=ALU.mult),
                          [gqk.b], [gqk.b])
                with ExitStack() as pw:
                    stg = [self.tile(pw, f"wstg1{i}", [128, 4096], F32) for i in range(2)]
                    self.stg_i = 0
                    for n in range(3):
                        w_ = 512 if n < 2 else 256
                        self.load_cast_w(stg, win, slice(n * 512, n * 512 + w_), self.w_in_cd[:, n * 512:n * 512 + w_], 8, w_, n)
                    fw.barrier()
                ssl_ = [self.tile(p1, f"ss1{i}", [128, 1], F32) for i in range(2)]
                xnl_ = [self.tile(p1, f"xn1{i}", [128, D], BF16) for i in range(2)]
                scr_l = [dict(ssl=[ssl_[i]], xnl=[xnl_[i]], sq=self.tile(p1, f"sq1{i}", [128, 640], F32),
                              ssq=self.tile(p1, f"ssq1{i}", [128, 10], F32), qn=self.tile(p1, f"qn1{i}", [128, 640], F32),
                              qr=self.tile(p1, f"qr1{i}", [128, 640], BF16), rt=self.tile(p1, f"rt1{i}", [128, 2, 320], F32)) for i in range(2)]
                xt = [self.tile(p1, f"xt1{i}", [128, D], F32) for i in range(2)]
                hT = self.tile(p1, "hT1", [128, 8, 512], BF16)
                qkv = [self.tile(p1, f"qkv1{i}", [128, 768], F32) for i in range(2)]
                chunks = [(0, 2)] + [(2 + 4 * i, 4) for i in range(8)]
                for ci, (t0, ntl) in enumerate(chunks):
                    h = hT
                    ntok = ntl * 128
                    is_ctx = t0 < 2
                    cls = 1 if is_ctx else 0
                    def norm_fn(tl):
                        t = t0 + tl
                        x_ = xt[tl % 2]
                        fw.q_sync.dma(x_[:], self.xres[t * 128:(t + 1) * 128, :], reads=[self.xres_b[t]], writes=[x_.b])
                        self.norm_tile_to_hT(x_, h, tl * 128, l, 1, cls, scr_l[tl % 2])

                    def qkv_fn(tl):
                        t = t0 + tl
                        qk_ = qkv[tl % 2]
                        for (c0, w_) in ((0, 512), (512, 256)):
                            if is_ctx and c0 == 0:
                                continue
                            p = self.ps_get()
                            for k in range(8):
                                fw.pe.op(lambda: nc.tensor.matmul(p[:, 0:w_], lhsT=h[:, k, tl * 128:(tl + 1) * 128], rhs=win[:, k, c0:c0 + w_],
                                                                  start=(k == 0), stop=(k == 7)), [h.b, win.b], [p.b], inc=(k == 7))
                            if c0 == 0:
                                fw.act.op(lambda: nc.scalar.copy(out=qk_[:, 0:512].rearrange("p (j a d) -> p a j d", a=2, d=64),
                                                                 in_=p[:, 0:512].rearrange("p (a j d) -> p a j d", a=2, j=4)), [p.b], [qk_.b])
                            else:
                                fw.act.op(lambda: nc.scalar.copy(out=qk_[:, c0:c0 + w_], in_=p[:, 0:w_]), [p.b], [qk_.b])
                            self.ps_put(p)
                        self.qk_post(qk_, t, gqk, scr_l[tl % 2], qT, kT, not is_ctx, None if is_ctx else t - 2)
                        self.v_fill(qk_, t, Vp)

                    for tl0 in range(0, ntl, 2):
                        ILV.run([(lambda tl=tl: norm_fn(tl)) for tl in range(tl0, min(tl0 + 2, ntl))])
                    for tl0 in range(0, ntl, 2):
                        ILV.run([(lambda tl=tl: qkv_fn(tl)) for tl in range(tl0, min(tl0 + 2, ntl))])
                    if not is_ctx:
                        tok0 = (t0 - 2) * 128
                        for j in range(4):
                            pu = self.ps_get()
                            for k in range(8):
                                fw.pe.op(lambda: nc.tensor.matmul(pu[:, 0:ntok], lhsT=win[:, k, 768 + j * 128:768 + (j + 1) * 128], rhs=h[:, k, 0:ntok],
                                                                  start=(k == 0), stop=(k == 7)), [win.b, h.b], [pu.b], inc=(k == 7))
                            fw.dve.op(lambda: nc.vector.tensor_copy(out=uT[:, j, PADU + tok0:PADU + tok0 + ntok], in_=pu[:, 0:ntok]), [pu.b], [uT.b])
                            self.ps_put(pu)
                fw.barrier()
            if self.stop == "l1p1":
                pattn.close()
                return
            with ExitStack() as p2:
                wm = self.tile(p2, "wm", [128, 2, 512], BF16)
                sk = self.tile(p2, "sk", [128, 2, 512], F32)
                with ExitStack() as pw:
                    wmf = self.tile(pw, "wmf", [128, 2, 512], F32)
                    fw.q_sync.dma(wmf[:], self.wmask, writes=[wmf.b])
                    fw.dve.op(lambda: nc.vector.tensor_copy(out=wm[:], in_=wmf[:]), [wmf.b], [wm.b])
                    fw.q_sync.dma(sk[:], self.sink_rep, writes=[sk.b])
                    fw.act.op(lambda: nc.scalar.activation(out=sk[:], in_=sk[:], func=AF.Exp), [sk.b], [sk.b])
                    fw.barrier()
                scr = dict(pexp=[self.tile(p2, f"pexp1{i}", [128, 1024], BF16) for i in range(4)], pi=0,
                           rec=[self.tile(p2, f"rec1{i}", [128, 512], F32) for i in range(2)],
                           posb=[[self.tile(p2, f"posb1{i}{k}", [128, 512], F32) for k in range(2)] for i in range(2)])
                blocks = []
                for t in range(2, NT):
                    kts = [(0, None), (1, None)]
                    if t > 2:
                        kts.append((t - 1, 0))
                    kts.append((t, None))
                    if t < NT - 1:
                        kts.append((t + 1, 1))
                    for kv in range(2):
                        blocks.append((kv, t, kts))
                self.attention(blocks, qT, kT, Vp, oT, scr, masks=wm, sinkrow=sk)
                fw.barrier()
            pattn.close()
            if self.debug:
                self.dbg_dump_bf(ph, "oT1", oT[:, :, 256:384], [128, 4, 128], oT.b)
                self.dbg_dump_bf(ph, "oT1b", oT[:, :, 640:768], [128, 4, 128], oT.b)
            dT = self.tile(ph, "dT", [128, 4, T], BF16)
            with ExitStack() as p3:
                pwf = self.tile(p3, "pwf", [128, 4, 128], F32)
                pwb = self.tile(p3, "pwb", [128, 4, 128], BF16)
                psc = self.tile(p3, "psc", [128, 4], F32)
                pfx = self.tile(p3, "pfx", [128, 4, 32], F32)
                fw.q_sync.dma(pwf[:], self.pool_w.rearrange("g c d -> c g d"), writes=[pwf.b])
                fw.q_sync.dma(psc[:], self.pool_scT, writes=[psc.b])
                fw.q_sync.dma(pfx[:], self.poolfix, writes=[pfx.b])
                fw.dve.op(lambda: nc.vector.tensor_copy(out=pwb[:], in_=pwf[:]), [pwf.b], [pwb.b])
                ta = [self.tile(p3, f"pta{i}", [128, 528], F32) for i in range(2)]
                pp_ = [self.tile(p3, f"ppb{i}", [128, 512], BF16) for i in range(2)]
                cnt = 0
                for ci in range(8):
                    tok0 = ci * 512
                    for j, w in enumerate((2, 4, 8, 16)):
                        base = PADU + tok0 - w // 2
                        ln = 512 + w - 1
                        cur = uT[:, j, base:base + ln]
                        curb = uT.b
                        step = 1
                        k = 0
                        while step < w:
                            dst = ta[k % 2]
                            e_, ee = (fw.dve, nc.vector) if (cnt % 2 == 0) else (fw.pool, nc.gpsimd)
                            cnt += 1
                            cc, cb_ = cur, curb
                            e_.op(lambda: ee.tensor_tensor(out=dst[:, 0:ln - step], in0=cc[:, 0:ln - step], in1=cc[:, step:ln], op=ALU.add), [cb_], [dst.b])
                            ln -= step
                            step *= 2
                            cur, curb = dst[:, 0:ln], dst.b
                            k += 1
                        pb_ = pp_[j % 2]
                        uc = uT[:, j, PADU + tok0:PADU + tok0 + 512]
                        sdst = ta[k % 2]
                        fw.dve.op(lambda: nc.vector.scalar_tensor_tensor(out=pb_[:], in0=cur[:, 0:512], scalar=1.0 / w, in1=uc, op0=ALU.mult, op1=ALU.subtract),
                                  [curb, uT.b], [pb_.b])
                        for (cond, lo, fo) in ((ci == 0, 0, 0), (ci == 7, 496, 16)):
                            if cond:
                                fw.dve.op(lambda: nc.vector.tensor_tensor(out=sdst[:, 0:16], in0=cur[:, lo:lo + 16], in1=pfx[:, j, fo:fo + 16], op=ALU.mult),
                                          [curb, pfx.b], [sdst.b])
                                fw.dve.op(lambda: nc.vector.tensor_tensor(out=pb_[:, lo:lo + 16], in0=sdst[:, 0:16], in1=uc[:, lo:lo + 16], op=ALU.subtract),
                                          [sdst.b, uT.b], [pb_.b])
                        py = self.ps_get()
                        fw.pe.op(lambda: nc.tensor.matmul(py[:], lhsT=pwb[:, j, :], rhs=pb_[:], start=True, stop=True), [pwb.b, pb_.b], [py.b])
                        fw.act.op(lambda: nc.scalar.activation(out=dT[:, j, NCTX + tok0:NCTX + tok0 + 512], in_=py[:], func=AF.Copy, scale=psc[:, j:j + 1]),
                                  [py.b, psc.b], [dT.b])
                        self.ps_put(py)
                fw.barrier()
            if self.debug:
                self.dbg_dump_bf(ph, "dT", dT[:, :, 256:384], [128, 4, 128], dT.b)
                self.dbg_dump_bf(ph, "dTe", dT[:, :, T - 128:T], [128, 4, 128], dT.b)
            if self.stop == "l1p3":
                return
            with ExitStack() as p4:
                self.out_proj(1, lambda c: (oT, c) if c < 4 else (dT, c - 4), self.w_out_cd, list(range(2, NT)), p4)
                fw.barrier()


def _rope_tables():
    rows = S // 64
    row = np.repeat(np.arange(rows, dtype=np.float32), 64)
    col = np.tile(np.arange(64, dtype=np.float32), rows)
    inv = (10000.0 ** (-np.arange(16, dtype=np.float32) / 16)).astype(np.float32)
    ang = np.concatenate([row[:, None] * inv, col[:, None] * inv], axis=-1).astype(np.float32)
    return np.cos(ang).astype(np.float32), np.sin(ang).astype(np.float32)


def _fm(v, chunks):
    return np.ascontiguousarray(np.asarray(v, np.float32).reshape(chunks, 128).T)


def make_in_maps(inp, cores):
    f = lambda a: np.ascontiguousarray(np.asarray(a, dtype=np.float32))
    cos, sin = _rope_tables()
    cosT = np.ascontiguousarray(cos.reshape(32, 128, 32).transpose(1, 0, 2))
    sinT = np.ascontiguousarray(sin.reshape(32, 128, 32).transpose(1, 0, 2))
    r = np.arange(128)
    mprev = (r[None, :] <= r[:, None]).astype(np.float32)
    mnext = (r[:, None] <= r[None, :]).astype(np.float32)
    wmask = np.stack([np.tile(mprev, (1, 4)), np.tile(mnext, (1, 4))], axis=1)
    shared = {
        "mod_w": f(inp["mod_w"]),
        "mod_bT": np.ascontiguousarray(f(inp["mod_b"]).reshape(2, 48, 128).transpose(2, 0, 1)),
        "ln1gT": np.ascontiguousarray(f(inp["ln1_g"]).reshape(2, 8, 128).transpose(2, 0, 1)),
        "ln2gT": np.ascontiguousarray(f(inp["ln2_g"]).reshape(2, 8, 128).transpose(2, 0, 1)),
        "w_in_ab": f(inp["w_in_ab"][0]), "w_out_ab": f(inp["w_out_ab"][0]),
        "gqk_a": np.ascontiguousarray(np.broadcast_to(np.concatenate([np.tile(f(inp["q_norm_a"][0]), 8), np.tile(f(inp["k_norm_a"][0]), 2)])[None, :], (128, 640))),
        "convwT": np.ascontiguousarray(f(inp["conv_w"][0]).reshape(31, 4, 128).transpose(2, 1, 0)),
        "convv": np.ascontiguousarray(np.stack([_fm(inp["conv_b"][0], 4), _fm(inp["conv_ln_g"][0], 4), _fm(inp["conv_ln_b"][0], 4)], axis=1)),
        "w_in_cd": f(inp["w_in_cd"][0]), "w_out_cd": f(inp["w_out_cd"][0]),
        "gqk_c": np.ascontiguousarray(np.broadcast_to(np.concatenate([np.tile(f(inp["q_norm_c"][0]), 8), np.tile(f(inp["k_norm_c"][0]), 2)])[None, :], (128, 640))),
        "sink_rep": np.ascontiguousarray(np.broadcast_to(np.repeat(f(inp["sink_c"][0]).reshape(2, 4), 128, axis=1)[None], (128, 2, 512))),
        "pool_w": f(inp["pool_w"][0]),
        "pool_scT": _fm(inp["pool_scale"][0], 4),
        "poolfix": _poolfix(),
        "rt_w": np.ascontiguousarray(np.concatenate([f(inp["rt_grp_w"]), f(inp["rt_exp_w"])], axis=2)),
        "rt_b": np.ascontiguousarray(np.broadcast_to(np.concatenate([f(inp["rt_grp_b"]), f(inp["rt_exp_b"])], axis=1)[None], (128, 2, 36))),
        "ex_gate": np.ascontiguousarray(f(inp["ex_gate"]).reshape(2, 32, 8, 128, 512).transpose(0, 1, 3, 2, 4).reshape(2, 4096, 4096)),
        "ex_up": np.ascontiguousarray(f(inp["ex_up"]).reshape(2, 32, 8, 128, 512).transpose(0, 1, 3, 2, 4).reshape(2, 4096, 4096)),
        "ex_down": np.ascontiguousarray(f(inp["ex_down"]).reshape(2, 32, 4, 128, 1024).transpose(0, 1, 3, 2, 4).reshape(2, 4096, 4096)),
        "pidx": np.arange(128, dtype=np.float32).reshape(128, 1),
        "mconst": np.ascontiguousarray(np.broadcast_to(np.concatenate([
            np.arange(32), 128.0 * np.arange(35), np.arange(100), np.arange(32) % 2, np.arange(100) % 2,
            128.0 * np.arange(1, 69), 2.0 * np.arange(1, 51), np.arange(16)]).astype(np.float32)[None], (128, 433))),
        "umat": np.ascontiguousarray((r[:, None] < r[None, :]).astype(np.float32)),
        "ident": np.eye(128, dtype=np.float32), "cosT": cosT, "sinT": sinT, "wmask": np.ascontiguousarray(wmask),
    }
    maps = []
    for b in cores:
        m = dict(shared)
        m["x"] = f(inp["x"][b])
        m["ctx"] = f(inp["ctx"][b])
        c2 = np.stack([f(inp["c"][b]), f(inp["c_ctx"])], axis=1)
        m["c2T"] = np.ascontiguousarray(c2.reshape(8, 128, 2).transpose(1, 0, 2))
        maps.append(m)
    return maps


def _poolfix():
    out = np.ones((4, 32), np.float32)
    for gi, w in enumerate((2, 4, 8, 16)):
        for i, t in enumerate(list(range(16)) + list(range(S - 16, S))):
            lo = min(max(t - w // 2, 0), S)
            hi = min(max(t - w // 2 + w, 0), S)
            out[gi, i] = 1.0 / float(hi - lo)
    return np.ascontiguousarray(np.broadcast_to(out[None], (128, 4, 32)))


_NC_CACHE = {}


def kernel(**inputs):
    if "nc" not in _NC_CACHE:
        _NC_CACHE["nc"] = Builder(debug=False).build()
    nc = _NC_CACHE["nc"]
    maps = make_in_maps(inputs, list(range(8)))
    res = run_bass_kernel_spmd(nc, maps, core_ids=list(range(8)))
    return np.stack([np.asarray(r["y"], dtype=np.float32) for r in res.results], axis=0)
```

```python
import os
import numpy as np
from contextlib import ExitStack
from collections import deque
import concourse.bass as bass
import concourse.mybir as mybir
from concourse.bass_utils import run_bass_kernel_spmd

F32 = mybir.dt.float32
BF16 = mybir.dt.bfloat16
I32 = mybir.dt.int32
ALU = mybir.AluOpType
AF = mybir.ActivationFunctionType
AX = mybir.AxisListType

D = 1024
S = 4096
NCTX = 256
T = S + NCTX
NT = T // 128
EPS = 1e-6
GLU_OFF_CTX = 15
GLU_OFF_LAT = 15 + NCTX + 15
GLU_LEN = GLU_OFF_LAT + S + 15


class Buf:
    __slots__ = ("name", "w", "r")

    def __init__(self, name=""):
        self.name = name
        self.w = None
        self.r = {}


class Tile:
    __slots__ = ("t", "b")

    def __init__(self, t, b):
        self.t, self.b = t, b

    def __getitem__(self, k):
        return self.t[k]


import threading


class Interleaver:
    def __init__(self):
        self.active = False

    def run(self, fns):
        if len(fns) == 1:
            fns[0]()
            return
        n = len(fns)
        self.ev = [threading.Event() for _ in range(n)]
        self.alive = [True] * n
        self.err = []
        self.tid = {}
        done = threading.Event()

        def worker(i):
            self.ev[i].wait()
            self.ev[i].clear()
            try:
                fns[i]()
            except BaseException as e:
                self.err.append(e)
            self.alive[i] = False
            nxt = self._next(i)
            if nxt is None:
                done.set()
            else:
                self.ev[nxt].set()

        ths = [threading.Thread(target=worker, args=(i,)) for i in range(n)]
        self.active = True
        for i, th in enumerate(ths):
            th.start()
            self.tid[th.ident] = i
        self.ev[0].set()
        done.wait()
        for th in ths:
            th.join()
        self.active = False
        if self.err:
            raise self.err[0]

    def _next(self, i):
        n = len(self.alive)
        for d in range(1, n + 1):
            j = (i + d) % n
            if self.alive[j] and j != i:
                return j
        return None

    def yield_point(self):
        if not self.active:
            return
        i = self.tid.get(threading.get_ident())
        if i is None:
            return
        nxt = self._next(i)
        if nxt is None:
            return
        self.ev[nxt].set()
        self.ev[i].wait()
        self.ev[i].clear()


ILV = Interleaver()


class Eng:
    def __init__(self, key, e, sem):
        self.key, self.e, self.sem = key, e, sem
        self.n = 0
        self.known = {}

    def _wait(self, sem, val):
        if self.known.get(sem, 0) >= val:
            return
        self.known[sem] = val
        self.e.wait_ge(sem, val)

    def op(self, ins_fn, reads=(), writes=(), inc=True):
        for b in reads:
            if b.w is not None:
                self._wait(b.w[0], b.w[1])
        strict = (self.key == "pool")
        for b in writes:
            if b.w is not None and (strict or b.w[2] != self.key):
                self._wait(b.w[0], b.w[1])
            for sem, (val, k) in b.r.items():
                if strict or k != self.key:
                    self._wait(sem, val)
        ins = ins_fn()
        if inc:
            self.n += 1
            ins.then_inc(self.sem, 1)
            tok = (self.sem, self.n, self.key)
        else:
            tok = (self.sem, self.n + 1, self.key)
        for b in reads:
            b.r[tok[0]] = (tok[1], tok[2])
        for b in writes:
            b.w = tok
            b.r = {}
        if inc:
            ILV.yield_point()
        return ins


class DmaQ:
    def __init__(self, key, e, sems):
        self.key, self.e, self.sems = key, e, sems
        self.cnt = [0] * len(sems)
        self.i = 0
        self.known = {}

    def _wait(self, sem, val):
        if self.known.get(sem, 0) >= val:
            return
        self.known[sem] = val
        self.e.wait_ge(sem, val)

    def dma(self, out, in_, reads=(), writes=(), **kw):
        for b in reads:
            if b.w is not None:
                self._wait(b.w[0], b.w[1])
        for b in writes:
            if b.w is not None:
                self._wait(b.w[0], b.w[1])
            for sem, (val, k) in b.r.items():
                self._wait(sem, val)
        s = self.i % len(self.sems)
        self.i += 1
        sem = self.sems[s]
        if self.cnt[s] > 0:
            self._wait(sem, 16 * self.cnt[s])
        self.cnt[s] += 1
        ins = self.e.dma_start(out=out, in_=in_, **kw)
        ins.then_inc(sem, 16)
        tok = (sem, 16 * self.cnt[s], self.key + str(s))
        for b in reads:
            b.r[tok[0]] = (tok[1], tok[2])
        for b in writes:
            b.w = tok
            b.r = {}
        return ins


def _dma_generic(self, fn, reads=(), writes=()):
    for b in reads:
        if b.w is not None:
            self._wait(b.w[0], b.w[1])
    for b in writes:
        if b.w is not None:
            self._wait(b.w[0], b.w[1])
        for sem, (val, k) in b.r.items():
            self._wait(sem, val)
    s = self.i % len(self.sems)
    self.i += 1
    sem = self.sems[s]
    if self.cnt[s] > 0:
        self._wait(sem, 16 * self.cnt[s])
    self.cnt[s] += 1
    ins = fn()
    ins.then_inc(sem, 16)
    tok = (sem, 16 * self.cnt[s], self.key + str(s))
    for b in reads:
        b.r[tok[0]] = (tok[1], tok[2])
    for b in writes:
        b.w = tok
        b.r = {}
    return ins


DmaQ.dma_fn = _dma_generic


class FW:
    def __init__(self, nc, stack, n_dma_sems=10):
        self.nc = nc
        mk = lambda nm: stack.enter_context(nc.semaphore(nm))
        self.pe = Eng("pe", nc.tensor, mk("s_pe"))
        self.act = Eng("act", nc.scalar, mk("s_act"))
        self.dve = Eng("dve", nc.vector, mk("s_dve"))
        self.pool = Eng("pool", nc.gpsimd, mk("s_pool"))
        self.q_sync = DmaQ("qs", nc.sync, [mk(f"s_qs{i}") for i in range(n_dma_sems)])
        self.q_pool = DmaQ("qp", nc.gpsimd, [mk(f"s_qp{i}") for i in range(n_dma_sems)])
        self.q_pool.known = self.pool.known
        self.engs = [self.pe, self.act, self.dve, self.pool]
        self.qs = [self.q_sync, self.q_pool]

    def barrier(self):
        toks = []
        for e in self.engs:
            if e.n > 0:
                toks.append((e.sem, e.n))
        for q in self.qs:
            for s, c in zip(q.sems, q.cnt):
                if c > 0:
                    toks.append((s, 16 * c))
        for e in self.engs + [self.q_sync]:
            for sem, val in toks:
                e._wait(sem, val)


class Builder:
    def __init__(self, debug=False, stop=None):
        self.debug = debug
        self.stop = stop
        self.nc = bass.Bass("TRN2", target_bir_lowering=False)
        self.dbg_out = {}

    def dram_in(self, name, shape, dt=F32):
        return self.nc.dram_tensor(name, list(shape), dt, kind="ExternalInput").ap()

    def tile(self, st, name, shape, dt):
        self._tn = getattr(self, "_tn", 0) + 1
        name = f"{name}_{self._tn}"
        t = st.enter_context(self.nc.sbuf_tensor(name, list(shape), dt))
        return Tile(t, Buf(name))

    def ps_get(self):
        return self.psq.popleft()

    def ps_put(self, p):
        self.psq.append(p)

    def dbg(self, name, shape):
        if not self.debug:
            return None
        ap = self.nc.dram_tensor("dbg_" + name, list(shape), F32, kind="ExternalOutput").ap()
        self.dbg_out[name] = (ap, Buf("dbg_" + name))
        return ap

    def cut(self, n):
        return int(os.environ.get("P1_CUT", "-1")) == n

    def tok_in(self, t):
        if t < 2:
            return self.ctx_in[t * 128:(t + 1) * 128, :]
        return self.x_in[(t - 2) * 128:(t - 1) * 128, :]

    def build(self):
        nc = self.nc
        di = self.dram_in
        self.x_in = di("x", [S, D])
        self.ctx_in = di("ctx", [NCTX, D])
        self.c2T = di("c2T", [128, 8, 2])
        self.mod_w = di("mod_w", [2, D, 6 * D])
        self.mod_bT = di("mod_bT", [128, 2, 48])
        self.ln1gT = di("ln1gT", [128, 2, 8])
        self.ln2gT = di("ln2gT", [128, 2, 8])
        self.w_in_ab = di("w_in_ab", [D, 1792])
        self.w_out_ab = di("w_out_ab", [D, D])
        self.gqk_a = di("gqk_a", [128, 640])
        self.convwT = di("convwT", [128, 4, 31])
        self.convv = di("convv", [128, 3, 4])
        self.w_in_cd = di("w_in_cd", [D, 1280])
        self.w_out_cd = di("w_out_cd", [D, D])
        self.gqk_c = di("gqk_c", [128, 640])
        self.sink_rep = di("sink_rep", [128, 2, 512])
        self.pool_w = di("pool_w", [4, 128, 128])
        self.pool_scT = di("pool_scT", [128, 4])
        self.poolfix = di("poolfix", [128, 4, 32])
        self.rt_w = di("rt_w", [2, D, 36])
        self.rt_b = di("rt_b", [128, 2, 36])
        self.ex_gate = di("ex_gate", [2, 32 * 128, 4096])
        self.ex_up = di("ex_up", [2, 32 * 128, 4096])
        self.ex_down = di("ex_down", [2, 32 * 128, 4096])
        self.pidx = di("pidx", [128, 1])
        self.ident_in = di("ident", [128, 128])
        self.cosT = di("cosT", [128, 32, 32])
        self.sinT = di("sinT", [128, 32, 32])
        self.wmask = di("wmask", [128, 2, 512])
        self.mconst = di("mconst", [128, 433])
        self.umat = di("umat", [128, 128])
        self.y_out = nc.dram_tensor("y", [S, D], F32, kind="ExternalOutput").ap()
        self.xres = nc.dram_tensor("xres", [T, D], F32, kind="Internal").ap()
        self.xres_b = [Buf(f"xres{t}") for t in range(NT)]
        self.y_b = [Buf(f"y{t}") for t in range(NT)]

        with ExitStack() as st:
            self.fw = FW(nc, st)
            fw = self.fw
            self.PS = []
            self.PSB = []
            for i in range(4):
                pb_ = st.enter_context(nc.psum_tensor(f"psb{i}", [128, 1024], F32))
                h0 = Tile(pb_[:, 0:512], Buf(f"ps{2 * i}"))
                h1 = Tile(pb_[:, 512:1024], Buf(f"ps{2 * i + 1}"))
                self.PS += [h0, h1]
                self.PSB.append((pb_, h0, h1))
            self.psq = deque(self.PS)
            g = self.g = {}
            g["identf"] = self.tile(st, "identf", [128, 128], F32)
            g["identb"] = self.tile(st, "identb", [128, 128], BF16)
            g["onesf"] = self.tile(st, "onesf", [128, 128], F32)
            g["modT"] = self.tile(st, "modT", [128, 2, 48, 2], F32)
            g["A1"] = self.tile(st, "A1", [128, 2, 8, 2], F32)
            g["A2"] = self.tile(st, "A2", [128, 2, 8, 2], F32)
            g["epsc"] = self.tile(st, "epsc", [128, 1], F32)
            fw.q_sync.dma(g["identf"][:], self.ident_in, writes=[g["identf"].b])
            fw.dve.op(lambda: nc.vector.tensor_copy(out=g["identb"][:], in_=g["identf"][:]), [g["identf"].b], [g["identb"].b])
            fw.dve.op(lambda: nc.vector.memset(g["onesf"][:], 1.0), [], [g["onesf"].b])
            fw.dve.op(lambda: nc.vector.memset(g["epsc"][:], EPS), [], [g["epsc"].b])

            self.bc_reg = nc.gpsimd.alloc_register("bcreg")
            nc.gpsimd.reg_mov(self.bc_reg, 8191)
            self.phase_mod()
            if self.stop != "p0":
                self.layer0_mixer()
            if self.stop is None or self.stop in ("m0", "l1", "m1"):
                (self.moe2 if os.environ.get("MOE_DENSE") != "1" else self.moe)(0)
            if self.stop is None or self.stop in ("l1", "m1"):
                self.layer1_mixer()
            if self.stop is None or self.stop in ("m1",):
                (self.moe2 if os.environ.get("MOE_DENSE") != "1" else self.moe)(1)

            for b in self.y_b[2:]:
                if b.w is not None:
                    fw.q_sync._wait(b.w[0], b.w[1])
            for name, (ap, b) in self.dbg_out.items():
                if b.w is not None:
                    fw.q_sync._wait(b.w[0], b.w[1])
            fw.barrier()
        return nc

    def rstd_from_ss(self, ss, n_inv, cols):
        nc, fw = self.nc, self.fw
        fw.dve.op(lambda: nc.vector.tensor_scalar(out=ss[:, 0:cols], in0=ss[:, 0:cols], scalar1=n_inv, scalar2=EPS,
                                                  op0=ALU.mult, op1=ALU.add), [ss.b], [ss.b])
        fw.act.op(lambda: nc.scalar.sqrt(out=ss[:, 0:cols], in_=ss[:, 0:cols]), [ss.b], [ss.b])
        fw.dve.op(lambda: nc.vector.reciprocal(out=ss[:, 0:cols], in_=ss[:, 0:cols]), [ss.b], [ss.b])

    def load_cast_w(self, st_pool, dst, dst_cols, src_ap, kc, ncols, eng_i):
        nc, fw = self.nc, self.fw
        stg = st_pool[self.stg_i % len(st_pool)]
        self.stg_i += 1
        fw.q_sync.dma(stg[:, 0:kc * ncols].rearrange("p (k n) -> p k n", k=kc),
                      src_ap.rearrange("(k p) n -> p k n", p=128), writes=[stg.b])
        src = stg[:, 0:kc * ncols].rearrange("p (k n) -> p k n", k=kc)
        if eng_i % 2 == 0:
            fw.act.op(lambda: nc.scalar.copy(out=dst[:, 0:kc, dst_cols], in_=src), [stg.b], [dst.b])
        else:
            fw.dve.op(lambda: nc.vector.tensor_copy(out=dst[:, 0:kc, dst_cols], in_=src), [stg.b], [dst.b])

    def phase_mod(self):
        nc, fw, g = self.nc, self.fw, self.g
        with ExitStack() as ph:
            c2 = self.tile(ph, "c2", [128, 8, 2], F32)
            sil = self.tile(ph, "sil", [128, 8, 2], BF16)
            mb = self.tile(ph, "mb", [128, 2, 48], F32)
            l1g = self.tile(ph, "l1g", [128, 2, 8], F32)
            l2g = self.tile(ph, "l2g", [128, 2, 8], F32)
            stg = [self.tile(ph, f"mstg{i}", [128, 4096], F32) for i in range(2)]
            wb = [self.tile(ph, f"mwb{i}", [128, 8, 512], BF16) for i in range(2)]
            self.stg_i = 0
            fw.q_sync.dma(c2[:], self.c2T, writes=[c2.b])
            fw.q_sync.dma(mb[:], self.mod_bT, writes=[mb.b])
            fw.q_sync.dma(l1g[:], self.ln1gT, writes=[l1g.b])
            fw.q_sync.dma(l2g[:], self.ln2gT, writes=[l2g.b])
            fw.act.op(lambda: nc.scalar.activation(out=sil[:], in_=c2[:], func=AF.Silu), [c2.b], [sil.b])
            for l in range(2):
                pm = self.ps_get()
                pmv = pm[:, 0:96].rearrange("p (c j) -> p c j", j=2)
                for n in range(12):
                    w = wb[n % 2]
                    self.load_cast_w(stg, w, slice(0, 512), self.mod_w[l, :, n * 512:(n + 1) * 512], 8, 512, n)
                    for q in range(4):
                        cc = n * 4 + q
                        for k in range(8):
                            fw.pe.op(lambda: nc.tensor.matmul(pmv[:, cc, :], lhsT=w[:, k, q * 128:(q + 1) * 128], rhs=sil[:, k, :],
                                                              start=(k == 0), stop=(k == 7)),
                                     [w.b, sil.b], [pm.b], inc=(k == 7 and q == 3))
                mT = g["modT"]
                fw.dve.op(lambda: nc.vector.tensor_tensor(out=mT[:, l, :, :], in0=pmv,
                                                          in1=mb[:, l, :].unsqueeze(2).to_broadcast([128, 48, 2]), op=ALU.add),
                          [pm.b, mb.b], [mT.b])
                self.ps_put(pm)
                fw.dve.op(lambda: nc.vector.scalar_tensor_tensor(out=g["A1"][:, l, :, :], in0=mT[:, l, 8:16, :], scalar=1.0,
                                                                 in1=l1g[:, l, :].unsqueeze(2).to_broadcast([128, 8, 2]),
                                                                 op0=ALU.add, op1=ALU.mult), [mT.b, l1g.b], [g["A1"].b])
                fw.dve.op(lambda: nc.vector.scalar_tensor_tensor(out=g["A2"][:, l, :, :], in0=mT[:, l, 32:40, :], scalar=1.0,
                                                                 in1=l2g[:, l, :].unsqueeze(2).to_broadcast([128, 8, 2]),
                                                                 op0=ALU.add, op1=ALU.mult), [mT.b, l2g.b], [g["A2"].b])
            if self.debug:
                ap = self.dbg("modT", [128, 2 * 48 * 2])
                fw.q_pool.dma(ap, g["modT"][:].rearrange("p l c j -> p (l c j)"), reads=[g["modT"].b], writes=[self.dbg_out["modT"][1]])
            fw.barrier()

    def bcast_tile(self, dst, col_ap_fn, tmp, extra=()):
        nc, fw, g = self.nc, self.fw, self.g
        for c in range(8):
            fw.dve.op(lambda: nc.vector.tensor_scalar(out=tmp[:], in0=g["onesf"][:], scalar1=col_ap_fn(c), scalar2=None, op0=ALU.mult),
                      [g["onesf"].b, g["modT"].b] + list(extra), [tmp.b])
            p = self.ps_get()
            fw.pe.op(lambda: nc.tensor.transpose(out=p[:, 0:128], in_=tmp[:], identity=g["identf"][:]), [tmp.b, g["identf"].b], [p.b])
            fw.act.op(lambda: nc.scalar.copy(out=dst[:, c * 128:(c + 1) * 128], in_=p[:, 0:128]), [p.b], [dst.b])
            self.ps_put(p)

    def norm_tile_to_hT(self, xt, hT, col0, l, which, cls, scr):
        nc, fw, g = self.nc, self.fw, self.g
        ni = scr.get("ni", 0)
        scr["ni"] = ni + 1
        ss, xn = scr["ssl"][ni % len(scr["ssl"])], scr["xnl"][ni % len(scr["xnl"])]
        fw.act.op(lambda: nc.scalar.activation(out=xn[:], in_=xt[:], func=AF.Square, accum_out=ss[:, 0:1]), [xt.b], [xn.b, ss.b])
        self.rstd_from_ss(ss, 1.0 / D, 1)
        fw.act.op(lambda: nc.scalar.activation(out=xn[:], in_=xt[:], func=AF.Copy, scale=ss[:, 0:1]), [xt.b, ss.b], [xn.b])
        p = self.ps_get()
        pv = p[:].bitcast(BF16).rearrange("p (c q) -> p c q", c=8)
        for c in range(8):
            fw.pe.op(lambda: nc.tensor.transpose(out=pv[:, c, :], in_=xn[:, c * 128:(c + 1) * 128], identity=g["identb"][:]),
                     [xn.b, g["identb"].b], [p.b], inc=(c == 7))
        A = g["A1"] if which == 1 else g["A2"]
        boff = 0 if which == 1 else 24
        for c in range(8):
            fw.dve.op(lambda: nc.vector.tensor_scalar(out=hT[:, c, col0:col0 + 128], in0=pv[:, c, :], scalar1=A[:, l, c, cls:cls + 1],
                                                      scalar2=g["modT"][:, l, boff + c, cls:cls + 1], op0=ALU.mult, op1=ALU.add),
                      [p.b, A.b, g["modT"].b], [hT.b])
        self.ps_put(p)

    def qk_post(self, qkv, t, gqk, scr, qT, kT, do_q, lat_idx):
        nc, fw, g = self.nc, self.fw, self.g
        sq, ssq, qn, qr, tmp = scr["sq"], scr["ssq"], scr["qn"], scr["qr"], scr["rt"]
        h0 = 0 if do_q else 8
        nh = 10 - h0
        c0 = h0 * 64
        q3 = lambda tl: tl[:, c0:640].rearrange("p (h d) -> p h d", d=64)
        fw.pool.op(lambda: nc.gpsimd.tensor_tensor(out=sq[:, c0:640], in0=qkv[:, c0:640], in1=qkv[:, c0:640], op=ALU.mult), [qkv.b], [sq.b])
        fw.dve.op(lambda: nc.vector.tensor_reduce(out=ssq[:, h0:10], in_=q3(sq), axis=AX.X, op=ALU.add), [sq.b], [ssq.b])
        fw.dve.op(lambda: nc.vector.tensor_scalar(out=ssq[:, h0:10], in0=ssq[:, h0:10], scalar1=1.0 / 64, scalar2=EPS,
                                                  op0=ALU.mult, op1=ALU.add), [ssq.b], [ssq.b])
        fw.act.op(lambda: nc.scalar.sqrt(out=ssq[:, h0:10], in_=ssq[:, h0:10]), [ssq.b], [ssq.b])
        fw.dve.op(lambda: nc.vector.reciprocal(out=ssq[:, h0:10], in_=ssq[:, h0:10]), [ssq.b], [ssq.b])
        fw.dve.op(lambda: nc.vector.tensor_tensor(out=q3(qn), in0=q3(qkv), in1=ssq[:, h0:10].unsqueeze(2).to_broadcast([128, nh, 64]),
                                                  op=ALU.mult), [qkv.b, ssq.b], [qn.b])
        if lat_idx is None:
            fw.pool.op(lambda: nc.gpsimd.tensor_tensor(out=qr[:, c0:640], in0=qn[:, c0:640], in1=gqk[:, c0:640], op=ALU.mult),
                       [qn.b, gqk.b], [qr.b])
        else:
            fw.pool.op(lambda: nc.gpsimd.tensor_tensor(out=qn[:, c0:640], in0=qn[:, c0:640], in1=gqk[:, c0:640], op=ALU.mult),
                       [qn.b, gqk.b], [qn.b])
            cosb = g["cos"][:, lat_idx, :].unsqueeze(1).to_broadcast([128, nh, 32])
            sinb = g["sin"][:, lat_idx, :].unsqueeze(1).to_broadcast([128, nh, 32])
            x1 = q3(qn)[:, :, 0:32]
            x2 = q3(qn)[:, :, 32:64]
            sq2 = sq[:, 0:640].rearrange("p (k n) -> p k n", k=2)
            t3 = lambda k: (sq2 if k < 2 else tmp)[:, k % 2, c0 // 2:320].rearrange("p (h d) -> p h d", d=32)
            fw.dve.op(lambda: nc.vector.tensor_tensor(out=t3(0), in0=x1, in1=cosb, op=ALU.mult), [qn.b, g["cos"].b], [sq.b])
            fw.pool.op(lambda: nc.gpsimd.tensor_tensor(out=t3(1), in0=x2, in1=sinb, op=ALU.mult), [qn.b, g["sin"].b], [sq.b])
            fw.dve.op(lambda: nc.vector.tensor_tensor(out=t3(2), in0=x2, in1=cosb, op=ALU.mult), [qn.b, g["cos"].b], [tmp.b])
            fw.pool.op(lambda: nc.gpsimd.tensor_tensor(out=t3(3), in0=x1, in1=sinb, op=ALU.mult), [qn.b, g["sin"].b], [tmp.b])
            fw.dve.op(lambda: nc.vector.tensor_tensor(out=q3(qr)[:, :, 0:32], in0=t3(0), in1=t3(1), op=ALU.subtract), [sq.b], [qr.b])
            fw.pool.op(lambda: nc.gpsimd.tensor_tensor(out=q3(qr)[:, :, 32:64], in0=t3(2), in1=t3(3), op=ALU.add), [tmp.b], [qr.b])
        if self.cut(30):
            return
        p = self.ps_get()
        pv = p[:].bitcast(BF16).rearrange("p (c q) -> p c q", c=8)
        if do_q:
            for j in range(4):
                fw.pe.op(lambda: nc.tensor.transpose(out=pv[:, j, :], in_=qr[:, j * 128:(j + 1) * 128], identity=g["identb"][:]),
                         [qr.b, g["identb"].b], [p.b], inc=False)
        fw.pe.op(lambda: nc.tensor.transpose(out=pv[:, 4, :], in_=qr[:, 512:640], identity=g["identb"][:]),
                 [qr.b, g["identb"].b], [p.b])
        if self.cut(31):
            return
        if do_q:
            fw.dve.op(lambda: nc.vector.tensor_copy(out=qT[:, t, :, :], in_=pv[:, 0:4, :]), [p.b], [qT.b])
        if self.cut(32):
            return
        fw.dve.op(lambda: nc.vector.tensor_copy(out=kT[0:64, 0, t * 128:(t + 1) * 128], in_=pv[0:64, 4, :]), [p.b], [kT.b])
        fw.dve.op(lambda: nc.vector.tensor_copy(out=kT[64:128, 1, t * 128:(t + 1) * 128], in_=pv[64:128, 4, :]), [p.b], [kT.b])
        self.ps_put(p)

    def v_fill(self, qkv, t, Vp):
        nc, fw = self.nc, self.fw
        fw.act.op(lambda: nc.scalar.copy(out=Vp[:, t, 0:64], in_=qkv[:, 640:704]), [qkv.b], [Vp.b])
        fw.act.op(lambda: nc.scalar.copy(out=Vp[:, t, 136:200], in_=qkv[:, 704:768]), [qkv.b], [Vp.b])

    def v_init(self, Vp):
        nc, fw = self.nc, self.fw
        fw.pool.op(lambda: nc.gpsimd.memset(Vp[:], 0.0), [], [Vp.b])
        fw.pool.op(lambda: nc.gpsimd.memset(Vp[:, :, 64:65], 1.0), [], [Vp.b])
        fw.pool.op(lambda: nc.gpsimd.memset(Vp[:, :, 104:105], 1.0), [], [Vp.b])

    def attention(self, blocks, qT, kT, Vp, oT, scr, masks=None, sinkrow=None, LA=2):
        nc, fw, g = self.nc, self.fw, self.g
        steps = []
        for b0 in range(0, len(blocks), 2):
            (kv0_, qb, kts), (kv1_, qb1, kts1) = blocks[b0], blocks[b0 + 1]
            assert (kv0_, kv1_) == (0, 1) and qb == qb1
            for i, (kt, mi) in enumerate(kts):
                steps.append(dict(bi=b0, qb=qb, kt=kt, mi=mi, first=(i == 0), last=(i == len(kts) - 1)))
        assert len(self.psq) == 8
        spairs = self.PSB[0:2]
        for (_, h0, h1) in spairs:
            self.psq.remove(h0)
            self.psq.remove(h1)
        po_of = {}
        cnt = dict(s=0)

        pending = []

        def finish_block(kv, qb, bi, po, idx):
            r0 = kv * 64
            dr = 64 if kv == 0 else 32
            M = 65 if kv == 0 else 128
            slot = (bi // 2) % 2
            posb = scr["posb"][slot][kv]
            rec = scr["rec"][slot]
            fw.dve.op(lambda: nc.vector.tensor_copy(out=posb[0:M, :], in_=po[0:M, :]), [po.b], [posb.b])
            self.ps_put(po)
            if sinkrow is not None:
                fw.dve.op(lambda: nc.vector.tensor_tensor(out=rec[dr:dr + 1, :], in0=posb[dr:dr + 1, :], in1=sinkrow[dr:dr + 1, kv, :], op=ALU.add),
                          [posb.b, sinkrow.b], [rec.b])
                fw.dve.op(lambda: nc.vector.reciprocal(out=rec[dr:dr + 1, :], in_=rec[dr:dr + 1, :]), [rec.b], [rec.b])
            else:
                fw.dve.op(lambda: nc.vector.reciprocal(out=rec[dr:dr + 1, :], in_=posb[dr:dr + 1, :]), [posb.b], [rec.b])
            pending.append((idx + 4, kv, qb, posb, rec))

        def finalize(kv, qb, posb, rec):
            r0 = kv * 64
            dr = 64 if kv == 0 else 32
            pb = self.ps_get()
            Mb = 64 if kv == 0 else 128
            fw.pe.op(lambda: nc.tensor.matmul(pb[0:Mb, :], lhsT=g["onesf"][dr:dr + 1, 0:Mb], rhs=rec[dr:dr + 1, :], start=True, stop=True),
                     [g["onesf"].b, rec.b], [pb.b])
            fw.dve.op(lambda: nc.vector.tensor_tensor(out=oT[r0:r0 + 64, :, qb * 128:(qb + 1) * 128],
                                                      in0=posb[r0:r0 + 64, :].rearrange("p (j q) -> p j q", j=4),
                                                      in1=pb[r0:r0 + 64, :].rearrange("p (j q) -> p j q", j=4), op=ALU.mult),
                      [posb.b, pb.b], [oT.b])
            self.ps_put(pb)

        def emit_S(st):
            qb, kt = st["qb"], st["kt"]
            big, h0, h1 = spairs[cnt["s"] % 2]
            cnt["s"] += 1
            rhs_q = qT[:, qb, :, :].rearrange("p j q -> p (j q)")
            fw.pe.op(lambda: nc.tensor.matmul(h0[:], lhsT=kT[:, 0, kt * 128:(kt + 1) * 128], rhs=rhs_q, start=True, stop=True),
                     [kT.b, qT.b], [h0.b], inc=False)
            fw.pe.op(lambda: nc.tensor.matmul(h1[:], lhsT=kT[:, 1, kt * 128:(kt + 1) * 128], rhs=rhs_q, start=True, stop=True),
                     [kT.b, qT.b], [h1.b])
            pe_ = scr["pexp"][scr["pi"] % len(scr["pexp"])]
            scr["pi"] += 1
            fw.act.op(lambda: nc.scalar.activation(out=pe_[:], in_=big[:, 0:1024], func=AF.Exp), [h0.b, h1.b], [pe_.b])
            if st["mi"] is not None:
                fw.dve.op(lambda: nc.vector.tensor_tensor(out=pe_[:].rearrange("p (a q) -> p a q", a=2), in0=pe_[:].rearrange("p (a q) -> p a q", a=2),
                                                          in1=masks[:, st["mi"], :].unsqueeze(1).to_broadcast([128, 2, 512]), op=ALU.mult),
                          [pe_.b, masks.b], [pe_.b])
            st["pe"] = pe_

        def emit_PV(st):
            qb, kt, bi = st["qb"], st["kt"], st["bi"]
            if st["first"]:
                po_of[bi] = (self.ps_get(), self.ps_get())
            pe_ = st["pe"]
            for kv in range(2):
                po = po_of[bi][kv]
                M = 65 if kv == 0 else 128
                fw.pe.op(lambda: nc.tensor.matmul(po[0:M, :], lhsT=Vp[:, kt, kv * 72:kv * 72 + M], rhs=pe_[:, kv * 512:(kv + 1) * 512],
                                                  start=st["first"], stop=st["last"]), [Vp.b, pe_.b], [po.b], inc=(st["last"] or kv == 1))
            if st["last"]:
                for kv in range(2):
                    finish_block(kv, qb, bi, po_of[bi][kv], st["idx"])
                del po_of[bi]

        n = len(steps)
        for idx in range(n + LA):
            if idx < n:
                emit_S(steps[idx])
            if idx - LA >= 0:
                steps[idx - LA]["idx"] = idx
                emit_PV(steps[idx - LA])
            while pending and pending[0][0] <= idx:
                _, kv, qb_, posb, rec = pending.pop(0)
                finalize(kv, qb_, posb, rec)
        while pending:
            _, kv, qb_, posb, rec = pending.pop(0)
            finalize(kv, qb_, posb, rec)
        for (_, h0, h1) in spairs:
            self.psq.append(h0)
            self.psq.append(h1)

    def out_proj(self, l, cat_fn, wsrc, tiles, ph):
        nc, fw, g = self.nc, self.fw, self.g
        classes = [1, 0] if l == 0 else [0]
        wo = {}
        GB = self.tile(ph, "GB", [128, D], F32)
        tmp = self.tile(ph, "bct", [128, 128], F32)
        stg = [self.tile(ph, f"ostg{i}", [128, D], F32) for i in range(2)]
        for cls in classes:
            wo[cls] = self.tile(ph, f"wo{cls}", [128, 8, D], BF16)
            self.bcast_tile(GB, lambda c: g["modT"][:, l, 16 + c, cls:cls + 1], tmp)
            for c in range(8):
                s_ = stg[c % 2]
                if c < 4:
                    fw.q_sync.dma(s_[0:64, :], wsrc[c * 64:(c + 1) * 64, :], writes=[s_.b])
                    fw.q_sync.dma(s_[64:128, :], wsrc[(c + 4) * 64:(c + 5) * 64, :], writes=[s_.b])
                else:
                    fw.q_sync.dma(s_[:], wsrc[c * 128:(c + 1) * 128, :], writes=[s_.b])
                fw.dve.op(lambda: nc.vector.tensor_tensor(out=wo[cls][:, c, :], in0=s_[:], in1=GB[:], op=ALU.mult), [s_.b, GB.b], [wo[cls].b])
        xt = [self.tile(ph, f"oxt{i}", [128, D], F32) for i in range(3)]

        def op_fn(i, t):
            cls = 1 if t < 2 else 0
            x_ = xt[i % 3]
            src = self.tok_in(t) if l == 0 else self.xres[t * 128:(t + 1) * 128, :]
            rb = [] if l == 0 else [self.xres_b[t]]
            fw.q_sync.dma(x_[:], src, reads=rb, writes=[x_.b])
            for half in range(2):
                p = self.ps_get()
                for c in range(8):
                    ct, ci = cat_fn(c)
                    fw.pe.op(lambda: nc.tensor.matmul(p[:], lhsT=ct[:, ci, t * 128:(t + 1) * 128], rhs=wo[cls][:, c, half * 512:(half + 1) * 512],
                                                      start=(c == 0), stop=(c == 7)), [ct.b, wo[cls].b], [p.b], inc=(c == 7))
                fw.dve.op(lambda: nc.vector.tensor_tensor(out=x_[:, half * 512:(half + 1) * 512], in0=p[:], in1=x_[:, half * 512:(half + 1) * 512],
                                                          op=ALU.add), [p.b, x_.b], [x_.b])
                self.ps_put(p)
            fw.q_pool.dma(self.xres[t * 128:(t + 1) * 128, :], x_[:], reads=[x_.b], writes=[self.xres_b[t]])
            if self.debug and t in (0, 2, NT - 1):
                nm = f"x1_{l}_{t}"
                ap = self.dbg(nm, [128, D])
                fw.q_pool.dma(ap, x_[:], reads=[x_.b], writes=[self.dbg_out[nm][1]])

        for i0 in range(0, len(tiles), 3):
            grp = list(range(i0, min(i0 + 3, len(tiles))))
            ILV.run([(lambda i=i: op_fn(i, tiles[i])) for i in grp])

    def layer0_mixer(self):
        nc, fw, g = self.nc, self.fw, self.g
        l = 0
        with ExitStack() as ph:
            gluT = self.tile(ph, "gluT", [128, 4, GLU_LEN], BF16)
            oT = self.tile(ph, "oT", [128, 4, T], BF16)
            pattn = ExitStack()
            qT = self.tile(pattn, "qT", [128, NT, 4, 128], BF16)
            kT = self.tile(pattn, "kT", [128, 2, T], BF16)
            Vp = self.tile(pattn, "Vp", [128, NT, 200], BF16)
            self.v_init(Vp)
            fw.pool.op(lambda: nc.gpsimd.memset(kT[:], 0.0), [], [kT.b])
            fw.pool.op(lambda: nc.gpsimd.memset(gluT[:], 0.0), [], [gluT.b])
            with ExitStack() as p1:
                win = self.tile(p1, "win", [128, 8, 1792], BF16)
                gqk = self.tile(p1, "gqk", [128, 640], F32)
                g["cos"] = self.tile(p1, "cos", [128, 32, 32], F32)
                g["sin"] = self.tile(p1, "sin", [128, 32, 32], F32)
                fw.q_sync.dma(gqk[:], self.gqk_a, writes=[gqk.b])
                fw.q_sync.dma(g["cos"][:], self.cosT, writes=[g["cos"].b])
                fw.q_sync.dma(g["sin"][:], self.sinT, writes=[g["sin"].b])
                fw.dve.op(lambda: nc.vector.tensor_scalar(out=gqk[:, 0:512], in0=gqk[:, 0:512], scalar1=0.125, scalar2=None, op0=ALU.mult),
                          [gqk.b], [gqk.b])
                with ExitStack() as pw:
                    stg = [self.tile(pw, f"wstg{i}", [128, 4096], F32) for i in range(1)]
                    self.stg_i = 0
                    for n in range(4):
                        w_ = 512 if n < 3 else 256
                        self.load_cast_w(stg, win, slice(n * 512, n * 512 + w_), self.w_in_ab[:, n * 512:n * 512 + w_], 8, w_, n)
                    fw.barrier()
                if self.cut(0):
                    return
                scr = dict(ssl=[self.tile(p1, f"ss{i}", [128, 1], F32) for i in range(2)],
                           xnl=[self.tile(p1, f"xn{i}", [128, D], BF16) for i in range(2)], sq=self.tile(p1, "sq", [128, 640], F32),
                           ssq=self.tile(p1, "ssq", [128, 10], F32), qn=self.tile(p1, "qn", [128, 640], F32),
                           qr=self.tile(p1, "qr", [128, 640], BF16), rt=self.tile(p1, "rt", [128, 2, 320], F32))
                xt = [self.tile(p1, f"xt{i}", [128, D], F32) for i in range(1)]
                hT = [self.tile(p1, f"hT{i}", [128, 8, 512], BF16) for i in range(1)]
                qkv = [self.tile(p1, f"qkv{i}", [128, 768], F32) for i in range(1)]
                sig = [self.tile(p1, f"sig{i}", [128, 512], BF16) for i in range(1)]
                chunks = [(0, 2)] + [(2 + 4 * i, 4) for i in range(8)]
                oflat = oT[:].rearrange("p a t -> p (a t)")
                coff = [0]

                def carve(n_elems, dt, shape3=None):
                    nb = n_elems * (4 if dt == F32 else 2) // 2
                    ap = oflat[:, coff[0]:coff[0] + nb]
                    coff[0] += nb
                    if dt == F32:
                        ap = ap.bitcast(F32)
                    if shape3 is not None:
                        ap = ap.rearrange("p (a b) -> p a b", a=shape3[0])
                    return Tile(ap, Buf("carve"))

                scr_b = dict(ssl=[scr["ssl"][1]], xnl=[scr["xnl"][1]], sq=carve(640, F32), ssq=carve(16, F32), qn=carve(640, F32),
                             qr=carve(640, BF16), rt=carve(640, F32, (2, 320)))
                scr_a = dict(scr)
                scr_a["ssl"] = [scr["ssl"][0]]
                scr_a["xnl"] = [scr["xnl"][0]]
                scr_l = [scr_a, scr_b]
                xt2 = [xt[0], carve(D, F32)]
                qkv2 = [qkv[0], carve(768, F32)]
                for ci, (t0, ntl) in enumerate(chunks):
                    h = hT[0]
                    ntok = ntl * 128
                    cls = 1 if t0 < 2 else 0

                    def norm_fn(tl):
                        t = t0 + tl
                        x_ = xt2[tl % 2]
                        fw.q_sync.dma(x_[:], self.tok_in(t), writes=[x_.b])
                        self.norm_tile_to_hT(x_, h, tl * 128, l, 1, cls, scr_l[tl % 2])

                    def qkv_fn(tl):
                        t = t0 + tl
                        qk_ = qkv2[tl % 2]
                        for (c0, w_) in ((0, 512), (512, 256)):
                            p = self.ps_get()
                            for k in range(8):
                                fw.pe.op(lambda: nc.tensor.matmul(p[:, 0:w_], lhsT=h[:, k, tl * 128:(tl + 1) * 128], rhs=win[:, k, c0:c0 + w_],
                                                                  start=(k == 0), stop=(k == 7)), [h.b, win.b], [p.b], inc=(k == 7))
                            if c0 == 0:
                                fw.act.op(lambda: nc.scalar.copy(out=qk_[:, 0:512].rearrange("p (j a d) -> p a j d", a=2, d=64),
                                                                 in_=p[:, 0:512].rearrange("p (a j d) -> p a j d", a=2, j=4)), [p.b], [qk_.b])
                            else:
                                fw.act.op(lambda: nc.scalar.copy(out=qk_[:, c0:c0 + w_], in_=p[:, 0:w_]), [p.b], [qk_.b])
                            self.ps_put(p)
                        self.qk_post(qk_, t, gqk, scr_l[tl % 2], qT, kT, True, None if t < 2 else t - 2)
                        self.v_fill(qk_, t, Vp)

                    for tl0 in range(0, ntl, 2):
                        ILV.run([(lambda tl=tl: norm_fn(tl)) for tl in range(tl0, min(tl0 + 2, ntl))])
                    for tl0 in range(0, ntl, 2):
                        ILV.run([(lambda tl=tl: qkv_fn(tl)) for tl in range(tl0, min(tl0 + 2, ntl))])
                    off = GLU_OFF_CTX if t0 < 2 else GLU_OFF_LAT + (t0 - 2) * 128
                    for j in range(4):
                        pa = self.ps_get()
                        pg = self.ps_get()
                        for (pp, cb) in ((pa, 768 + j * 128), (pg, 1280 + j * 128)):
                            for k in range(8):
                                fw.pe.op(lambda: nc.tensor.matmul(pp[:, 0:ntok], lhsT=win[:, k, cb:cb + 128], rhs=h[:, k, 0:ntok],
                                                                  start=(k == 0), stop=(k == 7)), [win.b, h.b], [pp.b], inc=(k == 7))
                        sg = sig[0]
                        fw.act.op(lambda: nc.scalar.activation(out=sg[:, 0:ntok], in_=pg[:, 0:ntok], func=AF.Sigmoid), [pg.b], [sg.b])
                        fw.dve.op(lambda: nc.vector.tensor_tensor(out=gluT[:, j, off:off + ntok], in0=pa[:, 0:ntok], in1=sg[:, 0:ntok], op=ALU.mult),
                                  [pa.b, sg.b], [gluT.b])
                        self.ps_put(pa)
                        self.ps_put(pg)
                    if self.cut(4) or (self.cut(5) and ci == 1):
                        return
                fw.barrier()
            if self.debug:
                self.dbg_dump_bf(ph, "qT", qT[:, 2, :, :], [128, 4, 128], qT.b)
                self.dbg_dump_bf(ph, "kT", kT[:, 0, 0:512], [128, 512], kT.b)
                self.dbg_dump_bf(ph, "glu", gluT[:, :, GLU_OFF_LAT:GLU_OFF_LAT + 128], [128, 4, 128], gluT.b)
            if self.stop == "p1":
                return
            with ExitStack() as p2:
                scr = dict(pexp=[self.tile(p2, f"pexp{i}", [128, 1024], BF16) for i in range(4)], pi=0,
                           rec=[self.tile(p2, f"rec{i}", [128, 512], F32) for i in range(2)],
                           posb=[[self.tile(p2, f"posb{i}{k}", [128, 512], F32) for k in range(2)] for i in range(2)])
                blocks = []
                for qb in range(NT):
                    kts = [(0, None), (1, None)] if qb < 2 else [(k, None) for k in range(NT)]
                    for kv in range(2):
                        blocks.append((kv, qb, kts))
                self.attention(blocks, qT, kT, Vp, oT, scr)
                fw.barrier()
            if self.debug:
                self.dbg_dump_bf(ph, "oT", oT[:, :, 256:384], [128, 4, 128], oT.b)
                self.dbg_dump_bf(ph, "oTc", oT[:, :, 0:128], [128, 4, 128], oT.b)
            pattn.close()
            if self.stop == "p2":
                return
            bT = self.tile(ph, "bT", [128, 4, T], BF16)
            with ExitStack() as p3:
                cw = self.tile(p3, "cw", [128, 4, 31], F32)
                cv = self.tile(p3, "cv", [128, 3, 4], F32)
                diag = self.tile(p3, "diag", [128, 4, 31, 128], BF16)
                onesM = self.tile(p3, "onesM", [128, 128], F32)
                fw.q_sync.dma(cw[:], self.convwT, writes=[cw.b])
                fw.q_sync.dma(cv[:], self.convv, writes=[cv.b])
                fw.dve.op(lambda: nc.vector.memset(onesM[:], 1.0 / 512), [], [onesM.b])
                for j in range(4):
                    for tap in range(31):
                        e_ = fw.dve if (tap % 2 == 0) else fw.pool
                        ee = nc.vector if (tap % 2 == 0) else nc.gpsimd
                        e_.op(lambda: ee.tensor_scalar(out=diag[:, j, tap, :], in0=g["identb"][:], scalar1=cw[:, j, tap:tap + 1], scalar2=None,
                                                       op0=ALU.mult), [g["identb"].b, cw.b], [diag.b])
                ysb = [self.tile(p3, f"ysb{i}", [128, 4, 512], F32) for i in range(2)]
                ysq = [self.tile(p3, f"ysq{i}", [128, 4, 512], F32) for i in range(2)]
                mean_l = [self.tile(p3, f"mean{i}", [128, 512], F32) for i in range(2)]
                rstd_l = [self.tile(p3, f"rstd{i}", [128, 512], F32) for i in range(2)]
                tmp_l = [[self.tile(p3, f"ctmp{i}{k}", [128, 512], F32) for k in range(2)] for i in range(2)]
                chunks = [(GLU_OFF_CTX, 0, 256)] + [(GLU_OFF_LAT + 512 * i, 256 + 512 * i, 512) for i in range(8)]

                def conv_fn(ci):
                    off, tok0, ntok = chunks[ci]
                    mean, rstd, tmp = mean_l[ci % 2], rstd_l[ci % 2], tmp_l[ci % 2]
                    y_, q_ = ysb[ci % 2], ysq[ci % 2]
                    for j in range(4):
                        p = self.ps_get()
                        for tap in range(31):
                            fw.pe.op(lambda: nc.tensor.matmul(p[:, 0:ntok], lhsT=diag[:, j, tap, :], rhs=gluT[:, j, off + tap - 15:off + tap - 15 + ntok],
                                                              start=(tap == 0), stop=(tap == 30)), [diag.b, gluT.b], [p.b], inc=(tap == 30))
                        fw.act.op(lambda: nc.scalar.activation(out=y_[:, j, 0:ntok], in_=p[:, 0:ntok], func=AF.Identity, bias=cv[:, 0, j:j + 1]),
                                  [p.b, cv.b], [y_.b])
                        self.ps_put(p)
                        fw.pool.op(lambda: nc.gpsimd.tensor_tensor(out=q_[:, j, 0:ntok], in0=y_[:, j, 0:ntok], in1=y_[:, j, 0:ntok], op=ALU.mult),
                                   [y_.b], [q_.b])
                    pm = self.ps_get()
                    pq = self.ps_get()
                    for (pp, src) in ((pm, y_), (pq, q_)):
                        for j in range(4):
                            fw.pe.op(lambda: nc.tensor.matmul(pp[:, 0:ntok], lhsT=onesM[:], rhs=src[:, j, 0:ntok], start=(j == 0), stop=(j == 3)),
                                     [onesM.b, src.b], [pp.b], inc=(j == 3))
                    fw.act.op(lambda: nc.scalar.copy(out=mean[:, 0:ntok], in_=pm[:, 0:ntok]), [pm.b], [mean.b])
                    self.ps_put(pm)
                    fw.pool.op(lambda: nc.gpsimd.tensor_tensor(out=rstd[:, 0:ntok], in0=mean[:, 0:ntok], in1=mean[:, 0:ntok], op=ALU.mult),
                               [mean.b], [rstd.b])
                    fw.dve.op(lambda: nc.vector.tensor_tensor(out=rstd[:, 0:ntok], in0=pq[:, 0:ntok], in1=rstd[:, 0:ntok], op=ALU.subtract),
                              [pq.b, rstd.b], [rstd.b])
                    self.ps_put(pq)
                    fw.dve.op(lambda: nc.vector.tensor_scalar(out=rstd[:, 0:ntok], in0=rstd[:, 0:ntok], scalar1=EPS, scalar2=None, op0=ALU.add),
                              [rstd.b], [rstd.b])
                    fw.act.op(lambda: nc.scalar.sqrt(out=rstd[:, 0:ntok], in_=rstd[:, 0:ntok]), [rstd.b], [rstd.b])
                    fw.dve.op(lambda: nc.vector.reciprocal(out=rstd[:, 0:ntok], in_=rstd[:, 0:ntok]), [rstd.b], [rstd.b])
                    for j in range(4):
                        t_ = tmp[j % 2]
                        fw.pool.op(lambda: nc.gpsimd.tensor_tensor(out=t_[:, 0:ntok], in0=y_[:, j, 0:ntok], in1=mean[:, 0:ntok], op=ALU.subtract),
                                   [y_.b, mean.b], [t_.b])
                        fw.dve.op(lambda: nc.vector.tensor_tensor(out=t_[:, 0:ntok], in0=t_[:, 0:ntok], in1=rstd[:, 0:ntok], op=ALU.mult),
                                  [t_.b, rstd.b], [t_.b])
                        fw.act.op(lambda: nc.scalar.activation(out=bT[:, j, tok0:tok0 + ntok], in_=t_[:, 0:ntok], func=AF.Silu,
                                                               scale=cv[:, 1, j:j + 1], bias=cv[:, 2, j:j + 1]), [t_.b, cv.b], [bT.b])

                conv_fn(0)
                for c0_ in range(1, 9, 2):
                    ILV.run([(lambda ci=ci: conv_fn(ci)) for ci in (c0_, c0_ + 1)])
                fw.barrier()
            if self.debug:
                self.dbg_dump_bf(ph, "bT", bT[:, :, 256:384], [128, 4, 128], bT.b)
            if self.stop == "p3":
                return
            with ExitStack() as p4:
                self.out_proj(0, lambda c: (oT, c) if c < 4 else (bT, c - 4), self.w_out_ab, list(range(NT)), p4)
                fw.barrier()

    def dbg_dump_bf(self, ph, name, src_ap, shape, buf):
        nc, fw = self.nc, self.fw
        n = int(np.prod(shape[1:]))
        ap = self.dbg(name, [128, n])
        with ExitStack() as ds:
            tf = self.tile(ds, "dbgt_" + name, shape, F32)
            fw.dve.op(lambda: nc.vector.tensor_copy(out=tf[:], in_=src_ap), [buf], [tf.b])
            flat = tf[:] if len(shape) == 2 else tf[:].rearrange("p a b -> p (a b)")
            fw.q_pool.dma(ap, flat, reads=[tf.b], writes=[self.dbg_out[name][1]])
            fw.barrier()

    def moe(self, l):
        nc, fw, g = self.nc, self.fw, self.g
        if l == 0:
            groups = [list(range(0, 10)), list(range(10, 18)), list(range(18, 26)), list(range(26, 34))]
        else:
            groups = [list(range(2 + 8 * i, 10 + 8 * i)) for i in range(4)]
        ngrp = int(os.environ.get("MOE_GROUPS", "4"))
        nexp = int(os.environ.get("MOE_EXPERTS", "32"))
        classes = [0, 1] if l == 0 else [0]
        with ExitStack() as ph:
            wr = self.tile(ph, "wr", [128, 8, 36], F32)
            rb = self.tile(ph, "rb", [128, 36], F32)
            fw.q_sync.dma(wr[:], self.rt_w[l].rearrange("(k p) n -> p k n", p=128), writes=[wr.b])
            fw.q_sync.dma(rb[:], self.rt_b[:, l, :], writes=[rb.b])
            G2B = {}
            tmpb = self.tile(ph, "bct2", [128, 128], F32)
            for cls in classes:
                G2B[cls] = self.tile(ph, f"G2B{cls}", [128, D], F32)
                self.bcast_tile(G2B[cls], lambda c: g["modT"][:, l, 40 + c, cls:cls + 1], tmpb)
            stg = [self.tile(ph, f"estg{i}", [128, 4096], F32) for i in range(2)]
            wg = [self.tile(ph, f"wg{i}", [128, 8, 512], BF16) for i in range(2)]
            wu = [self.tile(ph, f"wu{i}", [128, 8, 512], BF16) for i in range(2)]
            wd = {cls: [self.tile(ph, f"wd{cls}_{i}", [128, 4, D], BF16) for i in range(2)] for cls in classes}
            xg = self.tile(ph, "xg", [128, 10, D], F32)
            h2T = self.tile(ph, "h2T", [128, 8, 1280], BF16)
            Wt = self.tile(ph, "Wt", [128, 10, 32], F32)
            xn = self.tile(ph, "xn2", [128, D], F32)
            hTf = self.tile(ph, "hTf", [128, 8, 128], F32)
            ss = self.tile(ph, "ss2", [128, 1], F32)
            r_ = {k: self.tile(ph, "r_" + k, [128, n], F32) for k, n in
                  dict(lg=36, gmax=1, ngmax=1, goh=4, ge=4, gsum=1, pen=4, em=32, m8=8, d=1, ed=1, p1=1, p2=1, t1=32, t2=32).items()}
            hid = [self.tile(ph, f"hid{i}", [128, 4, 512], BF16) for i in range(2)]
            sgl = [self.tile(ph, f"sgl{i}", [128, 512], F32) for i in range(2)]
            self.stg_i = 0

            def load_expert(e, need_ctx):
                i = e % 2
                self.load_cast_w(stg, wg[i], slice(0, 512), self.ex_gate[l, e], 8, 512, 0)
                self.load_cast_w(stg, wu[i], slice(0, 512), self.ex_up[l, e], 8, 512, 0)
                s_ = stg[self.stg_i % 2]
                self.stg_i += 1
                fw.q_sync.dma(s_[:].rearrange("p (k n) -> p k n", k=4), self.ex_down[l, e].rearrange("(k p) n -> p k n", p=128), writes=[s_.b])
                for cls in classes:
                    if cls == 1 and not need_ctx:
                        continue
                    fw.dve.op(lambda: nc.vector.tensor_tensor(out=wd[cls][i][:], in0=s_[:].rearrange("p (k n) -> p k n", k=4),
                                                              in1=G2B[cls][:].unsqueeze(1).to_broadcast([128, 4, D]), op=ALU.mult),
                              [s_.b, G2B[cls].b], [wd[cls][i].b])

            for gi, tiles in enumerate(groups[:ngrp]):
                has_ctx = (l == 0 and gi == 0)
                load_expert(0, has_ctx)
                for ti, t in enumerate(tiles):
                    cls = 1 if t < 2 else 0
                    fw.q_sync.dma(xg[:, ti, :], self.xres[t * 128:(t + 1) * 128, :], reads=[self.xres_b[t]], writes=[xg.b])
                    fw.act.op(lambda: nc.scalar.activation(out=xn[:], in_=xg[:, ti, :], func=AF.Square, accum_out=ss[:, 0:1]), [xg.b], [xn.b, ss.b])
                    self.rstd_from_ss(ss, 1.0 / D, 1)
                    fw.act.op(lambda: nc.scalar.activation(out=xn[:], in_=xg[:, ti, :], func=AF.Copy, scale=ss[:, 0:1]), [xg.b, ss.b], [xn.b])
                    for hb in range(2):
                        p = self.ps_get()
                        pv = p[:].rearrange("p (c q) -> p c q", c=4)
                        for c4 in range(4):
                            c = hb * 4 + c4
                            fw.pe.op(lambda: nc.tensor.transpose(out=pv[:, c4, :], in_=xn[:, c * 128:(c + 1) * 128], identity=g["identf"][:]),
                                     [xn.b, g["identf"].b], [p.b], inc=(c4 == 3))
                        for c4 in range(4):
                            c = hb * 4 + c4
                            if hb == 0:
                                fw.dve.op(lambda: nc.vector.tensor_scalar(out=hTf[:, c, :], in0=pv[:, c4, :], scalar1=g["A2"][:, l, c, cls:cls + 1],
                                                                          scalar2=g["modT"][:, l, 24 + c, cls:cls + 1], op0=ALU.mult, op1=ALU.add),
                                          [p.b, g["A2"].b, g["modT"].b], [hTf.b])
                            else:
                                fw.act.op(lambda: nc.scalar.activation(out=hTf[:, c, :], in_=pv[:, c4, :], func=AF.Identity,
                                                                       scale=g["A2"][:, l, c, cls:cls + 1], bias=g["modT"][:, l, 24 + c, cls:cls + 1]),
                                          [p.b, g["A2"].b, g["modT"].b], [hTf.b])
                        self.ps_put(p)
                    fw.pool.op(lambda: nc.gpsimd.tensor_copy(out=h2T[:, :, ti * 128:(ti + 1) * 128], in_=hTf[:]), [hTf.b], [h2T.b])
                    pr = self.ps_get()
                    for c in range(8):
                        fw.pe.op(lambda: nc.tensor.matmul(pr[:, 0:36], lhsT=hTf[:, c, :], rhs=wr[:, c, :], start=(c == 0), stop=(c == 7)),
                                 [hTf.b, wr.b], [pr.b], inc=(c == 7))
                    self.route(pr, rb, r_, Wt, ti)
                    self.ps_put(pr)
                if self.debug and gi == 0:
                    self.dbg_dump_bf(ph, f"Wt{l}", Wt[:, 0:4, :], [128, 4, 32], Wt.b)
                if has_ctx:
                    chunks = [(0, 2), (2, 4), (6, 4)]
                else:
                    chunks = [(0, 4), (4, 4)]
                for e in range(nexp):
                    if e + 1 < nexp:
                        load_expert(e + 1, has_ctx)
                    i = e % 2
                    for ci, (tl0, ntl) in enumerate(chunks):
                        cls = 1 if (has_ctx and ci == 0) else 0
                        ntok = ntl * 128
                        c0 = tl0 * 128
                        hd = hid[(e * len(chunks) + ci) % 2]
                        for f in range(4):
                            pg = self.ps_get()
                            pu = self.ps_get()
                            for (pp, w_) in ((pg, wg[i]), (pu, wu[i])):
                                for k in range(8):
                                    fw.pe.op(lambda: nc.tensor.matmul(pp[:, 0:ntok], lhsT=w_[:, k, f * 128:(f + 1) * 128], rhs=h2T[:, k, c0:c0 + ntok],
                                                                      start=(k == 0), stop=(k == 7)), [w_.b, h2T.b], [pp.b], inc=(k == 7))
                            sg = sgl[f % 2]
                            fw.act.op(lambda: nc.scalar.activation(out=sg[:, 0:ntok], in_=pg[:, 0:ntok], func=AF.Silu), [pg.b], [sg.b])
                            fw.dve.op(lambda: nc.vector.tensor_tensor(out=hd[:, f, 0:ntok], in0=pu[:, 0:ntok], in1=sg[:, 0:ntok], op=ALU.mult),
                                      [pu.b, sg.b], [hd.b])
                            self.ps_put(pg)
                            self.ps_put(pu)
                        for tl in range(ntl):
                            ti = tl0 + tl
                            for half in range(2):
                                pd = self.ps_get()
                                for f in range(4):
                                    fw.pe.op(lambda: nc.tensor.matmul(pd[:], lhsT=hd[:, f, tl * 128:(tl + 1) * 128],
                                                                      rhs=wd[cls][i][:, f, half * 512:(half + 1) * 512], start=(f == 0), stop=(f == 3)),
                                             [hd.b, wd[cls][i].b], [pd.b], inc=(f == 3))
                                fw.dve.op(lambda: nc.vector.scalar_tensor_tensor(out=xg[:, ti, half * 512:(half + 1) * 512], in0=pd[:],
                                                                                 scalar=Wt[:, ti, e:e + 1], in1=xg[:, ti, half * 512:(half + 1) * 512],
                                                                                 op0=ALU.mult, op1=ALU.add), [pd.b, Wt.b, xg.b], [xg.b])
                                self.ps_put(pd)
                for ti, t in enumerate(tiles):
                    if l == 0:
                        fw.q_pool.dma(self.xres[t * 128:(t + 1) * 128, :], xg[:, ti, :], reads=[xg.b], writes=[self.xres_b[t]])
                    else:
                        fw.q_pool.dma(self.y_out[(t - 2) * 128:(t - 1) * 128, :], xg[:, ti, :], reads=[xg.b], writes=[self.y_b[t]])
                    if self.debug and t in (0, 2, NT - 1):
                        nm = f"x2_{l}_{t}"
                        ap = self.dbg(nm, [128, D])
                        fw.q_pool.dma(ap, xg[:, ti, :], reads=[xg.b], writes=[self.dbg_out[nm][1]])
            fw.barrier()

    def moe2(self, l):
        nc, fw, g = self.nc, self.fw, self.g
        V = nc.vector
        tiles = list(range(NT)) if l == 0 else list(range(2, NT))
        ntl = len(tiles)
        NA = 2 * ntl
        NB = (2 * ntl * 128 + 32 * 127 + 127) // 128
        classes = [0, 1] if l == 0 else [0]
        hs = nc.dram_tensor(f"hs{l}", [NB * 128, D], BF16, kind="Internal").ap()
        ys = nc.dram_tensor(f"ys{l}", [NB * 128, D], F32, kind="Internal").ap()
        blkE_d = nc.dram_tensor(f"blkE{l}", [1, NB], I32, kind="Internal").ap()
        hs_bs = [Buf() for _ in range(NA)]
        ys_bs = [Buf() for _ in range(NB)]
        blkE_b = Buf()
        dd = lambda fn, rd, wr_: fw.dve.op(fn, [x.b for x in rd], [x.b for x in wr_])
        with ExitStack() as pm:
            DESTi = self.tile(pm, "DESTi", [128, NA], I32)
            W12 = self.tile(pm, "W12", [128, NA], F32)
            WIDX = self.tile(pm, "WIDX", [128, NB], I32)
            G2B = {}
            for cls in classes:
                G2B[cls] = self.tile(pm, f"G2Bm{cls}", [128, D], F32)
            with ExitStack() as ph:
                tmpb = self.tile(ph, "bct3", [128, 128], F32)
                A2B, B2B = {}, {}
                for cls in classes:
                    A2B[cls] = self.tile(ph, f"A2B{cls}", [128, D], F32)
                    B2B[cls] = self.tile(ph, f"B2B{cls}", [128, D], F32)
                    self.bcast_tile(G2B[cls], lambda c: g["modT"][:, l, 40 + c, cls:cls + 1], tmpb)
                    self.bcast_tile(A2B[cls], lambda c: g["A2"][:, l, c, cls:cls + 1], tmpb, extra=[g["A2"].b])
                    self.bcast_tile(B2B[cls], lambda c: g["modT"][:, l, 24 + c, cls:cls + 1], tmpb)
                wr = self.tile(ph, "wr", [128, 8, 36], F32)
                rb = self.tile(ph, "rb", [128, 36], F32)
                mc = self.tile(ph, "mc", [128, 433], F32)
                Uf = self.tile(ph, "Uf", [128, 128], F32)
                Ub = self.tile(ph, "Ub", [128, 128], BF16)
                onesb = self.tile(ph, "onesb", [128, 128], BF16)
                fw.q_sync.dma(wr[:], self.rt_w[l].rearrange("(k p) n -> p k n", p=128), writes=[wr.b])
                fw.q_sync.dma(rb[:], self.rt_b[:, l, :], writes=[rb.b])
                fw.q_sync.dma(mc[:], self.mconst, writes=[mc.b])
                fw.q_sync.dma(Uf[:], self.umat, writes=[Uf.b])
                dd(lambda: V.tensor_copy(out=Ub[:], in_=Uf[:]), [Uf], [Ub])
                dd(lambda: V.memset(onesb[:], 1.0), [], [onesb])
                zt = self.tile(ph, "zt", [128, 4096], BF16)
                fw.pool.op(lambda: nc.gpsimd.memset(zt[:], 0.0), [], [zt.b])
                hz_b = []
                for r0 in range(0, NB * 128, 512):
                    nr = min(512, NB * 128 - r0)
                    hb_ = Buf("hz")
                    fw.q_pool.dma(hs[r0:r0 + nr, :].rearrange("(p k) d -> p (k d)", p=128), zt[:, 0:(nr // 128) * D], reads=[zt.b], writes=[hb_])
                    hz_b.append(hb_)
                h2tm = self.tile(ph, "h2tm", [128, ntl, D], BF16)
                OH = self.tile(ph, "OH", [128, NA, 32], BF16)
                RK = self.tile(ph, "RK", [128, NA], F32)
                run = self.tile(ph, "run", [128, 32], F32)
                dd(lambda: V.memset(run[:], 0.0), [], [run])
                xt = [self.tile(ph, f"mxt{i}", [128, D], F32) for i in range(2)]
                xn_l = [self.tile(ph, f"mxn{i}", [128, D], F32) for i in range(2)]
                xm_l = [self.tile(ph, f"mxm{i}", [128, D], F32) for i in range(2)]
                hTf_l = [self.tile(ph, f"mhTf{i}", [128, 8, 128], F32) for i in range(2)]
                ss_l = [self.tile(ph, f"mss{i}", [128, 1], F32) for i in range(2)]
                r_l = [{k: self.tile(ph, f"r{i}_" + k, [128, n], F32) for k, n in
                        dict(lg=36, gmax=1, ngmax=1, goh=4, ge=4, gsum=1, pen=4, em=32, m8=8, d=1, ed=1, p1=1, p2=1, t1=32, t2=32, rf=32).items()}
                       for i in range(2)]
                def tile_fn(ti, t):
                    cls = 1 if t < 2 else 0
                    xn, xm, hTf, ss, r_ = xn_l[ti % 2], xm_l[ti % 2], hTf_l[ti % 2], ss_l[ti % 2], r_l[ti % 2]
                    x_ = xt[ti % 2]
                    fw.q_sync.dma(x_[:], self.xres[t * 128:(t + 1) * 128, :], reads=[self.xres_b[t]], writes=[x_.b])
                    fw.act.op(lambda: nc.scalar.activation(out=xn[:], in_=x_[:], func=AF.Square, accum_out=ss[:, 0:1]), [x_.b], [xn.b, ss.b])
                    self.rstd_from_ss(ss, 1.0 / D, 1)
                    fw.act.op(lambda: nc.scalar.activation(out=xn[:], in_=x_[:], func=AF.Copy, scale=ss[:, 0:1]), [x_.b, ss.b], [xn.b])
                    fw.pool.op(lambda: nc.gpsimd.tensor_tensor(out=xm[:], in0=xn[:], in1=A2B[cls][:], op=ALU.mult), [xn.b, A2B[cls].b], [xm.b])
                    fw.pool.op(lambda: nc.gpsimd.tensor_tensor(out=h2tm[:, ti, :], in0=xm[:], in1=B2B[cls][:], op=ALU.add), [xm.b, B2B[cls].b], [h2tm.b])
                    for hb in range(2):
                        p = self.ps_get()
                        pv = p[:].rearrange("p (c q) -> p c q", c=4)
                        for c4 in range(4):
                            c = hb * 4 + c4
                            fw.pe.op(lambda: nc.tensor.transpose(out=pv[:, c4, :], in_=xn[:, c * 128:(c + 1) * 128], identity=g["identf"][:]),
                                     [xn.b, g["identf"].b], [p.b], inc=(c4 == 3))
                        for c4 in range(4):
                            c = hb * 4 + c4
                            if hb == 0:
                                fw.dve.op(lambda: V.tensor_scalar(out=hTf[:, c, :], in0=pv[:, c4, :], scalar1=g["A2"][:, l, c, cls:cls + 1],
                                                                  scalar2=g["modT"][:, l, 24 + c, cls:cls + 1], op0=ALU.mult, op1=ALU.add),
                                          [p.b, g["A2"].b, g["modT"].b], [hTf.b])
                            else:
                                fw.act.op(lambda: nc.scalar.activation(out=hTf[:, c, :], in_=pv[:, c4, :], func=AF.Identity,
                                                                       scale=g["A2"][:, l, c, cls:cls + 1], bias=g["modT"][:, l, 24 + c, cls:cls + 1]),
                                          [p.b, g["A2"].b, g["modT"].b], [hTf.b])
                        self.ps_put(p)
                    pr = self.ps_get()
                    for c in range(8):
                        fw.pe.op(lambda: nc.tensor.matmul(pr[:, 0:36], lhsT=hTf[:, c, :], rhs=wr[:, c, :], start=(c == 0), stop=(c == 7)),
                                 [hTf.b, wr.b], [pr.b], inc=(c == 7))
                    self.route(pr, rb, r_, None, ti, OH=OH, W12=W12)
                    self.ps_put(pr)

                def rank_fn(ti):
                    r_ = r_l[ti % 2]
                    for k in range(2):
                        a = 2 * ti + k
                        pk = self.ps_get()
                        fw.pe.op(lambda: nc.tensor.matmul(pk[:, 0:32], lhsT=Ub[:], rhs=OH[:, a, :], start=True, stop=True), [Ub.b, OH.b], [pk.b], inc=False)
                        fw.pe.op(lambda: nc.tensor.matmul(pk[:, 32:64], lhsT=onesb[:], rhs=OH[:, a, :], start=True, stop=True), [onesb.b, OH.b], [pk.b])
                        rf = r_["rf"]
                        dd(lambda: V.tensor_tensor(out=rf[:], in0=pk[:, 0:32], in1=run[:], op=ALU.add), [pk, run], [rf])
                        dd(lambda: V.tensor_tensor(out=rf[:], in0=rf[:], in1=OH[:, a, :], op=ALU.mult), [rf, OH], [rf])
                        dd(lambda: V.tensor_reduce(out=RK[:, a:a + 1], in_=rf[:], axis=AX.X, op=ALU.add), [rf], [RK])
                        dd(lambda: V.tensor_tensor(out=run[:], in0=pk[:, 32:64], in1=run[:], op=ALU.add), [pk, run], [run])
                        self.ps_put(pk)
                for ti0 in range(0, ntl, 2):
                    pair = list(range(ti0, min(ti0 + 2, ntl)))
                    ILV.run([(lambda ti=ti: tile_fn(ti, tiles[ti])) for ti in pair])
                    for ti in pair:
                        rank_fn(ti)
                cmpf = self.tile(ph, "cmp", [128, 5120], F32)
                v3 = lambda n_a, n_b: cmpf[:, 0:n_a * n_b].rearrange("p (a b) -> p a b", b=n_b)
                T_ = lambda nm, n: self.tile(ph, nm, [128, n], F32)
                nblk, exc, nbp, mm, pbx = T_("nblk", 32), T_("exc", 32), T_("nbp", 32), T_("mm", 32), T_("pbx", 32)
                sc = [T_(f"scan{i}", 32) for i in range(2)]
                blkf, dstf = T_("blkf", NB), T_("dstf", NA)
                selX, selM, selS, jf, ta_, tb_ = T_("selX", NA), T_("selM", NA), T_("selS", NA), T_("jf", NA), T_("ta_", NA), T_("tb_", NA)
                pend, pbase, m2k, lsk = T_("pend", 16), T_("pbase", 16), T_("m2k", 16), T_("lsk", 16)
                kidx, pbp, m2p, lsp, o_, q_, par_, int_ = (T_(n_, NB) for n_ in ("kidx", "pbp", "m2p", "lsp", "o_", "q_", "par_", "int_"))
                IOTA32, THR, IOTAB, PAR32, PARB, THR2, THR3, IOTA16 = (mc[:, 0:32], mc[:, 32:67], mc[:, 67:67 + NB], mc[:, 167:199],
                                                                     mc[:, 199:199 + NB], mc[:, 299:367], mc[:, 367:417], mc[:, 417:433])
                c3 = v3(32, 35)
                dd(lambda: V.tensor_tensor(out=c3, in0=run[:].unsqueeze(2).to_broadcast([128, 32, 35]),
                                           in1=THR.unsqueeze(1).to_broadcast([128, 32, 35]), op=ALU.is_gt), [run, mc], [cmpf])
                dd(lambda: V.tensor_reduce(out=nblk[:], in_=c3, axis=AX.X, op=ALU.add), [cmpf], [nblk])
                dd(lambda: V.tensor_copy(out=sc[0][:], in_=nblk[:]), [nblk], [sc[0]])
                cur = 0
                for sh in (1, 2, 4, 8, 16):
                    a_, b_ = sc[cur], sc[1 - cur]
                    dd(lambda: V.tensor_copy(out=b_[:, 0:sh], in_=a_[:, 0:sh]), [a_], [b_])
                    dd(lambda: V.tensor_tensor(out=b_[:, sh:32], in0=a_[:, sh:32], in1=a_[:, 0:32 - sh], op=ALU.add), [a_], [b_])
                    cur = 1 - cur
                inc_ = sc[cur]
                dd(lambda: V.tensor_tensor(out=exc[:], in0=inc_[:], in1=nblk[:], op=ALU.subtract), [inc_, nblk], [exc])
                pr2 = lambda tl: tl[:].rearrange("p (k s) -> p k s", s=2)
                dd(lambda: V.tensor_copy(out=pr2(nbp)[:, :, 0:1], in_=pr2(nblk)[:, :, 1:2]), [nblk], [nbp])
                dd(lambda: V.tensor_copy(out=pr2(nbp)[:, :, 1:2], in_=pr2(nblk)[:, :, 0:1]), [nblk], [nbp])
                dd(lambda: V.tensor_tensor(out=mm[:], in0=nblk[:], in1=nbp[:], op=ALU.min), [nblk, nbp], [mm])
                dd(lambda: V.tensor_scalar(out=pr2(pbx)[:, :, 0:1], in0=pr2(exc)[:, :, 0:1], scalar1=128.0, scalar2=None, op0=ALU.mult), [exc], [pbx])
                dd(lambda: V.tensor_scalar(out=pr2(pbx)[:, :, 1:2], in0=pr2(exc)[:, :, 0:1], scalar1=128.0, scalar2=None, op0=ALU.mult), [exc], [pbx])
                c4_ = v3(NA, 32)
                for (dst_, vec_, vb_) in ((selX, pbx[:], pbx.b), (selM, mm[:], mm.b), (selS, PAR32, mc.b)):
                    dd(lambda: V.tensor_tensor(out=c4_, in0=OH[:], in1=vec_.unsqueeze(1).to_broadcast([128, NA, 32]), op=ALU.mult), [OH, Tile(None, vb_)], [cmpf])
                    dd(lambda: V.tensor_reduce(out=dst_[:], in_=c4_, axis=AX.X, op=ALU.add), [cmpf], [dst_])
                c5_ = v3(NA, 68)
                dd(lambda: V.tensor_tensor(out=c5_, in0=RK[:].unsqueeze(2).to_broadcast([128, NA, 68]),
                                           in1=THR2.unsqueeze(1).to_broadcast([128, NA, 68]), op=ALU.is_ge), [RK, mc], [cmpf])
                dd(lambda: V.tensor_reduce(out=jf[:], in_=c5_, axis=AX.X, op=ALU.add), [cmpf], [jf])
                dd(lambda: V.scalar_tensor_tensor(out=ta_[:], in0=jf[:], scalar=2.0, in1=selS[:], op0=ALU.mult, op1=ALU.add), [jf, selS], [ta_])
                dd(lambda: V.tensor_tensor(out=tb_[:], in0=selM[:], in1=jf[:], op=ALU.add), [selM, jf], [tb_])
                dd(lambda: V.tensor_tensor(out=ta_[:], in0=ta_[:], in1=tb_[:], op=ALU.min), [ta_, tb_], [ta_])
                dd(lambda: V.tensor_tensor(out=ta_[:], in0=ta_[:], in1=jf[:], op=ALU.subtract), [ta_, jf], [ta_])
                dd(lambda: V.scalar_tensor_tensor(out=dstf[:], in0=ta_[:], scalar=128.0, in1=selX[:], op0=ALU.mult, op1=ALU.add), [ta_, selX], [dstf])
                dd(lambda: V.tensor_tensor(out=dstf[:], in0=dstf[:], in1=RK[:], op=ALU.add), [dstf, RK], [dstf])
                dd(lambda: V.tensor_copy(out=DESTi[:], in_=dstf[:]), [dstf], [DESTi])
                dd(lambda: V.tensor_copy(out=pend[:], in_=pr2(inc_)[:, :, 1]), [inc_], [pend])
                dd(lambda: V.tensor_copy(out=pbase[:], in_=pr2(exc)[:, :, 0]), [exc], [pbase])
                dd(lambda: V.tensor_scalar(out=m2k[:], in0=pr2(mm)[:, :, 0], scalar1=2.0, scalar2=None, op0=ALU.mult), [mm], [m2k])
                dd(lambda: V.tensor_tensor(out=lsk[:], in0=pr2(nblk)[:, :, 1], in1=pr2(nblk)[:, :, 0], op=ALU.is_gt), [nblk], [lsk])
                c6_ = v3(NB, 16)
                dd(lambda: V.tensor_tensor(out=c6_, in0=pend[:].unsqueeze(1).to_broadcast([128, NB, 16]),
                                           in1=IOTAB.unsqueeze(2).to_broadcast([128, NB, 16]), op=ALU.is_le), [pend, mc], [cmpf])
                dd(lambda: V.tensor_reduce(out=kidx[:], in_=c6_, axis=AX.X, op=ALU.add), [cmpf], [kidx])
                dd(lambda: V.tensor_scalar(out=kidx[:], in0=kidx[:], scalar1=15.0, scalar2=None, op0=ALU.min), [kidx], [kidx])
                ohk = self.tile(ph, "ohk", [128, NB, 16], F32)
                dd(lambda: V.tensor_tensor(out=ohk[:], in0=kidx[:].unsqueeze(2).to_broadcast([128, NB, 16]),
                                           in1=IOTA16.unsqueeze(1).to_broadcast([128, NB, 16]), op=ALU.is_equal), [kidx, mc], [ohk])
                for (dst_, vec_) in ((pbp, pbase), (m2p, m2k), (lsp, lsk)):
                    dd(lambda: V.tensor_tensor(out=c6_, in0=ohk[:], in1=vec_[:].unsqueeze(1).to_broadcast([128, NB, 16]), op=ALU.mult), [ohk, vec_], [cmpf])
                    dd(lambda: V.tensor_reduce(out=dst_[:], in_=c6_, axis=AX.X, op=ALU.add), [cmpf], [dst_])
                dd(lambda: V.tensor_tensor(out=o_[:], in0=IOTAB, in1=pbp[:], op=ALU.subtract), [mc, pbp], [o_])
                c7_ = v3(NB, 50)
                dd(lambda: V.tensor_tensor(out=c7_, in0=o_[:].unsqueeze(2).to_broadcast([128, NB, 50]),
                                           in1=THR3.unsqueeze(1).to_broadcast([128, NB, 50]), op=ALU.is_ge), [o_, mc], [cmpf])
                dd(lambda: V.tensor_reduce(out=q_[:], in_=c7_, axis=AX.X, op=ALU.add), [cmpf], [q_])
                dd(lambda: V.scalar_tensor_tensor(out=par_[:], in0=q_[:], scalar=-2.0, in1=o_[:], op0=ALU.mult, op1=ALU.add), [q_, o_], [par_])
                dd(lambda: V.tensor_tensor(out=int_[:], in0=o_[:], in1=m2p[:], op=ALU.is_lt), [o_, m2p], [int_])
                dd(lambda: V.tensor_tensor(out=par_[:], in0=par_[:], in1=lsp[:], op=ALU.subtract), [par_, lsp], [par_])
                dd(lambda: V.tensor_tensor(out=par_[:], in0=par_[:], in1=int_[:], op=ALU.mult), [par_, int_], [par_])
                dd(lambda: V.tensor_tensor(out=par_[:], in0=par_[:], in1=lsp[:], op=ALU.add), [par_, lsp], [par_])
                dd(lambda: V.scalar_tensor_tensor(out=blkf[:], in0=kidx[:], scalar=2.0, in1=par_[:], op0=ALU.mult, op1=ALU.add), [kidx, par_], [blkf])
                pix = self.tile(ph, "pix", [128, 1], F32)
                fw.q_sync.dma(pix[:], self.pidx, writes=[pix.b])
                dd(lambda: V.tensor_scalar(out=blkf[:], in0=blkf[:], scalar1=128.0, scalar2=pix[:, 0:1], op0=ALU.mult, op1=ALU.add), [blkf, pix], [blkf])
                if l == 1:
                    dd(lambda: V.tensor_scalar(out=blkf[:], in0=blkf[:], scalar1=4096.0, scalar2=None, op0=ALU.add), [blkf], [blkf])
                same2 = self.tile(ph, "same2", [128, NB], F32)
                dd(lambda: V.tensor_tensor(out=same2[:, 2:NB], in0=blkf[:, 2:NB], in1=blkf[:, 0:NB - 2], op=ALU.is_equal), [blkf], [same2])
                dd(lambda: V.tensor_scalar(out=same2[:, 2:NB], in0=same2[:, 2:NB], scalar1=1.0e6, scalar2=None, op0=ALU.mult), [same2], [same2])
                dd(lambda: V.tensor_tensor(out=blkf[:, 2:NB], in0=blkf[:, 2:NB], in1=same2[:, 2:NB], op=ALU.add), [blkf, same2], [blkf])
                dd(lambda: V.tensor_copy(out=WIDX[:], in_=blkf[:]), [blkf], [WIDX])
                if self.debug:
                    self.dbg_dump_bf(ph, f"dst{l}", dstf[:, 0:8], [128, 8], dstf.b)
                    self.dbg_dump_bf(ph, f"blk{l}", blkf[:, 0:NB], [128, NB], blkf.b)
                    self.dbg_dump_bf(ph, f"cnt{l}", run[:, 0:32], [128, 32], run.b)
                if os.environ.get("MOE_PROBE") == "1":
                    def tryv(nm, f):
                        try:
                            f(); print("PROBE ok", nm, flush=True)
                        except Exception as e:
                            print("PROBE fail", nm, repr(e)[:120], flush=True)
                    tryv("base", lambda: nc.gpsimd.indirect_dma_start(out=hs[:, :], out_offset=bass.IndirectOffsetOnAxis(ap=DESTi[:, 0:1], axis=0),
                                                                      in_=h2tm[:, 0, :], in_offset=None, bounds_check=NB * 128 - 1, oob_is_err=False))
                    tryv("xn_f32_ys", lambda: nc.gpsimd.indirect_dma_start(out=ys[:, :], out_offset=bass.IndirectOffsetOnAxis(ap=DESTi[:, 0:1], axis=0),
                                                                      in_=xn[:, :], in_offset=None, bounds_check=NB * 128 - 1, oob_is_err=False))
                    tryv("blki_idx", lambda: nc.gpsimd.indirect_dma_start(out=hs[:, :], out_offset=bass.IndirectOffsetOnAxis(ap=blki[:, 0:1], axis=0),
                                                                      in_=h2tm[:, 0, :], in_offset=None, bounds_check=NB * 128 - 1, oob_is_err=False))
                    tryv("gather", lambda: nc.gpsimd.indirect_dma_start(out=xn[:, :], out_offset=None, in_=ys[:, :],
                                                                      in_offset=bass.IndirectOffsetOnAxis(ap=DESTi[:, 0:1], axis=0), bounds_check=NB * 128 - 1, oob_is_err=False))
                    tryv("plain", lambda: nc.gpsimd.dma_start(out=ys[0:128, :], in_=xn[:, :]))
                for a in range(NA):
                    if os.environ.get("MOE_PROBE") == "1":
                        print("PROBE scatter a", a, flush=True)
                    fw.q_pool.dma_fn(lambda: nc.gpsimd.indirect_dma_start(
                        out=hs[:, :], out_offset=bass.IndirectOffsetOnAxis(ap=DESTi[:, a:a + 1], axis=0),
                        in_=h2tm[:, a // 2, :], in_offset=None),
                        reads=[h2tm.b, DESTi.b] + hz_b, writes=[hs_bs[a]])
                fw.barrier()
            with ExitStack() as ph:
                stg = {k: [self.tile(ph, f"bs{k}{i}", [128, 4096], F32) for i in range(2)] for k in "gud"}
                wgt = {k: [self.tile(ph, f"bw{k}{i}", [128, 4096], BF16) for i in range(2)] for k in "gud"}
                xb = [self.tile(ph, f"xb{i}", [128, D], BF16) for i in range(4)]
                xbT = [self.tile(ph, f"xbT{i}", [128, 8, 128], BF16) for i in range(2)]
                sg_l = [self.tile(ph, f"bsg{i}", [128, 512], F32) for i in range(2)]
                hid_l = [self.tile(ph, f"bhid{i}", [128, 512], BF16) for i in range(2)]
                hidT_l = [self.tile(ph, f"bhidT{i}", [128, 4, 128], BF16) for i in range(2)]
                ysb = [self.tile(ph, f"ysb{i}", [128, D], F32) for i in range(2)]
                srcs = dict(g=self.ex_gate, u=self.ex_up, d=self.ex_down)

                def gathers(i):
                    b2 = i % 2
                    for k in "gud":
                        fw.q_pool.dma_fn(lambda: nc.gpsimd.indirect_dma_start(
                            out=stg[k][b2][:, :], out_offset=None, in_=srcs[k].rearrange("l r n -> (l r) n"),
                            in_offset=bass.IndirectOffsetOnAxis(ap=WIDX[:, i:i + 1], axis=0),
                            bounds_check=self.bc_reg, oob_is_err=False),
                            reads=[WIDX.b], writes=[stg[k][b2].b])

                def casts(i):
                    b2 = i % 2
                    fw.dve.op(lambda: V.tensor_copy(out=wgt["g"][b2][:], in_=stg["g"][b2][:]), [stg["g"][b2].b], [wgt["g"][b2].b])
                    fw.act.op(lambda: nc.scalar.copy(out=wgt["u"][b2][:], in_=stg["u"][b2][:]), [stg["u"][b2].b], [wgt["u"][b2].b])
                    fw.dve.op(lambda: V.tensor_copy(out=wgt["d"][b2][:, 0:2048], in_=stg["d"][b2][:, 0:2048]), [stg["d"][b2].b], [wgt["d"][b2].b])
                    fw.act.op(lambda: nc.scalar.copy(out=wgt["d"][b2][:, 2048:4096], in_=stg["d"][b2][:, 2048:4096]), [stg["d"][b2].b], [wgt["d"][b2].b])

                def xload(i):
                    fw.q_sync.dma(xb[i % 4][:], hs[i * 128:(i + 1) * 128, :], reads=hs_bs, writes=[xb[i % 4].b])

                def block_fn(i):
                    b2 = i % 2
                    sg, hid, hidT = sg_l[b2], hid_l[b2], hidT_l[b2]
                    wg_ = wgt["g"][b2][:].rearrange("p (k n) -> p k n", k=8)
                    wu_ = wgt["u"][b2][:].rearrange("p (k n) -> p k n", k=8)
                    wd_ = wgt["d"][b2][:].rearrange("p (k n) -> p k n", k=4)
                    p = self.ps_get()
                    pv = p[:].bitcast(BF16).rearrange("p (c q) -> p c q", c=8)
                    for c in range(8):
                        fw.pe.op(lambda: nc.tensor.transpose(out=pv[:, c, :], in_=xb[i % 4][:, c * 128:(c + 1) * 128], identity=g["identb"][:]),
                                 [xb[i % 4].b, g["identb"].b], [p.b], inc=(c == 7))
                    dd(lambda: V.tensor_copy(out=xbT[b2][:], in_=pv), [p], [xbT[b2]])
                    self.ps_put(p)
                    pg = self.ps_get()
                    pu = self.ps_get()
                    for (pp, w_, wb_) in ((pg, wg_, wgt["g"][b2].b), (pu, wu_, wgt["u"][b2].b)):
                        for k in range(8):
                            fw.pe.op(lambda: nc.tensor.matmul(pp[:], lhsT=xbT[b2][:, k, :], rhs=w_[:, k, :], start=(k == 0), stop=(k == 7)),
                                     [xbT[b2].b, wb_], [pp.b], inc=(k == 7))
                    fw.act.op(lambda: nc.scalar.activation(out=sg[:], in_=pg[:], func=AF.Silu), [pg.b], [sg.b])
                    dd(lambda: V.tensor_tensor(out=hid[:], in0=pu[:], in1=sg[:], op=ALU.mult), [pu, sg], [hid])
                    self.ps_put(pg)
                    self.ps_put(pu)
                    p = self.ps_get()
                    pv = p[:].bitcast(BF16).rearrange("p (c q) -> p c q", c=8)
                    for c in range(4):
                        fw.pe.op(lambda: nc.tensor.transpose(out=pv[:, c, :], in_=hid[:, c * 128:(c + 1) * 128], identity=g["identb"][:]),
                                 [hid.b, g["identb"].b], [p.b], inc=(c == 3))
                    dd(lambda: V.tensor_copy(out=hidT[:], in_=pv[:, 0:4, :]), [p], [hidT])
                    self.ps_put(p)
                    y_ = ysb[b2]
                    pds = []
                    for half in range(2):
                        pd = self.ps_get()
                        pds.append(pd)
                        for f in range(4):
                            fw.pe.op(lambda: nc.tensor.matmul(pd[:], lhsT=hidT[:, f, :], rhs=wd_[:, f, half * 512:(half + 1) * 512],
                                                              start=(f == 0), stop=(f == 3)), [hidT.b, wgt["d"][b2].b], [pd.b], inc=(f == 3))
                    if i + 2 < NB:
                        casts(i + 2)
                    fw.act.op(lambda: nc.scalar.copy(out=y_[:, 0:512], in_=pds[0][:]), [pds[0].b], [y_.b])
                    dd(lambda: V.tensor_copy(out=y_[:, 512:1024], in_=pds[1][:]), [pds[1]], [y_])
                    self.ps_put(pds[0])
                    self.ps_put(pds[1])
                    fw.q_sync.dma(ys[i * 128:(i + 1) * 128, :], y_[:], reads=[y_.b], writes=[ys_bs[i]])

                gathers(0)
                gathers(1)
                for j in range(2):
                    xload(j)
                casts(0)
                casts(1)
                for i0_ in range(0, NB, 2):
                    for j in (i0_ + 2, i0_ + 3):
                        if j < NB:
                            gathers(j)
                            xload(j)
                    ILV.run([(lambda i=i: block_fn(i)) for i in (i0_, i0_ + 1) if i < NB])
                fw.barrier()
            with ExitStack() as ph:
                xt = [self.tile(ph, f"cxt{i}", [128, D], F32) for i in range(3)]
                y1 = [self.tile(ph, f"cy1{i}", [128, D], F32) for i in range(3)]
                y2 = [self.tile(ph, f"cy2{i}", [128, D], F32) for i in range(3)]
                def comb_fn(ti, t):
                    cls = 1 if t < 2 else 0
                    x_, a1, a2 = xt[ti % 3], y1[ti % 3], y2[ti % 3]
                    fw.q_sync.dma(x_[:], self.xres[t * 128:(t + 1) * 128, :], reads=[self.xres_b[t]], writes=[x_.b])
                    for k, yy in ((0, a1), (1, a2)):
                        a = 2 * ti + k
                        fw.q_pool.dma_fn(lambda: nc.gpsimd.indirect_dma_start(
                            out=yy[:, :], out_offset=None, in_=ys[:, :],
                            in_offset=bass.IndirectOffsetOnAxis(ap=DESTi[:, a:a + 1], axis=0)),
                            reads=ys_bs + [DESTi.b], writes=[yy.b])
                    dd(lambda: V.tensor_scalar(out=a1[:], in0=a1[:], scalar1=W12[:, 2 * ti:2 * ti + 1], scalar2=None, op0=ALU.mult), [a1, W12], [a1])
                    dd(lambda: V.scalar_tensor_tensor(out=a1[:], in0=a2[:], scalar=W12[:, 2 * ti + 1:2 * ti + 2], in1=a1[:], op0=ALU.mult, op1=ALU.add),
                       [a2, W12, a1], [a1])
                    dd(lambda: V.tensor_tensor(out=a1[:], in0=a1[:], in1=G2B[cls][:], op=ALU.mult), [a1, G2B[cls]], [a1])
                    dd(lambda: V.tensor_tensor(out=x_[:], in0=x_[:], in1=a1[:], op=ALU.add), [x_, a1], [x_])
                    if l == 0:
                        fw.q_sync.dma(self.xres[t * 128:(t + 1) * 128, :], x_[:], reads=[x_.b], writes=[self.xres_b[t]])
                    else:
                        fw.q_sync.dma(self.y_out[(t - 2) * 128:(t - 1) * 128, :], x_[:], reads=[x_.b], writes=[self.y_b[t]])
                    if self.debug and t in (0, 2, NT - 1):
                        nm = f"x2_{l}_{t}"
                        ap = self.dbg(nm, [128, D])
                        fw.q_sync.dma(ap, x_[:], reads=[x_.b], writes=[self.dbg_out[nm][1]])

                for ti0 in range(0, ntl, 3):
                    pair = list(range(ti0, min(ti0 + 3, ntl)))
                    ILV.run([(lambda ti=ti: comb_fn(ti, tiles[ti])) for ti in pair])
                fw.barrier()

    def route(self, pr, rb, r_, Wt, ti, OH=None, W12=None):
        nc, fw = self.nc, self.fw
        V = nc.vector
        d = lambda fn, rd, wr_: fw.dve.op(fn, [x.b for x in rd], [x.b for x in wr_])
        lg, gmax, ngmax, goh, ge, gsum, pen, em, m8 = (r_[k] for k in ("lg", "gmax", "ngmax", "goh", "ge", "gsum", "pen", "em", "m8"))
        dd, ed, p1, p2, t1, t2 = (r_[k] for k in ("d", "ed", "p1", "p2", "t1", "t2"))
        d(lambda: V.tensor_tensor(out=lg[:], in0=pr[:, 0:36], in1=rb[:], op=ALU.add), [pr, rb], [lg])
        d(lambda: V.tensor_reduce(out=gmax[:], in_=lg[:, 0:4], axis=AX.X, op=ALU.max), [lg], [gmax])
        d(lambda: V.tensor_scalar(out=ngmax[:], in0=gmax[:], scalar1=-1.0, scalar2=None, op0=ALU.mult), [gmax], [ngmax])
        d(lambda: V.tensor_scalar(out=pen[:], in0=lg[:, 0:4], scalar1=gmax[:, 0:1], scalar2=None, op0=ALU.is_ge), [lg, gmax], [pen])
        d(lambda: V.tensor_scalar(out=pen[:], in0=pen[:], scalar1=1e30, scalar2=-1e30, op0=ALU.mult, op1=ALU.add), [pen], [pen])
        fw.act.op(lambda: nc.scalar.activation(out=ge[:], in_=lg[:, 0:4], func=AF.Exp, bias=ngmax[:, 0:1], accum_out=gsum[:, 0:1]),
                  [lg.b, ngmax.b], [ge.b, gsum.b])
        d(lambda: V.tensor_tensor(out=em[:].rearrange("p (a b) -> p a b", a=4), in0=lg[:, 4:36].rearrange("p (a b) -> p a b", a=4),
                                  in1=pen[:].unsqueeze(2).to_broadcast([128, 4, 8]), op=ALU.add), [lg, pen], [em])
        d(lambda: V.max(out=m8[:], in_=em[:]), [em], [m8])
        d(lambda: V.tensor_tensor(out=dd[:], in0=m8[:, 1:2], in1=m8[:, 0:1], op=ALU.subtract), [m8], [dd])
        fw.act.op(lambda: nc.scalar.activation(out=ed[:], in_=dd[:], func=AF.Exp), [dd.b], [ed.b])
        d(lambda: V.tensor_scalar(out=p1[:], in0=ed[:], scalar1=1.0, scalar2=None, op0=ALU.add), [ed], [p1])
        d(lambda: V.tensor_tensor(out=p1[:], in0=p1[:], in1=gsum[:], op=ALU.mult), [p1, gsum], [p1])
        d(lambda: V.reciprocal(out=p1[:], in_=p1[:]), [p1], [p1])
        d(lambda: V.tensor_tensor(out=p2[:], in0=p1[:], in1=ed[:], op=ALU.mult), [p1, ed], [p2])
        if OH is not None:
            for k in range(2):
                a = 2 * ti + k
                d(lambda: V.tensor_scalar(out=OH[:, a, :], in0=em[:], scalar1=m8[:, k:k + 1], scalar2=None, op0=ALU.is_equal), [em, m8], [OH])
                pk_ = p1 if k == 0 else p2
                d(lambda: V.tensor_copy(out=W12[:, a:a + 1], in_=pk_[:]), [pk_], [W12])
            return
        d(lambda: V.tensor_scalar(out=t1[:], in0=em[:], scalar1=m8[:, 0:1], scalar2=p1[:, 0:1], op0=ALU.is_equal, op1=ALU.mult), [em, m8, p1], [t1])
        d(lambda: V.tensor_scalar(out=t2[:], in0=em[:], scalar1=m8[:, 1:2], scalar2=p2[:, 0:1], op0=ALU.is_equal, op1=ALU.mult), [em, m8, p2], [t2])
        d(lambda: V.tensor_tensor(out=Wt[:, ti, :], in0=t1[:], in1=t2[:], op=ALU.add), [t1, t2], [Wt])

    def layer1_mixer(self):
        nc, fw, g = self.nc, self.fw, self.g
        l = 1
        PADU = 16
        with ExitStack() as ph:
            oT = self.tile(ph, "oT1", [128, 4, T], BF16)
            uT = self.tile(ph, "uT", [128, 4, S + 2 * PADU], BF16)
            pattn = ExitStack()
            qT = self.tile(pattn, "qT1", [128, NT, 4, 128], BF16)
            kT = self.tile(pattn, "kT1", [128, 2, T], BF16)
            Vp = self.tile(pattn, "Vp1", [128, NT, 200], BF16)
            self.v_init(Vp)
            fw.pool.op(lambda: nc.gpsimd.memset(kT[:], 0.0), [], [kT.b])
            fw.pool.op(lambda: nc.gpsimd.memset(uT[:], 0.0), [], [uT.b])
            with ExitStack() as p1:
                win = self.tile(p1, "win1", [128, 8, 1280], BF16)
                gqk = self.tile(p1, "gqk1", [128, 640], F32)
                g["cos"] = self.tile(p1, "cos1", [128, 32, 32], F32)
                g["sin"] = self.tile(p1, "sin1", [128, 32, 32], F32)
                fw.q_sync.dma(gqk[:], self.gqk_c, writes=[gqk.b])
                fw.q_sync.dma(g["cos"][:], self.cosT, writes=[g["cos"].b])
                fw.q_sync.dma(g["sin"][:], self.sinT, writes=[g["sin"].b])
                fw.dve.op(lambda: nc.vector.tensor_scalar(out=gqk[:, 0:512], in0=gqk[:, 0:512], scalar1=0.125, scalar2=None, op0=ALU.mult),
                          [gqk.b], [gqk.b])
                with ExitStack() as pw:
                    stg = [self.tile(pw, f"wstg1{i}", [128, 4096], F32) for i in range(2)]
                    self.stg_i = 0
                    for n in range(3):
                        w_ = 512 if n < 2 else 256
                        self.load_cast_w(stg, win, slice(n * 512, n * 512 + w_), self.w_in_cd[:, n * 512:n * 512 + w_], 8, w_, n)
                    fw.barrier()
                ssl_ = [self.tile(p1, f"ss1{i}", [128, 1], F32) for i in range(2)]
                xnl_ = [self.tile(p1, f"xn1{i}", [128, D], BF16) for i in range(2)]
                scr_l = [dict(ssl=[ssl_[i]], xnl=[xnl_[i]], sq=self.tile(p1, f"sq1{i}", [128, 640], F32),
                              ssq=self.tile(p1, f"ssq1{i}", [128, 10], F32), qn=self.tile(p1, f"qn1{i}", [128, 640], F32),
                              qr=self.tile(p1, f"qr1{i}", [128, 640], BF16), rt=self.tile(p1, f"rt1{i}", [128, 2, 320], F32)) for i in range(2)]
                xt = [self.tile(p1, f"xt1{i}", [128, D], F32) for i in range(2)]
                hT = self.tile(p1, "hT1", [128, 8, 512], BF16)
                qkv = [self.tile(p1, f"qkv1{i}", [128, 768], F32) for i in range(2)]
                chunks = [(0, 2)] + [(2 + 4 * i, 4) for i in range(8)]
                for ci, (t0, ntl) in enumerate(chunks):
                    h = hT
                    ntok = ntl * 128
                    is_ctx = t0 < 2
                    cls = 1 if is_ctx else 0
                    def norm_fn(tl):
                        t = t0 + tl
                        x_ = xt[tl % 2]
                        fw.q_sync.dma(x_[:], self.xres[t * 128:(t + 1) * 128, :], reads=[self.xres_b[t]], writes=[x_.b])
                        self.norm_tile_to_hT(x_, h, tl * 128, l, 1, cls, scr_l[tl % 2])

                    def qkv_fn(tl):
                        t = t0 + tl
                        qk_ = qkv[tl % 2]
                        for (c0, w_) in ((0, 512), (512, 256)):
                            if is_ctx and c0 == 0:
                                continue
                            p = self.ps_get()
                            for k in range(8):
                                fw.pe.op(lambda: nc.tensor.matmul(p[:, 0:w_], lhsT=h[:, k, tl * 128:(tl + 1) * 128], rhs=win[:, k, c0:c0 + w_],
                                                                  start=(k == 0), stop=(k == 7)), [h.b, win.b], [p.b], inc=(k == 7))
                            if c0 == 0:
                                fw.act.op(lambda: nc.scalar.copy(out=qk_[:, 0:512].rearrange("p (j a d) -> p a j d", a=2, d=64),
                                                                 in_=p[:, 0:512].rearrange("p (a j d) -> p a j d", a=2, j=4)), [p.b], [qk_.b])
                            else:
                                fw.act.op(lambda: nc.scalar.copy(out=qk_[:, c0:c0 + w_], in_=p[:, 0:w_]), [p.b], [qk_.b])
                            self.ps_put(p)
                        self.qk_post(qk_, t, gqk, scr_l[tl % 2], qT, kT, not is_ctx, None if is_ctx else t - 2)
                        self.v_fill(qk_, t, Vp)

                    for tl0 in range(0, ntl, 2):
                        ILV.run([(lambda tl=tl: norm_fn(tl)) for tl in range(tl0, min(tl0 + 2, ntl))])
                    for tl0 in range(0, ntl, 2):
                        ILV.run([(lambda tl=tl: qkv_fn(tl)) for tl in range(tl0, min(tl0 + 2, ntl))])
                    if not is_ctx:
                        tok0 = (t0 - 2) * 128
                        for j in range(4):
                            pu = self.ps_get()
                            for k in range(8):
                                fw.pe.op(lambda: nc.tensor.matmul(pu[:, 0:ntok], lhsT=win[:, k, 768 + j * 128:768 + (j + 1) * 128], rhs=h[:, k, 0:ntok],
                                                                  start=(k == 0), stop=(k == 7)), [win.b, h.b], [pu.b], inc=(k == 7))
                            fw.dve.op(lambda: nc.vector.tensor_copy(out=uT[:, j, PADU + tok0:PADU + tok0 + ntok], in_=pu[:, 0:ntok]), [pu.b], [uT.b])
                            self.ps_put(pu)
                fw.barrier()
            if self.stop == "l1p1":
                pattn.close()
                return
            with ExitStack() as p2:
                wm = self.tile(p2, "wm", [128, 2, 512], BF16)
                sk = self.tile(p2, "sk", [128, 2, 512], F32)
                with ExitStack() as pw:
                    wmf = self.tile(pw, "wmf", [128, 2, 512], F32)
                    fw.q_sync.dma(wmf[:], self.wmask, writes=[wmf.b])
                    fw.dve.op(lambda: nc.vector.tensor_copy(out=wm[:], in_=wmf[:]), [wmf.b], [wm.b])
                    fw.q_sync.dma(sk[:], self.sink_rep, writes=[sk.b])
                    fw.act.op(lambda: nc.scalar.activation(out=sk[:], in_=sk[:], func=AF.Exp), [sk.b], [sk.b])
                    fw.barrier()
                scr = dict(pexp=[self.tile(p2, f"pexp1{i}", [128, 1024], BF16) for i in range(4)], pi=0,
                           rec=[self.tile(p2, f"rec1{i}", [128, 512], F32) for i in range(2)],
                           posb=[[self.tile(p2, f"posb1{i}{k}", [128, 512], F32) for k in range(2)] for i in range(2)])
                blocks = []
                for t in range(2, NT):
                    kts = [(0, None), (1, None)]
                    if t > 2:
                        kts.append((t - 1, 0))
                    kts.append((t, None))
                    if t < NT - 1:
                        kts.append((t + 1, 1))
                    for kv in range(2):
                        blocks.append((kv, t, kts))
                self.attention(blocks, qT, kT, Vp, oT, scr, masks=wm, sinkrow=sk)
                fw.barrier()
            pattn.close()
            if self.debug:
                self.dbg_dump_bf(ph, "oT1", oT[:, :, 256:384], [128, 4, 128], oT.b)
                self.dbg_dump_bf(ph, "oT1b", oT[:, :, 640:768], [128, 4, 128], oT.b)
            dT = self.tile(ph, "dT", [128, 4, T], BF16)
            with ExitStack() as p3:
                pwf = self.tile(p3, "pwf", [128, 4, 128], F32)
                pwb = self.tile(p3, "pwb", [128, 4, 128], BF16)
                psc = self.tile(p3, "psc", [128, 4], F32)
                pfx = self.tile(p3, "pfx", [128, 4, 32], F32)
                fw.q_sync.dma(pwf[:], self.pool_w.rearrange("g c d -> c g d"), writes=[pwf.b])
                fw.q_sync.dma(psc[:], self.pool_scT, writes=[psc.b])
                fw.q_sync.dma(pfx[:], self.poolfix, writes=[pfx.b])
                fw.dve.op(lambda: nc.vector.tensor_copy(out=pwb[:], in_=pwf[:]), [pwf.b], [pwb.b])
                ta = [self.tile(p3, f"pta{i}", [128, 528], F32) for i in range(2)]
                pp_ = [self.tile(p3, f"ppb{i}", [128, 512], BF16) for i in range(2)]
                cnt = 0
                for ci in range(8):
                    tok0 = ci * 512
                    for j, w in enumerate((2, 4, 8, 16)):
                        base = PADU + tok0 - w // 2
                        ln = 512 + w - 1
                        cur = uT[:, j, base:base + ln]
                        curb = uT.b
                        step = 1
                        k = 0
                        while step < w:
                            dst = ta[k % 2]
                            e_, ee = (fw.dve, nc.vector) if (cnt % 2 == 0) else (fw.pool, nc.gpsimd)
                            cnt += 1
                            cc, cb_ = cur, curb
                            e_.op(lambda: ee.tensor_tensor(out=dst[:, 0:ln - step], in0=cc[:, 0:ln - step], in1=cc[:, step:ln], op=ALU.add), [cb_], [dst.b])
                            ln -= step
                            step *= 2
                            cur, curb = dst[:, 0:ln], dst.b
                            k += 1
                        pb_ = pp_[j % 2]
                        uc = uT[:, j, PADU + tok0:PADU + tok0 + 512]
                        sdst = ta[k % 2]
                        fw.dve.op(lambda: nc.vector.scalar_tensor_tensor(out=pb_[:], in0=cur[:, 0:512], scalar=1.0 / w, in1=uc, op0=ALU.mult, op1=ALU.subtract),
                                  [curb, uT.b], [pb_.b])
                        for (cond, lo, fo) in ((ci == 0, 0, 0), (ci == 7, 496, 16)):
                            if cond:
                                fw.dve.op(lambda: nc.vector.tensor_tensor(out=sdst[:, 0:16], in0=cur[:, lo:lo + 16], in1=pfx[:, j, fo:fo + 16], op=ALU.mult),
                                          [curb, pfx.b], [sdst.b])
                                fw.dve.op(lambda: nc.vector.tensor_tensor(out=pb_[:, lo:lo + 16], in0=sdst[:, 0:16], in1=uc[:, lo:lo + 16], op=ALU.subtract),
                                          [sdst.b, uT.b], [pb_.b])
                        py = self.ps_get()
                        fw.pe.op(lambda: nc.tensor.matmul(py[:], lhsT=pwb[:, j, :], rhs=pb_[:], start=True, stop=True), [pwb.b, pb_.b], [py.b])
                        fw.act.op(lambda: nc.scalar.activation(out=dT[:, j, NCTX + tok0:NCTX + tok0 + 512], in_=py[:], func=AF.Copy, scale=psc[:, j:j + 1]),
                                  [py.b, psc.b], [dT.b])
                        self.ps_put(py)
                fw.barrier()
            if self.debug:
                self.dbg_dump_bf(ph, "dT", dT[:, :, 256:384], [128, 4, 128], dT.b)
                self.dbg_dump_bf(ph, "dTe", dT[:, :, T - 128:T], [128, 4, 128], dT.b)
            if self.stop == "l1p3":
                return
            with ExitStack() as p4:
                self.out_proj(1, lambda c: (oT, c) if c < 4 else (dT, c - 4), self.w_out_cd, list(range(2, NT)), p4)
                fw.barrier()


def _rope_tables():
    rows = S // 64
    row = np.repeat(np.arange(rows, dtype=np.float32), 64)
    col = np.tile(np.arange(64, dtype=np.float32), rows)
    inv = (10000.0 ** (-np.arange(16, dtype=np.float32) / 16)).astype(np.float32)
    ang = np.concatenate([row[:, None] * inv, col[:, None] * inv], axis=-1).astype(np.float32)
    return np.cos(ang).astype(np.float32), np.sin(ang).astype(np.float32)


def _fm(v, chunks):
    return np.ascontiguousarray(np.asarray(v, np.float32).reshape(chunks, 128).T)


def make_in_maps(inp, cores):
    f = lambda a: np.ascontiguousarray(np.asarray(a, dtype=np.float32))
    cos, sin = _rope_tables()
    cosT = np.ascontiguousarray(cos.reshape(32, 128, 32).transpose(1, 0, 2))
    sinT = np.ascontiguousarray(sin.reshape(32, 128, 32).transpose(1, 0, 2))
    r = np.arange(128)
    mprev = (r[None, :] <= r[:, None]).astype(np.float32)
    mnext = (r[:, None] <= r[None, :]).astype(np.float32)
    wmask = np.stack([np.tile(mprev, (1, 4)), np.tile(mnext, (1, 4))], axis=1)
    shared = {
        "mod_w": f(inp["mod_w"]),
        "mod_bT": np.ascontiguousarray(f(inp["mod_b"]).reshape(2, 48, 128).transpose(2, 0, 1)),
        "ln1gT": np.ascontiguousarray(f(inp["ln1_g"]).reshape(2, 8, 128).transpose(2, 0, 1)),
        "ln2gT": np.ascontiguousarray(f(inp["ln2_g"]).reshape(2, 8, 128).transpose(2, 0, 1)),
        "w_in_ab": f(inp["w_in_ab"][0]), "w_out_ab": f(inp["w_out_ab"][0]),
        "gqk_a": np.ascontiguousarray(np.broadcast_to(np.concatenate([np.tile(f(inp["q_norm_a"][0]), 8), np.tile(f(inp["k_norm_a"][0]), 2)])[None, :], (128, 640))),
        "convwT": np.ascontiguousarray(f(inp["conv_w"][0]).reshape(31, 4, 128).transpose(2, 1, 0)),
        "convv": np.ascontiguousarray(np.stack([_fm(inp["conv_b"][0], 4), _fm(inp["conv_ln_g"][0], 4), _fm(inp["conv_ln_b"][0], 4)], axis=1)),
        "w_in_cd": f(inp["w_in_cd"][0]), "w_out_cd": f(inp["w_out_cd"][0]),
        "gqk_c": np.ascontiguousarray(np.broadcast_to(np.concatenate([np.tile(f(inp["q_norm_c"][0]), 8), np.tile(f(inp["k_norm_c"][0]), 2)])[None, :], (128, 640))),
        "sink_rep": np.ascontiguousarray(np.broadcast_to(np.repeat(f(inp["sink_c"][0]).reshape(2, 4), 128, axis=1)[None], (128, 2, 512))),
        "pool_w": f(inp["pool_w"][0]),
        "pool_scT": _fm(inp["pool_scale"][0], 4),
        "poolfix": _poolfix(),
        "rt_w": np.ascontiguousarray(np.concatenate([f(inp["rt_grp_w"]), f(inp["rt_exp_w"])], axis=2)),
        "rt_b": np.ascontiguousarray(np.broadcast_to(np.concatenate([f(inp["rt_grp_b"]), f(inp["rt_exp_b"])], axis=1)[None], (128, 2, 36))),
        "ex_gate": np.ascontiguousarray(f(inp["ex_gate"]).reshape(2, 32, 8, 128, 512).transpose(0, 1, 3, 2, 4).reshape(2, 4096, 4096)),
        "ex_up": np.ascontiguousarray(f(inp["ex_up"]).reshape(2, 32, 8, 128, 512).transpose(0, 1, 3, 2, 4).reshape(2, 4096, 4096)),
        "ex_down": np.ascontiguousarray(f(inp["ex_down"]).reshape(2, 32, 4, 128, 1024).transpose(0, 1, 3, 2, 4).reshape(2, 4096, 4096)),
        "pidx": np.arange(128, dtype=np.float32).reshape(128, 1),
        "mconst": np.ascontiguousarray(np.broadcast_to(np.concatenate([
            np.arange(32), 128.0 * np.arange(35), np.arange(100), np.arange(32) % 2, np.arange(100) % 2,
            128.0 * np.arange(1, 69), 2.0 * np.arange(1, 51), np.arange(16)]).astype(np.float32)[None], (128, 433))),
        "umat": np.ascontiguousarray((r[:, None] < r[None, :]).astype(np.float32)),
        "ident": np.eye(128, dtype=np.float32), "cosT": cosT, "sinT": sinT, "wmask": np.ascontiguousarray(wmask),
    }
    maps = []
    for b in cores:
        m = dict(shared)
        m["x"] = f(inp["x"][b])
        m["ctx"] = f(inp["ctx"][b])
        c2 = np.stack([f(inp["c"][b]), f(inp["c_ctx"])], axis=1)
        m["c2T"] = np.ascontiguousarray(c2.reshape(8, 128, 2).transpose(1, 0, 2))
        maps.append(m)
    return maps


def _poolfix():
    out = np.ones((4, 32), np.float32)
    for gi, w in enumerate((2, 4, 8, 16)):
        for i, t in enumerate(list(range(16)) + list(range(S - 16, S))):
            lo = min(max(t - w // 2, 0), S)
            hi = min(max(t - w // 2 + w, 0), S)
            out[gi, i] = 1.0 / float(hi - lo)
    return np.ascontiguousarray(np.broadcast_to(out[None], (128, 4, 32)))


_NC_CACHE = {}


def kernel(**inputs):
    if "nc" not in _NC_CACHE:
        _NC_CACHE["nc"] = Builder(debug=False).build()
    nc = _NC_CACHE["nc"]
    maps = make_in_maps(inputs, list(range(8)))
    res = run_bass_kernel_spmd(nc, maps, core_ids=list(range(8)))
    return np.stack([np.asarray(r["y"], dtype=np.float32) for r in res.results], axis=0)
```

```python
import os
import numpy as np
from contextlib import ExitStack
from collections import deque
import concourse.bass as bass
import concourse.mybir as mybir
from concourse.bass_utils import run_bass_kernel_spmd

F32 = mybir.dt.float32
BF16 = mybir.dt.bfloat16
I32 = mybir.dt.int32
ALU = mybir.AluOpType
AF = mybir.ActivationFunctionType
AX = mybir.AxisListType

D = 1024
S = 4096
NCTX = 256
T = S + NCTX
NT = T // 128
EPS = 1e-6
GLU_OFF_CTX = 15
GLU_OFF_LAT = 15 + NCTX + 15
GLU_LEN = GLU_OFF_LAT + S + 15


class Buf:
    __slots__ = ("name", "w", "r")

    def __init__(self, name=""):
        self.name = name
        self.w = None
        self.r = {}


class Tile:
    __slots__ = ("t", "b")

    def __init__(self, t, b):
        self.t, self.b = t, b

    def __getitem__(self, k):
        return self.t[k]


import threading


class Interleaver:
    def __init__(self):
        self.active = False

    def run(self, fns):
        if len(fns) == 1:
            fns[0]()
            return
        n = len(fns)
        self.ev = [threading.Event() for _ in range(n)]
        self.alive = [True] * n
        self.err = []
        self.tid = {}
        done = threading.Event()

        def worker(i):
            self.ev[i].wait()
            self.ev[i].clear()
            try:
                fns[i]()
            except BaseException as e:
                self.err.append(e)
            self.alive[i] = False
            nxt = self._next(i)
            if nxt is None:
                done.set()
            else:
                self.ev[nxt].set()

        ths = [threading.Thread(target=worker, args=(i,)) for i in range(n)]
        self.active = True
        for i, th in enumerate(ths):
            th.start()
            self.tid[th.ident] = i
        self.ev[0].set()
        done.wait()
        for th in ths:
            th.join()
        self.active = False
        if self.err:
            raise self.err[0]

    def _next(self, i):
        n = len(self.alive)
        for d in range(1, n + 1):
            j = (i + d) % n
            if self.alive[j] and j != i:
                return j
        return None

    def yield_point(self):
        if not self.active:
            return
        i = self.tid.get(threading.get_ident())
        if i is None:
            return
        nxt = self._next(i)
        if nxt is None:
            return
        self.ev[nxt].set()
        self.ev[i].wait()
        self.ev[i].clear()


ILV = Interleaver()


class Eng:
    def __init__(self, key, e, sem):
        self.key, self.e, self.sem = key, e, sem
        self.n = 0
        self.known = {}

    def _wait(self, sem, val):
        if self.known.get(sem, 0) >= val:
            return
        self.known[sem] = val
        self.e.wait_ge(sem, val)

    def op(self, ins_fn, reads=(), writes=(), inc=True):
        for b in reads:
            if b.w is not None:
                self._wait(b.w[0], b.w[1])
        strict = (self.key == "pool")
        for b in writes:
            if b.w is not None and (strict or b.w[2] != self.key):
                self._wait(b.w[0], b.w[1])
            for sem, (val, k) in b.r.items():
                if strict or k != self.key:
                    self._wait(sem, val)
        ins = ins_fn()
        if inc:
            self.n += 1
            ins.then_inc(self.sem, 1)
            tok = (self.sem, self.n, self.key)
        else:
            tok = (self.sem, self.n + 1, self.key)
        for b in reads:
            b.r[tok[0]] = (tok[1], tok[2])
        for b in writes:
            b.w = tok
            b.r = {}
        if inc:
            ILV.yield_point()
        return ins


class DmaQ:
    def __init__(self, key, e, sems):
        self.key, self.e, self.sems = key, e, sems
        self.cnt = [0] * len(sems)
        self.i = 0
        self.known = {}

    def _wait(self, sem, val):
        if self.known.get(sem, 0) >= val:
            return
        self.known[sem] = val
        self.e.wait_ge(sem, val)

    def dma(self, out, in_, reads=(), writes=(), **kw):
        for b in reads:
            if b.w is not None:
                self._wait(b.w[0], b.w[1])
        for b in writes:
            if b.w is not None:
                self._wait(b.w[0], b.w[1])
            for sem, (val, k) in b.r.items():
                self._wait(sem, val)
        s = self.i % len(self.sems)
        self.i += 1
        sem = self.sems[s]
        if self.cnt[s] > 0:
            self._wait(sem, 16 * self.cnt[s])
        self.cnt[s] += 1
        ins = self.e.dma_start(out=out, in_=in_, **kw)
        ins.then_inc(sem, 16)
        tok = (sem, 16 * self.cnt[s], self.key + str(s))
        for b in reads:
            b.r[tok[0]] = (tok[1], tok[2])
        for b in writes:
            b.w = tok
            b.r = {}
        return ins


def _dma_generic(self, fn, reads=(), writes=()):
    for b in reads:
        if b.w is not None:
            self._wait(b.w[0], b.w[1])
    for b in writes:
        if b.w is not None:
            self._wait(b.w[0], b.w[1])
        for sem, (val, k) in b.r.items():
            self._wait(sem, val)
    s = self.i % len(self.sems)
    self.i += 1
    sem = self.sems[s]
    if self.cnt[s] > 0:
        self._wait(sem, 16 * self.cnt[s])
    self.cnt[s] += 1
    ins = fn()
    ins.then_inc(sem, 16)
    tok = (sem, 16 * self.cnt[s], self.key + str(s))
    for b in reads:
        b.r[tok[0]] = (tok[1], tok[2])
    for b in writes:
        b.w = tok
        b.r = {}
    return ins


DmaQ.dma_fn = _dma_generic


class FW:
    def __init__(self, nc, stack, n_dma_sems=10):
        self.nc = nc
        mk = lambda nm: stack.enter_context(nc.semaphore(nm))
        self.pe = Eng("pe", nc.tensor, mk("s_pe"))
        self.act = Eng("act", nc.scalar, mk("s_act"))
        self.dve = Eng("dve", nc.vector, mk("s_dve"))
        self.pool = Eng("pool", nc.gpsimd, mk("s_pool"))
        self.q_sync = DmaQ("qs", nc.sync, [mk(f"s_qs{i}") for i in range(n_dma_sems)])
        self.q_pool = DmaQ("qp", nc.gpsimd, [mk(f"s_qp{i}") for i in range(n_dma_sems)])
        self.q_pool.known = self.pool.known
        self.engs = [self.pe, self.act, self.dve, self.pool]
        self.qs = [self.q_sync, self.q_pool]

    def barrier(self):
        toks = []
        for e in self.engs:
            if e.n > 0:
                toks.append((e.sem, e.n))
        for q in self.qs:
            for s, c in zip(q.sems, q.cnt):
                if c > 0:
                    toks.append((s, 16 * c))
        for e in self.engs + [self.q_sync]:
            for sem, val in toks:
                e._wait(sem, val)


class Builder:
    def __init__(self, debug=False, stop=None):
        self.debug = debug
        self.stop = stop
        self.nc = bass.Bass("TRN2", target_bir_lowering=False)
        self.dbg_out = {}

    def dram_in(self, name, shape, dt=F32):
        return self.nc.dram_tensor(name, list(shape), dt, kind="ExternalInput").ap()

    def tile(self, st, name, shape, dt):
        self._tn = getattr(self, "_tn", 0) + 1
        name = f"{name}_{self._tn}"
        t = st.enter_context(self.nc.sbuf_tensor(name, list(shape), dt))
        return Tile(t, Buf(name))

    def ps_get(self):
        return self.psq.popleft()

    def ps_put(self, p):
        self.psq.append(p)

    def dbg(self, name, shape):
        if not self.debug:
            return None
        ap = self.nc.dram_tensor("dbg_" + name, list(shape), F32, kind="ExternalOutput").ap()
        self.dbg_out[name] = (ap, Buf("dbg_" + name))
        return ap

    def cut(self, n):
        return int(os.environ.get("P1_CUT", "-1")) == n

    def tok_in(self, t):
        if t < 2:
            return self.ctx_in[t * 128:(t + 1) * 128, :]
        return self.x_in[(t - 2) * 128:(t - 1) * 128, :]

    def build(self):
        nc = self.nc
        di = self.dram_in
        self.x_in = di("x", [S, D])
        self.ctx_in = di("ctx", [NCTX, D])
        self.c2T = di("c2T", [128, 8, 2])
        self.mod_w = di("mod_w", [2, D, 6 * D])
        self.mod_bT = di("mod_bT", [128, 2, 48])
        self.ln1gT = di("ln1gT", [128, 2, 8])
        self.ln2gT = di("ln2gT", [128, 2, 8])
        self.w_in_ab = di("w_in_ab", [D, 1792])
        self.w_out_ab = di("w_out_ab", [D, D])
        self.gqk_a = di("gqk_a", [128, 640])
        self.convwT = di("convwT", [128, 4, 31])
        self.convv = di("convv", [128, 3, 4])
        self.w_in_cd = di("w_in_cd", [D, 1280])
        self.w_out_cd = di("w_out_cd", [D, D])
        self.gqk_c = di("gqk_c", [128, 640])
        self.sink_rep = di("sink_rep", [128, 2, 512])
        self.pool_w = di("pool_w", [4, 128, 128])
        self.pool_scT = di("pool_scT", [128, 4])
        self.poolfix = di("poolfix", [128, 4, 32])
        self.rt_w = di("rt_w", [2, D, 36])
        self.rt_b = di("rt_b", [128, 2, 36])
        self.ex_gate = di("ex_gate", [2, 32 * 128, 4096])
        self.ex_up = di("ex_up", [2, 32 * 128, 4096])
        self.ex_down = di("ex_down", [2, 32 * 128, 4096])
        self.pidx = di("pidx", [128, 1])
        self.ident_in = di("ident", [128, 128])
        self.cosT = di("cosT", [128, 32, 32])
        self.sinT = di("sinT", [128, 32, 32])
        self.wmask = di("wmask", [128, 2, 512])
        self.mconst = di("mconst", [128, 433])
        self.umat = di("umat", [128, 128])
        self.y_out = nc.dram_tensor("y", [S, D], F32, kind="ExternalOutput").ap()
        self.xres = nc.dram_tensor("xres", [T, D], F32, kind="Internal").ap()
        self.xres_b = [Buf(f"xres{t}") for t in range(NT)]
        self.y_b = [Buf(f"y{t}") for t in range(NT)]

        with ExitStack() as st:
            self.fw = FW(nc, st)
            fw = self.fw
            self.PS = []
            self.PSB = []
            for i in range(4):
                pb_ = st.enter_context(nc.psum_tensor(f"psb{i}", [128, 1024], F32))
                h0 = Tile(pb_[:, 0:512], Buf(f"ps{2 * i}"))
                h1 = Tile(pb_[:, 512:1024], Buf(f"ps{2 * i + 1}"))
                self.PS += [h0, h1]
                self.PSB.append((pb_, h0, h1))
            self.psq = deque(self.PS)
            g = self.g = {}
            g["identf"] = self.tile(st, "identf", [128, 128], F32)
            g["identb"] = self.tile(st, "identb", [128, 128], BF16)
            g["onesf"] = self.tile(st, "onesf", [128, 128], F32)
            g["modT"] = self.tile(st, "modT", [128, 2, 48, 2], F32)
            g["A1"] = self.tile(st, "A1", [128, 2, 8, 2], F32)
            g["A2"] = self.tile(st, "A2", [128, 2, 8, 2], F32)
            g["epsc"] = self.tile(st, "epsc", [128, 1], F32)
            fw.q_sync.dma(g["identf"][:], self.ident_in, writes=[g["identf"].b])
            fw.dve.op(lambda: nc.vector.tensor_copy(out=g["identb"][:], in_=g["identf"][:]), [g["identf"].b], [g["identb"].b])
            fw.dve.op(lambda: nc.vector.memset(g["onesf"][:], 1.0), [], [g["onesf"].b])
            fw.dve.op(lambda: nc.vector.memset(g["epsc"][:], EPS), [], [g["epsc"].b])

            self.bc_reg = nc.gpsimd.alloc_register("bcreg")
            nc.gpsimd.reg_mov(self.bc_reg, 8191)
            self.phase_mod()
            if self.stop != "p0":
                self.layer0_mixer()
            if self.stop is None or self.stop in ("m0", "l1", "m1"):
                (self.moe2 if os.environ.get("MOE_DENSE") != "1" else self.moe)(0)
            if self.stop is None or self.stop in ("l1", "m1"):
                self.layer1_mixer()
            if self.stop is None or self.stop in ("m1",):
                (self.moe2 if os.environ.get("MOE_DENSE") != "1" else self.moe)(1)

            for b in self.y_b[2:]:
                if b.w is not None:
                    fw.q_sync._wait(b.w[0], b.w[1])
            for name, (ap, b) in self.dbg_out.items():
                if b.w is not None:
                    fw.q_sync._wait(b.w[0], b.w[1])
            fw.barrier()
        return nc

    def rstd_from_ss(self, ss, n_inv, cols):
        nc, fw = self.nc, self.fw
        fw.dve.op(lambda: nc.vector.tensor_scalar(out=ss[:, 0:cols], in0=ss[:, 0:cols], scalar1=n_inv, scalar2=EPS,
                                                  op0=ALU.mult, op1=ALU.add), [ss.b], [ss.b])
        fw.act.op(lambda: nc.scalar.sqrt(out=ss[:, 0:cols], in_=ss[:, 0:cols]), [ss.b], [ss.b])
        fw.dve.op(lambda: nc.vector.reciprocal(out=ss[:, 0:cols], in_=ss[:, 0:cols]), [ss.b], [ss.b])

    def load_cast_w(self, st_pool, dst, dst_cols, src_ap, kc, ncols, eng_i):
        nc, fw = self.nc, self.fw
        stg = st_pool[self.stg_i % len(st_pool)]
        self.stg_i += 1
        fw.q_sync.dma(stg[:, 0:kc * ncols].rearrange("p (k n) -> p k n", k=kc),
                      src_ap.rearrange("(k p) n -> p k n", p=128), writes=[stg.b])
        src = stg[:, 0:kc * ncols].rearrange("p (k n) -> p k n", k=kc)
        if eng_i % 2 == 0:
            fw.act.op(lambda: nc.scalar.copy(out=dst[:, 0:kc, dst_cols], in_=src), [stg.b], [dst.b])
        else:
            fw.dve.op(lambda: nc.vector.tensor_copy(out=dst[:, 0:kc, dst_cols], in_=src), [stg.b], [dst.b])

    def phase_mod(self):
        nc, fw, g = self.nc, self.fw, self.g
        with ExitStack() as ph:
            c2 = self.tile(ph, "c2", [128, 8, 2], F32)
            sil = self.tile(ph, "sil", [128, 8, 2], BF16)
            mb = self.tile(ph, "mb", [128, 2, 48], F32)
            l1g = self.tile(ph, "l1g", [128, 2, 8], F32)
            l2g = self.tile(ph, "l2g", [128, 2, 8], F32)
            stg = [self.tile(ph, f"mstg{i}", [128, 4096], F32) for i in range(2)]
            wb = [self.tile(ph, f"mwb{i}", [128, 8, 512], BF16) for i in range(2)]
            self.stg_i = 0
            fw.q_sync.dma(c2[:], self.c2T, writes=[c2.b])
            fw.q_sync.dma(mb[:], self.mod_bT, writes=[mb.b])
            fw.q_sync.dma(l1g[:], self.ln1gT, writes=[l1g.b])
            fw.q_sync.dma(l2g[:], self.ln2gT, writes=[l2g.b])
            fw.act.op(lambda: nc.scalar.activation(out=sil[:], in_=c2[:], func=AF.Silu), [c2.b], [sil.b])
            for l in range(2):
                pm = self.ps_get()
                pmv = pm[:, 0:96].rearrange("p (c j) -> p c j", j=2)
                for n in range(12):
                    w = wb[n % 2]
                    self.load_cast_w(stg, w, slice(0, 512), self.mod_w[l, :, n * 512:(n + 1) * 512], 8, 512, n)
                    for q in range(4):
                        cc = n * 4 + q
                        for k in range(8):
                            fw.pe.op(lambda: nc.tensor.matmul(pmv[:, cc, :], lhsT=w[:, k, q * 128:(q + 1) * 128], rhs=sil[:, k, :],
                                                              start=(k == 0), stop=(k == 7)),
                                     [w.b, sil.b], [pm.b], inc=(k == 7 and q == 3))
                mT = g["modT"]
                fw.dve.op(lambda: nc.vector.tensor_tensor(out=mT[:, l, :, :], in0=pmv,
                                                          in1=mb[:, l, :].unsqueeze(2).to_broadcast([128, 48, 2]), op=ALU.add),
                          [pm.b, mb.b], [mT.b])
                self.ps_put(pm)
                fw.dve.op(lambda: nc.vector.scalar_tensor_tensor(out=g["A1"][:, l, :, :], in0=mT[:, l, 8:16, :], scalar=1.0,
                                                                 in1=l1g[:, l, :].unsqueeze(2).to_broadcast([128, 8, 2]),
                                                                 op0=ALU.add, op1=ALU.mult), [mT.b, l1g.b], [g["A1"].b])
                fw.dve.op(lambda: nc.vector.scalar_tensor_tensor(out=g["A2"][:, l, :, :], in0=mT[:, l, 32:40, :], scalar=1.0,
                                                                 in1=l2g[:, l, :].unsqueeze(2).to_broadcast([128, 8, 2]),
                                                                 op0=ALU.add, op1=ALU.mult), [mT.b, l2g.b], [g["A2"].b])
            if self.debug:
                ap = self.dbg("modT", [128, 2 * 48 * 2])
                fw.q_pool.dma(ap, g["modT"][:].rearrange("p l c j -> p (l c j)"), reads=[g["modT"].b], writes=[self.dbg_out["modT"][1]])
            fw.barrier()

    def bcast_tile(self, dst, col_ap_fn, tmp, extra=()):
        nc, fw, g = self.nc, self.fw, self.g
        for c in range(8):
            fw.dve.op(lambda: nc.vector.tensor_scalar(out=tmp[:], in0=g["onesf"][:], scalar1=col_ap_fn(c), scalar2=None, op0=ALU.mult),
                      [g["onesf"].b, g["modT"].b] + list(extra), [tmp.b])
            p = self.ps_get()
            fw.pe.op(lambda: nc.tensor.transpose(out=p[:, 0:128], in_=tmp[:], identity=g["identf"][:]), [tmp.b, g["identf"].b], [p.b])
            fw.act.op(lambda: nc.scalar.copy(out=dst[:, c * 128:(c + 1) * 128], in_=p[:, 0:128]), [p.b], [dst.b])
            self.ps_put(p)

    def norm_tile_to_hT(self, xt, hT, col0, l, which, cls, scr):
        nc, fw, g = self.nc, self.fw, self.g
        ni = scr.get("ni", 0)
        scr["ni"] = ni + 1
        ss, xn = scr["ssl"][ni % len(scr["ssl"])], scr["xnl"][ni % len(scr["xnl"])]
        fw.act.op(lambda: nc.scalar.activation(out=xn[:], in_=xt[:], func=AF.Square, accum_out=ss[:, 0:1]), [xt.b], [xn.b, ss.b])
        self.rstd_from_ss(ss, 1.0 / D, 1)
        fw.act.op(lambda: nc.scalar.activation(out=xn[:], in_=xt[:], func=AF.Copy, scale=ss[:, 0:1]), [xt.b, ss.b], [xn.b])
        p = self.ps_get()
        pv = p[:].bitcast(BF16).rearrange("p (c q) -> p c q", c=8)
        for c in range(8):
            fw.pe.op(lambda: nc.tensor.transpose(out=pv[:, c, :], in_=xn[:, c * 128:(c + 1) * 128], identity=g["identb"][:]),
                     [xn.b, g["identb"].b], [p.b], inc=(c == 7))
        A = g["A1"] if which == 1 else g["A2"]
        boff = 0 if which == 1 else 24
        for c in range(8):
            fw.dve.op(lambda: nc.vector.tensor_scalar(out=hT[:, c, col0:col0 + 128], in0=pv[:, c, :], scalar1=A[:, l, c, cls:cls + 1],
                                                      scalar2=g["modT"][:, l, boff + c, cls:cls + 1], op0=ALU.mult, op1=ALU.add),
                      [p.b, A.b, g["modT"].b], [hT.b])
        self.ps_put(p)

    def qk_post(self, qkv, t, gqk, scr, qT, kT, do_q, lat_idx):
        nc, fw, g = self.nc, self.fw, self.g
        sq, ssq, qn, qr, tmp = scr["sq"], scr["ssq"], scr["qn"], scr["qr"], scr["rt"]
        h0 = 0 if do_q else 8
        nh = 10 - h0
        c0 = h0 * 64
        q3 = lambda tl: tl[:, c0:640].rearrange("p (h d) -> p h d", d=64)
        fw.pool.op(lambda: nc.gpsimd.tensor_tensor(out=sq[:, c0:640], in0=qkv[:, c0:640], in1=qkv[:, c0:640], op=ALU.mult), [qkv.b], [sq.b])
        fw.dve.op(lambda: nc.vector.tensor_reduce(out=ssq[:, h0:10], in_=q3(sq), axis=AX.X, op=ALU.add), [sq.b], [ssq.b])
        fw.dve.op(lambda: nc.vector.tensor_scalar(out=ssq[:, h0:10], in0=ssq[:, h0:10], scalar1=1.0 / 64, scalar2=EPS,
                                                  op0=ALU.mult, op1=ALU.add), [ssq.b], [ssq.b])
        fw.act.op(lambda: nc.scalar.sqrt(out=ssq[:, h0:10], in_=ssq[:, h0:10]), [ssq.b], [ssq.b])
        fw.dve.op(lambda: nc.vector.reciprocal(out=ssq[:, h0:10], in_=ssq[:, h0:10]), [ssq.b], [ssq.b])
        fw.dve.op(lambda: nc.vector.tensor_tensor(out=q3(qn), in0=q3(qkv), in1=ssq[:, h0:10].unsqueeze(2).to_broadcast([128, nh, 64]),
                                                  op=ALU.mult), [qkv.b, ssq.b], [qn.b])
        if lat_idx is None:
            fw.pool.op(lambda: nc.gpsimd.tensor_tensor(out=qr[:, c0:640], in0=qn[:, c0:640], in1=gqk[:, c0:640], op=ALU.mult),
                       [qn.b, gqk.b], [qr.b])
        else:
            fw.pool.op(lambda: nc.gpsimd.tensor_tensor(out=qn[:, c0:640], in0=qn[:, c0:640], in1=gqk[:, c0:640], op=ALU.mult),
                       [qn.b, gqk.b], [qn.b])
            cosb = g["cos"][:, lat_idx, :].unsqueeze(1).to_broadcast([128, nh, 32])
            sinb = g["sin"][:, lat_idx, :].unsqueeze(1).to_broadcast([128, nh, 32])
            x1 = q3(qn)[:, :, 0:32]
            x2 = q3(qn)[:, :, 32:64]
            sq2 = sq[:, 0:640].rearrange("p (k n) -> p k n", k=2)
            t3 = lambda k: (sq2 if k < 2 else tmp)[:, k % 2, c0 // 2:320].rearrange("p (h d) -> p h d", d=32)
            fw.dve.op(lambda: nc.vector.tensor_tensor(out=t3(0), in0=x1, in1=cosb, op=ALU.mult), [qn.b, g["cos"].b], [sq.b])
            fw.pool.op(lambda: nc.gpsimd.tensor_tensor(out=t3(1), in0=x2, in1=sinb, op=ALU.mult), [qn.b, g["sin"].b], [sq.b])
            fw.dve.op(lambda: nc.vector.tensor_tensor(out=t3(2), in0=x2, in1=cosb, op=ALU.mult), [qn.b, g["cos"].b], [tmp.b])
            fw.pool.op(lambda: nc.gpsimd.tensor_tensor(out=t3(3), in0=x1, in1=sinb, op=ALU.mult), [qn.b, g["sin"].b], [tmp.b])
            fw.dve.op(lambda: nc.vector.tensor_tensor(out=q3(qr)[:, :, 0:32], in0=t3(0), in1=t3(1), op=ALU.subtract), [sq.b], [qr.b])
            fw.pool.op(lambda: nc.gpsimd.tensor_tensor(out=q3(qr)[:, :, 32:64], in0=t3(2), in1=t3(3), op=ALU.add), [tmp.b], [qr.b])
        if self.cut(30):
            return
        p = self.ps_get()
        pv = p[:].bitcast(BF16).rearrange("p (c q) -> p c q", c=8)
        if do_q:
            for j in range(4):
                fw.pe.op(lambda: nc.tensor.transpose(out=pv[:, j, :], in_=qr[:, j * 128:(j + 1) * 128], identity=g["identb"][:]),
                         [qr.b, g["identb"].b], [p.b], inc=False)
        fw.pe.op(lambda: nc.tensor.transpose(out=pv[:, 4, :], in_=qr[:, 512:640], identity=g["identb"][:]),
                 [qr.b, g["identb"].b], [p.b])
        if self.cut(31):
            return
        if do_q:
            fw.dve.op(lambda: nc.vector.tensor_copy(out=qT[:, t, :, :], in_=pv[:, 0:4, :]), [p.b], [qT.b])
        if self.cut(32):
            return
        fw.dve.op(lambda: nc.vector.tensor_copy(out=kT[0:64, 0, t * 128:(t + 1) * 128], in_=pv[0:64, 4, :]), [p.b], [kT.b])
        fw.dve.op(lambda: nc.vector.tensor_copy(out=kT[64:128, 1, t * 128:(t + 1) * 128], in_=pv[64:128, 4, :]), [p.b], [kT.b])
        self.ps_put(p)

    def v_fill(self, qkv, t, Vp):
        nc, fw = self.nc, self.fw
        fw.act.op(lambda: nc.scalar.copy(out=Vp[:, t, 0:64], in_=qkv[:, 640:704]), [qkv.b], [Vp.b])
        fw.act.op(lambda: nc.scalar.copy(out=Vp[:, t, 136:200], in_=qkv[:, 704:768]), [qkv.b], [Vp.b])

    def v_init(self, Vp):
        nc, fw = self.nc, self.fw
        fw.pool.op(lambda: nc.gpsimd.memset(Vp[:], 0.0), [], [Vp.b])
        fw.pool.op(lambda: nc.gpsimd.memset(Vp[:, :, 64:65], 1.0), [], [Vp.b])
        fw.pool.op(lambda: nc.gpsimd.memset(Vp[:, :, 104:105], 1.0), [], [Vp.b])

    def attention(self, blocks, qT, kT, Vp, oT, scr, masks=None, sinkrow=None, LA=2):
        nc, fw, g = self.nc, self.fw, self.g
        steps = []
        for b0 in range(0, len(blocks), 2):
            (kv0_, qb, kts), (kv1_, qb1, kts1) = blocks[b0], blocks[b0 + 1]
            assert (kv0_, kv1_) == (0, 1) and qb == qb1
            for i, (kt, mi) in enumerate(kts):
                steps.append(dict(bi=b0, qb=qb, kt=kt, mi=mi, first=(i == 0), last=(i == len(kts) - 1)))
        assert len(self.psq) == 8
        spairs = self.PSB[0:2]
        for (_, h0, h1) in spairs:
            self.psq.remove(h0)
            self.psq.remove(h1)
        po_of = {}
        cnt = dict(s=0)

        pending = []

        def finish_block(kv, qb, bi, po, idx):
            r0 = kv * 64
            dr = 64 if kv == 0 else 32
            M = 65 if kv == 0 else 128
            slot = (bi // 2) % 2
            posb = scr["posb"][slot][kv]
            rec = scr["rec"][slot]
            fw.dve.op(lambda: nc.vector.tensor_copy(out=posb[0:M, :], in_=po[0:M, :]), [po.b], [posb.b])
            self.ps_put(po)
            if sinkrow is not None:
                fw.dve.op(lambda: nc.vector.tensor_tensor(out=rec[dr:dr + 1, :], in0=posb[dr:dr + 1, :], in1=sinkrow[dr:dr + 1, kv, :], op=ALU.add),
                          [posb.b, sinkrow.b], [rec.b])
                fw.dve.op(lambda: nc.vector.reciprocal(out=rec[dr:dr + 1, :], in_=rec[dr:dr + 1, :]), [rec.b], [rec.b])
            else:
                fw.dve.op(lambda: nc.vector.reciprocal(out=rec[dr:dr + 1, :], in_=posb[dr:dr + 1, :]), [posb.b], [rec.b])
            pending.append((idx + 4, kv, qb, posb, rec))

        def finalize(kv, qb, posb, rec):
            r0 = kv * 64
            dr = 64 if kv == 0 else 32
            pb = self.ps_get()
            Mb = 64 if kv == 0 else 128
            fw.pe.op(lambda: nc.tensor.matmul(pb[0:Mb, :], lhsT=g["onesf"][dr:dr + 1, 0:Mb], rhs=rec[dr:dr + 1, :], start=True, stop=True),
                     [g["onesf"].b, rec.b], [pb.b])
            fw.dve.op(lambda: nc.vector.tensor_tensor(out=oT[r0:r0 + 64, :, qb * 128:(qb + 1) * 128],
                                                      in0=posb[r0:r0 + 64, :].rearrange("p (j q) -> p j q", j=4),
                                                      in1=pb[r0:r0 + 64, :].rearrange("p (j q) -> p j q", j=4), op=ALU.mult),
                      [posb.b, pb.b], [oT.b])
            self.ps_put(pb)

        def emit_S(st):
            qb, kt = st["qb"], st["kt"]
            big, h0, h1 = spairs[cnt["s"] % 2]
            cnt["s"] += 1
            rhs_q = qT[:, qb, :, :].rearrange("p j q -> p (j q)")
            fw.pe.op(lambda: nc.tensor.matmul(h0[:], lhsT=kT[:, 0, kt * 128:(kt + 1) * 128], rhs=rhs_q, start=True, stop=True),
                     [kT.b, qT.b], [h0.b], inc=False)
            fw.pe.op(lambda: nc.tensor.matmul(h1[:], lhsT=kT[:, 1, kt * 128:(kt + 1) * 128], rhs=rhs_q, start=True, stop=True),
                     [kT.b, qT.b], [h1.b])
            pe_ = scr["pexp"][scr["pi"] % len(scr["pexp"])]
            scr["pi"] += 1
            fw.act.op(lambda: nc.scalar.activation(out=pe_[:], in_=big[:, 0:1024], func=AF.Exp), [h0.b, h1.b], [pe_.b])
            if st["mi"] is not None:
                fw.pool.op(lambda: nc.gpsimd.tensor_tensor(out=pe_[:].rearrange("p (a q) -> p a q", a=2), in0=pe_[:].rearrange("p (a q) -> p a q", a=2),
                                                           in1=masks[:, st["mi"], :].unsqueeze(1).to_broadcast([128, 2, 512]), op=ALU.mult),
                           [pe_.b, masks.b], [pe_.b])
            st["pe"] = pe_

        def emit_PV(st):
            qb, kt, bi = st["qb"], st["kt"], st["bi"]
            if st["first"]:
                po_of[bi] = (self.ps_get(), self.ps_get())
            pe_ = st["pe"]
            for kv in range(2):
                po = po_of[bi][kv]
                M = 65 if kv == 0 else 128
                fw.pe.op(lambda: nc.tensor.matmul(po[0:M, :], lhsT=Vp[:, kt, kv * 72:kv * 72 + M], rhs=pe_[:, kv * 512:(kv + 1) * 512],
                                                  start=st["first"], stop=st["last"]), [Vp.b, pe_.b], [po.b], inc=(st["last"] or kv == 1))
            if st["last"]:
                for kv in range(2):
                    finish_block(kv, qb, bi, po_of[bi][kv], st["idx"])
                del po_of[bi]

        n = len(steps)
        for idx in range(n + LA):
            if idx < n:
                emit_S(steps[idx])
            if idx - LA >= 0:
                steps[idx - LA]["idx"] = idx
                emit_PV(steps[idx - LA])
            while pending and pending[0][0] <= idx:
                _, kv, qb_, posb, rec = pending.pop(0)
                finalize(kv, qb_, posb, rec)
        while pending:
            _, kv, qb_, posb, rec = pending.pop(0)
            finalize(kv, qb_, posb, rec)
        for (_, h0, h1) in spairs:
            self.psq.append(h0)
            self.psq.append(h1)

    def out_proj(self, l, cat_fn, wsrc, tiles, ph):
        nc, fw, g = self.nc, self.fw, self.g
        classes = [1, 0] if l == 0 else [0]
        wo = {}
        GB = self.tile(ph, "GB", [128, D], F32)
        tmp = self.tile(ph, "bct", [128, 128], F32)
        stg = [self.tile(ph, f"ostg{i}", [128, D], F32) for i in range(2)]
        for cls in classes:
            wo[cls] = self.tile(ph, f"wo{cls}", [128, 8, D], BF16)
            self.bcast_tile(GB, lambda c: g["modT"][:, l, 16 + c, cls:cls + 1], tmp)
            for c in range(8):
                s_ = stg[c % 2]
                if c < 4:
                    fw.q_sync.dma(s_[0:64, :], wsrc[c * 64:(c + 1) * 64, :], writes=[s_.b])
                    fw.q_sync.dma(s_[64:128, :], wsrc[(c + 4) * 64:(c + 5) * 64, :], writes=[s_.b])
                else:
                    fw.q_sync.dma(s_[:], wsrc[c * 128:(c + 1) * 128, :], writes=[s_.b])
                fw.dve.op(lambda: nc.vector.tensor_tensor(out=wo[cls][:, c, :], in0=s_[:], in1=GB[:], op=ALU.mult), [s_.b, GB.b], [wo[cls].b])
        xt = [self.tile(ph, f"oxt{i}", [128, D], F32) for i in range(3)]

        def op_fn(i, t):
            cls = 1 if t < 2 else 0
            x_ = xt[i % 3]
            src = self.tok_in(t) if l == 0 else self.xres[t * 128:(t + 1) * 128, :]
            rb = [] if l == 0 else [self.xres_b[t]]
            fw.q_sync.dma(x_[:], src, reads=rb, writes=[x_.b])
            for half in range(2):
                p = self.ps_get()
                for c in range(8):
                    ct, ci = cat_fn(c)
                    fw.pe.op(lambda: nc.tensor.matmul(p[:], lhsT=ct[:, ci, t * 128:(t + 1) * 128], rhs=wo[cls][:, c, half * 512:(half + 1) * 512],
                                                      start=(c == 0), stop=(c == 7)), [ct.b, wo[cls].b], [p.b], inc=(c == 7))
                fw.dve.op(lambda: nc.vector.tensor_tensor(out=x_[:, half * 512:(half + 1) * 512], in0=p[:], in1=x_[:, half * 512:(half + 1) * 512],
                                                          op=ALU.add), [p.b, x_.b], [x_.b])
                self.ps_put(p)
            fw.q_pool.dma(self.xres[t * 128:(t + 1) * 128, :], x_[:], reads=[x_.b], writes=[self.xres_b[t]])
            if self.debug and t in (0, 2, NT - 1):
                nm = f"x1_{l}_{t}"
                ap = self.dbg(nm, [128, D])
                fw.q_pool.dma(ap, x_[:], reads=[x_.b], writes=[self.dbg_out[nm][1]])

        for i0 in range(0, len(tiles), 3):
            grp = list(range(i0, min(i0 + 3, len(tiles))))
            ILV.run([(lambda i=i: op_fn(i, tiles[i])) for i in grp])

    def layer0_mixer(self):
        nc, fw, g = self.nc, self.fw, self.g
        l = 0
        with ExitStack() as ph:
            gluT = self.tile(ph, "gluT", [128, 4, GLU_LEN], BF16)
            oT = self.tile(ph, "oT", [128, 4, T], BF16)
            pattn = ExitStack()
            qT = self.tile(pattn, "qT", [128, NT, 4, 128], BF16)
            kT = self.tile(pattn, "kT", [128, 2, T], BF16)
            Vp = self.tile(pattn, "Vp", [128, NT, 200], BF16)
            self.v_init(Vp)
            fw.pool.op(lambda: nc.gpsimd.memset(kT[:], 0.0), [], [kT.b])
            fw.pool.op(lambda: nc.gpsimd.memset(gluT[:], 0.0), [], [gluT.b])
            with ExitStack() as p1:
                win = self.tile(p1, "win", [128, 8, 1792], BF16)
                gqk = self.tile(p1, "gqk", [128, 640], F32)
                g["cos"] = self.tile(p1, "cos", [128, 32, 32], F32)
                g["sin"] = self.tile(p1, "sin", [128, 32, 32], F32)
                fw.q_sync.dma(gqk[:], self.gqk_a, writes=[gqk.b])
                fw.q_sync.dma(g["cos"][:], self.cosT, writes=[g["cos"].b])
                fw.q_sync.dma(g["sin"][:], self.sinT, writes=[g["sin"].b])
                fw.dve.op(lambda: nc.vector.tensor_scalar(out=gqk[:, 0:512], in0=gqk[:, 0:512], scalar1=0.125, scalar2=None, op0=ALU.mult),
                          [gqk.b], [gqk.b])
                with ExitStack() as pw:
                    stg = [self.tile(pw, f"wstg{i}", [128, 4096], F32) for i in range(1)]
                    self.stg_i = 0
                    for n in range(4):
                        w_ = 512 if n < 3 else 256
                        self.load_cast_w(stg, win, slice(n * 512, n * 512 + w_), self.w_in_ab[:, n * 512:n * 512 + w_], 8, w_, n)
                    fw.barrier()
                if self.cut(0):
                    return
                scr = dict(ssl=[self.tile(p1, f"ss{i}", [128, 1], F32) for i in range(2)],
                           xnl=[self.tile(p1, f"xn{i}", [128, D], BF16) for i in range(2)], sq=self.tile(p1, "sq", [128, 640], F32),
                           ssq=self.tile(p1, "ssq", [128, 10], F32), qn=self.tile(p1, "qn", [128, 640], F32),
                           qr=self.tile(p1, "qr", [128, 640], BF16), rt=self.tile(p1, "rt", [128, 2, 320], F32))
                xt = [self.tile(p1, f"xt{i}", [128, D], F32) for i in range(1)]
                hT = [self.tile(p1, f"hT{i}", [128, 8, 512], BF16) for i in range(1)]
                qkv = [self.tile(p1, f"qkv{i}", [128, 768], F32) for i in range(1)]
                sig = [self.tile(p1, f"sig{i}", [128, 512], BF16) for i in range(1)]
                chunks = [(0, 2)] + [(2 + 4 * i, 4) for i in range(8)]
                oflat = oT[:].rearrange("p a t -> p (a t)")
                coff = [0]

                def carve(n_elems, dt, shape3=None):
                    nb = n_elems * (4 if dt == F32 else 2) // 2
                    ap = oflat[:, coff[0]:coff[0] + nb]
                    coff[0] += nb
                    if dt == F32:
                        ap = ap.bitcast(F32)
                    if shape3 is not None:
                        ap = ap.rearrange("p (a b) -> p a b", a=shape3[0])
                    return Tile(ap, Buf("carve"))

                scr_b = dict(ssl=[scr["ssl"][1]], xnl=[scr["xnl"][1]], sq=carve(640, F32), ssq=carve(16, F32), qn=carve(640, F32),
                             qr=carve(640, BF16), rt=carve(640, F32, (2, 320)))
                scr_a = dict(scr)
                scr_a["ssl"] = [scr["ssl"][0]]
                scr_a["xnl"] = [scr["xnl"][0]]
                scr_l = [scr_a, scr_b]
                xt2 = [xt[0], carve(D, F32)]
                qkv2 = [qkv[0], carve(768, F32)]
                for ci, (t0, ntl) in enumerate(chunks):
                    h = hT[0]
                    ntok = ntl * 128
                    cls = 1 if t0 < 2 else 0

                    def norm_fn(tl):
                        t = t0 + tl
                        x_ = xt2[tl % 2]
                        fw.q_sync.dma(x_[:], self.tok_in(t), writes=[x_.b])
                        self.norm_tile_to_hT(x_, h, tl * 128, l, 1, cls, scr_l[tl % 2])

                    def qkv_fn(tl):
                        t = t0 + tl
                        qk_ = qkv2[tl % 2]
                        for (c0, w_) in ((0, 512), (512, 256)):
                            p = self.ps_get()
                            for k in range(8):
                                fw.pe.op(lambda: nc.tensor.matmul(p[:, 0:w_], lhsT=h[:, k, tl * 128:(tl + 1) * 128], rhs=win[:, k, c0:c0 + w_],
                                                                  start=(k == 0), stop=(k == 7)), [h.b, win.b], [p.b], inc=(k == 7))
                            if c0 == 0:
                                fw.act.op(lambda: nc.scalar.copy(out=qk_[:, 0:512].rearrange("p (j a d) -> p a j d", a=2, d=64),
                                                                 in_=p[:, 0:512].rearrange("p (a j d) -> p a j d", a=2, j=4)), [p.b], [qk_.b])
                            else:
                                fw.act.op(lambda: nc.scalar.copy(out=qk_[:, c0:c0 + w_], in_=p[:, 0:w_]), [p.b], [qk_.b])
                            self.ps_put(p)
                        self.qk_post(qk_, t, gqk, scr_l[tl % 2], qT, kT, True, None if t < 2 else t - 2)
                        self.v_fill(qk_, t, Vp)

                    for tl0 in range(0, ntl, 2):
                        ILV.run([(lambda tl=tl: norm_fn(tl)) for tl in range(tl0, min(tl0 + 2, ntl))])
                    for tl0 in range(0, ntl, 2):
                        ILV.run([(lambda tl=tl: qkv_fn(tl)) for tl in range(tl0, min(tl0 + 2, ntl))])
                    off = GLU_OFF_CTX if t0 < 2 else GLU_OFF_LAT + (t0 - 2) * 128
                    for j in range(4):
                        pa = self.ps_get()
                        pg = self.ps_get()
                        for (pp, cb) in ((pa, 768 + j * 128), (pg, 1280 + j * 128)):
                            for k in range(8):
                                fw.pe.op(lambda: nc.tensor.matmul(pp[:, 0:ntok], lhsT=win[:, k, cb:cb + 128], rhs=h[:, k, 0:ntok],
                                                                  start=(k == 0), stop=(k == 7)), [win.b, h.b], [pp.b], inc=(k == 7))
                        sg = sig[0]
                        fw.act.op(lambda: nc.scalar.activation(out=sg[:, 0:ntok], in_=pg[:, 0:ntok], func=AF.Sigmoid), [pg.b], [sg.b])
                        fw.dve.op(lambda: nc.vector.tensor_tensor(out=gluT[:, j, off:off + ntok], in0=pa[:, 0:ntok], in1=sg[:, 0:ntok], op=ALU.mult),
                                  [pa.b, sg.b], [gluT.b])
                        self.ps_put(pa)
                        self.ps_put(pg)
                    if self.cut(4) or (self.cut(5) and ci == 1):
                        return
                fw.barrier()
            if self.debug:
                self.dbg_dump_bf(ph, "qT", qT[:, 2, :, :], [128, 4, 128], qT.b)
                self.dbg_dump_bf(ph, "kT", kT[:, 0, 0:512], [128, 512], kT.b)
                self.dbg_dump_bf(ph, "glu", gluT[:, :, GLU_OFF_LAT:GLU_OFF_LAT + 128], [128, 4, 128], gluT.b)
            if self.stop == "p1":
                return
            with ExitStack() as p2:
                scr = dict(pexp=[self.tile(p2, f"pexp{i}", [128, 1024], BF16) for i in range(4)], pi=0,
                           rec=[self.tile(p2, f"rec{i}", [128, 512], F32) for i in range(2)],
                           posb=[[self.tile(p2, f"posb{i}{k}", [128, 512], F32) for k in range(2)] for i in range(2)])
                blocks = []
                for qb in range(NT):
                    kts = [(0, None), (1, None)] if qb < 2 else [(k, None) for k in range(NT)]
                    for kv in range(2):
                        blocks.append((kv, qb, kts))
                self.attention(blocks, qT, kT, Vp, oT, scr)
                fw.barrier()
            if self.debug:
                self.dbg_dump_bf(ph, "oT", oT[:, :, 256:384], [128, 4, 128], oT.b)
                self.dbg_dump_bf(ph, "oTc", oT[:, :, 0:128], [128, 4, 128], oT.b)
            pattn.close()
            if self.stop == "p2":
                return
            bT = self.tile(ph, "bT", [128, 4, T], BF16)
            with ExitStack() as p3:
                cw = self.tile(p3, "cw", [128, 4, 31], F32)
                cv = self.tile(p3, "cv", [128, 3, 4], F32)
                diag = self.tile(p3, "diag", [128, 4, 31, 128], BF16)
                onesM = self.tile(p3, "onesM", [128, 128], F32)
                fw.q_sync.dma(cw[:], self.convwT, writes=[cw.b])
                fw.q_sync.dma(cv[:], self.convv, writes=[cv.b])
                fw.dve.op(lambda: nc.vector.memset(onesM[:], 1.0 / 512), [], [onesM.b])
                for j in range(4):
                    for tap in range(31):
                        e_ = fw.dve if (tap % 2 == 0) else fw.pool
                        ee = nc.vector if (tap % 2 == 0) else nc.gpsimd
                        e_.op(lambda: ee.tensor_scalar(out=diag[:, j, tap, :], in0=g["identb"][:], scalar1=cw[:, j, tap:tap + 1], scalar2=None,
                                                       op0=ALU.mult), [g["identb"].b, cw.b], [diag.b])
                ysb = [self.tile(p3, f"ysb{i}", [128, 4, 512], F32) for i in range(2)]
                ysq = [self.tile(p3, f"ysq{i}", [128, 4, 512], F32) for i in range(2)]
                mean_l = [self.tile(p3, f"mean{i}", [128, 512], F32) for i in range(2)]
                rstd_l = [self.tile(p3, f"rstd{i}", [128, 512], F32) for i in range(2)]
                tmp_l = [[self.tile(p3, f"ctmp{i}{k}", [128, 512], F32) for k in range(2)] for i in range(2)]
                chunks = [(GLU_OFF_CTX, 0, 256)] + [(GLU_OFF_LAT + 512 * i, 256 + 512 * i, 512) for i in range(8)]

                def conv_fn(ci):
                    off, tok0, ntok = chunks[ci]
                    mean, rstd, tmp = mean_l[ci % 2], rstd_l[ci % 2], tmp_l[ci % 2]
                    y_, q_ = ysb[ci % 2], ysq[ci % 2]
                    for j in range(4):
                        p = self.ps_get()
                        for tap in range(31):
                            fw.pe.op(lambda: nc.tensor.matmul(p[:, 0:ntok], lhsT=diag[:, j, tap, :], rhs=gluT[:, j, off + tap - 15:off + tap - 15 + ntok],
                                                              start=(tap == 0), stop=(tap == 30)), [diag.b, gluT.b], [p.b], inc=(tap == 30))
                        fw.act.op(lambda: nc.scalar.activation(out=y_[:, j, 0:ntok], in_=p[:, 0:ntok], func=AF.Identity, bias=cv[:, 0, j:j + 1]),
                                  [p.b, cv.b], [y_.b])
                        self.ps_put(p)
                        fw.pool.op(lambda: nc.gpsimd.tensor_tensor(out=q_[:, j, 0:ntok], in0=y_[:, j, 0:ntok], in1=y_[:, j, 0:ntok], op=ALU.mult),
                                   [y_.b], [q_.b])
                    pm = self.ps_get()
                    pq = self.ps_get()
                    for (pp, src) in ((pm, y_), (pq, q_)):
                        for j in range(4):
                            fw.pe.op(lambda: nc.tensor.matmul(pp[:, 0:ntok], lhsT=onesM[:], rhs=src[:, j, 0:ntok], start=(j == 0), stop=(j == 3)),
                                     [onesM.b, src.b], [pp.b], inc=(j == 3))
                    fw.act.op(lambda: nc.scalar.copy(out=mean[:, 0:ntok], in_=pm[:, 0:ntok]), [pm.b], [mean.b])
                    self.ps_put(pm)
                    fw.pool.op(lambda: nc.gpsimd.tensor_tensor(out=rstd[:, 0:ntok], in0=mean[:, 0:ntok], in1=mean[:, 0:ntok], op=ALU.mult),
                               [mean.b], [rstd.b])
                    fw.dve.op(lambda: nc.vector.tensor_tensor(out=rstd[:, 0:ntok], in0=pq[:, 0:ntok], in1=rstd[:, 0:ntok], op=ALU.subtract),
                              [pq.b, rstd.b], [rstd.b])
                    self.ps_put(pq)
                    fw.dve.op(lambda: nc.vector.tensor_scalar(out=rstd[:, 0:ntok], in0=rstd[:, 0:ntok], scalar1=EPS, scalar2=None, op0=ALU.add),
                              [rstd.b], [rstd.b])
                    fw.act.op(lambda: nc.scalar.sqrt(out=rstd[:, 0:ntok], in_=rstd[:, 0:ntok]), [rstd.b], [rstd.b])
                    fw.dve.op(lambda: nc.vector.reciprocal(out=rstd[:, 0:ntok], in_=rstd[:, 0:ntok]), [rstd.b], [rstd.b])
                    for j in range(4):
                        t_ = tmp[j % 2]
                        fw.pool.op(lambda: nc.gpsimd.tensor_tensor(out=t_[:, 0:ntok], in0=y_[:, j, 0:ntok], in1=mean[:, 0:ntok], op=ALU.subtract),
                                   [y_.b, mean.b], [t_.b])
                        fw.dve.op(lambda: nc.vector.tensor_tensor(out=t_[:, 0:ntok], in0=t_[:, 0:ntok], in1=rstd[:, 0:ntok], op=ALU.mult),
                                  [t_.b, rstd.b], [t_.b])
                        fw.act.op(lambda: nc.scalar.activation(out=bT[:, j, tok0:tok0 + ntok], in_=t_[:, 0:ntok], func=AF.Silu,
                                                               scale=cv[:, 1, j:j + 1], bias=cv[:, 2, j:j + 1]), [t_.b, cv.b], [bT.b])

                conv_fn(0)
                for c0_ in range(1, 9, 2):
                    ILV.run([(lambda ci=ci: conv_fn(ci)) for ci in (c0_, c0_ + 1)])
                fw.barrier()
            if self.debug:
                self.dbg_dump_bf(ph, "bT", bT[:, :, 256:384], [128, 4, 128], bT.b)
            if self.stop == "p3":
                return
            with ExitStack() as p4:
                self.out_proj(0, lambda c: (oT, c) if c < 4 else (bT, c - 4), self.w_out_ab, list(range(NT)), p4)
                fw.barrier()

    def dbg_dump_bf(self, ph, name, src_ap, shape, buf):
        nc, fw = self.nc, self.fw
        n = int(np.prod(shape[1:]))
        ap = self.dbg(name, [128, n])
        with ExitStack() as ds:
            tf = self.tile(ds, "dbgt_" + name, shape, F32)
            fw.dve.op(lambda: nc.vector.tensor_copy(out=tf[:], in_=src_ap), [buf], [tf.b])
            flat = tf[:] if len(shape) == 2 else tf[:].rearrange("p a b -> p (a b)")
            fw.q_pool.dma(ap, flat, reads=[tf.b], writes=[self.dbg_out[name][1]])
            fw.barrier()

    def moe(self, l):
        nc, fw, g = self.nc, self.fw, self.g
        if l == 0:
            groups = [list(range(0, 10)), list(range(10, 18)), list(range(18, 26)), list(range(26, 34))]
        else:
            groups = [list(range(2 + 8 * i, 10 + 8 * i)) for i in range(4)]
        ngrp = int(os.environ.get("MOE_GROUPS", "4"))
        nexp = int(os.environ.get("MOE_EXPERTS", "32"))
        classes = [0, 1] if l == 0 else [0]
        with ExitStack() as ph:
            wr = self.tile(ph, "wr", [128, 8, 36], F32)
            rb = self.tile(ph, "rb", [128, 36], F32)
            fw.q_sync.dma(wr[:], self.rt_w[l].rearrange("(k p) n -> p k n", p=128), writes=[wr.b])
            fw.q_sync.dma(rb[:], self.rt_b[:, l, :], writes=[rb.b])
            G2B = {}
            tmpb = self.tile(ph, "bct2", [128, 128], F32)
            for cls in classes:
                G2B[cls] = self.tile(ph, f"G2B{cls}", [128, D], F32)
                self.bcast_tile(G2B[cls], lambda c: g["modT"][:, l, 40 + c, cls:cls + 1], tmpb)
            stg = [self.tile(ph, f"estg{i}", [128, 4096], F32) for i in range(2)]
            wg = [self.tile(ph, f"wg{i}", [128, 8, 512], BF16) for i in range(2)]
            wu = [self.tile(ph, f"wu{i}", [128, 8, 512], BF16) for i in range(2)]
            wd = {cls: [self.tile(ph, f"wd{cls}_{i}", [128, 4, D], BF16) for i in range(2)] for cls in classes}
            xg = self.tile(ph, "xg", [128, 10, D], F32)
            h2T = self.tile(ph, "h2T", [128, 8, 1280], BF16)
            Wt = self.tile(ph, "Wt", [128, 10, 32], F32)
            xn = self.tile(ph, "xn2", [128, D], F32)
            hTf = self.tile(ph, "hTf", [128, 8, 128], F32)
            ss = self.tile(ph, "ss2", [128, 1], F32)
            r_ = {k: self.tile(ph, "r_" + k, [128, n], F32) for k, n in
                  dict(lg=36, gmax=1, ngmax=1, goh=4, ge=4, gsum=1, pen=4, em=32, m8=8, d=1, ed=1, p1=1, p2=1, t1=32, t2=32).items()}
            hid = [self.tile(ph, f"hid{i}", [128, 4, 512], BF16) for i in range(2)]
            sgl = [self.tile(ph, f"sgl{i}", [128, 512], F32) for i in range(2)]
            self.stg_i = 0

            def load_expert(e, need_ctx):
                i = e % 2
                self.load_cast_w(stg, wg[i], slice(0, 512), self.ex_gate[l, e], 8, 512, 0)
                self.load_cast_w(stg, wu[i], slice(0, 512), self.ex_up[l, e], 8, 512, 0)
                s_ = stg[self.stg_i % 2]
                self.stg_i += 1
                fw.q_sync.dma(s_[:].rearrange("p (k n) -> p k n", k=4), self.ex_down[l, e].rearrange("(k p) n -> p k n", p=128), writes=[s_.b])
                for cls in classes:
                    if cls == 1 and not need_ctx:
                        continue
                    fw.dve.op(lambda: nc.vector.tensor_tensor(out=wd[cls][i][:], in0=s_[:].rearrange("p (k n) -> p k n", k=4),
                                                              in1=G2B[cls][:].unsqueeze(1).to_broadcast([128, 4, D]), op=ALU.mult),
                              [s_.b, G2B[cls].b], [wd[cls][i].b])

            for gi, tiles in enumerate(groups[:ngrp]):
                has_ctx = (l == 0 and gi == 0)
                load_expert(0, has_ctx)
                for ti, t in enumerate(tiles):
                    cls = 1 if t < 2 else 0
                    fw.q_sync.dma(xg[:, ti, :], self.xres[t * 128:(t + 1) * 128, :], reads=[self.xres_b[t]], writes=[xg.b])
                    fw.act.op(lambda: nc.scalar.activation(out=xn[:], in_=xg[:, ti, :], func=AF.Square, accum_out=ss[:, 0:1]), [xg.b], [xn.b, ss.b])
                    self.rstd_from_ss(ss, 1.0 / D, 1)
                    fw.act.op(lambda: nc.scalar.activation(out=xn[:], in_=xg[:, ti, :], func=AF.Copy, scale=ss[:, 0:1]), [xg.b, ss.b], [xn.b])
                    for hb in range(2):
                        p = self.ps_get()
                        pv = p[:].rearrange("p (c q) -> p c q", c=4)
                        for c4 in range(4):
                            c = hb * 4 + c4
                            fw.pe.op(lambda: nc.tensor.transpose(out=pv[:, c4, :], in_=xn[:, c * 128:(c + 1) * 128], identity=g["identf"][:]),
                                     [xn.b, g["identf"].b], [p.b], inc=(c4 == 3))
                        for c4 in range(4):
                            c = hb * 4 + c4
                            if hb == 0:
                                fw.dve.op(lambda: nc.vector.tensor_scalar(out=hTf[:, c, :], in0=pv[:, c4, :], scalar1=g["A2"][:, l, c, cls:cls + 1],
                                                                          scalar2=g["modT"][:, l, 24 + c, cls:cls + 1], op0=ALU.mult, op1=ALU.add),
                                          [p.b, g["A2"].b, g["modT"].b], [hTf.b])
                            else:
                                fw.act.op(lambda: nc.scalar.activation(out=hTf[:, c, :], in_=pv[:, c4, :], func=AF.Identity,
                                                                       scale=g["A2"][:, l, c, cls:cls + 1], bias=g["modT"][:, l, 24 + c, cls:cls + 1]),
                                          [p.b, g["A2"].b, g["modT"].b], [hTf.b])
                        self.ps_put(p)
                    fw.pool.op(lambda: nc.gpsimd.tensor_copy(out=h2T[:, :, ti * 128:(ti + 1) * 128], in_=hTf[:]), [hTf.b], [h2T.b])
                    pr = self.ps_get()
                    for c in range(8):
                        fw.pe.op(lambda: nc.tensor.matmul(pr[:, 0:36], lhsT=hTf[:, c, :], rhs=wr[:, c, :], start=(c == 0), stop=(c == 7)),
                                 [hTf.b, wr.b], [pr.b], inc=(c == 7))
                    self.route(pr, rb, r_, Wt, ti)
                    self.ps_put(pr)
                if self.debug and gi == 0:
                    self.dbg_dump_bf(ph, f"Wt{l}", Wt[:, 0:4, :], [128, 4, 32], Wt.b)
                if has_ctx:
                    chunks = [(0, 2), (2, 4), (6, 4)]
                else:
                    chunks = [(0, 4), (4, 4)]
                for e in range(nexp):
                    if e + 1 < nexp:
                        load_expert(e + 1, has_ctx)
                    i = e % 2
                    for ci, (tl0, ntl) in enumerate(chunks):
                        cls = 1 if (has_ctx and ci == 0) else 0
                        ntok = ntl * 128
                        c0 = tl0 * 128
                        hd = hid[(e * len(chunks) + ci) % 2]
                        for f in range(4):
                            pg = self.ps_get()
                            pu = self.ps_get()
                            for (pp, w_) in ((pg, wg[i]), (pu, wu[i])):
                                for k in range(8):
                                    fw.pe.op(lambda: nc.tensor.matmul(pp[:, 0:ntok], lhsT=w_[:, k, f * 128:(f + 1) * 128], rhs=h2T[:, k, c0:c0 + ntok],
                                                                      start=(k == 0), stop=(k == 7)), [w_.b, h2T.b], [pp.b], inc=(k == 7))
                            sg = sgl[f % 2]
                            fw.act.op(lambda: nc.scalar.activation(out=sg[:, 0:ntok], in_=pg[:, 0:ntok], func=AF.Silu), [pg.b], [sg.b])
                            fw.dve.op(lambda: nc.vector.tensor_tensor(out=hd[:, f, 0:ntok], in0=pu[:, 0:ntok], in1=sg[:, 0:ntok], op=ALU.mult),
                                      [pu.b, sg.b], [hd.b])
                            self.ps_put(pg)
                            self.ps_put(pu)
                        for tl in range(ntl):
                            ti = tl0 + tl
                            for half in range(2):
                                pd = self.ps_get()
                                for f in range(4):
                                    fw.pe.op(lambda: nc.tensor.matmul(pd[:], lhsT=hd[:, f, tl * 128:(tl + 1) * 128],
                                                                      rhs=wd[cls][i][:, f, half * 512:(half + 1) * 512], start=(f == 0), stop=(f == 3)),
                                             [hd.b, wd[cls][i].b], [pd.b], inc=(f == 3))
                                fw.dve.op(lambda: nc.vector.scalar_tensor_tensor(out=xg[:, ti, half * 512:(half + 1) * 512], in0=pd[:],
                                                                                 scalar=Wt[:, ti, e:e + 1], in1=xg[:, ti, half * 512:(half + 1) * 512],
                                                                                 op0=ALU.mult, op1=ALU.add), [pd.b, Wt.b, xg.b], [xg.b])
                                self.ps_put(pd)
                for ti, t in enumerate(tiles):
                    if l == 0:
                        fw.q_pool.dma(self.xres[t * 128:(t + 1) * 128, :], xg[:, ti, :], reads=[xg.b], writes=[self.xres_b[t]])
                    else:
                        fw.q_pool.dma(self.y_out[(t - 2) * 128:(t - 1) * 128, :], xg[:, ti, :], reads=[xg.b], writes=[self.y_b[t]])
                    if self.debug and t in (0, 2, NT - 1):
                        nm = f"x2_{l}_{t}"
                        ap = self.dbg(nm, [128, D])
                        fw.q_pool.dma(ap, xg[:, ti, :], reads=[xg.b], writes=[self.dbg_out[nm][1]])
            fw.barrier()

    def moe2(self, l):
        nc, fw, g = self.nc, self.fw, self.g
        V = nc.vector
        tiles = list(range(NT)) if l == 0 else list(range(2, NT))
        ntl = len(tiles)
        NA = 2 * ntl
        NB = (2 * ntl * 128 + 32 * 127 + 127) // 128
        classes = [0, 1] if l == 0 else [0]
        hs = nc.dram_tensor(f"hs{l}", [NB * 128, D], BF16, kind="Internal").ap()
        ys = nc.dram_tensor(f"ys{l}", [NB * 128, D], F32, kind="Internal").ap()
        blkE_d = nc.dram_tensor(f"blkE{l}", [1, NB], I32, kind="Internal").ap()
        hs_bs = [Buf() for _ in range(NA)]
        ys_bs = [Buf() for _ in range(NB)]
        blkE_b = Buf()
        dd = lambda fn, rd, wr_: fw.dve.op(fn, [x.b for x in rd], [x.b for x in wr_])
        with ExitStack() as pm:
            DESTi = self.tile(pm, "DESTi", [128, NA], I32)
            W12 = self.tile(pm, "W12", [128, NA], F32)
            WIDX = self.tile(pm, "WIDX", [128, NB], I32)
            G2B = {}
            for cls in classes:
                G2B[cls] = self.tile(pm, f"G2Bm{cls}", [128, D], F32)
            with ExitStack() as ph:
                tmpb = self.tile(ph, "bct3", [128, 128], F32)
                A2B, B2B = {}, {}
                for cls in classes:
                    A2B[cls] = self.tile(ph, f"A2B{cls}", [128, D], F32)
                    B2B[cls] = self.tile(ph, f"B2B{cls}", [128, D], F32)
                    self.bcast_tile(G2B[cls], lambda c: g["modT"][:, l, 40 + c, cls:cls + 1], tmpb)
                    self.bcast_tile(A2B[cls], lambda c: g["A2"][:, l, c, cls:cls + 1], tmpb, extra=[g["A2"].b])
                    self.bcast_tile(B2B[cls], lambda c: g["modT"][:, l, 24 + c, cls:cls + 1], tmpb)
                wr = self.tile(ph, "wr", [128, 8, 36], F32)
                rb = self.tile(ph, "rb", [128, 36], F32)
                mc = self.tile(ph, "mc", [128, 433], F32)
                Uf = self.tile(ph, "Uf", [128, 128], F32)
                Ub = self.tile(ph, "Ub", [128, 128], BF16)
                onesb = self.tile(ph, "onesb", [128, 128], BF16)
                fw.q_sync.dma(wr[:], self.rt_w[l].rearrange("(k p) n -> p k n", p=128), writes=[wr.b])
                fw.q_sync.dma(rb[:], self.rt_b[:, l, :], writes=[rb.b])
                fw.q_sync.dma(mc[:], self.mconst, writes=[mc.b])
                fw.q_sync.dma(Uf[:], self.umat, writes=[Uf.b])
                dd(lambda: V.tensor_copy(out=Ub[:], in_=Uf[:]), [Uf], [Ub])
                dd(lambda: V.memset(onesb[:], 1.0), [], [onesb])
                zt = self.tile(ph, "zt", [128, 4096], BF16)
                fw.pool.op(lambda: nc.gpsimd.memset(zt[:], 0.0), [], [zt.b])
                hz_b = []
                for r0 in range(0, NB * 128, 512):
                    nr = min(512, NB * 128 - r0)
                    hb_ = Buf("hz")
                    fw.q_pool.dma(hs[r0:r0 + nr, :].rearrange("(p k) d -> p (k d)", p=128), zt[:, 0:(nr // 128) * D], reads=[zt.b], writes=[hb_])
                    hz_b.append(hb_)
                h2tm = self.tile(ph, "h2tm", [128, ntl, D], BF16)
                OH = self.tile(ph, "OH", [128, NA, 32], BF16)
                RK = self.tile(ph, "RK", [128, NA], F32)
                run = self.tile(ph, "run", [128, 32], F32)
                dd(lambda: V.memset(run[:], 0.0), [], [run])
                xt = [self.tile(ph, f"mxt{i}", [128, D], F32) for i in range(2)]
                xn_l = [self.tile(ph, f"mxn{i}", [128, D], F32) for i in range(2)]
                xm_l = [self.tile(ph, f"mxm{i}", [128, D], F32) for i in range(2)]
                hTf_l = [self.tile(ph, f"mhTf{i}", [128, 8, 128], F32) for i in range(2)]
                ss_l = [self.tile(ph, f"mss{i}", [128, 1], F32) for i in range(2)]
                r_l = [{k: self.tile(ph, f"r{i}_" + k, [128, n], F32) for k, n in
                        dict(lg=36, gmax=1, ngmax=1, goh=4, ge=4, gsum=1, pen=4, em=32, m8=8, d=1, ed=1, p1=1, p2=1, t1=32, t2=32, rf=32).items()}
                       for i in range(2)]
                def tile_fn(ti, t):
                    cls = 1 if t < 2 else 0
                    xn, xm, hTf, ss, r_ = xn_l[ti % 2], xm_l[ti % 2], hTf_l[ti % 2], ss_l[ti % 2], r_l[ti % 2]
                    x_ = xt[ti % 2]
                    fw.q_sync.dma(x_[:], self.xres[t * 128:(t + 1) * 128, :], reads=[self.xres_b[t]], writes=[x_.b])
                    fw.act.op(lambda: nc.scalar.activation(out=xn[:], in_=x_[:], func=AF.Square, accum_out=ss[:, 0:1]), [x_.b], [xn.b, ss.b])
                    self.rstd_from_ss(ss, 1.0 / D, 1)
                    fw.act.op(lambda: nc.scalar.activation(out=xn[:], in_=x_[:], func=AF.Copy, scale=ss[:, 0:1]), [x_.b, ss.b], [xn.b])
                    fw.pool.op(lambda: nc.gpsimd.tensor_tensor(out=xm[:], in0=xn[:], in1=A2B[cls][:], op=ALU.mult), [xn.b, A2B[cls].b], [xm.b])
                    fw.pool.op(lambda: nc.gpsimd.tensor_tensor(out=h2tm[:, ti, :], in0=xm[:], in1=B2B[cls][:], op=ALU.add), [xm.b, B2B[cls].b], [h2tm.b])
                    for hb in range(2):
                        p = self.ps_get()
                        pv = p[:].rearrange("p (c q) -> p c q", c=4)
                        for c4 in range(4):
                            c = hb * 4 + c4
                            fw.pe.op(lambda: nc.tensor.transpose(out=pv[:, c4, :], in_=xn[:, c * 128:(c + 1) * 128], identity=g["identf"][:]),
                                     [xn.b, g["identf"].b], [p.b], inc=(c4 == 3))
                        for c4 in range(4):
                            c = hb * 4 + c4
                            if hb == 0:
                                fw.dve.op(lambda: V.tensor_scalar(out=hTf[:, c, :], in0=pv[:, c4, :], scalar1=g["A2"][:, l, c, cls:cls + 1],
                                                                  scalar2=g["modT"][:, l, 24 + c, cls:cls + 1], op0=ALU.mult, op1=ALU.add),
                                          [p.b, g["A2"].b, g["modT"].b], [hTf.b])
                            else:
                                fw.act.op(lambda: nc.scalar.activation(out=hTf[:, c, :], in_=pv[:, c4, :], func=AF.Identity,
                                                                       scale=g["A2"][:, l, c, cls:cls + 1], bias=g["modT"][:, l, 24 + c, cls:cls + 1]),
                                          [p.b, g["A2"].b, g["modT"].b], [hTf.b])
                        self.ps_put(p)
                    pr = self.ps_get()
                    for c in range(8):
                        fw.pe.op(lambda: nc.tensor.matmul(pr[:, 0:36], lhsT=hTf[:, c, :], rhs=wr[:, c, :], start=(c == 0), stop=(c == 7)),
                                 [hTf.b, wr.b], [pr.b], inc=(c == 7))
                    self.route(pr, rb, r_, None, ti, OH=OH, W12=W12)
                    self.ps_put(pr)

                def rank_fn(ti):
                    r_ = r_l[ti % 2]
                    for k in range(2):
                        a = 2 * ti + k
                        pk = self.ps_get()
                        fw.pe.op(lambda: nc.tensor.matmul(pk[:, 0:32], lhsT=Ub[:], rhs=OH[:, a, :], start=True, stop=True), [Ub.b, OH.b], [pk.b], inc=False)
                        fw.pe.op(lambda: nc.tensor.matmul(pk[:, 32:64], lhsT=onesb[:], rhs=OH[:, a, :], start=True, stop=True), [onesb.b, OH.b], [pk.b])
                        rf = r_["rf"]
                        dd(lambda: V.tensor_tensor(out=rf[:], in0=pk[:, 0:32], in1=run[:], op=ALU.add), [pk, run], [rf])
                        dd(lambda: V.tensor_tensor(out=rf[:], in0=rf[:], in1=OH[:, a, :], op=ALU.mult), [rf, OH], [rf])
                        dd(lambda: V.tensor_reduce(out=RK[:, a:a + 1], in_=rf[:], axis=AX.X, op=ALU.add), [rf], [RK])
                        dd(lambda: V.tensor_tensor(out=run[:], in0=pk[:, 32:64], in1=run[:], op=ALU.add), [pk, run], [run])
                        self.ps_put(pk)
                for ti0 in range(0, ntl, 2):
                    pair = list(range(ti0, min(ti0 + 2, ntl)))
                    ILV.run([(lambda ti=ti: tile_fn(ti, tiles[ti])) for ti in pair])
                    for ti in pair:
                        rank_fn(ti)
                cmpf = self.tile(ph, "cmp", [128, 5120], F32)
                v3 = lambda n_a, n_b: cmpf[:, 0:n_a * n_b].rearrange("p (a b) -> p a b", b=n_b)
                T_ = lambda nm, n: self.tile(ph, nm, [128, n], F32)
                nblk, exc, nbp, mm, pbx = T_("nblk", 32), T_("exc", 32), T_("nbp", 32), T_("mm", 32), T_("pbx", 32)
                sc = [T_(f"scan{i}", 32) for i in range(2)]
                blkf, dstf = T_("blkf", NB), T_("dstf", NA)
                selX, selM, selS, jf, ta_, tb_ = T_("selX", NA), T_("selM", NA), T_("selS", NA), T_("jf", NA), T_("ta_", NA), T_("tb_", NA)
                pend, pbase, m2k, lsk = T_("pend", 16), T_("pbase", 16), T_("m2k", 16), T_("lsk", 16)
                kidx, pbp, m2p, lsp, o_, q_, par_, int_ = (T_(n_, NB) for n_ in ("kidx", "pbp", "m2p", "lsp", "o_", "q_", "par_", "int_"))
                IOTA32, THR, IOTAB, PAR32, PARB, THR2, THR3, IOTA16 = (mc[:, 0:32], mc[:, 32:67], mc[:, 67:67 + NB], mc[:, 167:199],
                                                                     mc[:, 199:199 + NB], mc[:, 299:367], mc[:, 367:417], mc[:, 417:433])
                c3 = v3(32, 35)
                dd(lambda: V.tensor_tensor(out=c3, in0=run[:].unsqueeze(2).to_broadcast([128, 32, 35]),
                                           in1=THR.unsqueeze(1).to_broadcast([128, 32, 35]), op=ALU.is_gt), [run, mc], [cmpf])
                dd(lambda: V.tensor_reduce(out=nblk[:], in_=c3, axis=AX.X, op=ALU.add), [cmpf], [nblk])
                dd(lambda: V.tensor_copy(out=sc[0][:], in_=nblk[:]), [nblk], [sc[0]])
                cur = 0
                for sh in (1, 2, 4, 8, 16):
                    a_, b_ = sc[cur], sc[1 - cur]
                    dd(lambda: V.tensor_copy(out=b_[:, 0:sh], in_=a_[:, 0:sh]), [a_], [b_])
                    dd(lambda: V.tensor_tensor(out=b_[:, sh:32], in0=a_[:, sh:32], in1=a_[:, 0:32 - sh], op=ALU.add), [a_], [b_])
                    cur = 1 - cur
                inc_ = sc[cur]
                dd(lambda: V.tensor_tensor(out=exc[:], in0=inc_[:], in1=nblk[:], op=ALU.subtract), [inc_, nblk], [exc])
                pr2 = lambda tl: tl[:].rearrange("p (k s) -> p k s", s=2)
                dd(lambda: V.tensor_copy(out=pr2(nbp)[:, :, 0:1], in_=pr2(nblk)[:, :, 1:2]), [nblk], [nbp])
                dd(lambda: V.tensor_copy(out=pr2(nbp)[:, :, 1:2], in_=pr2(nblk)[:, :, 0:1]), [nblk], [nbp])
                dd(lambda: V.tensor_tensor(out=mm[:], in0=nblk[:], in1=nbp[:], op=ALU.min), [nblk, nbp], [mm])
                dd(lambda: V.tensor_scalar(out=pr2(pbx)[:, :, 0:1], in0=pr2(exc)[:, :, 0:1], scalar1=128.0, scalar2=None, op0=ALU.mult), [exc], [pbx])
                dd(lambda: V.tensor_scalar(out=pr2(pbx)[:, :, 1:2], in0=pr2(exc)[:, :, 0:1], scalar1=128.0, scalar2=None, op0=ALU.mult), [exc], [pbx])
                c4_ = v3(NA, 32)
                for (dst_, vec_, vb_) in ((selX, pbx[:], pbx.b), (selM, mm[:], mm.b), (selS, PAR32, mc.b)):
                    dd(lambda: V.tensor_tensor(out=c4_, in0=OH[:], in1=vec_.unsqueeze(1).to_broadcast([128, NA, 32]), op=ALU.mult), [OH, Tile(None, vb_)], [cmpf])
                    dd(lambda: V.tensor_reduce(out=dst_[:], in_=c4_, axis=AX.X, op=ALU.add), [cmpf], [dst_])
                c5_ = v3(NA, 68)
                dd(lambda: V.tensor_tensor(out=c5_, in0=RK[:].unsqueeze(2).to_broadcast([128, NA, 68]),
                                           in1=THR2.unsqueeze(1).to_broadcast([128, NA, 68]), op=ALU.is_ge), [RK, mc], [cmpf])
                dd(lambda: V.tensor_reduce(out=jf[:], in_=c5_, axis=AX.X, op=ALU.add), [cmpf], [jf])
                dd(lambda: V.scalar_tensor_tensor(out=ta_[:], in0=jf[:], scalar=2.0, in1=selS[:], op0=ALU.mult, op1=ALU.add), [jf, selS], [ta_])
                dd(lambda: V.tensor_tensor(out=tb_[:], in0=selM[:], in1=jf[:], op=ALU.add), [selM, jf], [tb_])
                dd(lambda: V.tensor_tensor(out=ta_[:], in0=ta_[:], in1=tb_[:], op=ALU.min), [ta_, tb_], [ta_])
                dd(lambda: V.tensor_tensor(out=ta_[:], in0=ta_[:], in1=jf[:], op=ALU.subtract), [ta_, jf], [ta_])
                dd(lambda: V.scalar_tensor_tensor(out=dstf[:], in0=ta_[:], scalar=128.0, in1=selX[:], op0=ALU.mult, op1=ALU.add), [ta_, selX], [dstf])
                dd(lambda: V.tensor_tensor(out=dstf[:], in0=dstf[:], in1=RK[:], op=ALU.add), [dstf, RK], [dstf])
                dd(lambda: V.tensor_copy(out=DESTi[:], in_=dstf[:]), [dstf], [DESTi])
                dd(lambda: V.tensor_copy(out=pend[:], in_=pr2(inc_)[:, :, 1]), [inc_], [pend])
                dd(lambda: V.tensor_copy(out=pbase[:], in_=pr2(exc)[:, :, 0]), [exc], [pbase])
                dd(lambda: V.tensor_scalar(out=m2k[:], in0=pr2(mm)[:, :, 0], scalar1=2.0, scalar2=None, op0=ALU.mult), [mm], [m2k])
                dd(lambda: V.tensor_tensor(out=lsk[:], in0=pr2(nblk)[:, :, 1], in1=pr2(nblk)[:, :, 0], op=ALU.is_gt), [nblk], [lsk])
                c6_ = v3(NB, 16)
                dd(lambda: V.tensor_tensor(out=c6_, in0=pend[:].unsqueeze(1).to_broadcast([128, NB, 16]),
                                           in1=IOTAB.unsqueeze(2).to_broadcast([128, NB, 16]), op=ALU.is_le), [pend, mc], [cmpf])
                dd(lambda: V.tensor_reduce(out=kidx[:], in_=c6_, axis=AX.X, op=ALU.add), [cmpf], [kidx])
                dd(lambda: V.tensor_scalar(out=kidx[:], in0=kidx[:], scalar1=15.0, scalar2=None, op0=ALU.min), [kidx], [kidx])
                ohk = self.tile(ph, "ohk", [128, NB, 16], F32)
                dd(lambda: V.tensor_tensor(out=ohk[:], in0=kidx[:].unsqueeze(2).to_broadcast([128, NB, 16]),
                                           in1=IOTA16.unsqueeze(1).to_broadcast([128, NB, 16]), op=ALU.is_equal), [kidx, mc], [ohk])
                for (dst_, vec_) in ((pbp, pbase), (m2p, m2k), (lsp, lsk)):
                    dd(lambda: V.tensor_tensor(out=c6_, in0=ohk[:], in1=vec_[:].unsqueeze(1).to_broadcast([128, NB, 16]), op=ALU.mult), [ohk, vec_], [cmpf])
                    dd(lambda: V.tensor_reduce(out=dst_[:], in_=c6_, axis=AX.X, op=ALU.add), [cmpf], [dst_])
                dd(lambda: V.tensor_tensor(out=o_[:], in0=IOTAB, in1=pbp[:], op=ALU.subtract), [mc, pbp], [o_])
                c7_ = v3(NB, 50)
                dd(lambda: V.tensor_tensor(out=c7_, in0=o_[:].unsqueeze(2).to_broadcast([128, NB, 50]),
                                           in1=THR3.unsqueeze(1).to_broadcast([128, NB, 50]), op=ALU.is_ge), [o_, mc], [cmpf])
                dd(lambda: V.tensor_reduce(out=q_[:], in_=c7_, axis=AX.X, op=ALU.add), [cmpf], [q_])
                dd(lambda: V.scalar_tensor_tensor(out=par_[:], in0=q_[:], scalar=-2.0, in1=o_[:], op0=ALU.mult, op1=ALU.add), [q_, o_], [par_])
                dd(lambda: V.tensor_tensor(out=int_[:], in0=o_[:], in1=m2p[:], op=ALU.is_lt), [o_, m2p], [int_])
                dd(lambda: V.tensor_tensor(out=par_[:], in0=par_[:], in1=lsp[:], op=ALU.subtract), [par_, lsp], [par_])
                dd(lambda: V.tensor_tensor(out=par_[:], in0=par_[:], in1=int_[:], op=ALU.mult), [par_, int_], [par_])
                dd(lambda: V.tensor_tensor(out=par_[:], in0=par_[:], in1=lsp[:], op=ALU.add), [par_, lsp], [par_])
                dd(lambda: V.scalar_tensor_tensor(out=blkf[:], in0=kidx[:], scalar=2.0, in1=par_[:], op0=ALU.mult, op1=ALU.add), [kidx, par_], [blkf])
                pix = self.tile(ph, "pix", [128, 1], F32)
                fw.q_sync.dma(pix[:], self.pidx, writes=[pix.b])
                dd(lambda: V.tensor_scalar(out=blkf[:], in0=blkf[:], scalar1=128.0, scalar2=pix[:, 0:1], op0=ALU.mult, op1=ALU.add), [blkf, pix], [blkf])
                if l == 1:
                    dd(lambda: V.tensor_scalar(out=blkf[:], in0=blkf[:], scalar1=4096.0, scalar2=None, op0=ALU.add), [blkf], [blkf])
                same2 = self.tile(ph, "same2", [128, NB], F32)
                dd(lambda: V.tensor_tensor(out=same2[:, 2:NB], in0=blkf[:, 2:NB], in1=blkf[:, 0:NB - 2], op=ALU.is_equal), [blkf], [same2])
                dd(lambda: V.tensor_scalar(out=same2[:, 2:NB], in0=same2[:, 2:NB], scalar1=1.0e6, scalar2=None, op0=ALU.mult), [same2], [same2])
                dd(lambda: V.tensor_tensor(out=blkf[:, 2:NB], in0=blkf[:, 2:NB], in1=same2[:, 2:NB], op=ALU.add), [blkf, same2], [blkf])
                dd(lambda: V.tensor_copy(out=WIDX[:], in_=blkf[:]), [blkf], [WIDX])
                if self.debug:
                    self.dbg_dump_bf(ph, f"dst{l}", dstf[:, 0:8], [128, 8], dstf.b)
                    self.dbg_dump_bf(ph, f"blk{l}", blkf[:, 0:NB], [128, NB], blkf.b)
                    self.dbg_dump_bf(ph, f"cnt{l}", run[:, 0:32], [128, 32], run.b)
                if os.environ.get("MOE_PROBE") == "1":
                    def tryv(nm, f):
                        try:
                            f(); print("PROBE ok", nm, flush=True)
                        except Exception as e:
                            print("PROBE fail", nm, repr(e)[:120], flush=True)
                    tryv("base", lambda: nc.gpsimd.indirect_dma_start(out=hs[:, :], out_offset=bass.IndirectOffsetOnAxis(ap=DESTi[:, 0:1], axis=0),
                                                                      in_=h2tm[:, 0, :], in_offset=None, bounds_check=NB * 128 - 1, oob_is_err=False))
                    tryv("xn_f32_ys", lambda: nc.gpsimd.indirect_dma_start(out=ys[:, :], out_offset=bass.IndirectOffsetOnAxis(ap=DESTi[:, 0:1], axis=0),
                                                                      in_=xn[:, :], in_offset=None, bounds_check=NB * 128 - 1, oob_is_err=False))
                    tryv("blki_idx", lambda: nc.gpsimd.indirect_dma_start(out=hs[:, :], out_offset=bass.IndirectOffsetOnAxis(ap=blki[:, 0:1], axis=0),
                                                                      in_=h2tm[:, 0, :], in_offset=None, bounds_check=NB * 128 - 1, oob_is_err=False))
                    tryv("gather", lambda: nc.gpsimd.indirect_dma_start(out=xn[:, :], out_offset=None, in_=ys[:, :],
                                                                      in_offset=bass.IndirectOffsetOnAxis(ap=DESTi[:, 0:1], axis=0), bounds_check=NB * 128 - 1, oob_is_err=False))
                    tryv("plain", lambda: nc.gpsimd.dma_start(out=ys[0:128, :], in_=xn[:, :]))
                for a in range(NA):
                    if os.environ.get("MOE_PROBE") == "1":
                        print("PROBE scatter a", a, flush=True)
                    fw.q_pool.dma_fn(lambda: nc.gpsimd.indirect_dma_start(
                        out=hs[:, :], out_offset=bass.IndirectOffsetOnAxis(ap=DESTi[:, a:a + 1], axis=0),
                        in_=h2tm[:, a // 2, :], in_offset=None),
                        reads=[h2tm.b, DESTi.b] + hz_b, writes=[hs_bs[a]])
                fw.barrier()
            with ExitStack() as ph:
                stg = {k: [self.tile(ph, f"bs{k}{i}", [128, 4096], F32) for i in range(2)] for k in "gud"}
                wgt = {k: [self.tile(ph, f"bw{k}{i}", [128, 4096], BF16) for i in range(2)] for k in "gud"}
                xb = [self.tile(ph, f"xb{i}", [128, D], BF16) for i in range(4)]
                xbT = [self.tile(ph, f"xbT{i}", [128, 8, 128], BF16) for i in range(2)]
                sg_l = [self.tile(ph, f"bsg{i}", [128, 512], F32) for i in range(2)]
                hid_l = [self.tile(ph, f"bhid{i}", [128, 512], BF16) for i in range(2)]
                hidT_l = [self.tile(ph, f"bhidT{i}", [128, 4, 128], BF16) for i in range(2)]
                ysb = [self.tile(ph, f"ysb{i}", [128, D], F32) for i in range(2)]
                srcs = dict(g=self.ex_gate, u=self.ex_up, d=self.ex_down)

                def gathers(i):
                    b2 = i % 2
                    for k in "gud":
                        fw.q_pool.dma_fn(lambda: nc.gpsimd.indirect_dma_start(
                            out=stg[k][b2][:, :], out_offset=None, in_=srcs[k].rearrange("l r n -> (l r) n"),
                            in_offset=bass.IndirectOffsetOnAxis(ap=WIDX[:, i:i + 1], axis=0),
                            bounds_check=self.bc_reg, oob_is_err=False),
                            reads=[WIDX.b], writes=[stg[k][b2].b])

                def casts(i):
                    b2 = i % 2
                    fw.dve.op(lambda: V.tensor_copy(out=wgt["g"][b2][:], in_=stg["g"][b2][:]), [stg["g"][b2].b], [wgt["g"][b2].b])
                    fw.act.op(lambda: nc.scalar.copy(out=wgt["u"][b2][:], in_=stg["u"][b2][:]), [stg["u"][b2].b], [wgt["u"][b2].b])
                    fw.dve.op(lambda: V.tensor_copy(out=wgt["d"][b2][:, 0:2048], in_=stg["d"][b2][:, 0:2048]), [stg["d"][b2].b], [wgt["d"][b2].b])
                    fw.act.op(lambda: nc.scalar.copy(out=wgt["d"][b2][:, 2048:4096], in_=stg["d"][b2][:, 2048:4096]), [stg["d"][b2].b], [wgt["d"][b2].b])

                def xload(i):
                    fw.q_sync.dma(xb[i % 4][:], hs[i * 128:(i + 1) * 128, :], reads=hs_bs, writes=[xb[i % 4].b])

                def block_fn(i):
                    b2 = i % 2
                    sg, hid, hidT = sg_l[b2], hid_l[b2], hidT_l[b2]
                    wg_ = wgt["g"][b2][:].rearrange("p (k n) -> p k n", k=8)
                    wu_ = wgt["u"][b2][:].rearrange("p (k n) -> p k n", k=8)
                    wd_ = wgt["d"][b2][:].rearrange("p (k n) -> p k n", k=4)
                    p = self.ps_get()
                    pv = p[:].bitcast(BF16).rearrange("p (c q) -> p c q", c=8)
                    for c in range(8):
                        fw.pe.op(lambda: nc.tensor.transpose(out=pv[:, c, :], in_=xb[i % 4][:, c * 128:(c + 1) * 128], identity=g["identb"][:]),
                                 [xb[i % 4].b, g["identb"].b], [p.b], inc=(c == 7))
                    dd(lambda: V.tensor_copy(out=xbT[b2][:], in_=pv), [p], [xbT[b2]])
                    self.ps_put(p)
                    pg = self.ps_get()
                    pu = self.ps_get()
                    for (pp, w_, wb_) in ((pg, wg_, wgt["g"][b2].b), (pu, wu_, wgt["u"][b2].b)):
                        for k in range(8):
                            fw.pe.op(lambda: nc.tensor.matmul(pp[:], lhsT=xbT[b2][:, k, :], rhs=w_[:, k, :], start=(k == 0), stop=(k == 7)),
                                     [xbT[b2].b, wb_], [pp.b], inc=(k == 7))
                    fw.act.op(lambda: nc.scalar.activation(out=sg[:], in_=pg[:], func=AF.Silu), [pg.b], [sg.b])
                    dd(lambda: V.tensor_tensor(out=hid[:], in0=pu[:], in1=sg[:], op=ALU.mult), [pu, sg], [hid])
                    self.ps_put(pg)
                    self.ps_put(pu)
                    p = self.ps_get()
                    pv = p[:].bitcast(BF16).rearrange("p (c q) -> p c q", c=8)
                    for c in range(4):
                        fw.pe.op(lambda: nc.tensor.transpose(out=pv[:, c, :], in_=hid[:, c * 128:(c + 1) * 128], identity=g["identb"][:]),
                                 [hid.b, g["identb"].b], [p.b], inc=(c == 3))
                    dd(lambda: V.tensor_copy(out=hidT[:], in_=pv[:, 0:4, :]), [p], [hidT])
                    self.ps_put(p)
                    y_ = ysb[b2]
                    pds = []
                    for half in range(2):
                        pd = self.ps_get()
                        pds.append(pd)
                        for f in range(4):
                            fw.pe.op(lambda: nc.tensor.matmul(pd[:], lhsT=hidT[:, f, :], rhs=wd_[:, f, half * 512:(half + 1) * 512],
                                                              start=(f == 0), stop=(f == 3)), [hidT.b, wgt["d"][b2].b], [pd.b], inc=(f == 3))
                    if i + 2 < NB:
                        casts(i + 2)
                    fw.act.op(lambda: nc.scalar.copy(out=y_[:, 0:512], in_=pds[0][:]), [pds[0].b], [y_.b])
                    dd(lambda: V.tensor_copy(out=y_[:, 512:1024], in_=pds[1][:]), [pds[1]], [y_])
                    self.ps_put(pds[0])
                    self.ps_put(pds[1])
                    fw.q_sync.dma(ys[i * 128:(i + 1) * 128, :], y_[:], reads=[y_.b], writes=[ys_bs[i]])

                gathers(0)
                gathers(1)
                for j in range(2):
                    xload(j)
                casts(0)
                casts(1)
                for i0_ in range(0, NB, 2):
                    for j in (i0_ + 2, i0_ + 3):
                        if j < NB:
                            gathers(j)
                            xload(j)
                    ILV.run([(lambda i=i: block_fn(i)) for i in (i0_, i0_ + 1) if i < NB])
                fw.barrier()
            with ExitStack() as ph:
                xt = [self.tile(ph, f"cxt{i}", [128, D], F32) for i in range(3)]
                y1 = [self.tile(ph, f"cy1{i}", [128, D], F32) for i in range(3)]
                y2 = [self.tile(ph, f"cy2{i}", [128, D], F32) for i in range(3)]
                def comb_fn(ti, t):
                    cls = 1 if t < 2 else 0
                    x_, a1, a2 = xt[ti % 3], y1[ti % 3], y2[ti % 3]
                    fw.q_sync.dma(x_[:], self.xres[t * 128:(t + 1) * 128, :], reads=[self.xres_b[t]], writes=[x_.b])
                    for k, yy in ((0, a1), (1, a2)):
                        a = 2 * ti + k
                        fw.q_pool.dma_fn(lambda: nc.gpsimd.indirect_dma_start(
                            out=yy[:, :], out_offset=None, in_=ys[:, :],
                            in_offset=bass.IndirectOffsetOnAxis(ap=DESTi[:, a:a + 1], axis=0)),
                            reads=ys_bs + [DESTi.b], writes=[yy.b])
                    dd(lambda: V.tensor_scalar(out=a1[:], in0=a1[:], scalar1=W12[:, 2 * ti:2 * ti + 1], scalar2=None, op0=ALU.mult), [a1, W12], [a1])
                    dd(lambda: V.scalar_tensor_tensor(out=a1[:], in0=a2[:], scalar=W12[:, 2 * ti + 1:2 * ti + 2], in1=a1[:], op0=ALU.mult, op1=ALU.add),
                       [a2, W12, a1], [a1])
                    dd(lambda: V.tensor_tensor(out=a1[:], in0=a1[:], in1=G2B[cls][:], op=ALU.mult), [a1, G2B[cls]], [a1])
                    dd(lambda: V.tensor_tensor(out=x_[:], in0=x_[:], in1=a1[:], op=ALU.add), [x_, a1], [x_])
                    if l == 0:
                        fw.q_sync.dma(self.xres[t * 128:(t + 1) * 128, :], x_[:], reads=[x_.b], writes=[self.xres_b[t]])
                    else:
                        fw.q_sync.dma(self.y_out[(t - 2) * 128:(t - 1) * 128, :], x_[:], reads=[x_.b], writes=[self.y_b[t]])
                    if self.debug and t in (0, 2, NT - 1):
                        nm = f"x2_{l}_{t}"
                        ap = self.dbg(nm, [128, D])
                        fw.q_sync.dma(ap, x_[:], reads=[x_.b], writes=[self.dbg_out[nm][1]])

                for ti0 in range(0, ntl, 3):
                    pair = list(range(ti0, min(ti0 + 3, ntl)))
                    ILV.run([(lambda ti=ti: comb_fn(ti, tiles[ti])) for ti in pair])
                fw.barrier()

    def route(self, pr, rb, r_, Wt, ti, OH=None, W12=None):
        nc, fw = self.nc, self.fw
        V = nc.vector
        d = lambda fn, rd, wr_: fw.dve.op(fn, [x.b for x in rd], [x.b for x in wr_])
        lg, gmax, ngmax, goh, ge, gsum, pen, em, m8 = (r_[k] for k in ("lg", "gmax", "ngmax", "goh", "ge", "gsum", "pen", "em", "m8"))
        dd, ed, p1, p2, t1, t2 = (r_[k] for k in ("d", "ed", "p1", "p2", "t1", "t2"))
        d(lambda: V.tensor_tensor(out=lg[:], in0=pr[:, 0:36], in1=rb[:], op=ALU.add), [pr, rb], [lg])
        d(lambda: V.tensor_reduce(out=gmax[:], in_=lg[:, 0:4], axis=AX.X, op=ALU.max), [lg], [gmax])
        d(lambda: V.tensor_scalar(out=ngmax[:], in0=gmax[:], scalar1=-1.0, scalar2=None, op0=ALU.mult), [gmax], [ngmax])
        d(lambda: V.tensor_scalar(out=pen[:], in0=lg[:, 0:4], scalar1=gmax[:, 0:1], scalar2=None, op0=ALU.is_ge), [lg, gmax], [pen])
        d(lambda: V.tensor_scalar(out=pen[:], in0=pen[:], scalar1=1e30, scalar2=-1e30, op0=ALU.mult, op1=ALU.add), [pen], [pen])
        fw.act.op(lambda: nc.scalar.activation(out=ge[:], in_=lg[:, 0:4], func=AF.Exp, bias=ngmax[:, 0:1], accum_out=gsum[:, 0:1]),
                  [lg.b, ngmax.b], [ge.b, gsum.b])
        d(lambda: V.tensor_tensor(out=em[:].rearrange("p (a b) -> p a b", a=4), in0=lg[:, 4:36].rearrange("p (a b) -> p a b", a=4),
                                  in1=pen[:].unsqueeze(2).to_broadcast([128, 4, 8]), op=ALU.add), [lg, pen], [em])
        d(lambda: V.max(out=m8[:], in_=em[:]), [em], [m8])
        d(lambda: V.tensor_tensor(out=dd[:], in0=m8[:, 1:2], in1=m8[:, 0:1], op=ALU.subtract), [m8], [dd])
        fw.act.op(lambda: nc.scalar.activation(out=ed[:], in_=dd[:], func=AF.Exp), [dd.b], [ed.b])
        d(lambda: V.tensor_scalar(out=p1[:], in0=ed[:], scalar1=1.0, scalar2=None, op0=ALU.add), [ed], [p1])
        d(lambda: V.tensor_tensor(out=p1[:], in0=p1[:], in1=gsum[:], op=ALU.mult), [p1, gsum], [p1])
        d(lambda: V.reciprocal(out=p1[:], in_=p1[:]), [p1], [p1])
        d(lambda: V.tensor_tensor(out=p2[:], in0=p1[:], in1=ed[:], op=ALU.mult), [p1, ed], [p2])
        if OH is not None:
            for k in range(2):
                a = 2 * ti + k
                d(lambda: V.tensor_scalar(out=OH[:, a, :], in0=em[:], scalar1=m8[:, k:k + 1], scalar2=None, op0=ALU.is_equal), [em, m8], [OH])
                pk_ = p1 if k == 0 else p2
                d(lambda: V.tensor_copy(out=W12[:, a:a + 1], in_=pk_[:]), [pk_], [W12])
            return
        d(lambda: V.tensor_scalar(out=t1[:], in0=em[:], scalar1=m8[:, 0:1], scalar2=p1[:, 0:1], op0=ALU.is_equal, op1=ALU.mult), [em, m8, p1], [t1])
        d(lambda: V.tensor_scalar(out=t2[:], in0=em[:], scalar1=m8[:, 1:2], scalar2=p2[:, 0:1], op0=ALU.is_equal, op1=ALU.mult), [em, m8, p2], [t2])
        d(lambda: V.tensor_tensor(out=Wt[:, ti, :], in0=t1[:], in1=t2[:], op=ALU.add), [t1, t2], [Wt])

    def layer1_mixer(self):
        nc, fw, g = self.nc, self.fw, self.g
        l = 1
        PADU = 16
        with ExitStack() as ph:
            oT = self.tile(ph, "oT1", [128, 4, T], BF16)
            uT = self.tile(ph, "uT", [128, 4, S + 2 * PADU], BF16)
            pattn = ExitStack()
            qT = self.tile(pattn, "qT1", [128, NT, 4, 128], BF16)
            kT = self.tile(pattn, "kT1", [128, 2, T], BF16)
            Vp = self.tile(pattn, "Vp1", [128, NT, 200], BF16)
            self.v_init(Vp)
            fw.pool.op(lambda: nc.gpsimd.memset(kT[:], 0.0), [], [kT.b])
            fw.pool.op(lambda: nc.gpsimd.memset(uT[:], 0.0), [], [uT.b])
            with ExitStack() as p1:
                win = self.tile(p1, "win1", [128, 8, 1280], BF16)
                gqk = self.tile(p1, "gqk1", [128, 640], F32)
                g["cos"] = self.tile(p1, "cos1", [128, 32, 32], F32)
                g["sin"] = self.tile(p1, "sin1", [128, 32, 32], F32)
                fw.q_sync.dma(gqk[:], self.gqk_c, writes=[gqk.b])
                fw.q_sync.dma(g["cos"][:], self.cosT, writes=[g["cos"].b])
                fw.q_sync.dma(g["sin"][:], self.sinT, writes=[g["sin"].b])
                fw.dve.op(lambda: nc.vector.tensor_scalar(out=gqk[:, 0:512], in0=gqk[:, 0:512], scalar1=0.125, scalar2=None, op0=ALU.mult),
                          [gqk.b], [gqk.b])
                with ExitStack() as pw:
                    stg = [self.tile(pw, f"wstg1{i}", [128, 4096], F32) for i in range(2)]
                    self.stg_i = 0
                    for n in range(3):
                        w_ = 512 if n < 2 else 256
                        self.load_cast_w(stg, win, slice(n * 512, n * 512 + w_), self.w_in_cd[:, n * 512:n * 512 + w_], 8, w_, n)
                    fw.barrier()
                ssl_ = [self.tile(p1, f"ss1{i}", [128, 1], F32) for i in range(2)]
                xnl_ = [self.tile(p1, f"xn1{i}", [128, D], BF16) for i in range(2)]
                scr_l = [dict(ssl=[ssl_[i]], xnl=[xnl_[i]], sq=self.tile(p1, f"sq1{i}", [128, 640], F32),
                              ssq=self.tile(p1, f"ssq1{i}", [128, 10], F32), qn=self.tile(p1, f"qn1{i}", [128, 640], F32),
                              qr=self.tile(p1, f"qr1{i}", [128, 640], BF16), rt=self.tile(p1, f"rt1{i}", [128, 2, 320], F32)) for i in range(2)]
                xt = [self.tile(p1, f"xt1{i}", [128, D], F32) for i in range(2)]
                hT = self.tile(p1, "hT1", [128, 8, 512], BF16)
                qkv = [self.tile(p1, f"qkv1{i}", [128, 768], F32) for i in range(2)]
                chunks = [(0, 2)] + [(2 + 4 * i, 4) for i in range(8)]
                for ci, (t0, ntl) in enumerate(chunks):
                    h = hT
                    ntok = ntl * 128
                    is_ctx = t0 < 2
                    cls = 1 if is_ctx else 0
                    def norm_fn(tl):
                        t = t0 + tl
                        x_ = xt[tl % 2]
                        fw.q_sync.dma(x_[:], self.xres[t * 128:(t + 1) * 128, :], reads=[self.xres_b[t]], writes=[x_.b])
                        self.norm_tile_to_hT(x_, h, tl * 128, l, 1, cls, scr_l[tl % 2])

                    def qkv_fn(tl):
                        t = t0 + tl
                        qk_ = qkv[tl % 2]
                        for (c0, w_) in ((0, 512), (512, 256)):
                            if is_ctx and c0 == 0:
                                continue
                            p = self.ps_get()
                            for k in range(8):
                                fw.pe.op(lambda: nc.tensor.matmul(p[:, 0:w_], lhsT=h[:, k, tl * 128:(tl + 1) * 128], rhs=win[:, k, c0:c0 + w_],
                                                                  start=(k == 0), stop=(k == 7)), [h.b, win.b], [p.b], inc=(k == 7))
                            if c0 == 0:
                                fw.act.op(lambda: nc.scalar.copy(out=qk_[:, 0:512].rearrange("p (j a d) -> p a j d", a=2, d=64),
                                                                 in_=p[:, 0:512].rearrange("p (a j d) -> p a j d", a=2, j=4)), [p.b], [qk_.b])
                            else:
                                fw.act.op(lambda: nc.scalar.copy(out=qk_[:, c0:c0 + w_], in_=p[:, 0:w_]), [p.b], [qk_.b])
                            self.ps_put(p)
                        self.qk_post(qk_, t, gqk, scr_l[tl % 2], qT, kT, not is_ctx, None if is_ctx else t - 2)
                        self.v_fill(qk_, t, Vp)

                    for tl0 in range(0, ntl, 2):
                        ILV.run([(lambda tl=tl: norm_fn(tl)) for tl in range(tl0, min(tl0 + 2, ntl))])
                    for tl0 in range(0, ntl, 2):
                        ILV.run([(lambda tl=tl: qkv_fn(tl)) for tl in range(tl0, min(tl0 + 2, ntl))])
                    if not is_ctx:
                        tok0 = (t0 - 2) * 128
                        for j in range(4):
                            pu = self.ps_get()
                            for k in range(8):
                                fw.pe.op(lambda: nc.tensor.matmul(pu[:, 0:ntok], lhsT=win[:, k, 768 + j * 128:768 + (j + 1) * 128], rhs=h[:, k, 0:ntok],
                                                                  start=(k == 0), stop=(k == 7)), [win.b, h.b], [pu.b], inc=(k == 7))
                            fw.dve.op(lambda: nc.vector.tensor_copy(out=uT[:, j, PADU + tok0:PADU + tok0 + ntok], in_=pu[:, 0:ntok]), [pu.b], [uT.b])
                            self.ps_put(pu)
                fw.barrier()
            if self.stop == "l1p1":
                pattn.close()
                return
            with ExitStack() as p2:
                wm = self.tile(p2, "wm", [128, 2, 512], BF16)
                sk = self.tile(p2, "sk", [128, 2, 512], F32)
                with ExitStack() as pw:
                    wmf = self.tile(pw, "wmf", [128, 2, 512], F32)
                    fw.q_sync.dma(wmf[:], self.wmask, writes=[wmf.b])
                    fw.dve.op(lambda: nc.vector.tensor_copy(out=wm[:], in_=wmf[:]), [wmf.b], [wm.b])
                    fw.q_sync.dma(sk[:], self.sink_rep, writes=[sk.b])
                    fw.act.op(lambda: nc.scalar.activation(out=sk[:], in_=sk[:], func=AF.Exp), [sk.b], [sk.b])
                    fw.barrier()
                scr = dict(pexp=[self.tile(p2, f"pexp1{i}", [128, 1024], BF16) for i in range(4)], pi=0,
                           rec=[self.tile(p2, f"rec1{i}", [128, 512], F32) for i in range(2)],
                           posb=[[self.tile(p2, f"posb1{i}{k}", [128, 512], F32) for k in range(2)] for i in range(2)])
                blocks = []
                for t in range(2, NT):
                    kts = [(0, None), (1, None)]
                    if t > 2:
                        kts.append((t - 1, 0))
                    kts.append((t, None))
                    if t < NT - 1:
                        kts.append((t + 1, 1))
                    for kv in range(2):
                        blocks.append((kv, t, kts))
                self.attention(blocks, qT, kT, Vp, oT, scr, masks=wm, sinkrow=sk)
                fw.barrier()
            pattn.close()
            if self.debug:
                self.dbg_dump_bf(ph, "oT1", oT[:, :, 256:384], [128, 4, 128], oT.b)
                self.dbg_dump_bf(ph, "oT1b", oT[:, :, 640:768], [128, 4, 128], oT.b)
            dT = self.tile(ph, "dT", [128, 4, T], BF16)
            with ExitStack() as p3:
                pwf = self.tile(p3, "pwf", [128, 4, 128], F32)
                pwb = self.tile(p3, "pwb", [128, 4, 128], BF16)
                psc = self.tile(p3, "psc", [128, 4], F32)
                pfx = self.tile(p3, "pfx", [128, 4, 32], F32)
                fw.q_sync.dma(pwf[:], self.pool_w.rearrange("g c d -> c g d"), writes=[pwf.b])
                fw.q_sync.dma(psc[:], self.pool_scT, writes=[psc.b])
                fw.q_sync.dma(pfx[:], self.poolfix, writes=[pfx.b])
                fw.dve.op(lambda: nc.vector.tensor_copy(out=pwb[:], in_=pwf[:]), [pwf.b], [pwb.b])
                ta = [self.tile(p3, f"pta{i}", [128, 528], F32) for i in range(2)]
                pp_ = [self.tile(p3, f"ppb{i}", [128, 512], BF16) for i in range(2)]
                cnt = 0
                for ci in range(8):
                    tok0 = ci * 512
                    for j, w in enumerate((2, 4, 8, 16)):
                        base = PADU + tok0 - w // 2
                        ln = 512 + w - 1
                        cur = uT[:, j, base:base + ln]
                        curb = uT.b
                        step = 1
                        k = 0
                        while step < w:
                            dst = ta[k % 2]
                            e_, ee = (fw.dve, nc.vector) if (cnt % 2 == 0) else (fw.pool, nc.gpsimd)
                            cnt += 1
                            cc, cb_ = cur, curb
                            e_.op(lambda: ee.tensor_tensor(out=dst[:, 0:ln - step], in0=cc[:, 0:ln - step], in1=cc[:, step:ln], op=ALU.add), [cb_], [dst.b])
                            ln -= step
                            step *= 2
                            cur, curb = dst[:, 0:ln], dst.b
                            k += 1
                        pb_ = pp_[j % 2]
                        uc = uT[:, j, PADU + tok0:PADU + tok0 + 512]
                        sdst = ta[k % 2]
                        fw.dve.op(lambda: nc.vector.scalar_tensor_tensor(out=pb_[:], in0=cur[:, 0:512], scalar=1.0 / w, in1=uc, op0=ALU.mult, op1=ALU.subtract),
                                  [curb, uT.b], [pb_.b])
                        for (cond, lo, fo) in ((ci == 0, 0, 0), (ci == 7, 496, 16)):
                            if cond:
                                fw.dve.op(lambda: nc.vector.tensor_tensor(out=sdst[:, 0:16], in0=cur[:, lo:lo + 16], in1=pfx[:, j, fo:fo + 16], op=ALU.mult),
                                          [curb, pfx.b], [sdst.b])
                                fw.dve.op(lambda: nc.vector.tensor_tensor(out=pb_[:, lo:lo + 16], in0=sdst[:, 0:16], in1=uc[:, lo:lo + 16], op=ALU.subtract),
                                          [sdst.b, uT.b], [pb_.b])
                        py = self.ps_get()
                        fw.pe.op(lambda: nc.tensor.matmul(py[:], lhsT=pwb[:, j, :], rhs=pb_[:], start=True, stop=True), [pwb.b, pb_.b], [py.b])
                        fw.act.op(lambda: nc.scalar.activation(out=dT[:, j, NCTX + tok0:NCTX + tok0 + 512], in_=py[:], func=AF.Copy, scale=psc[:, j:j + 1]),
                                  [py.b, psc.b], [dT.b])
                        self.ps_put(py)
                fw.barrier()
            if self.debug:
                self.dbg_dump_bf(ph, "dT", dT[:, :, 256:384], [128, 4, 128], dT.b)
                self.dbg_dump_bf(ph, "dTe", dT[:, :, T - 128:T], [128, 4, 128], dT.b)
            if self.stop == "l1p3":
                return
            with ExitStack() as p4:
                self.out_proj(1, lambda c: (oT, c) if c < 4 else (dT, c - 4), self.w_out_cd, list(range(2, NT)), p4)
                fw.barrier()


def _rope_tables():
    rows = S // 64
    row = np.repeat(np.arange(rows, dtype=np.float32), 64)
    col = np.tile(np.arange(64, dtype=np.float32), rows)
    inv = (10000.0 ** (-np.arange(16, dtype=np.float32) / 16)).astype(np.float32)
    ang = np.concatenate([row[:, None] * inv, col[:, None] * inv], axis=-1).astype(np.float32)
    return np.cos(ang).astype(np.float32), np.sin(ang).astype(np.float32)


def _fm(v, chunks):
    return np.ascontiguousarray(np.asarray(v, np.float32).reshape(chunks, 128).T)


def make_in_maps(inp, cores):
    f = lambda a: np.ascontiguousarray(np.asarray(a, dtype=np.float32))
    cos, sin = _rope_tables()
    cosT = np.ascontiguousarray(cos.reshape(32, 128, 32).transpose(1, 0, 2))
    sinT = np.ascontiguousarray(sin.reshape(32, 128, 32).transpose(1, 0, 2))
    r = np.arange(128)
    mprev = (r[None, :] <= r[:, None]).astype(np.float32)
    mnext = (r[:, None] <= r[None, :]).astype(np.float32)
    wmask = np.stack([np.tile(mprev, (1, 4)), np.tile(mnext, (1, 4))], axis=1)
    shared = {
        "mod_w": f(inp["mod_w"]),
        "mod_bT": np.ascontiguousarray(f(inp["mod_b"]).reshape(2, 48, 128).transpose(2, 0, 1)),
        "ln1gT": np.ascontiguousarray(f(inp["ln1_g"]).reshape(2, 8, 128).transpose(2, 0, 1)),
        "ln2gT": np.ascontiguousarray(f(inp["ln2_g"]).reshape(2, 8, 128).transpose(2, 0, 1)),
        "w_in_ab": f(inp["w_in_ab"][0]), "w_out_ab": f(inp["w_out_ab"][0]),
        "gqk_a": np.ascontiguousarray(np.broadcast_to(np.concatenate([np.tile(f(inp["q_norm_a"][0]), 8), np.tile(f(inp["k_norm_a"][0]), 2)])[None, :], (128, 640))),
        "convwT": np.ascontiguousarray(f(inp["conv_w"][0]).reshape(31, 4, 128).transpose(2, 1, 0)),
        "convv": np.ascontiguousarray(np.stack([_fm(inp["conv_b"][0], 4), _fm(inp["conv_ln_g"][0], 4), _fm(inp["conv_ln_b"][0], 4)], axis=1)),
        "w_in_cd": f(inp["w_in_cd"][0]), "w_out_cd": f(inp["w_out_cd"][0]),
        "gqk_c": np.ascontiguousarray(np.broadcast_to(np.concatenate([np.tile(f(inp["q_norm_c"][0]), 8), np.tile(f(inp["k_norm_c"][0]), 2)])[None, :], (128, 640))),
        "sink_rep": np.ascontiguousarray(np.broadcast_to(np.repeat(f(inp["sink_c"][0]).reshape(2, 4), 128, axis=1)[None], (128, 2, 512))),
        "pool_w": f(inp["pool_w"][0]),
        "pool_scT": _fm(inp["pool_scale"][0], 4),
        "poolfix": _poolfix(),
        "rt_w": np.ascontiguousarray(np.concatenate([f(inp["rt_grp_w"]), f(inp["rt_exp_w"])], axis=2)),
        "rt_b": np.ascontiguousarray(np.broadcast_to(np.concatenate([f(inp["rt_grp_b"]), f(inp["rt_exp_b"])], axis=1)[None], (128, 2, 36))),
        "ex_gate": np.ascontiguousarray(f(inp["ex_gate"]).reshape(2, 32, 8, 128, 512).transpose(0, 1, 3, 2, 4).reshape(2, 4096, 4096)),
        "ex_up": np.ascontiguousarray(f(inp["ex_up"]).reshape(2, 32, 8, 128, 512).transpose(0, 1, 3, 2, 4).reshape(2, 4096, 4096)),
        "ex_down": np.ascontiguousarray(f(inp["ex_down"]).reshape(2, 32, 4, 128, 1024).transpose(0, 1, 3, 2, 4).reshape(2, 4096, 4096)),
        "pidx": np.arange(128, dtype=np.float32).reshape(128, 1),
        "mconst": np.ascontiguousarray(np.broadcast_to(np.concatenate([
            np.arange(32), 128.0 * np.arange(35), np.arange(100), np.arange(32) % 2, np.arange(100) % 2,
            128.0 * np.arange(1, 69), 2.0 * np.arange(1, 51), np.arange(16)]).astype(np.float32)[None], (128, 433))),
        "umat": np.ascontiguousarray((r[:, None] < r[None, :]).astype(np.float32)),
        "ident": np.eye(128, dtype=np.float32), "cosT": cosT, "sinT": sinT, "wmask": np.ascontiguousarray(wmask),
    }
    maps = []
    for b in cores:
        m = dict(shared)
        m["x"] = f(inp["x"][b])
        m["ctx"] = f(inp["ctx"][b])
        c2 = np.stack([f(inp["c"][b]), f(inp["c_ctx"])], axis=1)
        m["c2T"] = np.ascontiguousarray(c2.reshape(8, 128, 2).transpose(1, 0, 2))
        maps.append(m)
    return maps


def _poolfix():
    out = np.ones((4, 32), np.float32)
    for gi, w in enumerate((2, 4, 8, 16)):
        for i, t in enumerate(list(range(16)) + list(range(S - 16, S))):
            lo = min(max(t - w // 2, 0), S)
            hi = min(max(t - w // 2 + w, 0), S)
            out[gi, i] = 1.0 / float(hi - lo)
    return np.ascontiguousarray(np.broadcast_to(out[None], (128, 4, 32)))


_NC_CACHE = {}


def kernel(**inputs):
    if "nc" not in _NC_CACHE:
        _NC_CACHE["nc"] = Builder(debug=False).build()
    nc = _NC_CACHE["nc"]
    maps = make_in_maps(inputs, list(range(8)))
    res = run_bass_kernel_spmd(nc, maps, core_ids=list(range(8)))
    return np.stack([np.asarray(r["y"], dtype=np.float32) for r in res.results], axis=0)
```
